# Optimizing a Trainium2 kernel written in Bass

```python
import math
import jax, jax.numpy as jnp
from jax import lax
import numpy as np

D_MODEL = 1024
BATCH = 2
SEQ = 8192
DEPTH = 4

N_MIXERS = 4
PLE_DIM = 256
ALPHA = (2.0 * DEPTH) ** 0.25
BETA = (8.0 * DEPTH) ** -0.25
LN_EPS = 1e-5

RG_WIDTH = D_MODEL
RG_BLOCK = 256
RG_BLOCKS = RG_WIDTH // RG_BLOCK
RG_CONV = 4
RG_C = 8.0

HG_HEADS = 8
HG_DK = D_MODEL // HG_HEADS
HG_DV = D_MODEL // HG_HEADS
HG_KDIM = HG_HEADS * HG_DK
HG_VDIM = HG_HEADS * HG_DV

RET_HEADS = 4
RET_DK = D_MODEL // RET_HEADS
RET_DV = 2 * D_MODEL // RET_HEADS
RET_KDIM = RET_HEADS * RET_DK
RET_VDIM = RET_HEADS * RET_DV
RET_CHUNK = 64
ROPE_BASE = 10000.0

GLA_HEADS = 4
GLA_DK = D_MODEL // 2 // GLA_HEADS
GLA_DV = D_MODEL // GLA_HEADS
GLA_KDIM = GLA_HEADS * GLA_DK
GLA_VDIM = GLA_HEADS * GLA_DV
GLA_RANK = 16
GLA_TAU = 16.0

GATE_CHUNK = 32

FFN_DENSE = 2816
N_EXPERTS = 8
TOP_K = 2
FFN_EXPERT = 3584

N_RGLRU = (DEPTH + 3) // 4
N_HGRN = (DEPTH + 2) // 4
N_RET = (DEPTH + 1) // 4
N_GLA = DEPTH // 4
N_DENSE = (DEPTH + 1) // 2
N_MOE = DEPTH // 2

kernel_name = "hybrid_interleaved_rglru_hgrn2_retnet_gla_moe_deepnorm"

F32 = jnp.float32


def layer_norm(x, g, b):
    xf = x.astype(F32)
    mu = jnp.mean(xf, -1, keepdims=True)
    var = jnp.mean(jnp.square(xf - mu), -1, keepdims=True)
    return ((xf - mu) * lax.rsqrt(var + LN_EPS) * g + b).astype(x.dtype)


def head_rms_norm(o):
    return o * lax.rsqrt(jnp.mean(jnp.square(o), -1, keepdims=True) + LN_EPS)


def head_group_norm(o):
    mu = jnp.mean(o, -1, keepdims=True)
    var = jnp.mean(jnp.square(o - mu), -1, keepdims=True)
    return (o - mu) * lax.rsqrt(var + LN_EPS)


def split_heads(t, h):
    b, s, c = t.shape
    return t.reshape(b, s, h, c // h).transpose(0, 2, 1, 3)


def merge_heads(t):
    b, h, s, d = t.shape
    return t.transpose(0, 2, 1, 3).reshape(b, s, h * d)


def rotary(t):
    s, d = t.shape[-2], t.shape[-1]
    inv = ROPE_BASE ** (-jnp.arange(0, d, 2, dtype=F32) / d)
    ang = jnp.arange(s, dtype=F32)[:, None] * inv[None, :]
    cos, sin = jnp.cos(ang), jnp.sin(ang)
    t = t.astype(F32)
    t1, t2 = t[..., : d // 2], t[..., d // 2:]
    return jnp.concatenate([t1 * cos - t2 * sin, t1 * sin + t2 * cos], -1)


def causal_depthwise_conv(x, w, b):
    k = w.shape[0]
    y = lax.conv_general_dilated(
        x, w[:, None, :], window_strides=(1,), padding=[(k - 1, 0)],
        dimension_numbers=("NWC", "WIO", "NWC"), feature_group_count=x.shape[-1])
    return y + b


def _linear_recurrence_combine(left, right):
    a1, b1 = left
    a2, b2 = right
    return a1 * a2, a2 * b1 + b2


def chunk_gated_linear_attention(q, k, v, log_g, chunk):
    q, k, v, log_g = (t.astype(F32) for t in (q, k, v, log_g))
    b_, h_, s_, dk = q.shape
    dv = v.shape[-1]
    n = s_ // chunk

    def to_chunks(t):
        return t.reshape(b_, h_, n, chunk, t.shape[-1]).transpose(2, 0, 1, 3, 4)

    causal = jnp.tril(jnp.ones((chunk, chunk), bool))

    def step(state, inp):
        qi, ki, vi, gi = inp
        cum = jnp.cumsum(gi, axis=2)
        ref = cum[:, :, chunk // 2: chunk // 2 + 1]
        last = cum[:, :, -1:]
        inter = jnp.einsum("bhcd,bhde->bhce", qi * jnp.exp(cum), state)
        scores = jnp.einsum("bhcd,bhsd->bhcs", qi * jnp.exp(cum - ref), ki * jnp.exp(ref - cum))
        intra = jnp.einsum("bhcs,bhse->bhce", jnp.where(causal, scores, 0.0), vi)
        new_state = (state * jnp.exp(jnp.swapaxes(last, -1, -2))
                     + jnp.einsum("bhcd,bhce->bhde", ki * jnp.exp(last - cum), vi))
        return new_state, inter + intra

    init = jnp.zeros((b_, h_, dk, dv), F32)
    _, out = lax.scan(step, init, tuple(map(to_chunks, (q, k, v, log_g))))
    return out.transpose(1, 2, 0, 3, 4).reshape(b_, h_, s_, dv)


def chunk_retention(q, k, v, log_gamma, chunk):
    q, k, v = (t.astype(F32) for t in (q, k, v))
    b_, h_, s_, dk = q.shape
    dv = v.shape[-1]
    n = s_ // chunk
    pos = jnp.arange(chunk, dtype=F32)
    lg = log_gamma[:, None]
    decay_q = jnp.exp(lg * (pos + 1.0))[None, :, :, None]
    decay_k = jnp.exp(lg * (chunk - 1.0 - pos))[None, :, :, None]
    rel = pos[:, None] - pos[None, :]
    dmat = jnp.where(rel >= 0, jnp.exp(lg[:, :, None] * jnp.maximum(rel, 0.0)), 0.0)[None]
    gamma_chunk = jnp.exp(lg * chunk)[None, :, :, None]

    def to_chunks(t):
        return t.reshape(b_, h_, n, chunk, t.shape[-1]).transpose(2, 0, 1, 3, 4)

    def step(state, inp):
        qi, ki, vi = inp
        inter = jnp.einsum("bhcd,bhde->bhce", qi, state) * decay_q
        intra = jnp.einsum("bhcs,bhse->bhce", jnp.einsum("bhcd,bhsd->bhcs", qi, ki) * dmat, vi)
        new_state = state * gamma_chunk + jnp.einsum("bhcd,bhce->bhde", ki * decay_k, vi)
        return new_state, inter + intra

    init = jnp.zeros((b_, h_, dk, dv), F32)
    _, out = lax.scan(step, init, tuple(map(to_chunks, (q, k, v))))
    return out.transpose(1, 2, 0, 3, 4).reshape(b_, h_, s_, dv)


def rglru_mixer(x, w_in, conv_w, conv_b, w_a, b_a, w_x, b_x, lam, w_out):
    b_, s_, _ = x.shape
    gate_br, rec_br = jnp.split(x @ w_in, 2, axis=-1)
    gate_br = jax.nn.gelu(gate_br, approximate=True)
    u = causal_depthwise_conv(rec_br, conv_w, conv_b)
    ub = u.reshape(b_, s_, RG_BLOCKS, RG_BLOCK)
    r = jax.nn.sigmoid(jnp.einsum("bsnc,ncd->bsnd", ub, w_a).reshape(b_, s_, RG_WIDTH) + b_a)
    i = jax.nn.sigmoid(jnp.einsum("bsnc,ncd->bsnd", ub, w_x).reshape(b_, s_, RG_WIDTH) + b_x)
    log_a = -RG_C * jax.nn.softplus(-lam.astype(F32)) * r.astype(F32)
    a = jnp.exp(log_a)
    inp = jnp.sqrt(-jnp.expm1(2.0 * log_a)) * (i.astype(F32) * u.astype(F32))
    _, h = lax.associative_scan(_linear_recurrence_combine, (a, inp), axis=1)
    return (h.astype(x.dtype) * gate_br) @ w_out


def hgrn2_mixer(x, w_in, lb, w_out):
    q, fz, i, g = jnp.split(x @ w_in, [HG_KDIM, 2 * HG_KDIM, 2 * HG_KDIM + HG_VDIM], axis=-1)
    q = jax.nn.silu(q)
    f = lb + (1.0 - lb) * jax.nn.sigmoid(fz.astype(F32))
    k = 1.0 - f
    o = chunk_gated_linear_attention(split_heads(q, HG_HEADS), split_heads(k, HG_HEADS),
                                     split_heads(i, HG_HEADS), split_heads(jnp.log(f), HG_HEADS),
                                     GATE_CHUNK)
    o = merge_heads(head_rms_norm(o)).astype(x.dtype) * jax.nn.silu(g)
    return o @ w_out


def retention_mixer(x, w_in, w_out):
    q, k, v, g = jnp.split(x @ w_in, [RET_KDIM, 2 * RET_KDIM, 2 * RET_KDIM + RET_VDIM], axis=-1)
    q = rotary(split_heads(q, RET_HEADS))
    k = rotary(split_heads(k, RET_HEADS)) * (RET_DK ** -0.5)
    log_gamma = jnp.log1p(-jnp.exp2(-5.0 - jnp.arange(RET_HEADS, dtype=F32)))
    o = chunk_retention(q, k, split_heads(v, RET_HEADS), log_gamma, RET_CHUNK)
    o = merge_heads(head_group_norm(o)).astype(x.dtype)
    return (jax.nn.silu(g) * o) @ w_out


def gla_mixer(x, w_in, w_gate, b_gate, w_out):
    q, k, v, r, gl = jnp.split(
        x @ w_in, [GLA_KDIM, 2 * GLA_KDIM, 2 * GLA_KDIM + GLA_VDIM, 2 * GLA_KDIM + 2 * GLA_VDIM], axis=-1)
    log_alpha = jax.nn.log_sigmoid((gl @ w_gate + b_gate).astype(F32)) / GLA_TAU
    q = q * (GLA_DK ** -0.5)
    o = chunk_gated_linear_attention(split_heads(q, GLA_HEADS), split_heads(k, GLA_HEADS),
                                     split_heads(v, GLA_HEADS), split_heads(log_alpha, GLA_HEADS),
                                     GATE_CHUNK)
    o = merge_heads(head_rms_norm(o)).astype(x.dtype) * jax.nn.silu(r)
    return o @ w_out


def swiglu(x, w_in, w_out):
    g, u = jnp.split(x @ w_in, 2, axis=-1)
    return (jax.nn.silu(g) * u) @ w_out


def moe_swiglu(x, w_router, w_in, w_out):
    logits = (x @ w_router).astype(F32)
    top_vals, top_idx = lax.top_k(logits, TOP_K)
    weights = jax.nn.softmax(top_vals, axis=-1)
    gates = jnp.sum(jax.nn.one_hot(top_idx, N_EXPERTS, dtype=F32) * weights[..., None], axis=-2)
    y = jnp.zeros_like(x)
    for e in range(N_EXPERTS):
        y = y + gates[..., e:e + 1].astype(x.dtype) * swiglu(x, w_in[e], w_out[e])
    return y


def _nrm(key, shape, fan_in, scale=1.0):
    return jax.random.normal(key, shape, F32) * (scale * fan_in ** -0.5)


def setup_inputs(seed: int = 0) -> dict:
    key = jax.random.key(seed)
    ks = iter(jax.random.split(key, 48))
    D = D_MODEL
    small = lambda shape, s=0.01: s * jax.random.normal(next(ks), shape, F32)
    x = jax.random.normal(next(ks), (BATCH, SEQ, D), F32)
    p = jax.random.normal(next(ks), (DEPTH, BATCH, SEQ, PLE_DIM), F32)
    rg_w_in = _nrm(next(ks), (N_RGLRU, D, 2 * RG_WIDTH), D)
    rg_conv_w = _nrm(next(ks), (N_RGLRU, RG_CONV, RG_WIDTH), RG_CONV)
    rg_conv_b = small((N_RGLRU, RG_WIDTH))
    rg_w_a = _nrm(next(ks), (N_RGLRU, RG_BLOCKS, RG_BLOCK, RG_BLOCK), RG_BLOCK)
    rg_b_a = small((N_RGLRU, RG_WIDTH))
    rg_w_x = _nrm(next(ks), (N_RGLRU, RG_BLOCKS, RG_BLOCK, RG_BLOCK), RG_BLOCK)
    rg_b_x = small((N_RGLRU, RG_WIDTH))
    a_pow_c = jax.random.uniform(next(ks), (N_RGLRU, RG_WIDTH), F32, 0.9, 0.999)
    log_a = jnp.log(a_pow_c) / RG_C
    rg_lambda = log_a - jnp.log(-jnp.expm1(log_a))
    rg_w_out = _nrm(next(ks), (N_RGLRU, RG_WIDTH, D), RG_WIDTH, BETA)
    hg_w_in = _nrm(next(ks), (N_HGRN, D, 2 * HG_KDIM + 2 * HG_VDIM), D)
    hg_lb_logits = small((DEPTH, HG_KDIM), 0.5)
    hg_w_out = _nrm(next(ks), (N_HGRN, HG_VDIM, D), HG_VDIM, BETA)
    ret_w_in = _nrm(next(ks), (N_RET, D, 2 * RET_KDIM + 2 * RET_VDIM), D)
    ret_w_out = _nrm(next(ks), (N_RET, RET_VDIM, D), RET_VDIM, BETA)
    gla_w_in = _nrm(next(ks), (N_GLA, D, 2 * GLA_KDIM + 2 * GLA_VDIM + GLA_RANK), D)
    gla_w_gate = _nrm(next(ks), (N_GLA, GLA_RANK, GLA_KDIM), GLA_RANK)
    gla_b_gate = small((N_GLA, GLA_KDIM), 0.1)
    gla_w_out = _nrm(next(ks), (N_GLA, GLA_VDIM, D), GLA_VDIM, BETA)
    dense_w_in = _nrm(next(ks), (N_DENSE, D, 2 * FFN_DENSE), D)
    dense_w_out = _nrm(next(ks), (N_DENSE, FFN_DENSE, D), FFN_DENSE, BETA)
    moe_w_router = _nrm(next(ks), (N_MOE, D, N_EXPERTS), D)
    moe_w_in = _nrm(next(ks), (N_MOE, N_EXPERTS, D, 2 * FFN_EXPERT), D)
    moe_w_out = _nrm(next(ks), (N_MOE, N_EXPERTS, FFN_EXPERT, D), FFN_EXPERT, BETA)
    ple_w_proj = _nrm(next(ks), (DEPTH, PLE_DIM, D), PLE_DIM)
    ple_w_gate = _nrm(next(ks), (DEPTH, D, D), D)
    ln_mix_g = 1.0 + small((DEPTH, D))
    ln_mix_b = small((DEPTH, D))
    ln_ffn_g = 1.0 + small((DEPTH, D))
    ln_ffn_b = small((DEPTH, D))
    return {
        "x": x, "p": p,
        "rg_w_in": rg_w_in, "rg_conv_w": rg_conv_w, "rg_conv_b": rg_conv_b,
        "rg_w_a": rg_w_a, "rg_b_a": rg_b_a, "rg_w_x": rg_w_x, "rg_b_x": rg_b_x,
        "rg_lambda": rg_lambda, "rg_w_out": rg_w_out,
        "hg_w_in": hg_w_in, "hg_lb_logits": hg_lb_logits, "hg_w_out": hg_w_out,
        "ret_w_in": ret_w_in, "ret_w_out": ret_w_out,
        "gla_w_in": gla_w_in, "gla_w_gate": gla_w_gate, "gla_b_gate": gla_b_gate, "gla_w_out": gla_w_out,
        "dense_w_in": dense_w_in, "dense_w_out": dense_w_out,
        "moe_w_router": moe_w_router, "moe_w_in": moe_w_in, "moe_w_out": moe_w_out,
        "ple_w_proj": ple_w_proj, "ple_w_gate": ple_w_gate,
        "ln_mix_g": ln_mix_g, "ln_mix_b": ln_mix_b, "ln_ffn_g": ln_ffn_g, "ln_ffn_b": ln_ffn_b,
    }


def reference(x, p,
              rg_w_in, rg_conv_w, rg_conv_b, rg_w_a, rg_b_a, rg_w_x, rg_b_x, rg_lambda, rg_w_out,
              hg_w_in, hg_lb_logits, hg_w_out,
              ret_w_in, ret_w_out,
              gla_w_in, gla_w_gate, gla_b_gate, gla_w_out,
              dense_w_in, dense_w_out,
              moe_w_router, moe_w_in, moe_w_out,
              ple_w_proj, ple_w_gate,
              ln_mix_g, ln_mix_b, ln_ffn_g, ln_ffn_b):
    lb_sm = jax.nn.softmax(hg_lb_logits.astype(F32), axis=0)
    lower_bounds = jnp.cumsum(lb_sm, axis=0) - lb_sm[0:1]
    for i in range(DEPTH):
        kind, j = i % N_MIXERS, i // N_MIXERS
        if kind == 0:
            h = rglru_mixer(x, rg_w_in[j], rg_conv_w[j], rg_conv_b[j], rg_w_a[j], rg_b_a[j],
                            rg_w_x[j], rg_b_x[j], rg_lambda[j], rg_w_out[j])
        elif kind == 1:
            h = hgrn2_mixer(x, hg_w_in[j], lower_bounds[i], hg_w_out[j])
        elif kind == 2:
            h = retention_mixer(x, ret_w_in[j], ret_w_out[j])
        else:
            h = gla_mixer(x, gla_w_in[j], gla_w_gate[j], gla_b_gate[j], gla_w_out[j])
        x = layer_norm(ALPHA * x + h, ln_mix_g[i], ln_mix_b[i])
        if i % 2 == 0:
            f = swiglu(x, dense_w_in[i // 2], dense_w_out[i // 2])
        else:
            f = moe_swiglu(x, moe_w_router[i // 2], moe_w_in[i // 2], moe_w_out[i // 2])
        x = layer_norm(ALPHA * x + f, ln_ffn_g[i], ln_ffn_b[i])
        x = x + (p[i] @ ple_w_proj[i]) * jax.nn.sigmoid(x @ ple_w_gate[i])
    return x
```

```python
from contextlib import ExitStack
import math
import numpy as np
import ml_dtypes
import concourse.bass as bass
import concourse.mybir as mybir
from concourse.bass_utils import run_bass_kernel_spmd

F32 = mybir.dt.float32
BF16 = mybir.dt.bfloat16
AF = mybir.ActivationFunctionType
ALU = mybir.AluOpType
AX = mybir.AxisListType

NCORES = 8
D = 1024
SEQ = 8192
T = 2048
NT = 512
NTILE = T // NT
ALPHA = 8.0 ** 0.25
EPS = 1e-5
FFN_DENSE = 2816
FFN_EXPERT = 3584
ARENA_BYTES = 206 * 1024
DEBUG = False
CC_INC = 1
DEBUG_TI = 0


class Tok:
    __slots__ = ("name", "w", "r")

    def __init__(self, name):
        self.name = name
        self.w = None
        self.r = {}


class Prog:
    ENGS = ("pe", "act", "dve", "pool", "sp")

    def __init__(self, nc):
        self.nc = nc
        self.stack = ExitStack()
        self.streams = {e: [] for e in self.ENGS}
        self.cnt = {e: 0 for e in self.ENGS}
        self.waited = {e: {} for e in self.ENGS}
        self.needed = {e: set() for e in self.ENGS}
        self.slot_of = {}
        self.slot_total = []
        self.free_slots = []
        self.ntok = 0

    def _slot(self, key):
        if key not in self.slot_of:
            if self.free_slots:
                sl = self.free_slots.pop()
            else:
                sl = len(self.slot_total)
                self.slot_total.append(0)
            self.slot_of[key] = sl
        return self.slot_of[key]

    def release_keys(self):
        self.free_slots.extend(sorted(set(self.slot_of.values()), reverse=True))
        self.slot_of.clear()

    def tok(self, name=None):
        self.ntok += 1
        return Tok(name or f"t{self.ntok}")

    def toks(self, n, name="t"):
        return [self.tok(f"{name}{i}") for i in range(n)]

    def _deps(self, r, w):
        deps = []
        for t in r:
            if t.w is not None:
                deps.append(t.w)
        for t in w:
            if t.w is not None:
                deps.append(t.w)
            deps.extend(t.r.values())
        return deps

    def _emit_waits(self, eng, deps, skip_self=False):
        best = {}
        for d in deps:
            k = (d[0], d[1])
            if skip_self and d[0] == "e" and d[1] == eng:
                continue
            if best.get(k, 0) < d[2]:
                best[k] = d[2]
        wd = self.waited[eng]
        for k, v in best.items():
            if wd.get(k, 0) < v:
                wd[k] = v
                if k[0] == "e":
                    self.needed[k[1]].add(v)
                self.streams[eng].append(("wait", (k[0], k[1], v)))

    def op(self, eng, fn, r=(), w=(), skip_self=False):
        self._emit_waits(eng, self._deps(r, w), skip_self=skip_self)
        self.cnt[eng] += 1
        idx = self.cnt[eng]
        self.streams[eng].append(("op", fn, idx))
        me = ("e", eng, idx)
        for t in r:
            t.r[("e", eng)] = me
        for t in w:
            t.w = me
            t.r = {}
        return idx

    def dma(self, q, out, in_, r=(), w=(), key=None):
        self._emit_waits(q, self._deps(r, w))
        if key is None:
            key = (w[0] if len(w) else r[0])
        sl = self._slot(key)
        self.slot_total[sl] += 16
        c = self.slot_total[sl]
        self.streams[q].append(("dma", (out, in_), sl))
        me = ("d", sl, c)
        for t in r:
            t.r[("d", sl)] = me
        for t in w:
            t.w = me
            t.r = {}

    def collective(self, ins_ap, outs_ap, r, w):
        self._emit_waits("pool", self._deps(r, w))
        sl = self._slot(w[0])
        self.slot_total[sl] += CC_INC
        c = self.slot_total[sl]
        self.streams["pool"].append(("cc", (ins_ap, outs_ap), sl))
        me = ("d", sl, c)
        for t in r:
            t.r[("d", sl)] = me
        for t in w:
            t.w = me
            t.r = {}

    def barrier(self):
        for e in self.ENGS:
            deps = [("e", f, self.cnt[f]) for f in self.ENGS if f != e and self.cnt[f] > 0]
            deps += [("d", k, c) for k, c in enumerate(self.slot_total) if c > 0]
            self._emit_waits(e, deps)

    def finalize(self):
        nc = self.nc
        self.barrier()
        esem = {}
        for e in self.ENGS:
            if self.needed[e]:
                esem[e] = self.stack.enter_context(nc.semaphore(f"es_{e}"))
        dsem = {}
        for i in range(len(self.slot_total)):
            dsem[i] = self.stack.enter_context(nc.semaphore(f"ds_{i}"))
        rank = {}
        for e in self.ENGS:
            s = sorted(self.needed[e])
            rank[e] = {v: i + 1 for i, v in enumerate(s)}
        self.n_sems = len(esem) + len(dsem)

        def replay(e, h):
            for ent in self.streams[e]:
                if ent[0] == "wait":
                    kind, k, v = ent[1]
                    if kind == "e":
                        h.wait_ge(esem[k], rank[k][v])
                    else:
                        h.wait_ge(dsem[k], v)
                elif ent[0] == "op":
                    ins = ent[1](h)
                    if ent[2] in rank[e]:
                        ins.then_inc(esem[e], 1)
                elif ent[0] == "cc":
                    ins_ap, outs_ap = ent[1]
                    h.collective_compute("AllGather", ALU.bypass, replica_groups=[list(range(NCORES))],
                                         ins=[ins_ap], outs=[outs_ap]).then_inc(dsem[ent[2]], CC_INC)
                else:
                    out, in_ = ent[1]
                    h.dma_start(out=out, in_=in_).then_inc(dsem[ent[2]], 16)

        with nc.Block() as block:
            @block.tensor
            def _(h):
                replay("pe", h)

            @block.scalar
            def _(h):
                replay("act", h)

            @block.vector
            def _(h):
                replay("dve", h)

            @block.gpsimd
            def _(h):
                replay("pool", h)

            @block.sync
            def _(h):
                replay("sp", h)
        self.stack.close()


class Arena:
    def __init__(self, P, nbytes):
        self.t = P.stack.enter_context(P.nc.sbuf_tensor("arena", [128, nbytes // 4], F32))
        self.n = nbytes // 4
        self.off = 0

    def alloc(self, shape, dtype=F32, parts=128):
        n = 1
        for s in shape:
            n *= s
        words = (n + 1) // 2 if dtype == BF16 else n
        words = (words + 7) // 8 * 8
        assert self.off + words <= self.n, f"arena overflow: need {words*4}B at {self.off*4}B"
        v = self.t[0:parts, self.off:self.off + words]
        self.off += words
        if dtype == BF16:
            v = v.bitcast(BF16)
        v = v[:, 0:n]
        if len(shape) == 2:
            v = v.rearrange("p (a b) -> p a b", a=shape[0])
        elif len(shape) == 3:
            v = v.rearrange("p (a b c) -> p a b c", a=shape[0], b=shape[1])
        elif len(shape) == 4:
            v = v.rearrange("p (a b c d) -> p a b c d", a=shape[0], b=shape[1], c=shape[2])
        return v

    def mark(self):
        return self.off

    def reset(self, m):
        self.off = m


class Ring:
    def __init__(self, P, bufs, name):
        self.bufs = bufs
        self.toks = P.toks(len(bufs), name)
        self.i = 0

    def next(self):
        b, t = self.bufs[self.i], self.toks[self.i]
        self.i = (self.i + 1) % len(self.bufs)
        return b, t


def _vec_layout():
    lay = {}
    off = 0

    def add(name, n):
        nonlocal off
        lay[name] = (off, n)
        off += n
    for j in range(4):
        add(f"rg_conv_w{j}", 8)
    add("rg_conv_b", 8)
    add("rg_b_a", 8)
    add("rg_b_x", 8)
    add("rg_lambda", 8)
    for i in range(4):
        add(f"hg_lb{i}", 8)
    add("gla_b_gate", 4)
    for i in range(4):
        add(f"ln_mix_g{i}", 8)
        add(f"ln_mix_b{i}", 8)
        add(f"ln_ffn_g{i}", 8)
        add(f"ln_ffn_b{i}", 8)
    return lay, off


VLAY, NVEC = _vec_layout()


def _fm(v):
    v = np.asarray(v, np.float32).reshape(-1)
    return np.ascontiguousarray(v.reshape(-1, 128).T)


class KB:
    def __init__(self, stages, fused=False):
        self.nc = bass.Bass("TRN2", target_bir_lowering=False)
        self.P = Prog(self.nc)
        self.stages = stages
        self.fused = fused
        self.cur_q = 0
        self.scr = {}
        self.inputs = {}
        self.outputs = {}
        P = self.P
        self.A = Arena(P, ARENA_BYTES)
        A = self.A
        self.pb = [P.stack.enter_context(self.nc.psum_tensor(f"pb{i}", [128, 512], F32))[:] for i in range(7)]
        self.pbT = P.stack.enter_context(self.nc.psum_tensor("pbT", [128, 1024], BF16))[:]
        self.tpb = P.toks(7, "pb")
        self.tpbT = P.tok("pbT")
        self.x_f = A.alloc([8, T], F32)
        self.tx = P.toks(NTILE, "x")
        self.vecs = A.alloc([NVEC], F32)
        self.tvec = P.tok("vecs")
        self.ident_b = A.alloc([128], BF16)
        self.ident_f = A.alloc([128], F32)
        self.ones_b = A.alloc([128], BF16)
        self.sel = A.alloc([8], F32)
        self.tconst = P.tok("const")
        P.dma("sp", self.vecs, self.din("vecs", [128, NVEC]), w=[self.tvec])
        P.dma("sp", self.ident_f, self.din("ident_f", [128, 128]), w=[self.tconst])
        P.dma("sp", self.sel, self.din("keep" if fused else "sel", [128, 8]), w=[self.tconst])
        P.dma("pool", self.ident_b, self.inputs["ident_f"], w=[self.tconst])
        P.op("dve", lambda e: e.memset(self.ones_b, 1.0), w=[self.tconst])
        self.base_mark = A.mark()

    def din(self, name, shape, dtype=F32):
        if name not in self.inputs:
            self.inputs[name] = self.nc.dram_tensor(name, list(shape), dtype, kind="ExternalInput").ap()
        return self.inputs[name]

    def dout(self, name, shape, dtype=F32):
        if name not in self.outputs:
            self.outputs[name] = self.nc.dram_tensor(name, list(shape), dtype, kind="ExternalOutput").ap()
        return self.outputs[name]

    def vcol(self, name, i=0, n=1):
        o, _ = VLAY[name]
        return self.vecs[:, o + i:o + i + n]

    def set_pass(self, q):
        self.cur_q = q

    def carry_load(self, name, dst, toks, n):
        P = self.P
        if self.cur_q == 0:
            P.op("dve", lambda e: e.memset(dst, 0.0), w=list(toks))
            return
        sc, tsc = self.scr[name]
        P.dma("sp", dst, sc, r=[tsc], w=list(toks), key=toks[0])
        kq = self.sel[:, self.cur_q:self.cur_q + 1]
        P.op("dve", lambda e: e.tensor_scalar(out=dst, in0=dst, scalar1=kq, scalar2=None, op0=ALU.mult),
             r=[self.tconst], w=list(toks))

    def carry_save(self, name, src, toks, n):
        P = self.P
        if name not in self.scr:
            self.scr[name] = (self.nc.dram_tensor("scr_" + name, [128, n], F32).ap(), P.tok("scr_" + name))
        sc, tsc = self.scr[name]
        P.dma("sp", sc, src, r=list(toks), w=[tsc], key=tsc)

    def dbg(self, name, ap, tok, shape):
        o = self.dout(name, shape)
        self.P.dma("sp", o, ap, r=[tok])

    def phase_end(self):
        self.P.barrier()
        self.P.release_keys()
        self.A.reset(self.base_mark)

    def load_w(self, dst, src, tok):
        self.P.dma("pool", dst, src, w=[tok])

    def proj(self, ps, tps, wslot, tw, cols, xb, txb, n=NT, kcs=8, extra_r=()):
        P = self.P
        for kc in range(kcs):
            P.op("pe", lambda e, kc=kc: e.matmul(ps, lhsT=wslot[:, kc, cols], rhs=xb[:, kc, :],
                                                 start=(kc == 0), stop=(kc == kcs - 1)),
                 r=[tw, txb] + list(extra_r), w=[tps], skip_self=True)

    def load_x(self):
        xT = self.din(f"xT_{self.cur_q}" if self.fused else "xT", [128, 8, T])
        for ti in range(NTILE):
            self.P.dma("sp", self.x_f[:, :, ti * NT:(ti + 1) * NT], xT[:, :, ti * NT:(ti + 1) * NT], w=[self.tx[ti]])

    def store_x(self):
        yT = self.dout("yT", [128, 8, T])
        for ti in range(NTILE):
            self.P.dma("sp", yT[:, :, ti * NT:(ti + 1) * NT], self.x_f[:, :, ti * NT:(ti + 1) * NT], r=[self.tx[ti]])

    def ln_alloc(self):
        A, P = self.A, self.P
        d = dict(mean=A.alloc([NT]), var=A.alloc([NT]), rstd=A.alloc([NT]), nmr=A.alloc([NT]), t=P.tok("lnsm"))
        return d

    def layer_norm(self, z, tz, zb, tzb, zsq, tzsq, sm, gname, bname, out_f, tout, out_b=None, tout_b=None,
                   pbs=(5, 6)):
        P = self.P
        ps_s, ts_s = self.pb[pbs[0]], self.tpb[pbs[0]]
        ps_q, ts_q = self.pb[pbs[1]], self.tpb[pbs[1]]
        P.op("act", lambda e: e.copy(out=zb, in_=z), r=[tz], w=[tzb])
        P.op("act", lambda e: e.activation(out=zsq, in_=z, func=AF.Square), r=[tz], w=[tzsq])
        for kc in range(8):
            P.op("pe", lambda e, kc=kc: e.matmul(ps_s, lhsT=self.ones_b, rhs=zb[:, kc, :], start=(kc == 0), stop=(kc == 7)),
                 r=[tzb, self.tconst], w=[ts_s], skip_self=True)
        for kc in range(8):
            P.op("pe", lambda e, kc=kc: e.matmul(ps_q, lhsT=self.ones_b, rhs=zsq[:, kc, :], start=(kc == 0), stop=(kc == 7)),
                 r=[tzsq, self.tconst], w=[ts_q], skip_self=True)
        mean, var, rstd, nmr, tsm = sm["mean"], sm["var"], sm["rstd"], sm["nmr"], sm["t"]
        epsc, tcc = self._eps, self.tcc
        P.op("act", lambda e: e.mul(out=mean, in_=ps_s, mul=1.0 / D), r=[ts_s], w=[tsm])
        P.op("act", lambda e: e.activation(out=nmr, in_=ps_s, func=AF.Square, scale=1.0 / D), r=[ts_s], w=[tsm])
        P.op("dve", lambda e: e.scalar_tensor_tensor(out=var, in0=ps_q, scalar=1.0 / D, in1=nmr, op0=ALU.mult, op1=ALU.subtract),
             r=[ts_q, tsm], w=[tsm])
        P.op("act", lambda e: e.activation(out=var, in_=var, func=AF.Ln, bias=epsc, scale=1.0), r=[tsm, tcc], w=[tsm])
        P.op("act", lambda e: e.activation(out=rstd, in_=var, func=AF.Exp, scale=-0.5), r=[tsm], w=[tsm])
        P.op("dve", lambda e: e.scalar_tensor_tensor(out=nmr, in0=mean, scalar=-1.0, in1=rstd, op0=ALU.mult, op1=ALU.mult),
             r=[tsm], w=[tsm])
        P.op("dve", lambda e: e.tensor_tensor(out=z, in0=z, in1=rstd.unsqueeze(1).to_broadcast([128, 8, NT]), op=ALU.mult),
             r=[tsm, tz], w=[tz])
        P.op("dve", lambda e: e.tensor_tensor(out=z, in0=z, in1=nmr.unsqueeze(1).to_broadcast([128, 8, NT]), op=ALU.add),
             r=[tsm, tz], w=[tz])
        for kc in range(8):
            P.op("act", lambda e, kc=kc: e.activation(out=out_f[:, kc, :], in_=z[:, kc, :], func=AF.Identity,
                                                      scale=self.vcol(gname, kc), bias=self.vcol(bname, kc)),
                 r=[tz, self.tvec], w=[tout])
        if out_b is not None:
            for kc in range(8):
                P.op("dve", lambda e, kc=kc: e.tensor_scalar(out=out_b[:, kc, :], in0=z[:, kc, :], scalar1=self.vcol(gname, kc),
                                                             scalar2=self.vcol(bname, kc), op0=ALU.mult, op1=ALU.add),
                     r=[tz, self.tvec], w=[tout_b])

    def mixer_out(self, li, ti, m_b, tm, nvc, w_out, sc):
        P = self.P
        z, tz = sc["z"], sc["tz"]
        xt = self.x_f[:, :, ti * NT:(ti + 1) * NT]
        wv = w_out.rearrange("(c p) n -> p c n", p=128)
        sw = 128 if nvc > 8 else 256
        for half in range(1024 // sw):
            slot, tw = sc["wout_ring"].next()
            self.load_w(slot[:, 0:nvc, :], wv[:, :, half * sw:(half + 1) * sw], tw)
            for m2 in range(sw // 128):
                mo = half * (sw // 128) + m2
                pbi = mo % 2
                ps, tps = self.pb[pbi], self.tpb[pbi]
                for vc in range(nvc):
                    P.op("pe", lambda e, vc=vc, m2=m2, ps=ps, slot=slot: e.matmul(
                        ps, lhsT=slot[:, vc, m2 * 128:(m2 + 1) * 128], rhs=m_b[:, vc, :], start=(vc == 0), stop=(vc == nvc - 1)),
                        r=[tw, tm], w=[tps], skip_self=True)
                P.op("dve", lambda e, mo=mo, ps=ps: e.scalar_tensor_tensor(
                    out=z[:, mo, :], in0=xt[:, mo, :], scalar=ALPHA, in1=ps, op0=ALU.mult, op1=ALU.add),
                    r=[tps, self.tx[ti]], w=[tz])
        self.layer_norm(z, tz, sc["zb"], sc["tzb"], sc["zsq"], sc["tzsq"], sc["sm"], f"ln_mix_g{li}", f"ln_mix_b{li}",
                        xt, self.tx[ti])

    def mixer_scratch(self, nvc_max, zb, tzb, zsq, tzsq, sw=256):
        A, P = self.A, self.P
        sc = {}
        sc["z"] = A.alloc([8, NT]); sc["tz"] = P.tok("z")
        sc["zb"] = zb; sc["tzb"] = tzb
        sc["zsq"] = zsq; sc["tzsq"] = tzsq
        sc["sm"] = self.ln_alloc()
        sc["wout_ring"] = Ring(P, [A.alloc([nvc_max, sw], BF16) for _ in range(2)], "wout")
        return sc

    def new_consts(self):
        P = self.P
        c = self.A.alloc([4], F32)
        tc = P.tok("cc")
        P.op("dve", lambda e: e.memset(c[:, 0:1], EPS), w=[tc])
        P.op("dve", lambda e: e.memset(c[:, 1:2], 1.0), w=[tc])
        self._eps = c[:, 0:1]
        self._one = c[:, 1:2]
        self.tcc = tc

    def ffn_phase(self, li):
        P, A = self.P, self.A
        moe = (li % 2 == 1)
        NE = 8 if moe else 1
        F = FFN_EXPERT if moe else FFN_DENSE
        NF = F // 128
        G = 4
        self.new_consts()
        if moe:
            w_in_all = self.din(f"moe_w_in{li // 2}", [8, D, 2 * F])
            w_out_all = self.din(f"moe_w_out{li // 2}", [8, F, D])
            w_r = self.din(f"moe_w_router{li // 2}", [D, 8])
        else:
            w_in_all = self.din(f"dense_w_in{li // 2}", [1, D, 2 * F])
            w_out_all = self.din(f"dense_w_out{li // 2}", [1, F, D])
        wg_d = self.din(f"ple_w_gate{li}", [D, D]).rearrange("(c p) n -> p c n", p=128)
        wp_d = self.din(f"ple_w_proj{li}", [256, D]).rearrange("(c p) n -> p c n", p=128)
        pT = self.din(f"pT{li}_{self.cur_q}" if self.fused else f"pT{li}", [128, 2, T])

        TT = 1024
        xn_b = A.alloc([2, 8, NT], BF16)
        txn = P.toks(2, "xn")
        a_b = A.alloc([G, 2, NT], BF16)
        ta = P.tok("a")
        y = A.alloc([2, 8, NT], F32)
        ty = P.toks(2, "y")
        win_ring = Ring(P, [A.alloc([8, 2, 256], BF16) for _ in range(3)], "win")
        wout_ring = Ring(P, [A.alloc([G, D], BF16) for _ in range(2)], "wo")
        sg_ring = Ring(P, [A.alloc([NT], F32) for _ in range(2)], "sg")
        sm = self.ln_alloc()
        wg_ring = Ring(P, [A.alloc([8, 256], BF16) for _ in range(2)], "wg")
        wp_b = A.alloc([2, D], BF16)
        twp = P.tok("wp")
        p_b = A.alloc([2, NT], BF16)
        tp = P.tok("p")
        tmp_ring = Ring(P, [A.alloc([NT], F32) for _ in range(2)], "tmp")
        self.load_w(wp_b, wp_d, twp)
        if moe:
            wr_f = A.alloc([8, 8], F32)
            twr = P.tok("wr")
            P.dma("sp", wr_f, w_r.rearrange("(c p) n -> p c n", p=128), w=[twr])
            ones8 = A.alloc([128], F32, parts=8)
            P.op("dve", lambda e: e.memset(ones8, 1.0), w=[twr])
            gm = A.alloc([NT], F32, parts=8)
            tgm = P.tok("gm")
            lg_s = A.alloc([NT], F32, parts=8)
            tlg = P.tok("lg")
            lt = A.alloc([4, 8], F32)
            mx = A.alloc([4, 8], F32)
            ex = A.alloc([4, 8], F32)
            msk = A.alloc([4, 8], F32)
            den = A.alloc([4], F32)
            trt = P.tok("rt")
            g_fm = A.alloc([2, NT], F32, parts=8)
            tgf = P.tok("gfm")
            gate_b = A.alloc([2, NT], BF16)
            tgb = P.tok("gb")

        groups = [(f0, min(G, NF - f0)) for f0 in range(0, NF, G)]
        for tt in range(2):
            tiles = [tt * 2, tt * 2 + 1]
            for st in range(2):
                ti = tiles[st]
                P.op("act", lambda e, st=st, ti=ti: e.copy(out=xn_b[:, st], in_=self.x_f[:, :, ti * NT:(ti + 1) * NT]),
                     r=[self.tx[ti]], w=[txn[st]])
            if moe:
                for st in range(2):
                    ti = tiles[st]
                    xt = self.x_f[:, :, ti * NT:(ti + 1) * NT]
                    ps, tps = self.pb[6], self.tpb[6]
                    for kc in range(8):
                        P.op("pe", lambda e, kc=kc, xt=xt: e.matmul(ps[0:8, :], lhsT=wr_f[:, kc, :], rhs=xt[:, kc, :],
                                                                    start=(kc == 0), stop=(kc == 7)),
                             r=[twr, self.tx[ti]], w=[tps], skip_self=True)
                    P.op("act", lambda e: e.copy(out=lg_s, in_=ps[0:8, :]), r=[tps], w=[tlg])
                    pst = self.pb[6][:, 0:32].rearrange("p (a b) -> p a b", a=4)
                    for blk in range(4):
                        P.op("pe", lambda e, blk=blk: e.transpose(out=pst[:, blk, :], in_=lg_s[:, blk * 128:(blk + 1) * 128],
                                                                  identity=self.ident_f[0:8, 0:8]),
                             r=[tlg, self.tconst], w=[tps], skip_self=True)
                    P.op("dve", lambda e: e.tensor_copy(out=lt, in_=pst), r=[tps], w=[trt])
                    for blk in range(4):
                        P.op("dve", lambda e, blk=blk: e.max(out=mx[:, blk, :], in_=lt[:, blk, :]), r=[trt], w=[trt])
                    P.op("dve", lambda e: e.tensor_tensor(out=ex, in0=lt, in1=mx[:, :, 0:1].to_broadcast([128, 4, 8]), op=ALU.subtract),
                         r=[trt], w=[trt])
                    P.op("act", lambda e: e.activation(out=ex, in_=ex, func=AF.Exp), r=[trt], w=[trt])
                    P.op("dve", lambda e: e.tensor_tensor(out=msk, in0=lt, in1=mx[:, :, 1:2].to_broadcast([128, 4, 8]), op=ALU.is_ge),
                         r=[trt], w=[trt])
                    P.op("dve", lambda e: e.tensor_tensor(out=ex, in0=ex, in1=msk, op=ALU.mult), r=[trt], w=[trt])
                    P.op("dve", lambda e: e.tensor_reduce(out=den, in_=ex, axis=AX.X, op=ALU.add), r=[trt], w=[trt])
                    P.op("dve", lambda e: e.reciprocal(out=den, in_=den), r=[trt], w=[trt])
                    P.op("dve", lambda e: e.tensor_tensor(out=ex, in0=ex, in1=den.unsqueeze(2).to_broadcast([128, 4, 8]), op=ALU.mult),
                         r=[trt], w=[trt])
                    for blk in range(4):
                        P.op("pe", lambda e, blk=blk: e.transpose(out=ps[0:8, blk * 128:(blk + 1) * 128], in_=ex[:, blk, :],
                                                                  identity=self.ident_f),
                             r=[trt, self.tconst], w=[tps], skip_self=True)
                    P.op("act", lambda e, st=st: e.copy(out=g_fm[:, st, :], in_=ps[0:8, :]), r=[tps], w=[tgf])
            for ei in range(NE):
                w_in = w_in_all[ei].rearrange("(c p) n -> p c n", p=128)
                w_out = w_out_all[ei]
                if moe:
                    for st in range(2):
                        ps, tps = self.pb[6], self.tpb[6]
                        P.op("dve", lambda e, st=st, ei=ei: e.tensor_scalar(out=gm, in0=g_fm[:, st, :], scalar1=self.ident_f[0:8, ei:ei + 1],
                                                                          scalar2=None, op0=ALU.mult), r=[tgf, self.tconst], w=[tgm])
                        P.op("pe", lambda e: e.matmul(ps, lhsT=ones8, rhs=gm, start=True, stop=True),
                             r=[tgm, twr], w=[tps], skip_self=True)
                        P.op("act", lambda e, st=st: e.copy(out=gate_b[:, st, :], in_=ps), r=[tps], w=[tgb])
                for gi, (f0, g) in enumerate(groups):
                    for pr in range(0, g, 2):
                        npair = min(2, g - pr)
                        slot, tw = win_ring.next()
                        c0 = (f0 + pr) * 128
                        self.load_w(slot[:, :, 0, 0:npair * 128], w_in[:, :, c0:c0 + npair * 128], tw)
                        self.load_w(slot[:, :, 1, 0:npair * 128], w_in[:, :, F + c0:F + c0 + npair * 128], tw)
                        for ff in range(npair):
                            fi = pr + ff
                            for st in range(2):
                                bi = 2 * st
                                psg, tg_ = self.pb[bi], self.tpb[bi]
                                psu, tu_ = self.pb[bi + 1], self.tpb[bi + 1]
                                for kc in range(8):
                                    P.op("pe", lambda e, kc=kc, ff=ff, st=st, psg=psg, slot=slot: e.matmul(
                                        psg, lhsT=slot[:, kc, 0, ff * 128:(ff + 1) * 128], rhs=xn_b[:, st, kc, :],
                                        start=(kc == 0), stop=(kc == 7)), r=[tw, txn[st]], w=[tg_], skip_self=True)
                                for kc in range(8):
                                    P.op("pe", lambda e, kc=kc, ff=ff, st=st, psu=psu, slot=slot: e.matmul(
                                        psu, lhsT=slot[:, kc, 1, ff * 128:(ff + 1) * 128], rhs=xn_b[:, st, kc, :],
                                        start=(kc == 0), stop=(kc == 7)), r=[tw, txn[st]], w=[tu_], skip_self=True)
                                sg, tsg = sg_ring.next()
                                P.op("act", lambda e, sg=sg, psg=psg: e.activation(out=sg, in_=psg, func=AF.Silu), r=[tg_], w=[tsg])
                                if moe:
                                    P.op("dve", lambda e, sg=sg, st=st: e.tensor_tensor(out=sg, in0=sg, in1=gate_b[:, st, :], op=ALU.mult),
                                         r=[tsg, tgb], w=[tsg])
                                P.op("dve", lambda e, sg=sg, psu=psu, fi=fi, st=st: e.tensor_tensor(
                                    out=a_b[:, fi, st, :], in0=sg, in1=psu, op=ALU.mult), r=[tsg, tu_], w=[ta])
                    wslot, two = wout_ring.next()
                    self.load_w(wslot[:, 0:g, :], w_out[f0 * 128:(f0 + g) * 128, :].rearrange("(g p) n -> p g n", p=128), two)
                    first = (ei == 0 and gi == 0)
                    for st in range(2):
                        for mo in range(8):
                            bi = 4 + (mo % 2)
                            ps, tps = self.pb[bi], self.tpb[bi]
                            for fi in range(g):
                                P.op("pe", lambda e, fi=fi, mo=mo, st=st, ps=ps, wslot=wslot: e.matmul(
                                    ps, lhsT=wslot[:, fi, mo * 128:(mo + 1) * 128], rhs=a_b[:, fi, st, :],
                                    start=(fi == 0), stop=(fi == g - 1)), r=[two, ta], w=[tps], skip_self=True)
                            if first:
                                P.op("act", lambda e, mo=mo, st=st, ps=ps: e.copy(out=y[:, st, mo, :], in_=ps), r=[tps], w=[ty[st]])
                            else:
                                P.op("dve", lambda e, mo=mo, st=st, ps=ps: e.tensor_tensor(
                                    out=y[:, st, mo, :], in0=y[:, st, mo, :], in1=ps, op=ALU.add), r=[tps, ty[st]], w=[ty[st]])
            for st in range(2):
                ti = tiles[st]
                xt = self.x_f[:, :, ti * NT:(ti + 1) * NT]
                yz = y[:, st]
                P.op("dve", lambda e, yz=yz, xt=xt: e.scalar_tensor_tensor(out=yz, in0=xt, scalar=ALPHA, in1=yz,
                                                                           op0=ALU.mult, op1=ALU.add),
                     r=[self.tx[ti], ty[st]], w=[ty[st]])
                zb, zsq = xn_b[:, 0], xn_b[:, 1]
                self.layer_norm(yz, ty[st], zb, txn[0], zsq, txn[1], sm, f"ln_ffn_g{li}", f"ln_ffn_b{li}",
                                xt, self.tx[ti], out_b=zb, tout_b=txn[0], pbs=(5, 6))
                xb = zb
                self.load_w(p_b, pT[:, :, ti * NT:(ti + 1) * NT], tp)
                for q4 in range(4):
                    wgs, twg = wg_ring.next()
                    self.load_w(wgs, wg_d[:, :, q4 * 256:(q4 + 1) * 256], twg)
                    for m2 in range(2):
                        mo = q4 * 2 + m2
                        psg, tg_ = self.pb[0 + 2 * m2], self.tpb[0 + 2 * m2]
                        psp, tp_ = self.pb[1 + 2 * m2], self.tpb[1 + 2 * m2]
                        for kc in range(8):
                            P.op("pe", lambda e, kc=kc, m2=m2, psg=psg, wgs=wgs: e.matmul(
                                psg, lhsT=wgs[:, kc, m2 * 128:(m2 + 1) * 128], rhs=xb[:, kc, :], start=(kc == 0), stop=(kc == 7)),
                                r=[twg, txn[0]], w=[tg_], skip_self=True)
                        for k2 in range(2):
                            P.op("pe", lambda e, k2=k2, mo=mo, psp=psp: e.matmul(
                                psp, lhsT=wp_b[:, k2, mo * 128:(mo + 1) * 128], rhs=p_b[:, k2, :], start=(k2 == 0), stop=(k2 == 1)),
                                r=[twp, tp], w=[tp_], skip_self=True)
                        sg, tsg = sg_ring.next()
                        P.op("act", lambda e, sg=sg, psg=psg: e.activation(out=sg, in_=psg, func=AF.Sigmoid), r=[tg_], w=[tsg])
                        tmp, ttmp = tmp_ring.next()
                        P.op("dve", lambda e, sg=sg, psp=psp, tmp=tmp: e.tensor_tensor(out=tmp, in0=sg, in1=psp, op=ALU.mult),
                             r=[tsg, tp_], w=[ttmp])
                        P.op("dve", lambda e, mo=mo, xt=xt, tmp=tmp: e.tensor_tensor(out=xt[:, mo, :], in0=xt[:, mo, :], in1=tmp, op=ALU.add),
                             r=[ttmp, self.tx[ti]], w=[self.tx[ti]])
        self.phase_end()

    def exchange_out(self, li, src, tsrc, W):
        st = self.dout(f"st_loc{li}", [128, W])
        self.P.dma("sp", st, src, r=[tsrc])

    def rglru_pass(self, mode):
        P, A = self.P, self.A
        li = 0
        self.new_consts()
        w_in = self.din("rg_w_in", [D, 2 * D]).rearrange("(c p) n -> p c n", p=128)
        w_a = self.din("rg_w_a", [4, 256, 256])
        w_x = self.din("rg_w_x", [4, 256, 256])
        x_b = A.alloc([8, NT], BF16); txb = P.tok("xb")
        if not self.fused:
            xh = self.din("xh", [128, 8, 4])
            xh_b = A.alloc([8, 4], BF16); txh = P.tok("xh")
            self.load_w(xh_b, xh, txh)
        wa_b = A.alloc([4, 2, 256], BF16); wx_b = A.alloc([4, 2, 256], BF16); twax = P.tok("wax")
        for n in range(4):
            self.load_w(wa_b[:, n], w_a[n].rearrange("(c p) n -> p c n", p=128), twax)
            self.load_w(wx_b[:, n], w_x[n].rearrange("(c p) n -> p c n", p=128), twax)
        w_ring = Ring(P, [A.alloc([8, 256], BF16) for _ in range(3)], "wi")
        rec = A.alloc([2, NT + 3], F32); trec = P.tok("rec")
        halo = A.alloc([8, 3], F32); thalo = P.tok("halo")
        u = A.alloc([2, NT], F32); tu = P.tok("u")
        u_b = A.alloc([2, NT], BF16); tub = P.tok("ub")
        r_s = A.alloc([NT], F32); i_s = A.alloc([NT], F32); a_s = A.alloc([NT], F32); q_s = A.alloc([NT], F32)
        h_s = A.alloc([NT], F32)
        tg = P.tok("gates"); th = P.tok("h")
        hst = A.alloc([8], F32); thst = P.tok("hst")
        cl = A.alloc([8], F32); cl2 = A.alloc([8], F32); tcl = P.tok("cl")
        lam = self.vcol("rg_lambda", 0, 8)
        one_c = self._one
        P.op("act", lambda e: e.activation(out=cl, in_=lam, func=AF.Exp, scale=-1.0), r=[self.tvec], w=[tcl])
        P.op("act", lambda e: e.activation(out=cl, in_=cl, func=AF.Ln, bias=one_c.to_broadcast([128, 8]) if False else one_c, scale=1.0), r=[tcl, self.tcc], w=[tcl])
        P.op("dve", lambda e: e.tensor_scalar(out=cl2, in0=cl, scalar1=-16.0, scalar2=None, op0=ALU.mult), r=[tcl], w=[tcl])
        P.op("dve", lambda e: e.tensor_scalar(out=cl, in0=cl, scalar1=-8.0, scalar2=None, op0=ALU.mult), r=[tcl], w=[tcl])
        if mode == "A":
            P.op("dve", lambda e: e.memset(hst, 0.0), w=[thst])
            ptot = A.alloc([8], F32)
            P.op("dve", lambda e: e.memset(ptot, 1.0), w=[thst])
            pt_s = A.alloc([NT], F32)
            zeros = A.alloc([NT], F32)
            P.op("dve", lambda e: e.memset(zeros, 0.0), w=[thst])
        elif self.fused:
            self.carry_load("rg_h", hst, [thst], 8)
            self.carry_load("rg_halo", halo.rearrange("p a b -> p (a b)"), [thalo], 24)
            m_b = A.alloc([8, NT], BF16); tm = P.tok("m")
            gb = A.alloc([NT], F32); g2 = A.alloc([NT], F32); tgb = P.tok("gb")
            sc = self.mixer_scratch(8, x_b, txb, m_b, tm)
            w_out = self.din("rg_w_out", [D, D])
        else:
            st_all = self.din("st_all0", [8, 128, 16])
            sta = A.alloc([8, 16], F32); tsta = P.tok("sta")
            P.dma("sp", sta, st_all.rearrange("r p w -> p r w"), w=[tsta])
            P.op("dve", lambda e: e.memset(hst, 0.0), w=[thst])
            dsel = A.alloc([8], F32); hl = A.alloc([8], F32)
            for r in range(8):
                sr = self.sel[:, r:r + 1]
                P.op("dve", lambda e, r=r, sr=sr: e.tensor_scalar(out=dsel, in0=sta[:, r, 8:16], scalar1=-1.0, scalar2=sr,
                                                                 op0=ALU.add, op1=ALU.mult), r=[tsta, self.tconst], w=[thst])
                P.op("dve", lambda e: e.tensor_scalar(out=dsel, in0=dsel, scalar1=1.0, scalar2=None, op0=ALU.add), r=[thst], w=[thst])
                P.op("dve", lambda e, r=r, sr=sr: e.tensor_scalar(out=hl, in0=sta[:, r, 0:8], scalar1=sr, scalar2=None, op0=ALU.mult),
                     r=[tsta, self.tconst], w=[thst])
                P.op("dve", lambda e: e.tensor_tensor(out=hst, in0=hst, in1=dsel, op=ALU.mult), r=[thst], w=[thst])
                P.op("dve", lambda e: e.tensor_tensor(out=hst, in0=hst, in1=hl, op=ALU.add), r=[thst], w=[thst])
            m_b = A.alloc([8, NT], BF16); tm = P.tok("m")
            gb = A.alloc([NT], F32); g2 = A.alloc([NT], F32); tgb = P.tok("gb")
            sc = self.mixer_scratch(8, x_b, txb, m_b, tm)
            w_out = self.din("rg_w_out", [D, D])
        for n in range(4 if not self.fused else 0):
            slot, tw = w_ring.next()
            self.load_w(slot, w_in[:, :, D + n * 256:D + (n + 1) * 256], tw)
            for c2 in range(2):
                cc = 2 * n + c2
                ps, tps = self.pb[c2], self.tpb[c2]
                self.proj(ps[:, 0:4], tps, slot, tw, slice(c2 * 128, (c2 + 1) * 128), xh_b, txh)
                P.op("act", lambda e, cc=cc, ps=ps: e.copy(out=halo[:, cc, :], in_=ps[:, 0:3]), r=[tps], w=[thalo])
        for ti in range(NTILE):
            xt = self.x_f[:, :, ti * NT:(ti + 1) * NT]
            P.op("act", lambda e, xt=xt: e.copy(out=x_b, in_=xt), r=[self.tx[ti]], w=[txb])
            for n in range(4):
                slot, tw = w_ring.next()
                self.load_w(slot, w_in[:, :, D + n * 256:D + (n + 1) * 256], tw)
                if mode == "B":
                    gslot, tgw = w_ring.next()
                    self.load_w(gslot, w_in[:, :, n * 256:(n + 1) * 256], tgw)
                for c2 in range(2):
                    cc = 2 * n + c2
                    ps, tps = self.pb[c2], self.tpb[c2]
                    self.proj(ps, tps, slot, tw, slice(c2 * 128, (c2 + 1) * 128), x_b, txb)
                    P.op("dve", lambda e, cc=cc, c2=c2: e.tensor_copy(out=rec[:, c2, 0:3], in_=halo[:, cc, :]), r=[thalo], w=[trec])
                    P.op("act", lambda e, c2=c2, ps=ps: e.copy(out=rec[:, c2, 3:NT + 3], in_=ps), r=[tps], w=[trec])
                    P.op("dve", lambda e, cc=cc, c2=c2: e.tensor_copy(out=halo[:, cc, :], in_=rec[:, c2, NT:NT + 3]), r=[trec], w=[thalo])
                    P.op("act", lambda e, cc=cc, c2=c2: e.activation(out=u[:, c2, :], in_=rec[:, c2, 0:NT], func=AF.Identity,
                                                                     scale=self.vcol("rg_conv_w0", cc), bias=self.vcol("rg_conv_b", cc)),
                         r=[trec, self.tvec], w=[tu])
                    for j in range(1, 4):
                        P.op("dve", lambda e, cc=cc, c2=c2, j=j: e.scalar_tensor_tensor(
                            out=u[:, c2, :], in0=rec[:, c2, j:j + NT], scalar=self.vcol(f"rg_conv_w{j}", cc), in1=u[:, c2, :],
                            op0=ALU.mult, op1=ALU.add), r=[trec, tu, self.tvec], w=[tu])
                    P.op("act", lambda e, c2=c2: e.copy(out=u_b[:, c2, :], in_=u[:, c2, :]), r=[tu], w=[tub])
                for c2 in range(2):
                    cc = 2 * n + c2
                    psr, tpr = self.pb[2], self.tpb[2]
                    psi, tpi = self.pb[3], self.tpb[3]
                    for k2 in range(2):
                        P.op("pe", lambda e, k2=k2, c2=c2, n=n: e.matmul(psr, lhsT=wa_b[:, n, k2, c2 * 128:(c2 + 1) * 128], rhs=u_b[:, k2, :],
                                                                         start=(k2 == 0), stop=(k2 == 1)), r=[twax, tub], w=[tpr], skip_self=True)
                    for k2 in range(2):
                        P.op("pe", lambda e, k2=k2, c2=c2, n=n: e.matmul(psi, lhsT=wx_b[:, n, k2, c2 * 128:(c2 + 1) * 128], rhs=u_b[:, k2, :],
                                                                         start=(k2 == 0), stop=(k2 == 1)), r=[twax, tub], w=[tpi], skip_self=True)
                    P.op("act", lambda e, cc=cc: e.activation(out=r_s, in_=psr, func=AF.Sigmoid, bias=self.vcol("rg_b_a", cc), scale=1.0),
                         r=[tpr, self.tvec], w=[tg])
                    P.op("act", lambda e, cc=cc: e.activation(out=i_s, in_=psi, func=AF.Sigmoid, bias=self.vcol("rg_b_x", cc), scale=1.0),
                         r=[tpi, self.tvec], w=[tg])
                    P.op("act", lambda e, cc=cc: e.activation(out=a_s, in_=r_s, func=AF.Exp, scale=cl[:, cc:cc + 1]), r=[tg, tcl], w=[tg])
                    P.op("act", lambda e, cc=cc: e.activation(out=q_s, in_=r_s, func=AF.Exp, scale=cl2[:, cc:cc + 1]), r=[tg, tcl], w=[tg])
                    P.op("act", lambda e: e.activation(out=q_s, in_=q_s, func=AF.Sqrt, bias=one_c, scale=-1.0), r=[tg, self.tcc], w=[tg])
                    P.op("dve", lambda e: e.tensor_tensor(out=q_s, in0=q_s, in1=i_s, op=ALU.mult), r=[tg], w=[tg])
                    P.op("dve", lambda e, c2=c2: e.tensor_tensor(out=q_s, in0=q_s, in1=u[:, c2, :], op=ALU.mult), r=[tg, tu], w=[tg])
                    P.op("dve", lambda e, cc=cc: e.tensor_tensor_scan(out=h_s, data0=a_s, data1=q_s, initial=hst[:, cc:cc + 1],
                                                                      op0=ALU.mult, op1=ALU.add), r=[tg, thst], w=[th])
                    P.op("dve", lambda e, cc=cc: e.tensor_copy(out=hst[:, cc:cc + 1], in_=h_s[:, NT - 1:NT]), r=[th], w=[thst])
                    if mode == "A":
                        P.op("dve", lambda e, cc=cc: e.tensor_tensor_scan(out=pt_s, data0=a_s, data1=zeros, initial=ptot[:, cc:cc + 1],
                                                                          op0=ALU.mult, op1=ALU.add), r=[tg, thst, self.tconst], w=[th])
                        P.op("dve", lambda e, cc=cc: e.tensor_copy(out=ptot[:, cc:cc + 1], in_=pt_s[:, NT - 1:NT]), r=[th], w=[thst])
                    else:
                        psg, tpg = self.pb[4 + c2], self.tpb[4 + c2]
                        self.proj(psg, tpg, gslot, tgw, slice(c2 * 128, (c2 + 1) * 128), x_b, txb)
                        P.op("act", lambda e, psg=psg: e.activation(out=g2, in_=psg, func=AF.Square), r=[tpg], w=[tgb])
                        P.op("dve", lambda e: e.tensor_scalar(out=g2, in0=g2, scalar1=0.044715, scalar2=1.0, op0=ALU.mult, op1=ALU.add),
                             r=[tgb], w=[tgb])
                        P.op("dve", lambda e, psg=psg: e.tensor_tensor(out=g2, in0=g2, in1=psg, op=ALU.mult), r=[tgb, tpg], w=[tgb])
                        P.op("act", lambda e: e.activation(out=g2, in_=g2, func=AF.Sigmoid, scale=1.5957691216057308), r=[tgb], w=[tgb])
                        P.op("dve", lambda e, psg=psg: e.tensor_tensor(out=gb, in0=g2, in1=psg, op=ALU.mult), r=[tgb, tpg], w=[tgb])
                        P.op("dve", lambda e, cc=cc: e.tensor_tensor(out=m_b[:, cc, :], in0=gb, in1=h_s, op=ALU.mult), r=[tgb, th], w=[tm])
            if mode == "B":
                self.mixer_out(li, ti, m_b, tm, 8, w_out, sc)
        if mode == "A":
            stl = A.alloc([16], F32)
            P.op("dve", lambda e: e.tensor_copy(out=stl[:, 0:8], in_=hst), r=[thst], w=[th])
            P.op("dve", lambda e: e.tensor_copy(out=stl[:, 8:16], in_=ptot), r=[thst], w=[th])
            self.exchange_out(0, stl, th, 16)
        if self.fused:
            self.carry_save("rg_h", hst, [thst], 8)
            self.carry_save("rg_halo", halo.rearrange("p a b -> p (a b)"), [thalo], 24)
        self.phase_end()

    def gla_alloc(self, H, dv, mode):
        A, P = self.A, self.P
        c = dict(H=H, dv=dv, dvc=dv // 128, mode=mode)
        c["reset"] = A.alloc([NT], F32)
        c["maskT"] = A.alloc([128], F32)
        c["rowmask"] = A.alloc([4], F32)
        c["ttab"] = P.tok("gtab")
        P.dma("sp", c["reset"], self.din("tab_reset", [128, NT]), w=[c["ttab"]])
        P.dma("sp", c["maskT"], self.din("tab_maskT", [128, 128]), w=[c["ttab"]])
        P.dma("sp", c["rowmask"], self.din("tab_rowmask", [128, 4]), w=[c["ttab"]])
        c["S"] = A.alloc([H, dv], F32); c["tS"] = P.toks(H, "S")
        c["slg"] = A.alloc([H], F32); c["tslg"] = P.tok("slg")
        c["cum"] = A.alloc([NT], F32); c["E"] = A.alloc([NT], F32); c["E2"] = A.alloc([NT], F32)
        c["tcum"] = P.tok("cum"); c["tE"] = P.tok("E"); c["tE2"] = P.tok("E2")
        c["kd_b"] = A.alloc([NT], BF16); c["tkd"] = P.tok("kd")
        c["kdm"] = A.alloc([4, 4, 128], BF16); c["tkdm"] = P.toks(4, "kdm")
        c["dec"] = A.alloc([16], F32); c["tdec"] = P.tok("dec")
        c["red"] = A.alloc([1], F32)
        if mode == "B":
            c["qe_b"] = A.alloc([NT], BF16); c["ke_b"] = A.alloc([NT], BF16); c["qi_f"] = A.alloc([NT], F32)
            c["tqk"] = P.tok("qk")
            c["PT"] = Ring(P, [A.alloc([128], BF16) for _ in range(2)], "PT")
            c["o_s"] = A.alloc([c["dvc"], NT], F32); c["to"] = P.tok("o")
            c["osq"] = A.alloc([c["dvc"], NT], BF16); c["tosq"] = P.tok("osq")
            c["rn"] = A.alloc([NT], F32); c["trn"] = P.tok("rn")
        return c

    def gla_init_state(self, c, li):
        P, A = self.P, self.A
        H, dv = c["H"], c["dv"]
        W = H * dv + H
        c["W"] = W
        S, tS = c["S"], c["tS"]
        P.op("dve", lambda e: e.memset(c["slg"], 0.0), w=[c["tslg"]])
        if self.fused:
            self.carry_load(f"S{li}", S.rearrange("p a b -> p (a b)"), tS, H * dv)
            return
        for h in range(H):
            P.op("dve", lambda e, h=h: e.memset(S[:, h, :], 0.0), w=[tS[h]])
        if c["mode"] == "A":
            return
        st_all = self.din(f"st_all{li}", [8, 128, W])
        m = A.mark()
        sta = A.alloc([W], F32); tsta = P.tok("sta")
        dsel = A.alloc([H], F32); tds = P.tok("dsel")
        for r in range(8):
            sr = self.sel[:, r:r + 1]
            P.dma("sp", sta, st_all[r], w=[tsta])
            P.op("act", lambda e: e.activation(out=dsel, in_=sta[:, H * dv:H * dv + H], func=AF.Exp), r=[tsta], w=[tds])
            P.op("dve", lambda e, sr=sr: e.tensor_scalar(out=dsel, in0=dsel, scalar1=-1.0, scalar2=sr, op0=ALU.add, op1=ALU.mult),
                 r=[tds, self.tconst], w=[tds])
            P.op("dve", lambda e: e.tensor_scalar(out=dsel, in0=dsel, scalar1=1.0, scalar2=None, op0=ALU.add), r=[tds], w=[tds])
            P.op("dve", lambda e, sr=sr: e.tensor_scalar(out=sta[:, 0:H * dv], in0=sta[:, 0:H * dv], scalar1=sr, scalar2=None, op0=ALU.mult),
                 r=[tsta, self.tconst], w=[tsta])
            for h in range(H):
                P.op("dve", lambda e, h=h: e.scalar_tensor_tensor(out=S[:, h, :], in0=S[:, h, :], scalar=dsel[:, h:h + 1],
                                                                  in1=sta[:, h * dv:(h + 1) * dv], op0=ALU.mult, op1=ALU.add),
                     r=[tds, tsta, tS[h]], w=[tS[h]])
        P.barrier()
        A.reset(m)

    def gla_finish(self, c, li):
        if self.fused:
            self.carry_save(f"S{li}", c["S"].rearrange("p a b -> p (a b)"), c["tS"], c["H"] * c["dv"])
        elif c["mode"] == "A":
            self.gla_finish_A(c, li)

    def gla_finish_A(self, c, li):
        P, A = self.P, self.A
        H, dv = c["H"], c["dv"]
        stl = A.alloc([H * dv + H], F32); tst = P.tok("stl")
        for h in range(H):
            P.op("dve", lambda e, h=h: e.tensor_copy(out=stl[:, h * dv:(h + 1) * dv], in_=c["S"][:, h, :]), r=[c["tS"][h]], w=[tst])
        P.op("dve", lambda e: e.tensor_copy(out=stl[:, H * dv:H * dv + H], in_=c["slg"]), r=[c["tslg"]], w=[tst])
        self.exchange_out(li, stl, tst, H * dv + H)

    def gla_head(self, c, h, q_f, k_f, lg_f, tin, v_tok, tv):
        P = self.P
        mode, dv, dvc = c["mode"], c["dv"], c["dvc"]
        cum, E, E2 = c["cum"], c["E"], c["E2"]
        tcum, tE, tE2 = c["tcum"], c["tE"], c["tE2"]
        S, tS = c["S"][:, h, :], c["tS"][h]
        P.op("dve", lambda e: e.tensor_tensor_scan(out=cum, data0=c["reset"], data1=lg_f, initial=0.0, op0=ALU.mult, op1=ALU.add),
             r=[tin, c["ttab"]], w=[tcum])
        P.op("dve", lambda e: e.tensor_reduce(out=c["red"], in_=lg_f, axis=AX.X, op=ALU.add), r=[tin], w=[tE2])
        P.op("dve", lambda e: e.tensor_tensor(out=c["slg"][:, h:h + 1], in0=c["slg"][:, h:h + 1], in1=c["red"], op=ALU.add),
             r=[tE2, c["tslg"]], w=[c["tslg"]])
        cum3 = cum.rearrange("p (c t) -> p c t", t=32)
        E3 = E.rearrange("p (c t) -> p c t", t=32)
        lastb = cum3[:, :, 31:32].to_broadcast([128, 16, 32])
        refb = cum3[:, :, 16:17].to_broadcast([128, 16, 32])
        P.op("dve", lambda e: e.tensor_tensor(out=E3, in0=lastb, in1=cum3, op=ALU.subtract), r=[tcum], w=[tE])
        P.op("act", lambda e: e.activation(out=E, in_=E, func=AF.Exp), r=[tE], w=[tE])
        P.op("dve", lambda e: e.tensor_tensor(out=c["kd_b"], in0=k_f, in1=E, op=ALU.mult), r=[tE, tin], w=[c["tkd"]])
        P.op("act", lambda e: e.activation(out=c["dec"], in_=cum3[:, :, 31], func=AF.Exp), r=[tcum], w=[c["tdec"]])
        pT4 = self.pbT[:, 0:512].rearrange("p (b d) -> p b d", b=4)
        for blk in range(4):
            P.op("pe", lambda e, blk=blk: e.transpose(out=pT4[:, blk, :], in_=c["kd_b"][:, blk * 128:(blk + 1) * 128], identity=self.ident_b),
                 r=[c["tkd"], self.tconst], w=[self.tpbT], skip_self=True)
        for blk in range(4):
            P.op("dve", lambda e, blk=blk: e.tensor_tensor(
                out=c["kdm"][:, blk], in0=pT4[:, blk, :].unsqueeze(1).to_broadcast([128, 4, 128]),
                in1=c["rowmask"].unsqueeze(2).to_broadcast([128, 4, 128]), op=ALU.mult),
                r=[self.tpbT, c["ttab"]], w=[c["tkdm"][blk]])
        if mode == "B":
            qe_b, ke_b, qi_f, tqk = c["qe_b"], c["ke_b"], c["qi_f"], c["tqk"]
            E23 = E2.rearrange("p (c t) -> p c t", t=32)
            P.op("act", lambda e: e.activation(out=qi_f, in_=cum, func=AF.Exp), r=[tcum], w=[tqk])
            P.op("dve", lambda e: e.tensor_tensor(out=qi_f, in0=qi_f, in1=q_f, op=ALU.mult), r=[tqk, tin], w=[tqk])
            P.op("dve", lambda e: e.tensor_tensor(out=E23, in0=cum3, in1=refb, op=ALU.subtract), r=[tcum], w=[tE2])
            P.op("act", lambda e: e.activation(out=E, in_=E2, func=AF.Exp), r=[tE2, c["tkd"]], w=[tE])
            P.op("dve", lambda e: e.tensor_tensor(out=qe_b, in0=q_f, in1=E, op=ALU.mult), r=[tE, tin], w=[tqk])
            P.op("act", lambda e: e.activation(out=E2, in_=E2, func=AF.Exp, scale=-1.0), r=[tE2], w=[tE2])
            P.op("dve", lambda e: e.tensor_tensor(out=ke_b, in0=k_f, in1=E2, op=ALU.mult), r=[tE2, tin], w=[tqk])
        for blk in range(4):
            bs = slice(blk * 128, (blk + 1) * 128)
            if mode == "B":
                pss, tss = self.pb[2], self.tpb[2]
                P.op("pe", lambda e, bs=bs: e.matmul(pss[:, 0:128], lhsT=c["ke_b"][:, bs], rhs=c["qe_b"][:, bs], start=True, stop=True),
                     r=[c["tqk"]], w=[tss], skip_self=True)
                PT, tPT = c["PT"].next()
                P.op("dve", lambda e, PT=PT: e.tensor_tensor(out=PT, in0=pss[:, 0:128], in1=c["maskT"], op=ALU.mult),
                     r=[tss, c["ttab"]], w=[tPT])
                pso, tpo = self.pb[3 + blk % 2], self.tpb[3 + blk % 2]
                pso3 = pso[:, 0:dvc * 128].rearrange("p (a b) -> p a b", a=dvc)
                for ec in range(dvc):
                    P.op("pe", lambda e, ec=ec, blk=blk, PT=PT, pso3=pso3: e.matmul(
                        pso3[:, ec, :], lhsT=v_tok[:, blk, ec * 128:(ec + 1) * 128], rhs=PT, start=(ec == 0), stop=False, skip_group_check=True),
                        r=[tv, tPT], w=[tpo], skip_self=True)
            for i in range(4):
                ch = blk * 4 + i
                if mode == "B":
                    for ec in range(dvc):
                        P.op("pe", lambda e, ec=ec, i=i, ch=ch, pso3=pso3: e.matmul(
                            pso3[:, ec, i * 32:(i + 1) * 32], lhsT=S[:, ec * 128:(ec + 1) * 128], rhs=c["qi_f"][:, ch * 32:(ch + 1) * 32],
                            start=False, stop=(i == 3), skip_group_check=True),
                            r=[tS, c["tqk"]], w=[tpo], skip_self=True)
                psS, tpS = self.pb[5 + ch % 2], self.tpb[5 + ch % 2]
                P.op("pe", lambda e, blk=blk, i=i, psS=psS: e.matmul(psS[:, 0:dv], lhsT=c["kdm"][:, blk, i, :], rhs=v_tok[:, blk, :],
                                                                     start=True, stop=True),
                     r=[c["tkdm"][blk], tv], w=[tpS], skip_self=True)
                P.op("dve", lambda e, ch=ch, psS=psS: e.scalar_tensor_tensor(out=S, in0=S, scalar=c["dec"][:, ch:ch + 1], in1=psS[:, 0:dv],
                                                                           op0=ALU.mult, op1=ALU.add),
                     r=[tpS, c["tdec"], tS], w=[tS])
            if mode == "B":
                P.op("act", lambda e, bs=bs, pso3=pso3: e.copy(out=c["o_s"][:, :, bs], in_=pso3), r=[tpo], w=[c["to"]])

    def head_rms_gate(self, c, ps_g_list, m_b, tm, vc0):
        P = self.P
        dv, dvc = c["dv"], c["dvc"]
        o_s, to, osq, tosq, rn, trn = c["o_s"], c["to"], c["osq"], c["tosq"], c["rn"], c["trn"]
        epsc, tcc = self._eps, self.tcc
        P.op("act", lambda e: e.activation(out=osq, in_=o_s, func=AF.Square), r=[to], w=[tosq])
        psn, tpn = self.pb[2], self.tpb[2]
        for ec in range(dvc):
            P.op("pe", lambda e, ec=ec: e.matmul(psn, lhsT=self.ones_b, rhs=osq[:, ec, :], start=(ec == 0), stop=(ec == dvc - 1)),
                 r=[tosq, self.tconst], w=[tpn], skip_self=True)
        P.op("act", lambda e: e.activation(out=rn, in_=psn, func=AF.Ln, bias=epsc, scale=1.0 / dv), r=[tpn, tcc], w=[trn])
        P.op("act", lambda e: e.activation(out=rn, in_=rn, func=AF.Exp, scale=-0.5), r=[trn], w=[trn])
        for ec in range(dvc):
            psg, tpg = ps_g_list[ec]
            P.op("dve", lambda e, ec=ec: e.tensor_tensor(out=o_s[:, ec, :], in0=o_s[:, ec, :], in1=rn, op=ALU.mult), r=[trn, to], w=[to])
            sgt = c["E"]
            P.op("act", lambda e, psg=psg: e.activation(out=sgt, in_=psg, func=AF.Silu), r=[tpg, c["tkd"]], w=[c["tE"]])
            P.op("dve", lambda e, ec=ec: e.tensor_tensor(out=m_b[:, vc0 + ec, :], in0=o_s[:, ec, :], in1=sgt, op=ALU.mult),
                 r=[to, c["tE"]], w=[tm])

    def hgrn2_pass(self, mode):
        P, A = self.P, self.A
        li = 1
        self.new_consts()
        one_c = self._one
        w_in = self.din("hg_w_in", [D, 4 * D]).rearrange("(c p) n -> p c n", p=128)
        c = self.gla_alloc(8, 128, mode)
        lb = A.alloc([8], F32); oml = A.alloc([8], F32); tlb = P.tok("lb")
        et = A.alloc([4, 8], F32)
        for k in range(4):
            P.op("act", lambda e, k=k: e.activation(out=et[:, k, :], in_=self.vcol(f"hg_lb{k}", 0, 8), func=AF.Exp), r=[self.tvec], w=[tlb])
        P.op("dve", lambda e: e.tensor_tensor(out=oml, in0=et[:, 0, :], in1=et[:, 1, :], op=ALU.add), r=[tlb], w=[tlb])
        P.op("dve", lambda e: e.tensor_tensor(out=oml, in0=oml, in1=et[:, 2, :], op=ALU.add), r=[tlb], w=[tlb])
        P.op("dve", lambda e: e.tensor_tensor(out=oml, in0=oml, in1=et[:, 3, :], op=ALU.add), r=[tlb], w=[tlb])
        P.op("dve", lambda e: e.reciprocal(out=oml, in_=oml), r=[tlb], w=[tlb])
        P.op("dve", lambda e: e.tensor_copy(out=lb, in_=et[:, 1, :]), r=[tlb], w=[tlb])
        for k in range(2, li + 1):
            P.op("dve", lambda e, k=k: e.tensor_tensor(out=lb, in0=lb, in1=et[:, k, :], op=ALU.add), r=[tlb], w=[tlb])
        P.op("dve", lambda e: e.tensor_tensor(out=lb, in0=lb, in1=oml, op=ALU.mult), r=[tlb], w=[tlb])
        P.op("dve", lambda e: e.tensor_scalar(out=oml, in0=lb, scalar1=-1.0, scalar2=1.0, op0=ALU.mult, op1=ALU.add), r=[tlb], w=[tlb])
        self.gla_init_state(c, li)
        x_b = A.alloc([8, NT], BF16); txb = P.tok("xb")
        wv_ring = Ring(P, [A.alloc([8, 256], BF16) for _ in range(2)], "wv")
        v_tok = A.alloc([4, D], BF16); tv = P.tok("v")
        ncol = 3 if mode == "B" else 1
        wh_ring = Ring(P, [A.alloc([8, ncol, 256], BF16) for _ in range(2)], "wh")
        q_f = A.alloc([NT], F32); k_f = A.alloc([NT], F32); lg_f = A.alloc([NT], F32); tin = P.tok("qkl")
        if mode == "B":
            m_b = A.alloc([8, NT], BF16); tm = P.tok("m")
            sc = self.mixer_scratch(8, x_b, txb, m_b, tm)
            w_out = self.din("hg_w_out", [D, D])
        for ti in range(NTILE):
            xt = self.x_f[:, :, ti * NT:(ti + 1) * NT]
            P.op("act", lambda e, xt=xt: e.copy(out=x_b, in_=xt), r=[self.tx[ti]], w=[txb])
            for qt in range(4):
                ws, tw = wv_ring.next()
                self.load_w(ws, w_in[:, :, 2048 + qt * 256:2048 + (qt + 1) * 256], tw)
                for blk in range(4):
                    ps, tps = self.pb[blk % 2], self.tpb[blk % 2]
                    for kc in range(8):
                        P.op("pe", lambda e, kc=kc, blk=blk, ps=ps, ws=ws: e.matmul(
                            ps[:, 0:256], lhsT=x_b[:, kc, blk * 128:(blk + 1) * 128], rhs=ws[:, kc, :], start=(kc == 0), stop=(kc == 7)),
                            r=[txb, tw], w=[tps], skip_self=True)
                    P.op("act", lambda e, blk=blk, qt=qt, ps=ps: e.copy(out=v_tok[:, blk, qt * 256:(qt + 1) * 256], in_=ps[:, 0:256]),
                         r=[tps], w=[tv])
            for hp in range(4):
                ws, tw = wh_ring.next()
                self.load_w(ws[:, :, 0, :], w_in[:, :, 1024 + hp * 256:1024 + (hp + 1) * 256], tw)
                if mode == "B":
                    self.load_w(ws[:, :, 1, :], w_in[:, :, hp * 256:(hp + 1) * 256], tw)
                    self.load_w(ws[:, :, 2, :], w_in[:, :, 3072 + hp * 256:3072 + (hp + 1) * 256], tw)
                for h2 in range(2):
                    h = hp * 2 + h2
                    cs = slice(h2 * 128, (h2 + 1) * 128)
                    psf, tpf = self.pb[0], self.tpb[0]
                    self.proj(psf, tpf, ws[:, :, 0, :], tw, cs, x_b, txb)
                    P.op("act", lambda e: e.activation(out=k_f, in_=psf, func=AF.Sigmoid), r=[tpf], w=[tin])
                    P.op("act", lambda e, h=h: e.activation(out=k_f, in_=k_f, func=AF.Identity, scale=oml[:, h:h + 1], bias=lb[:, h:h + 1]),
                         r=[tin, tlb], w=[tin])
                    P.op("act", lambda e: e.activation(out=lg_f, in_=k_f, func=AF.Ln), r=[tin], w=[tin])
                    P.op("dve", lambda e: e.tensor_scalar(out=k_f, in0=k_f, scalar1=-1.0, scalar2=1.0, op0=ALU.mult, op1=ALU.add),
                         r=[tin], w=[tin])
                    if mode == "B":
                        psq, tpq = self.pb[1], self.tpb[1]
                        self.proj(psq, tpq, ws[:, :, 1, :], tw, cs, x_b, txb)
                        P.op("act", lambda e: e.activation(out=q_f, in_=psq, func=AF.Silu), r=[tpq], w=[tin])
                    self.gla_head(c, h, q_f, k_f, lg_f, tin, v_tok[:, :, h * 128:(h + 1) * 128], tv)
                    if mode == "B" and DEBUG and ti == DEBUG_TI and h == 0:
                        self.dbg("d_q", q_f, tin, [128, NT]); self.dbg("d_k", k_f, tin, [128, NT]); self.dbg("d_lg", lg_f, tin, [128, NT])
                        self.dbg("d_cum", c["cum"], c["tcum"], [128, NT]); self.dbg("d_o", c["o_s"][:, 0, :], c["to"], [128, NT])
                        self.dbg("d_S", c["S"][:, 0, :], c["tS"][0], [128, 128])
                        self.dbg("d_v", v_tok, tv, [128, 4, D], ) if False else None
                    if mode == "B":
                        psg, tpg = self.pb[0], self.tpb[0]
                        self.proj(psg, tpg, ws[:, :, 2, :], tw, cs, x_b, txb)
                        self.head_rms_gate(c, [(psg, tpg)], m_b, tm, h)
            if mode == "B":
                self.mixer_out(li, ti, m_b, tm, 8, w_out, sc)
        self.gla_finish(c, li)
        self.phase_end()

    def gla_pass(self, mode):
        P, A = self.P, self.A
        li = 3
        self.new_consts()
        one_c = self._one
        w_in = self.din("gla_w_in", [D, 3088]).rearrange("(c p) n -> p c n", p=128)
        c = self.gla_alloc(4, 256, mode)
        nbg = A.alloc([4], F32); tnb = P.tok("nbg")
        P.op("dve", lambda e: e.tensor_scalar(out=nbg, in0=self.vcol("gla_b_gate", 0, 4), scalar1=-1.0, scalar2=None, op0=ALU.mult),
             r=[self.tvec], w=[tnb])
        wgl = A.alloc([8, 16], BF16); twgl = P.tok("wgl")
        self.load_w(wgl, w_in[:, :, 3072:3088], twgl)
        wgate = A.alloc([512], BF16, parts=16)
        self.load_w(wgate, self.din("gla_w_gate", [16, 512]), twgl)
        gl_b = A.alloc([NT], BF16, parts=16); tgl = P.tok("gl")
        self.gla_init_state(c, li)
        x_b = A.alloc([8, NT], BF16); txb = P.tok("xb")
        wring = Ring(P, [A.alloc([8, 256], BF16) for _ in range(4)], "wr")
        v_tok = A.alloc([4, D], BF16); tv = P.tok("v")
        q_f = A.alloc([NT], F32); k_f = A.alloc([NT], F32); lg_f = A.alloc([NT], F32); tin = P.tok("qkl")
        if mode == "B":
            m_b = A.alloc([8, NT], BF16); tm = P.tok("m")
            sc = self.mixer_scratch(8, x_b, txb, m_b, tm)
            w_out = self.din("gla_w_out", [D, D])
        for ti in range(NTILE):
            xt = self.x_f[:, :, ti * NT:(ti + 1) * NT]
            P.op("act", lambda e, xt=xt: e.copy(out=x_b, in_=xt), r=[self.tx[ti]], w=[txb])
            ps, tps = self.pb[0], self.tpb[0]
            for kc in range(8):
                P.op("pe", lambda e, kc=kc: e.matmul(ps[0:16, :], lhsT=wgl[:, kc, :], rhs=x_b[:, kc, :], start=(kc == 0), stop=(kc == 7)),
                     r=[twgl, txb], w=[tps], skip_self=True)
            P.op("act", lambda e: e.copy(out=gl_b, in_=ps[0:16, :]), r=[tps], w=[tgl])
            for qt in range(4):
                ws, tw = wring.next()
                self.load_w(ws, w_in[:, :, 1024 + qt * 256:1024 + (qt + 1) * 256], tw)
                for blk in range(4):
                    ps, tps = self.pb[blk % 2], self.tpb[blk % 2]
                    for kc in range(8):
                        P.op("pe", lambda e, kc=kc, blk=blk, ps=ps, ws=ws: e.matmul(
                            ps[:, 0:256], lhsT=x_b[:, kc, blk * 128:(blk + 1) * 128], rhs=ws[:, kc, :], start=(kc == 0), stop=(kc == 7)),
                            r=[txb, tw], w=[tps], skip_self=True)
                    P.op("act", lambda e, blk=blk, qt=qt, ps=ps: e.copy(out=v_tok[:, blk, qt * 256:(qt + 1) * 256], in_=ps[:, 0:256]),
                         r=[tps], w=[tv])
            for hp in range(2):
                wk, twk = wring.next()
                self.load_w(wk, w_in[:, :, 512 + hp * 256:512 + (hp + 1) * 256], twk)
                if mode == "B":
                    wq, twq = wring.next()
                    self.load_w(wq, w_in[:, :, hp * 256:(hp + 1) * 256], twq)
                for h2 in range(2):
                    h = hp * 2 + h2
                    cs = slice(h2 * 128, (h2 + 1) * 128)
                    psz, tpz = self.pb[0], self.tpb[0]
                    P.op("pe", lambda e, h=h: e.matmul(psz, lhsT=wgate[:, h * 128:(h + 1) * 128], rhs=gl_b, start=True, stop=True),
                         r=[twgl, tgl], w=[tpz], skip_self=True)
                    P.op("act", lambda e, h=h: e.activation(out=lg_f, in_=psz, func=AF.Exp, scale=-1.0, bias=nbg[:, h:h + 1]),
                         r=[tpz, tnb], w=[tin])
                    P.op("act", lambda e: e.activation(out=lg_f, in_=lg_f, func=AF.Ln, bias=one_c, scale=1.0), r=[tin, self.tcc], w=[tin])
                    P.op("dve", lambda e: e.tensor_scalar(out=lg_f, in0=lg_f, scalar1=-1.0 / 16.0, scalar2=None, op0=ALU.mult), r=[tin], w=[tin])
                    psk, tpk = self.pb[1], self.tpb[1]
                    self.proj(psk, tpk, wk, twk, cs, x_b, txb)
                    P.op("act", lambda e: e.copy(out=k_f, in_=psk), r=[tpk], w=[tin])
                    if mode == "B":
                        psq, tpq = self.pb[0], self.tpb[0]
                        self.proj(psq, tpq, wq, twq, cs, x_b, txb)
                        P.op("act", lambda e: e.mul(out=q_f, in_=psq, mul=128.0 ** -0.5), r=[tpq], w=[tin])
                    self.gla_head(c, h, q_f, k_f, lg_f, tin, v_tok[:, :, h * 256:(h + 1) * 256], tv)
                    if mode == "B" and DEBUG and ti == DEBUG_TI and h == 0:
                        self.dbg("d_q", q_f, tin, [128, NT]); self.dbg("d_k", k_f, tin, [128, NT]); self.dbg("d_lg", lg_f, tin, [128, NT])
                        self.dbg("d_o", c["o_s"], c["to"], [128, 2, NT])
                        self.dbg("d_S", c["S"][:, 0, :], c["tS"][0], [128, 256])
                    if mode == "B":
                        wr_, twr_ = wring.next()
                        self.load_w(wr_, w_in[:, :, 2048 + h * 256:2048 + (h + 1) * 256], twr_)
                        pl = []
                        for ec in range(2):
                            psg, tpg = self.pb[ec], self.tpb[ec]
                            self.proj(psg, tpg, wr_, twr_, slice(ec * 128, (ec + 1) * 128), x_b, txb)
                            pl.append((psg, tpg))
                        self.head_rms_gate(c, pl, m_b, tm, h * 2)
            if mode == "B":
                self.mixer_out(li, ti, m_b, tm, 8, w_out, sc)
        self.gla_finish(c, li)
        self.phase_end()

    def ret_pass(self, mode):
        P, A = self.P, self.A
        li = 2
        self.new_consts()
        epsc, tcc = self._eps, self.tcc
        H = 4
        gam = [1.0 - 2.0 ** (-5.0 - h) for h in range(H)]
        w_in = self.din("ret_w_in", [D, 6144]).rearrange("(c p) n -> p c n", p=128)
        S = A.alloc([2, H, 512], F32); tS = P.toks(H, "S")
        if self.fused:
            self.carry_load("S2", S.rearrange("p a b c -> p (a b c)"), tS, 4096)
        else:
            for h in range(H):
                P.op("dve", lambda e, h=h: e.memset(S[:, :, h, :], 0.0), w=[tS[h]])
        if mode == "B" and not self.fused:
            st_all = self.din("st_all2", [8, 128, 4096])
            m = A.mark()
            sring = Ring(P, [A.alloc([512], F32) for _ in range(3)], "sta")
            dsel = A.alloc([8, H], F32); tds = P.tok("dsel")
            for r in range(8):
                for h in range(H):
                    P.op("dve", lambda e, r=r, h=h: e.tensor_scalar(out=dsel[:, r, h:h + 1], in0=self.sel[:, r:r + 1],
                                                                   scalar1=float(gam[h] ** T - 1.0), scalar2=1.0, op0=ALU.mult, op1=ALU.add),
                         r=[self.tconst], w=[tds])
            for r in range(8):
                for h in range(H):
                    for dc in range(2):
                        sb, tsb = sring.next()
                        o0 = (dc * H + h) * 512
                        P.dma("sp", sb, st_all[r][:, o0:o0 + 512], w=[tsb])
                        P.op("dve", lambda e, r=r, sb=sb: e.tensor_scalar(out=sb, in0=sb, scalar1=self.sel[:, r:r + 1], scalar2=None, op0=ALU.mult),
                             r=[tsb, self.tconst], w=[tsb])
                        P.op("dve", lambda e, r=r, h=h, dc=dc, sb=sb: e.scalar_tensor_tensor(
                            out=S[:, dc, h, :], in0=S[:, dc, h, :], scalar=dsel[:, r, h:h + 1], in1=sb, op0=ALU.mult, op1=ALU.add),
                            r=[tsb, tds, tS[h]], w=[tS[h]])
            P.barrier()
            A.reset(m)
        zeta = A.alloc([H, 128], F32); ttab = P.tok("rtab")
        P.dma("sp", zeta, self.din("tab_zeta", [128, H, 128]), w=[ttab])
        cos_t = A.alloc([NT], F32); sin_t = A.alloc([NT], F32); tcs = P.tok("cs")
        sfx = f"_{self.cur_q}" if self.fused else ""
        cosd = self.din("tabc_cos" + sfx, [128, T]); sind = self.din("tabc_sin" + sfx, [128, T])
        if mode == "B":
            dmat = A.alloc([H, 128], F32); xi = A.alloc([H, 128], F32)
            P.dma("sp", dmat, self.din("tab_dmat", [128, H, 128]), w=[ttab])
            P.dma("sp", xi, self.din("tab_xi", [128, H, 128]), w=[ttab])
            S_b = A.alloc([2, 512], BF16); tSb = P.tok("Sb")
        x_b = A.alloc([8, NT], BF16); txb = P.tok("xb")
        wring = Ring(P, [A.alloc([8, 256], BF16) for _ in range(4)], "wr")
        v_tok = A.alloc([4, 512], BF16); tv = P.tok("v")
        A1 = A.alloc([NT], F32); A2 = A.alloc([NT], F32); tA = P.tok("A12")
        k_r = A.alloc([2, NT], BF16); kz = A.alloc([2, NT], BF16); tkr = P.tok("kr"); tkz = P.tok("kz")
        kz_tok = A.alloc([4, 256], BF16); tkzt = P.tok("kzt")
        if mode == "B":
            q_r = A.alloc([2, NT], BF16); qx = A.alloc([2, NT], BF16); tqr = P.tok("qr"); tqx = P.tok("qx")
            PTr = Ring(P, [A.alloc([128], BF16) for _ in range(2)], "PT")
            o_s = A.alloc([4, NT], F32); to = P.tok("o")
            m_b = A.alloc([16, NT], BF16); tm = P.tok("m")
            sc = self.mixer_scratch(16, x_b, txb, m_b[:, 0:8], tm, sw=128)
            zreg = sc["z"].rearrange("p a b -> p (a b)").bitcast(BF16)
            o_b = zreg[:, 0:4 * NT].rearrange("p (a b) -> p a b", a=4)
            osq = zreg[:, 4 * NT:8 * NT].rearrange("p (a b) -> p a b", a=4)
            tz = sc["tz"]
            sm = sc["sm"]
            w_out = self.din("ret_w_out", [2 * D, D])

        def rotary(ps1, tp1, ps2, tp2, out_b, tout):
            P.op("dve", lambda e: e.tensor_tensor(out=A1, in0=ps1, in1=cos_t, op=ALU.mult), r=[tp1, tcs], w=[tA])
            P.op("dve", lambda e: e.tensor_tensor(out=A2, in0=ps2, in1=sin_t, op=ALU.mult), r=[tp2, tcs], w=[tA])
            P.op("dve", lambda e: e.tensor_tensor(out=out_b[:, 0, :], in0=A1, in1=A2, op=ALU.subtract), r=[tA], w=[tout])
            P.op("dve", lambda e: e.tensor_tensor(out=A1, in0=ps1, in1=sin_t, op=ALU.mult), r=[tp1, tcs, tout], w=[tA])
            P.op("dve", lambda e: e.tensor_tensor(out=A2, in0=ps2, in1=cos_t, op=ALU.mult), r=[tp2, tcs], w=[tA])
            P.op("dve", lambda e: e.tensor_tensor(out=out_b[:, 1, :], in0=A1, in1=A2, op=ALU.add), r=[tA], w=[tout])

        for ti in range(NTILE):
            xt = self.x_f[:, :, ti * NT:(ti + 1) * NT]
            P.op("act", lambda e, xt=xt: e.copy(out=x_b, in_=xt), r=[self.tx[ti]], w=[txb])
            P.dma("sp", cos_t, cosd[:, ti * NT:(ti + 1) * NT], w=[tcs])
            P.dma("sp", sin_t, sind[:, ti * NT:(ti + 1) * NT], w=[tcs])
            for h in range(H):
                for qt in range(2):
                    ws, tw = wring.next()
                    self.load_w(ws, w_in[:, :, 2048 + h * 512 + qt * 256:2048 + h * 512 + (qt + 1) * 256], tw)
                    for blk in range(4):
                        ps, tps = self.pb[blk % 2], self.tpb[blk % 2]
                        for kc in range(8):
                            P.op("pe", lambda e, kc=kc, blk=blk, ps=ps, ws=ws: e.matmul(
                                ps[:, 0:256], lhsT=x_b[:, kc, blk * 128:(blk + 1) * 128], rhs=ws[:, kc, :], start=(kc == 0), stop=(kc == 7)),
                                r=[txb, tw], w=[tps], skip_self=True)
                        P.op("act", lambda e, blk=blk, qt=qt, ps=ps: e.copy(out=v_tok[:, blk, qt * 256:(qt + 1) * 256], in_=ps[:, 0:256]),
                             r=[tps], w=[tv])
                wk, twk = wring.next()
                self.load_w(wk, w_in[:, :, 1024 + h * 256:1024 + (h + 1) * 256], twk)
                self.proj(self.pb[0], self.tpb[0], wk, twk, slice(0, 128), x_b, txb)
                self.proj(self.pb[1], self.tpb[1], wk, twk, slice(128, 256), x_b, txb)
                rotary(self.pb[0], self.tpb[0], self.pb[1], self.tpb[1], k_r, tkr)
                kz4 = kz.rearrange("p a (b c) -> p a b c", b=4)
                kr4 = k_r.rearrange("p a (b c) -> p a b c", b=4)
                for dc in range(2):
                    P.op("dve", lambda e, dc=dc, h=h: e.tensor_tensor(out=kz4[:, dc], in0=kr4[:, dc],
                                                                      in1=zeta[:, h, :].unsqueeze(1).to_broadcast([128, 4, 128]), op=ALU.mult),
                         r=[tkr, ttab], w=[tkz])
                pT = self.pbT.rearrange("p (b d) -> p b d", b=4)
                for blk in range(4):
                    for dc in range(2):
                        P.op("pe", lambda e, blk=blk, dc=dc: e.transpose(out=pT[:, blk, dc * 128:(dc + 1) * 128],
                                                                         in_=kz[:, dc, blk * 128:(blk + 1) * 128], identity=self.ident_b),
                             r=[tkz, self.tconst], w=[self.tpbT], skip_self=True)
                P.op("act", lambda e: e.copy(out=kz_tok, in_=pT), r=[self.tpbT], w=[tkzt])
                if mode == "B":
                    wq, twq = wring.next()
                    self.load_w(wq, w_in[:, :, h * 256:(h + 1) * 256], twq)
                    self.proj(self.pb[0], self.tpb[0], wq, twq, slice(0, 128), x_b, txb)
                    self.proj(self.pb[1], self.tpb[1], wq, twq, slice(128, 256), x_b, txb)
                    rotary(self.pb[0], self.tpb[0], self.pb[1], self.tpb[1], q_r, tqr)
                    qx4 = qx.rearrange("p a (b c) -> p a b c", b=4)
                    qr4 = q_r.rearrange("p a (b c) -> p a b c", b=4)
                    for dc in range(2):
                        P.op("dve", lambda e, dc=dc, h=h: e.tensor_tensor(out=qx4[:, dc], in0=qr4[:, dc],
                                                                          in1=xi[:, h, :].unsqueeze(1).to_broadcast([128, 4, 128]), op=ALU.mult),
                             r=[tqr, ttab], w=[tqx])
                    P.op("act", lambda e, h=h: e.copy(out=S_b, in_=S[:, :, h, :]), r=[tS[h]], w=[tSb])
                for blk in range(4):
                    bs = slice(blk * 128, (blk + 1) * 128)
                    if mode == "B":
                        pss, tss = self.pb[2], self.tpb[2]
                        for dc in range(2):
                            P.op("pe", lambda e, dc=dc, bs=bs: e.matmul(pss[:, 0:128], lhsT=k_r[:, dc, bs], rhs=q_r[:, dc, bs],
                                                                        start=(dc == 0), stop=(dc == 1)),
                                 r=[tkr, tqr], w=[tss], skip_self=True)
                        PT, tPT = PTr.next()
                        P.op("dve", lambda e, PT=PT, h=h: e.tensor_tensor(out=PT, in0=pss[:, 0:128], in1=dmat[:, h, :], op=ALU.mult),
                             r=[tss, ttab], w=[tPT])
                        pso, tpo = self.pb[3 + blk % 2], self.tpb[3 + blk % 2]
                        pso3 = pso.rearrange("p (a b) -> p a b", a=4)
                        for ec in range(4):
                            P.op("pe", lambda e, ec=ec, blk=blk, PT=PT, pso3=pso3: e.matmul(
                                pso3[:, ec, :], lhsT=v_tok[:, blk, ec * 128:(ec + 1) * 128], rhs=PT, start=(ec == 0), stop=False, skip_group_check=True),
                                r=[tv, tPT], w=[tpo], skip_self=True)
                            for dc in range(2):
                                P.op("pe", lambda e, ec=ec, dc=dc, bs=bs, pso3=pso3: e.matmul(
                                    pso3[:, ec, :], lhsT=S_b[:, dc, ec * 128:(ec + 1) * 128], rhs=qx[:, dc, bs],
                                    start=False, stop=(dc == 1), skip_group_check=True),
                                    r=[tSb, tqx], w=[tpo], skip_self=True)
                        P.op("act", lambda e, bs=bs, pso3=pso3: e.copy(out=o_s[:, :, bs], in_=pso3), r=[tpo], w=[to])
                    for dc in range(2):
                        psS, tpS = self.pb[5 + dc], self.tpb[5 + dc]
                        P.op("pe", lambda e, dc=dc, blk=blk, psS=psS: e.matmul(psS, lhsT=kz_tok[:, blk, dc * 128:(dc + 1) * 128], rhs=v_tok[:, blk, :],
                                                                               start=True, stop=True),
                             r=[tkzt, tv], w=[tpS], skip_self=True)
                        P.op("dve", lambda e, dc=dc, h=h, psS=psS: e.scalar_tensor_tensor(
                            out=S[:, dc, h, :], in0=S[:, dc, h, :], scalar=float(gam[h] ** 128), in1=psS, op0=ALU.mult, op1=ALU.add),
                            r=[tpS, tS[h]], w=[tS[h]])
                    if mode == "B" and blk < 3:
                        P.op("act", lambda e, h=h: e.copy(out=S_b, in_=S[:, :, h, :]), r=[tS[h]], w=[tSb])
                if mode == "B":
                    mean, var, rstd, nmr, tsm = sm["mean"], sm["var"], sm["rstd"], sm["nmr"], sm["t"]
                    P.op("act", lambda e: e.copy(out=o_b, in_=o_s), r=[to], w=[tz])
                    P.op("act", lambda e: e.activation(out=osq, in_=o_s, func=AF.Square), r=[to], w=[tz])
                    ps_s, ts_s = self.pb[2], self.tpb[2]
                    ps_q, ts_q = self.pb[0], self.tpb[0]
                    for ec in range(4):
                        P.op("pe", lambda e, ec=ec: e.matmul(ps_s, lhsT=self.ones_b, rhs=o_b[:, ec, :], start=(ec == 0), stop=(ec == 3)),
                             r=[tz, self.tconst], w=[ts_s], skip_self=True)
                    for ec in range(4):
                        P.op("pe", lambda e, ec=ec: e.matmul(ps_q, lhsT=self.ones_b, rhs=osq[:, ec, :], start=(ec == 0), stop=(ec == 3)),
                             r=[tz, self.tconst], w=[ts_q], skip_self=True)
                    P.op("act", lambda e: e.mul(out=mean, in_=ps_s, mul=1.0 / 512), r=[ts_s], w=[tsm])
                    P.op("act", lambda e: e.activation(out=nmr, in_=ps_s, func=AF.Square, scale=1.0 / 512), r=[ts_s], w=[tsm])
                    P.op("dve", lambda e: e.scalar_tensor_tensor(out=var, in0=ps_q, scalar=1.0 / 512, in1=nmr, op0=ALU.mult, op1=ALU.subtract),
                         r=[ts_q, tsm], w=[tsm])
                    P.op("act", lambda e: e.activation(out=var, in_=var, func=AF.Ln, bias=epsc, scale=1.0), r=[tsm, tcc], w=[tsm])
                    P.op("act", lambda e: e.activation(out=rstd, in_=var, func=AF.Exp, scale=-0.5), r=[tsm], w=[tsm])
                    P.op("dve", lambda e: e.scalar_tensor_tensor(out=nmr, in0=mean, scalar=-1.0, in1=rstd, op0=ALU.mult, op1=ALU.mult),
                         r=[tsm], w=[tsm])
                    P.op("dve", lambda e: e.tensor_tensor(out=o_s, in0=o_s, in1=rstd.unsqueeze(1).to_broadcast([128, 4, NT]), op=ALU.mult),
                         r=[tsm, to], w=[to])
                    P.op("dve", lambda e: e.tensor_tensor(out=o_s, in0=o_s, in1=nmr.unsqueeze(1).to_broadcast([128, 4, NT]), op=ALU.add),
                         r=[tsm, to], w=[to])
                    for e2 in range(2):
                        wg, twg = wring.next()
                        self.load_w(wg, w_in[:, :, 4096 + h * 512 + e2 * 256:4096 + h * 512 + (e2 + 1) * 256], twg)
                        for e1 in range(2):
                            ec = e2 * 2 + e1
                            psg, tpg = self.pb[e1], self.tpb[e1]
                            self.proj(psg, tpg, wg, twg, slice(e1 * 128, (e1 + 1) * 128), x_b, txb)
                            P.op("act", lambda e, psg=psg: e.activation(out=A1, in_=psg, func=AF.Silu), r=[tpg], w=[tA])
                            P.op("dve", lambda e, ec=ec, h=h: e.tensor_tensor(out=m_b[:, h * 4 + ec, :], in0=o_s[:, ec, :], in1=A1, op=ALU.mult),
                                 r=[to, tA], w=[tm])
            if mode == "B":
                self.mixer_out(li, ti, m_b, tm, 16, w_out, sc)
        if self.fused:
            self.carry_save("S2", S.rearrange("p a b c -> p (a b c)"), tS, 4096)
        elif mode == "A":
            st = self.dout("st_loc2", [128, 4096])
            for h in range(H):
                for dc in range(2):
                    o0 = (dc * H + h) * 512
                    P.dma("sp", st[:, o0:o0 + 512], S[:, dc, h, :], r=[tS[h]])
        self.phase_end()

    def finalize(self):
        self.P.finalize()
        return self.nc


class Host:
    def __init__(self, inputs):
        self.inp = {k: np.asarray(v) for k, v in inputs.items()}
        self.cache = {}
        v = np.zeros((128, NVEC), np.float32)

        def put(name, arr):
            o, n = VLAY[name]
            v[:, o:o + n] = _fm(arr)
        I = self.inp
        for j in range(4):
            put(f"rg_conv_w{j}", I["rg_conv_w"][0, j])
        put("rg_conv_b", I["rg_conv_b"][0])
        put("rg_b_a", I["rg_b_a"][0])
        put("rg_b_x", I["rg_b_x"][0])
        put("rg_lambda", I["rg_lambda"][0])
        for i in range(4):
            put(f"hg_lb{i}", I["hg_lb_logits"][i])
            put(f"ln_mix_g{i}", I["ln_mix_g"][i])
            put(f"ln_mix_b{i}", I["ln_mix_b"][i])
            put(f"ln_ffn_g{i}", I["ln_ffn_g"][i])
            put(f"ln_ffn_b{i}", I["ln_ffn_b"][i])
        put("gla_b_gate", I["gla_b_gate"][0])
        self.vecs = v
        self.ident = np.eye(128, dtype=np.float32)
        selE = np.zeros((8, 8, 128), np.float32)
        for e in range(8):
            selE[e, e, :] = 1.0
        self.selE = selE.reshape(8, 1024)

    @staticmethod
    def fm_act(a):
        t, c = a.shape
        return np.ascontiguousarray(a.T.reshape(c // 128, 128, t).transpose(1, 0, 2))

    def get(self, name, core, xcur=None, st_all=None):
        I = self.inp
        b, j = core // 4, core % 4
        t0 = j * T
        if name == "keep":
            k = np.zeros((128, 8), np.float32)
            for q in range(4):
                if q > 3 - j:
                    k[:, q] = 1.0
            return k
        if name[:-1].endswith("_") and name[-1].isdigit() and (name.startswith("xT_") or name.startswith("pT") or name.startswith("tabc_")):
            q = int(name[-1])
            ch = max(q - (3 - j), 0)
            t0 = ch * T
            base = name[:-2]
            if base == "xT":
                return self.fm_act(I["x"][b, t0:t0 + T])
            name = base
        if name == "xT":
            return xcur[core]
        if name == "vecs":
            return self.vecs
        if name == "ident_f":
            return self.ident
        if name == "selE":
            return self.selE
        if name == "tab_reset":
            r = np.ones((128, NT), np.float32)
            r[:, ::32] = 0.0
            return r
        if name == "tab_maskT":
            i = np.arange(128)
            return ((i[:, None] // 32 == i[None, :] // 32) & (i[:, None] <= i[None, :])).astype(np.float32)
        if name == "tab_rowmask":
            i = np.arange(128)
            return (i[:, None] // 32 == np.arange(4)[None, :]).astype(np.float32)
        if name in ("tab_zeta", "tab_xi", "tab_dmat"):
            out = np.zeros((128, 4, 128), np.float64)
            i = np.arange(128, dtype=np.float64)
            for h in range(4):
                g = 1.0 - 2.0 ** (-5.0 - h)
                if name == "tab_zeta":
                    out[:, h, :] = (g ** (127.0 - i))[None, :] / 16.0
                elif name == "tab_xi":
                    out[:, h, :] = (g ** (i + 1.0))[None, :]
                else:
                    rel = i[None, :] - i[:, None]
                    out[:, h, :] = np.where(rel >= 0, g ** np.maximum(rel, 0.0), 0.0) / 16.0
            return out.astype(np.float32)
        if name in ("tabc_cos", "tabc_sin"):
            inv = (np.float32(10000.0) ** (-(np.arange(0, 256, 2, dtype=np.float32)) / np.float32(256))).astype(np.float32)
            pos = np.arange(t0, t0 + T, dtype=np.float32)
            ang = (pos[None, :] * inv[:, None]).astype(np.float32).astype(np.float64)
            return (np.cos(ang) if name == "tabc_cos" else np.sin(ang)).astype(np.float32)
        if name == "sel":
            s = np.zeros((128, 8), np.float32)
            for r in range(8):
                if r // 4 == b and r % 4 < j:
                    s[:, r] = 1.0
            return s
        if name == "xh":
            h = np.zeros((4, D), np.float32)
            if j > 0:
                h[0:3] = I["x"][b, t0 - 3:t0]
            return self.fm_act(h)
        if name.startswith("pT"):
            li = int(name[2:])
            return self.fm_act(I["p"][li, b, t0:t0 + T])
        if name.startswith("st_all"):
            return st_all[name]
        if name in I and name not in ("x", "p"):
            a = I[name]
            return a[0] if a.shape[0] == 1 and name not in ("moe_w_in", "moe_w_out") else a
        for base in ("dense_w_in", "dense_w_out", "moe_w_router", "moe_w_in", "moe_w_out", "ple_w_proj", "ple_w_gate"):
            if name.startswith(base) and name[len(base):].isdigit():
                idx = int(name[len(base):])
                a = I[base]
                if base in ("dense_w_in", "dense_w_out"):
                    return a[idx:idx + 1]
                return a[idx]
        raise KeyError(name)


def run_launch(host, stage_list, xcur=None, st_all=None, trace=False, fused=False):
    kb = KB(stage_list, fused=fused)
    for st in stage_list:
        getattr(kb, st[0])(*st[1:])
    names = list(kb.inputs.keys())
    nc = kb.finalize()
    print("[kernel] instr counts", dict(kb.P.cnt), "sems", kb.P.n_sems, flush=True)
    in_maps = []
    for c in range(NCORES):
        m = {}
        for n in names:
            key = (n, c)
            per_core = n in ("xT", "sel", "xh", "keep") or n.startswith("xT_") or n.startswith("pT") or n.startswith("st_all") or n.startswith("tabc_")
            if per_core:
                m[n] = np.ascontiguousarray(host.get(n, c, xcur, st_all), dtype=np.float32)
            else:
                if n not in host.cache:
                    host.cache[n] = np.ascontiguousarray(host.get(n, 0, xcur, st_all), dtype=np.float32)
                m[n] = host.cache[n]
        in_maps.append(m)
    res = run_bass_kernel_spmd(nc, in_maps, core_ids=list(range(NCORES)), trace=trace)
    return res


PASSES = ["rglru_pass", "hgrn2_pass", "ret_pass", "gla_pass"]


def kernel(**inputs):
    host = Host(inputs)
    res = run_launch(host, fused_stages(), fused=True)
    out = np.zeros((2, SEQ, D), np.float32)
    for c in range(NCORES):
        out[c // 4, (c % 4) * T:(c % 4 + 1) * T] = res.results[c]["yT"].transpose(2, 1, 0).reshape(T, D)
    return out


def fused_stages():
    st = []
    for q in range(4):
        st += [("set_pass", q), ("load_x",), ("rglru_pass", "B"), ("ffn_phase", 0), ("hgrn2_pass", "B"), ("ffn_phase", 1),
               ("ret_pass", "B"), ("ffn_phase", 2)]
        if q == 3:
            st += [("gla_pass", "B"), ("ffn_phase", 3), ("store_x",)]
        else:
            st += [("gla_pass", "A")]
    return st


def kernel_unfused(**inputs):
    host = Host(inputs)
    x = np.asarray(inputs["x"], np.float32)
    xcur = [Host.fm_act(x[c // 4, (c % 4) * T:(c % 4 + 1) * T]) for c in range(NCORES)]
    res = run_launch(host, [("load_x",), (PASSES[0], "A")], xcur=xcur)
    st = np.stack([res.results[c]["st_loc0"] for c in range(NCORES)])
    for i in range(4):
        stages = [("load_x",), (PASSES[i], "B"), ("ffn_phase", i)]
        if i < 3:
            stages.append((PASSES[i + 1], "A"))
        stages.append(("store_x",))
        res = run_launch(host, stages, xcur=xcur, st_all={f"st_all{i}": st})
        xcur = [res.results[c]["yT"] for c in range(NCORES)]
        if i < 3:
            st = np.stack([res.results[c][f"st_loc{i + 1}"] for c in range(NCORES)])
    out = np.zeros((2, SEQ, D), np.float32)
    for c in range(NCORES):
        out[c // 4, (c % 4) * T:(c % 4 + 1) * T] = xcur[c].transpose(2, 1, 0).reshape(T, D)
    return out
```

```python
from contextlib import ExitStack
import math
import numpy as np
import ml_dtypes
import concourse.bass as bass
import concourse.mybir as mybir
from concourse.bass_utils import run_bass_kernel_spmd

F32 = mybir.dt.float32
BF16 = mybir.dt.bfloat16
AF = mybir.ActivationFunctionType
ALU = mybir.AluOpType
AX = mybir.AxisListType

NCORES = 8
D = 1024
SEQ = 8192
T = 2048
NT = 512
NTILE = T // NT
ALPHA = 8.0 ** 0.25
EPS = 1e-5
FFN_DENSE = 2816
FFN_EXPERT = 3584
ARENA_BYTES = 206 * 1024
DEBUG = False
CC_INC = 1
DEBUG_TI = 0


class Tok:
    __slots__ = ("name", "w", "r")

    def __init__(self, name):
        self.name = name
        self.w = None
        self.r = {}


class Prog:
    ENGS = ("pe", "act", "dve", "pool", "sp")

    def __init__(self, nc):
        self.nc = nc
        self.stack = ExitStack()
        self.streams = {e: [] for e in self.ENGS}
        self.cnt = {e: 0 for e in self.ENGS}
        self.waited = {e: {} for e in self.ENGS}
        self.needed = {e: set() for e in self.ENGS}
        self.slot_of = {}
        self.slot_total = []
        self.free_slots = []
        self.ntok = 0

    def _slot(self, key):
        if key not in self.slot_of:
            if self.free_slots:
                sl = self.free_slots.pop()
            else:
                sl = len(self.slot_total)
                self.slot_total.append(0)
            self.slot_of[key] = sl
        return self.slot_of[key]

    def release_keys(self):
        self.free_slots.extend(sorted(set(self.slot_of.values()), reverse=True))
        self.slot_of.clear()

    def tok(self, name=None):
        self.ntok += 1
        return Tok(name or f"t{self.ntok}")

    def toks(self, n, name="t"):
        return [self.tok(f"{name}{i}") for i in range(n)]

    def _deps(self, r, w):
        deps = []
        for t in r:
            if t.w is not None:
                deps.append(t.w)
        for t in w:
            if t.w is not None:
                deps.append(t.w)
            deps.extend(t.r.values())
        return deps

    def _emit_waits(self, eng, deps, skip_self=False):
        best = {}
        for d in deps:
            k = (d[0], d[1])
            if skip_self and d[0] == "e" and d[1] == eng:
                continue
            if best.get(k, 0) < d[2]:
                best[k] = d[2]
        wd = self.waited[eng]
        for k, v in best.items():
            if wd.get(k, 0) < v:
                wd[k] = v
                if k[0] == "e":
                    self.needed[k[1]].add(v)
                self.streams[eng].append(("wait", (k[0], k[1], v)))

    def op(self, eng, fn, r=(), w=(), skip_self=False):
        self._emit_waits(eng, self._deps(r, w), skip_self=skip_self)
        self.cnt[eng] += 1
        idx = self.cnt[eng]
        self.streams[eng].append(("op", fn, idx))
        me = ("e", eng, idx)
        for t in r:
            t.r[("e", eng)] = me
        for t in w:
            t.w = me
            t.r = {}
        return idx

    def raw(self, eng, fn):
        self.streams[eng].append(("raw", fn))

    def dma(self, q, out, in_, r=(), w=(), key=None):
        self._emit_waits(q, self._deps(r, w))
        if key is None:
            key = (w[0] if len(w) else r[0])
        sl = self._slot(key)
        self.slot_total[sl] += 16
        c = self.slot_total[sl]
        self.streams[q].append(("dma", (out, in_), sl))
        me = ("d", sl, c)
        for t in r:
            t.r[("d", sl)] = me
        for t in w:
            t.w = me
            t.r = {}

    def collective(self, ins_ap, outs_ap, r, w):
        self._emit_waits("pool", self._deps(r, w))
        sl = self._slot(w[0])
        self.slot_total[sl] += CC_INC
        c = self.slot_total[sl]
        self.streams["pool"].append(("cc", (ins_ap, outs_ap), sl))
        me = ("d", sl, c)
        for t in r:
            t.r[("d", sl)] = me
        for t in w:
            t.w = me
            t.r = {}

    def barrier(self):
        for e in self.ENGS:
            deps = [("e", f, self.cnt[f]) for f in self.ENGS if f != e and self.cnt[f] > 0]
            deps += [("d", k, c) for k, c in enumerate(self.slot_total) if c > 0]
            self._emit_waits(e, deps)

    def finalize(self):
        nc = self.nc
        self.barrier()
        esem = {}
        for e in self.ENGS:
            if self.needed[e]:
                esem[e] = self.stack.enter_context(nc.semaphore(f"es_{e}"))
        dsem = {}
        for i in range(len(self.slot_total)):
            dsem[i] = self.stack.enter_context(nc.semaphore(f"ds_{i}"))
        rank = {}
        for e in self.ENGS:
            s = sorted(self.needed[e])
            rank[e] = {v: i + 1 for i, v in enumerate(s)}
        self.n_sems = len(esem) + len(dsem)

        def replay(e, h):
            for ent in self.streams[e]:
                if ent[0] == "wait":
                    kind, k, v = ent[1]
                    if kind == "e":
                        h.wait_ge(esem[k], rank[k][v])
                    else:
                        h.wait_ge(dsem[k], v)
                elif ent[0] == "op":
                    ins = ent[1](h)
                    if ent[2] in rank[e]:
                        ins.then_inc(esem[e], 1)
                elif ent[0] == "raw":
                    ent[1](h)
                elif ent[0] == "cc":
                    ins_ap, outs_ap = ent[1]
                    h.collective_compute("AllGather", ALU.bypass, replica_groups=[list(range(NCORES))],
                                         ins=[ins_ap], outs=[outs_ap]).then_inc(dsem[ent[2]], CC_INC)
                else:
                    out, in_ = ent[1]
                    h.dma_start(out=out, in_=in_).then_inc(dsem[ent[2]], 16)

        with nc.Block() as block:
            @block.tensor
            def _(h):
                replay("pe", h)

            @block.scalar
            def _(h):
                replay("act", h)

            @block.vector
            def _(h):
                replay("dve", h)

            @block.gpsimd
            def _(h):
                replay("pool", h)

            @block.sync
            def _(h):
                replay("sp", h)
        self.stack.close()


class Arena:
    def __init__(self, P, nbytes):
        self.t = P.stack.enter_context(P.nc.sbuf_tensor("arena", [128, nbytes // 4], F32))
        self.n = nbytes // 4
        self.off = 0

    def alloc(self, shape, dtype=F32, parts=128):
        n = 1
        for s in shape:
            n *= s
        words = (n + 1) // 2 if dtype == BF16 else n
        words = (words + 7) // 8 * 8
        assert self.off + words <= self.n, f"arena overflow: need {words*4}B at {self.off*4}B"
        v = self.t[0:parts, self.off:self.off + words]
        self.off += words
        if dtype == BF16:
            v = v.bitcast(BF16)
        v = v[:, 0:n]
        if len(shape) == 2:
            v = v.rearrange("p (a b) -> p a b", a=shape[0])
        elif len(shape) == 3:
            v = v.rearrange("p (a b c) -> p a b c", a=shape[0], b=shape[1])
        elif len(shape) == 4:
            v = v.rearrange("p (a b c d) -> p a b c d", a=shape[0], b=shape[1], c=shape[2])
        return v

    def mark(self):
        return self.off

    def reset(self, m):
        self.off = m


class Ring:
    def __init__(self, P, bufs, name):
        self.bufs = bufs
        self.toks = P.toks(len(bufs), name)
        self.i = 0

    def next(self):
        b, t = self.bufs[self.i], self.toks[self.i]
        self.i = (self.i + 1) % len(self.bufs)
        return b, t


def _vec_layout():
    lay = {}
    off = 0

    def add(name, n):
        nonlocal off
        lay[name] = (off, n)
        off += n
    for j in range(4):
        add(f"rg_conv_w{j}", 8)
    add("rg_conv_b", 8)
    add("rg_b_a", 8)
    add("rg_b_x", 8)
    add("rg_lambda", 8)
    for i in range(4):
        add(f"hg_lb{i}", 8)
    add("gla_b_gate", 4)
    for i in range(4):
        add(f"ln_mix_g{i}", 8)
        add(f"ln_mix_b{i}", 8)
        add(f"ln_ffn_g{i}", 8)
        add(f"ln_ffn_b{i}", 8)
    return lay, off


VLAY, NVEC = _vec_layout()


def _fm(v):
    v = np.asarray(v, np.float32).reshape(-1)
    return np.ascontiguousarray(v.reshape(-1, 128).T)


class KB:
    def __init__(self, stages, fused=False):
        self.nc = bass.Bass("TRN2", target_bir_lowering=False)
        self.P = Prog(self.nc)
        self.stages = stages
        self.fused = fused
        self.cur_q = 0
        self.scr = {}
        self.inputs = {}
        self.outputs = {}
        P = self.P
        self.A = Arena(P, ARENA_BYTES)
        A = self.A
        self.pb = [P.stack.enter_context(self.nc.psum_tensor(f"pb{i}", [128, 512], F32))[:] for i in range(7)]
        self.pbT = P.stack.enter_context(self.nc.psum_tensor("pbT", [128, 1024], BF16))[:]
        self.tpb = P.toks(7, "pb")
        self.tpbT = P.tok("pbT")
        self.x_f = A.alloc([8, T], F32)
        self.tx = P.toks(NTILE, "x")
        self.vecs = A.alloc([NVEC], F32)
        self.tvec = P.tok("vecs")
        self.ident_b = A.alloc([128], BF16)
        self.ident_f = A.alloc([128], F32)
        self.ones_b = A.alloc([128], BF16)
        self.sel = A.alloc([8], F32)
        self.tconst = P.tok("const")
        P.dma("sp", self.vecs, self.din("vecs", [128, NVEC]), w=[self.tvec])
        P.dma("sp", self.ident_f, self.din("ident_f", [128, 128]), w=[self.tconst])
        P.dma("sp", self.sel, self.din("keep" if fused else "sel", [128, 8]), w=[self.tconst])
        P.dma("pool", self.ident_b, self.inputs["ident_f"], w=[self.tconst])
        P.op("dve", lambda e: e.memset(self.ones_b, 1.0), w=[self.tconst])
        self.base_mark = A.mark()

    def din(self, name, shape, dtype=F32):
        if name not in self.inputs:
            self.inputs[name] = self.nc.dram_tensor(name, list(shape), dtype, kind="ExternalInput").ap()
        return self.inputs[name]

    def dout(self, name, shape, dtype=F32):
        if name not in self.outputs:
            self.outputs[name] = self.nc.dram_tensor(name, list(shape), dtype, kind="ExternalOutput").ap()
        return self.outputs[name]

    def vcol(self, name, i=0, n=1):
        o, _ = VLAY[name]
        return self.vecs[:, o + i:o + i + n]

    def pin_lnexp(self):
        return

    def set_pass(self, q):
        self.cur_q = q

    def carry_load(self, name, dst, toks, n):
        P = self.P
        if self.cur_q == 0:
            P.op("dve", lambda e: e.memset(dst, 0.0), w=list(toks))
            return
        sc, tsc = self.scr[name]
        P.dma("sp", dst, sc, r=[tsc], w=list(toks), key=toks[0])
        kq = self.sel[:, self.cur_q:self.cur_q + 1]
        P.op("dve", lambda e: e.tensor_scalar(out=dst, in0=dst, scalar1=kq, scalar2=None, op0=ALU.mult),
             r=[self.tconst], w=list(toks))

    def carry_save(self, name, src, toks, n):
        P = self.P
        if name not in self.scr:
            self.scr[name] = (self.nc.dram_tensor("scr_" + name, [128, n], F32).ap(), P.tok("scr_" + name))
        sc, tsc = self.scr[name]
        P.dma("sp", sc, src, r=list(toks), w=[tsc], key=tsc)

    def dbg(self, name, ap, tok, shape):
        o = self.dout(name, shape)
        self.P.dma("sp", o, ap, r=[tok])

    def phase_end(self):
        self.P.barrier()
        self.P.release_keys()
        self.A.reset(self.base_mark)

    def load_w(self, dst, src, tok):
        self.P.dma("pool", dst, src, w=[tok])

    def proj(self, ps, tps, wslot, tw, cols, xb, txb, n=NT, kcs=8, extra_r=()):
        P = self.P
        for kc in range(kcs):
            P.op("pe", lambda e, kc=kc: e.matmul(ps, lhsT=wslot[:, kc, cols], rhs=xb[:, kc, :],
                                                 start=(kc == 0), stop=(kc == kcs - 1)),
                 r=[tw, txb] + list(extra_r), w=[tps], skip_self=True)

    def load_x(self):
        xT = self.din(f"xT_{self.cur_q}" if self.fused else "xT", [128, 8, T])
        for ti in range(NTILE):
            self.P.dma("sp", self.x_f[:, :, ti * NT:(ti + 1) * NT], xT[:, :, ti * NT:(ti + 1) * NT], w=[self.tx[ti]])

    def store_x(self):
        yT = self.dout("yT", [128, 8, T])
        for ti in range(NTILE):
            self.P.dma("sp", yT[:, :, ti * NT:(ti + 1) * NT], self.x_f[:, :, ti * NT:(ti + 1) * NT], r=[self.tx[ti]])

    def ln_alloc(self):
        A, P = self.A, self.P
        d = dict(mean=A.alloc([NT]), var=A.alloc([NT]), rstd=A.alloc([NT]), nmr=A.alloc([NT]), t=P.tok("lnsm"))
        return d

    def layer_norm(self, z, tz, zb, tzb, zsq, tzsq, sm, gname, bname, out_f, tout, out_b=None, tout_b=None,
                   pbs=(5, 6)):
        P = self.P
        tz = list(tz)
        ps_s, ts_s = self.pb[pbs[0]], self.tpb[pbs[0]]
        ps_q, ts_q = self.pb[pbs[1]], self.tpb[pbs[1]]
        P.op("dve", lambda e: e.tensor_copy(out=zb, in_=z), r=tz, w=[tzb])
        P.op("act", lambda e: e.activation(out=zsq, in_=z, func=AF.Square), r=tz, w=[tzsq])
        for kc in range(8):
            P.op("pe", lambda e, kc=kc: e.matmul(ps_s, lhsT=self.ones_b, rhs=zb[:, kc, :], start=(kc == 0), stop=(kc == 7)),
                 r=[tzb, self.tconst], w=[ts_s], skip_self=True)
        for kc in range(8):
            P.op("pe", lambda e, kc=kc: e.matmul(ps_q, lhsT=self.ones_b, rhs=zsq[:, kc, :], start=(kc == 0), stop=(kc == 7)),
                 r=[tzsq, self.tconst], w=[ts_q], skip_self=True)
        mean, var, rstd, nmr, tsm = sm["mean"], sm["var"], sm["rstd"], sm["nmr"], sm["t"]
        epsc, tcc = self._eps, self.tcc
        P.op("act", lambda e: e.mul(out=mean, in_=ps_s, mul=1.0 / D), r=[ts_s], w=[tsm])
        P.op("act", lambda e: e.activation(out=nmr, in_=ps_s, func=AF.Square, scale=1.0 / D), r=[ts_s], w=[tsm])
        P.op("dve", lambda e: e.scalar_tensor_tensor(out=var, in0=ps_q, scalar=1.0 / D, in1=nmr, op0=ALU.mult, op1=ALU.subtract),
             r=[ts_q, tsm], w=[tsm])
        self.pin_lnexp()
        P.op("act", lambda e: e.activation(out=var, in_=var, func=AF.Ln, bias=epsc, scale=1.0), r=[tsm, tcc], w=[tsm])
        P.op("act", lambda e: e.activation(out=rstd, in_=var, func=AF.Exp, scale=-0.5), r=[tsm], w=[tsm])
        P.op("dve", lambda e: e.scalar_tensor_tensor(out=nmr, in0=mean, scalar=-1.0, in1=rstd, op0=ALU.mult, op1=ALU.mult),
             r=[tsm], w=[tsm])
        for kc in range(8):
            P.op("dve", lambda e, kc=kc: e.tensor_tensor(out=z[:, kc, :], in0=z[:, kc, :], in1=rstd, op=ALU.mult),
                 r=[tsm, tz[kc]], w=[tz[kc]])
            P.op("dve", lambda e, kc=kc: e.tensor_tensor(out=z[:, kc, :], in0=z[:, kc, :], in1=nmr, op=ALU.add),
                 r=[tsm, tz[kc]], w=[tz[kc]])
            P.op("act", lambda e, kc=kc: e.activation(out=out_f[:, kc, :], in_=z[:, kc, :], func=AF.Identity,
                                                      scale=self.vcol(gname, kc), bias=self.vcol(bname, kc)),
                 r=[tz[kc], self.tvec], w=[tout])
            if out_b is not None:
                if kc % 2 == 0:
                    P.op("dve", lambda e, kc=kc: e.tensor_scalar(out=out_b[:, kc, :], in0=z[:, kc, :], scalar1=self.vcol(gname, kc),
                                                                 scalar2=self.vcol(bname, kc), op0=ALU.mult, op1=ALU.add),
                         r=[tz[kc], self.tvec], w=[tout_b])
                else:
                    P.op("act", lambda e, kc=kc: e.activation(out=out_b[:, kc, :], in_=z[:, kc, :], func=AF.Identity,
                                                              scale=self.vcol(gname, kc), bias=self.vcol(bname, kc)),
                         r=[tz[kc], self.tvec], w=[tout_b])

    def mixer_out(self, li, ti, m_b, tm, nvc, w_out, sc):
        P = self.P
        z, tz = sc["z"], sc["tz"]
        xt = self.x_f[:, :, ti * NT:(ti + 1) * NT]
        wv = w_out.rearrange("(c p) n -> p c n", p=128)
        sw = 128 if nvc > 8 else 256
        for half in range(1024 // sw):
            slot, tw = sc["wout_ring"].next()
            self.load_w(slot[:, 0:nvc, :], wv[:, :, half * sw:(half + 1) * sw], tw)
            for m2 in range(sw // 128):
                mo = half * (sw // 128) + m2
                pbi = mo % 2
                ps, tps = self.pb[pbi], self.tpb[pbi]
                for vc in range(nvc):
                    P.op("pe", lambda e, vc=vc, m2=m2, ps=ps, slot=slot: e.matmul(
                        ps, lhsT=slot[:, vc, m2 * 128:(m2 + 1) * 128], rhs=m_b[:, vc, :], start=(vc == 0), stop=(vc == nvc - 1)),
                        r=[tw, tm], w=[tps], skip_self=True)
                P.op("dve", lambda e, mo=mo, ps=ps: e.scalar_tensor_tensor(
                    out=z[:, mo, :], in0=xt[:, mo, :], scalar=ALPHA, in1=ps, op0=ALU.mult, op1=ALU.add),
                    r=[tps, self.tx[ti]], w=[tz[mo]])
        self.layer_norm(z, tz, sc["zb"], sc["tzb"], sc["zsq"], sc["tzsq"], sc["sm"], f"ln_mix_g{li}", f"ln_mix_b{li}",
                        xt, self.tx[ti])

    def mixer_scratch(self, nvc_max, zb, tzb, zsq, tzsq, sw=256):
        A, P = self.A, self.P
        sc = {}
        sc["z"] = A.alloc([8, NT]); sc["tz"] = P.toks(8, "z")
        sc["zb"] = zb; sc["tzb"] = tzb
        sc["zsq"] = zsq; sc["tzsq"] = tzsq
        sc["sm"] = self.ln_alloc()
        sc["wout_ring"] = Ring(P, [A.alloc([nvc_max, sw], BF16) for _ in range(2)], "wout")
        return sc

    def new_consts(self):
        P = self.P
        c = self.A.alloc([4], F32)
        tc = P.tok("cc")
        P.op("dve", lambda e: e.memset(c[:, 0:1], EPS), w=[tc])
        P.op("dve", lambda e: e.memset(c[:, 1:2], 1.0), w=[tc])
        self._eps = c[:, 0:1]
        self._one = c[:, 1:2]
        self.tcc = tc

    def ffn_phase(self, li):
        P, A = self.P, self.A
        moe = (li % 2 == 1)
        NE = 8 if moe else 1
        F = FFN_EXPERT if moe else FFN_DENSE
        NF = F // 128
        G = 4
        self.new_consts()
        if moe:
            w_in_all = self.din(f"moe_w_in{li // 2}", [8, D, 2 * F])
            w_out_all = self.din(f"moe_w_out{li // 2}", [8, F, D])
            w_r = self.din(f"moe_w_router{li // 2}", [D, 8])
        else:
            w_in_all = self.din(f"dense_w_in{li // 2}", [1, D, 2 * F])
            w_out_all = self.din(f"dense_w_out{li // 2}", [1, F, D])
        wg_d = self.din(f"ple_w_gate{li}", [D, D]).rearrange("(c p) n -> p c n", p=128)
        wp_d = self.din(f"ple_w_proj{li}", [256, D]).rearrange("(c p) n -> p c n", p=128)
        pT = self.din(f"pT{li}_{self.cur_q}" if self.fused else f"pT{li}", [128, 2, T])

        TT = 1024
        xn_b = A.alloc([2, 8, NT], BF16)
        txn = P.toks(2, "xn")
        a_b = A.alloc([G, 2, NT], BF16)
        ta = P.tok("a")
        y = A.alloc([2, 8, NT], F32)
        ty = [P.toks(8, f"y{st}") for st in range(2)]
        win_ring = Ring(P, [A.alloc([8, 2, 256], BF16) for _ in range(3)], "win")
        wout_ring = Ring(P, [A.alloc([G, D], BF16) for _ in range(2)], "wo")
        sg_ring = Ring(P, [A.alloc([NT], F32) for _ in range(2)], "sg")
        sm = self.ln_alloc()
        wg_ring = Ring(P, [A.alloc([8, 256], BF16) for _ in range(2)], "wg")
        wp_b = A.alloc([2, D], BF16)
        twp = P.tok("wp")
        p_b = A.alloc([2, NT], BF16)
        tp = P.tok("p")
        tmp_ring = Ring(P, [A.alloc([NT], F32) for _ in range(2)], "tmp")
        self.load_w(wp_b, wp_d, twp)
        if moe:
            wr_f = A.alloc([8, 8], F32)
            twr = P.tok("wr")
            P.dma("sp", wr_f, w_r.rearrange("(c p) n -> p c n", p=128), w=[twr])
            ones8 = A.alloc([128], F32, parts=8)
            P.op("dve", lambda e: e.memset(ones8, 1.0), w=[twr])
            gm = A.alloc([NT], F32, parts=8)
            tgm = P.tok("gm")
            lg_s = A.alloc([NT], F32, parts=8)
            tlg = P.tok("lg")
            lt = A.alloc([4, 8], F32)
            mx = A.alloc([4, 8], F32)
            ex = A.alloc([4, 8], F32)
            msk = A.alloc([4, 8], F32)
            den = A.alloc([4], F32)
            trt = P.tok("rt")
            g_fm = A.alloc([2, NT], F32, parts=8)
            tgf = P.tok("gfm")
            gate_b = A.alloc([2, NT], BF16)
            tgb = P.tok("gb")

        groups = [(f0, min(G, NF - f0)) for f0 in range(0, NF, G)]
        for tt in range(2):
            tiles = [tt * 2, tt * 2 + 1]
            for st in range(2):
                ti = tiles[st]
                P.op("act", lambda e, st=st, ti=ti: e.copy(out=xn_b[:, st], in_=self.x_f[:, :, ti * NT:(ti + 1) * NT]),
                     r=[self.tx[ti]], w=[txn[st]])
            if moe:
                for st in range(2):
                    ti = tiles[st]
                    xt = self.x_f[:, :, ti * NT:(ti + 1) * NT]
                    ps, tps = self.pb[6], self.tpb[6]
                    for kc in range(8):
                        P.op("pe", lambda e, kc=kc, xt=xt: e.matmul(ps[0:8, :], lhsT=wr_f[:, kc, :], rhs=xt[:, kc, :],
                                                                    start=(kc == 0), stop=(kc == 7)),
                             r=[twr, self.tx[ti]], w=[tps], skip_self=True)
                    P.op("act", lambda e: e.copy(out=lg_s, in_=ps[0:8, :]), r=[tps], w=[tlg])
                    pst = self.pb[6][:, 0:32].rearrange("p (a b) -> p a b", a=4)
                    for blk in range(4):
                        P.op("pe", lambda e, blk=blk: e.transpose(out=pst[:, blk, :], in_=lg_s[:, blk * 128:(blk + 1) * 128],
                                                                  identity=self.ident_f[0:8, 0:8]),
                             r=[tlg, self.tconst], w=[tps], skip_self=True)
                    P.op("dve", lambda e: e.tensor_copy(out=lt, in_=pst), r=[tps], w=[trt])
                    for blk in range(4):
                        P.op("dve", lambda e, blk=blk: e.max(out=mx[:, blk, :], in_=lt[:, blk, :]), r=[trt], w=[trt])
                    P.op("dve", lambda e: e.tensor_tensor(out=ex, in0=lt, in1=mx[:, :, 0:1].to_broadcast([128, 4, 8]), op=ALU.subtract),
                         r=[trt], w=[trt])
                    P.op("act", lambda e: e.activation(out=ex, in_=ex, func=AF.Exp), r=[trt], w=[trt])
                    P.op("dve", lambda e: e.tensor_tensor(out=msk, in0=lt, in1=mx[:, :, 1:2].to_broadcast([128, 4, 8]), op=ALU.is_ge),
                         r=[trt], w=[trt])
                    P.op("dve", lambda e: e.tensor_tensor(out=ex, in0=ex, in1=msk, op=ALU.mult), r=[trt], w=[trt])
                    P.op("dve", lambda e: e.tensor_reduce(out=den, in_=ex, axis=AX.X, op=ALU.add), r=[trt], w=[trt])
                    P.op("dve", lambda e: e.reciprocal(out=den, in_=den), r=[trt], w=[trt])
                    P.op("dve", lambda e: e.tensor_tensor(out=ex, in0=ex, in1=den.unsqueeze(2).to_broadcast([128, 4, 8]), op=ALU.mult),
                         r=[trt], w=[trt])
                    for blk in range(4):
                        P.op("pe", lambda e, blk=blk: e.transpose(out=ps[0:8, blk * 128:(blk + 1) * 128], in_=ex[:, blk, :],
                                                                  identity=self.ident_f),
                             r=[trt, self.tconst], w=[tps], skip_self=True)
                    P.op("act", lambda e, st=st: e.copy(out=g_fm[:, st, :], in_=ps[0:8, :]), r=[tps], w=[tgf])
            for ei in range(NE):
                w_in = w_in_all[ei].rearrange("(c p) n -> p c n", p=128)
                w_out = w_out_all[ei]
                if moe:
                    for st in range(2):
                        ps, tps = self.pb[6], self.tpb[6]
                        P.op("dve", lambda e, st=st, ei=ei: e.tensor_scalar(out=gm, in0=g_fm[:, st, :], scalar1=self.ident_f[0:8, ei:ei + 1],
                                                                          scalar2=None, op0=ALU.mult), r=[tgf, self.tconst], w=[tgm])
                        P.op("pe", lambda e: e.matmul(ps, lhsT=ones8, rhs=gm, start=True, stop=True),
                             r=[tgm, twr], w=[tps], skip_self=True)
                        P.op("act", lambda e, st=st: e.copy(out=gate_b[:, st, :], in_=ps), r=[tps], w=[tgb])
                for gi, (f0, g) in enumerate(groups):
                    for pr in range(0, g, 2):
                        npair = min(2, g - pr)
                        slot, tw = win_ring.next()
                        c0 = (f0 + pr) * 128
                        self.load_w(slot[:, :, 0, 0:npair * 128], w_in[:, :, c0:c0 + npair * 128], tw)
                        self.load_w(slot[:, :, 1, 0:npair * 128], w_in[:, :, F + c0:F + c0 + npair * 128], tw)
                        for ff in range(npair):
                            fi = pr + ff
                            for st in range(2):
                                bi = 2 * st
                                psg, tg_ = self.pb[bi], self.tpb[bi]
                                psu, tu_ = self.pb[bi + 1], self.tpb[bi + 1]
                                for kc in range(8):
                                    P.op("pe", lambda e, kc=kc, ff=ff, st=st, psg=psg, slot=slot: e.matmul(
                                        psg, lhsT=slot[:, kc, 0, ff * 128:(ff + 1) * 128], rhs=xn_b[:, st, kc, :],
                                        start=(kc == 0), stop=(kc == 7)), r=[tw, txn[st]], w=[tg_], skip_self=True)
                                for kc in range(8):
                                    P.op("pe", lambda e, kc=kc, ff=ff, st=st, psu=psu, slot=slot: e.matmul(
                                        psu, lhsT=slot[:, kc, 1, ff * 128:(ff + 1) * 128], rhs=xn_b[:, st, kc, :],
                                        start=(kc == 0), stop=(kc == 7)), r=[tw, txn[st]], w=[tu_], skip_self=True)
                                sg, tsg = sg_ring.next()
                                P.op("act", lambda e, sg=sg, psg=psg: e.activation(out=sg, in_=psg, func=AF.Silu), r=[tg_], w=[tsg])
                                if moe:
                                    P.op("dve", lambda e, sg=sg, st=st: e.tensor_tensor(out=sg, in0=sg, in1=gate_b[:, st, :], op=ALU.mult),
                                         r=[tsg, tgb], w=[tsg])
                                P.op("dve", lambda e, sg=sg, psu=psu, fi=fi, st=st: e.tensor_tensor(
                                    out=a_b[:, fi, st, :], in0=sg, in1=psu, op=ALU.mult), r=[tsg, tu_], w=[ta])
                    wslot, two = wout_ring.next()
                    self.load_w(wslot[:, 0:g, :], w_out[f0 * 128:(f0 + g) * 128, :].rearrange("(g p) n -> p g n", p=128), two)
                    first = (ei == 0 and gi == 0)
                    for st in range(2):
                        for mo in range(8):
                            bi = 4 + (mo % 2)
                            ps, tps = self.pb[bi], self.tpb[bi]
                            for fi in range(g):
                                P.op("pe", lambda e, fi=fi, mo=mo, st=st, ps=ps, wslot=wslot: e.matmul(
                                    ps, lhsT=wslot[:, fi, mo * 128:(mo + 1) * 128], rhs=a_b[:, fi, st, :],
                                    start=(fi == 0), stop=(fi == g - 1)), r=[two, ta], w=[tps], skip_self=True)
                            if first:
                                P.op("act", lambda e, mo=mo, st=st, ps=ps: e.copy(out=y[:, st, mo, :], in_=ps), r=[tps], w=[ty[st][mo]])
                            else:
                                P.op("dve", lambda e, mo=mo, st=st, ps=ps: e.tensor_tensor(
                                    out=y[:, st, mo, :], in0=y[:, st, mo, :], in1=ps, op=ALU.add), r=[tps, ty[st][mo]], w=[ty[st][mo]])
            for st in range(2):
                ti = tiles[st]
                xt = self.x_f[:, :, ti * NT:(ti + 1) * NT]
                yz = y[:, st]
                P.op("dve", lambda e, yz=yz, xt=xt: e.scalar_tensor_tensor(out=yz, in0=xt, scalar=ALPHA, in1=yz,
                                                                           op0=ALU.mult, op1=ALU.add),
                     r=[self.tx[ti]] + ty[st], w=ty[st])
                zb, zsq = xn_b[:, 0], xn_b[:, 1]
                self.layer_norm(yz, ty[st], zb, txn[0], zsq, txn[1], sm, f"ln_ffn_g{li}", f"ln_ffn_b{li}",
                                xt, self.tx[ti], out_b=zb, tout_b=txn[0], pbs=(5, 6))
                xb = zb
                self.load_w(p_b, pT[:, :, ti * NT:(ti + 1) * NT], tp)
                for q4 in range(4):
                    wgs, twg = wg_ring.next()
                    self.load_w(wgs, wg_d[:, :, q4 * 256:(q4 + 1) * 256], twg)
                    for m2 in range(2):
                        mo = q4 * 2 + m2
                        psg, tg_ = self.pb[0 + 2 * m2], self.tpb[0 + 2 * m2]
                        psp, tp_ = self.pb[1 + 2 * m2], self.tpb[1 + 2 * m2]
                        for kc in range(8):
                            P.op("pe", lambda e, kc=kc, m2=m2, psg=psg, wgs=wgs: e.matmul(
                                psg, lhsT=wgs[:, kc, m2 * 128:(m2 + 1) * 128], rhs=xb[:, kc, :], start=(kc == 0), stop=(kc == 7)),
                                r=[twg, txn[0]], w=[tg_], skip_self=True)
                        for k2 in range(2):
                            P.op("pe", lambda e, k2=k2, mo=mo, psp=psp: e.matmul(
                                psp, lhsT=wp_b[:, k2, mo * 128:(mo + 1) * 128], rhs=p_b[:, k2, :], start=(k2 == 0), stop=(k2 == 1)),
                                r=[twp, tp], w=[tp_], skip_self=True)
                        sg, tsg = sg_ring.next()
                        P.op("act", lambda e, sg=sg, psg=psg: e.activation(out=sg, in_=psg, func=AF.Sigmoid), r=[tg_], w=[tsg])
                        tmp, ttmp = tmp_ring.next()
                        P.op("dve", lambda e, sg=sg, psp=psp, tmp=tmp: e.tensor_tensor(out=tmp, in0=sg, in1=psp, op=ALU.mult),
                             r=[tsg, tp_], w=[ttmp])
                        P.op("dve", lambda e, mo=mo, xt=xt, tmp=tmp: e.tensor_tensor(out=xt[:, mo, :], in0=xt[:, mo, :], in1=tmp, op=ALU.add),
                             r=[ttmp, self.tx[ti]], w=[self.tx[ti]])
        self.phase_end()

    def exchange_out(self, li, src, tsrc, W):
        st = self.dout(f"st_loc{li}", [128, W])
        self.P.dma("sp", st, src, r=[tsrc])

    def rglru_pass(self, mode):
        P, A = self.P, self.A
        li = 0
        self.new_consts()
        w_in = self.din("rg_w_in", [D, 2 * D]).rearrange("(c p) n -> p c n", p=128)
        w_a = self.din("rg_w_a", [4, 256, 256])
        w_x = self.din("rg_w_x", [4, 256, 256])
        x_b = A.alloc([8, NT], BF16); txb = P.tok("xb")
        if not self.fused:
            xh = self.din("xh", [128, 8, 4])
            xh_b = A.alloc([8, 4], BF16); txh = P.tok("xh")
            self.load_w(xh_b, xh, txh)
        wa_b = A.alloc([4, 2, 256], BF16); wx_b = A.alloc([4, 2, 256], BF16); twax = P.tok("wax")
        for n in range(4):
            self.load_w(wa_b[:, n], w_a[n].rearrange("(c p) n -> p c n", p=128), twax)
            self.load_w(wx_b[:, n], w_x[n].rearrange("(c p) n -> p c n", p=128), twax)
        w_ring = Ring(P, [A.alloc([8, 256], BF16) for _ in range(3)], "wi")
        rec = A.alloc([2, NT + 3], F32); trec = P.tok("rec")
        halo = A.alloc([8, 3], F32); thalo = P.tok("halo")
        u = A.alloc([2, NT], F32); tu = P.tok("u")
        u_b = A.alloc([2, NT], BF16); tub = P.tok("ub")
        r_s = A.alloc([NT], F32); i_s = A.alloc([NT], F32); a_s = A.alloc([NT], F32); q_s = A.alloc([NT], F32)
        h_s = A.alloc([NT], F32)
        tg = P.tok("gates"); th = P.tok("h")
        hst = A.alloc([8], F32); thst = P.tok("hst")
        cl = A.alloc([8], F32); cl2 = A.alloc([8], F32); tcl = P.tok("cl")
        lam = self.vcol("rg_lambda", 0, 8)
        one_c = self._one
        P.op("act", lambda e: e.activation(out=cl, in_=lam, func=AF.Exp, scale=-1.0), r=[self.tvec], w=[tcl])
        P.op("act", lambda e: e.activation(out=cl, in_=cl, func=AF.Ln, bias=one_c.to_broadcast([128, 8]) if False else one_c, scale=1.0), r=[tcl, self.tcc], w=[tcl])
        P.op("dve", lambda e: e.tensor_scalar(out=cl2, in0=cl, scalar1=-16.0, scalar2=None, op0=ALU.mult), r=[tcl], w=[tcl])
        P.op("dve", lambda e: e.tensor_scalar(out=cl, in0=cl, scalar1=-8.0, scalar2=None, op0=ALU.mult), r=[tcl], w=[tcl])
        if mode == "A":
            P.op("dve", lambda e: e.memset(hst, 0.0), w=[thst])
            ptot = A.alloc([8], F32)
            P.op("dve", lambda e: e.memset(ptot, 1.0), w=[thst])
            pt_s = A.alloc([NT], F32)
            zeros = A.alloc([NT], F32)
            P.op("dve", lambda e: e.memset(zeros, 0.0), w=[thst])
        elif self.fused:
            self.carry_load("rg_h", hst, [thst], 8)
            self.carry_load("rg_halo", halo.rearrange("p a b -> p (a b)"), [thalo], 24)
            m_b = A.alloc([8, NT], BF16); tm = P.tok("m")
            gb = A.alloc([NT], F32); g2 = A.alloc([NT], F32); tgb = P.tok("gb")
            sc = self.mixer_scratch(8, x_b, txb, m_b, tm)
            w_out = self.din("rg_w_out", [D, D])
        else:
            st_all = self.din("st_all0", [8, 128, 16])
            sta = A.alloc([8, 16], F32); tsta = P.tok("sta")
            P.dma("sp", sta, st_all.rearrange("r p w -> p r w"), w=[tsta])
            P.op("dve", lambda e: e.memset(hst, 0.0), w=[thst])
            dsel = A.alloc([8], F32); hl = A.alloc([8], F32)
            for r in range(8):
                sr = self.sel[:, r:r + 1]
                P.op("dve", lambda e, r=r, sr=sr: e.tensor_scalar(out=dsel, in0=sta[:, r, 8:16], scalar1=-1.0, scalar2=sr,
                                                                 op0=ALU.add, op1=ALU.mult), r=[tsta, self.tconst], w=[thst])
                P.op("dve", lambda e: e.tensor_scalar(out=dsel, in0=dsel, scalar1=1.0, scalar2=None, op0=ALU.add), r=[thst], w=[thst])
                P.op("dve", lambda e, r=r, sr=sr: e.tensor_scalar(out=hl, in0=sta[:, r, 0:8], scalar1=sr, scalar2=None, op0=ALU.mult),
                     r=[tsta, self.tconst], w=[thst])
                P.op("dve", lambda e: e.tensor_tensor(out=hst, in0=hst, in1=dsel, op=ALU.mult), r=[thst], w=[thst])
                P.op("dve", lambda e: e.tensor_tensor(out=hst, in0=hst, in1=hl, op=ALU.add), r=[thst], w=[thst])
            m_b = A.alloc([8, NT], BF16); tm = P.tok("m")
            gb = A.alloc([NT], F32); g2 = A.alloc([NT], F32); tgb = P.tok("gb")
            sc = self.mixer_scratch(8, x_b, txb, m_b, tm)
            w_out = self.din("rg_w_out", [D, D])
        for n in range(4 if not self.fused else 0):
            slot, tw = w_ring.next()
            self.load_w(slot, w_in[:, :, D + n * 256:D + (n + 1) * 256], tw)
            for c2 in range(2):
                cc = 2 * n + c2
                ps, tps = self.pb[c2], self.tpb[c2]
                self.proj(ps[:, 0:4], tps, slot, tw, slice(c2 * 128, (c2 + 1) * 128), xh_b, txh)
                P.op("act", lambda e, cc=cc, ps=ps: e.copy(out=halo[:, cc, :], in_=ps[:, 0:3]), r=[tps], w=[thalo])
        for ti in range(NTILE):
            xt = self.x_f[:, :, ti * NT:(ti + 1) * NT]
            P.op("act", lambda e, xt=xt: e.copy(out=x_b, in_=xt), r=[self.tx[ti]], w=[txb])
            for n in range(4):
                slot, tw = w_ring.next()
                self.load_w(slot, w_in[:, :, D + n * 256:D + (n + 1) * 256], tw)
                if mode == "B":
                    gslot, tgw = w_ring.next()
                    self.load_w(gslot, w_in[:, :, n * 256:(n + 1) * 256], tgw)
                for c2 in range(2):
                    cc = 2 * n + c2
                    ps, tps = self.pb[c2], self.tpb[c2]
                    self.proj(ps, tps, slot, tw, slice(c2 * 128, (c2 + 1) * 128), x_b, txb)
                    P.op("dve", lambda e, cc=cc, c2=c2: e.tensor_copy(out=rec[:, c2, 0:3], in_=halo[:, cc, :]), r=[thalo], w=[trec])
                    P.op("act", lambda e, c2=c2, ps=ps: e.copy(out=rec[:, c2, 3:NT + 3], in_=ps), r=[tps], w=[trec])
                    P.op("dve", lambda e, cc=cc, c2=c2: e.tensor_copy(out=halo[:, cc, :], in_=rec[:, c2, NT:NT + 3]), r=[trec], w=[thalo])
                    P.op("act", lambda e, cc=cc, c2=c2: e.activation(out=u[:, c2, :], in_=rec[:, c2, 0:NT], func=AF.Identity,
                                                                     scale=self.vcol("rg_conv_w0", cc), bias=self.vcol("rg_conv_b", cc)),
                         r=[trec, self.tvec], w=[tu])
                    for j in range(1, 4):
                        P.op("dve", lambda e, cc=cc, c2=c2, j=j: e.scalar_tensor_tensor(
                            out=u[:, c2, :], in0=rec[:, c2, j:j + NT], scalar=self.vcol(f"rg_conv_w{j}", cc), in1=u[:, c2, :],
                            op0=ALU.mult, op1=ALU.add), r=[trec, tu, self.tvec], w=[tu])
                    P.op("act", lambda e, c2=c2: e.copy(out=u_b[:, c2, :], in_=u[:, c2, :]), r=[tu], w=[tub])
                for c2 in range(2):
                    cc = 2 * n + c2
                    psr, tpr = self.pb[2], self.tpb[2]
                    psi, tpi = self.pb[3], self.tpb[3]
                    for k2 in range(2):
                        P.op("pe", lambda e, k2=k2, c2=c2, n=n: e.matmul(psr, lhsT=wa_b[:, n, k2, c2 * 128:(c2 + 1) * 128], rhs=u_b[:, k2, :],
                                                                         start=(k2 == 0), stop=(k2 == 1)), r=[twax, tub], w=[tpr], skip_self=True)
                    for k2 in range(2):
                        P.op("pe", lambda e, k2=k2, c2=c2, n=n: e.matmul(psi, lhsT=wx_b[:, n, k2, c2 * 128:(c2 + 1) * 128], rhs=u_b[:, k2, :],
                                                                         start=(k2 == 0), stop=(k2 == 1)), r=[twax, tub], w=[tpi], skip_self=True)
                    P.op("act", lambda e, cc=cc: e.activation(out=r_s, in_=psr, func=AF.Sigmoid, bias=self.vcol("rg_b_a", cc), scale=1.0),
                         r=[tpr, self.tvec], w=[tg])
                    P.op("act", lambda e, cc=cc: e.activation(out=i_s, in_=psi, func=AF.Sigmoid, bias=self.vcol("rg_b_x", cc), scale=1.0),
                         r=[tpi, self.tvec], w=[tg])
                    P.op("act", lambda e, cc=cc: e.activation(out=a_s, in_=r_s, func=AF.Exp, scale=cl[:, cc:cc + 1]), r=[tg, tcl], w=[tg])
                    P.op("act", lambda e, cc=cc: e.activation(out=q_s, in_=r_s, func=AF.Exp, scale=cl2[:, cc:cc + 1]), r=[tg, tcl], w=[tg])
                    P.op("act", lambda e: e.activation(out=q_s, in_=q_s, func=AF.Sqrt, bias=one_c, scale=-1.0), r=[tg, self.tcc], w=[tg])
                    P.op("dve", lambda e: e.tensor_tensor(out=q_s, in0=q_s, in1=i_s, op=ALU.mult), r=[tg], w=[tg])
                    P.op("dve", lambda e, c2=c2: e.tensor_tensor(out=q_s, in0=q_s, in1=u[:, c2, :], op=ALU.mult), r=[tg, tu], w=[tg])
                    P.op("dve", lambda e, cc=cc: e.tensor_tensor_scan(out=h_s, data0=a_s, data1=q_s, initial=hst[:, cc:cc + 1],
                                                                      op0=ALU.mult, op1=ALU.add), r=[tg, thst], w=[th])
                    P.op("dve", lambda e, cc=cc: e.tensor_copy(out=hst[:, cc:cc + 1], in_=h_s[:, NT - 1:NT]), r=[th], w=[thst])
                    if mode == "A":
                        P.op("dve", lambda e, cc=cc: e.tensor_tensor_scan(out=pt_s, data0=a_s, data1=zeros, initial=ptot[:, cc:cc + 1],
                                                                          op0=ALU.mult, op1=ALU.add), r=[tg, thst, self.tconst], w=[th])
                        P.op("dve", lambda e, cc=cc: e.tensor_copy(out=ptot[:, cc:cc + 1], in_=pt_s[:, NT - 1:NT]), r=[th], w=[thst])
                    else:
                        psg, tpg = self.pb[4 + c2], self.tpb[4 + c2]
                        self.proj(psg, tpg, gslot, tgw, slice(c2 * 128, (c2 + 1) * 128), x_b, txb)
                        P.op("act", lambda e, psg=psg: e.activation(out=g2, in_=psg, func=AF.Square), r=[tpg], w=[tgb])
                        P.op("dve", lambda e: e.tensor_scalar(out=g2, in0=g2, scalar1=0.044715, scalar2=1.0, op0=ALU.mult, op1=ALU.add),
                             r=[tgb], w=[tgb])
                        P.op("dve", lambda e, psg=psg: e.tensor_tensor(out=g2, in0=g2, in1=psg, op=ALU.mult), r=[tgb, tpg], w=[tgb])
                        P.op("act", lambda e: e.activation(out=g2, in_=g2, func=AF.Sigmoid, scale=1.5957691216057308), r=[tgb], w=[tgb])
                        P.op("dve", lambda e, psg=psg: e.tensor_tensor(out=gb, in0=g2, in1=psg, op=ALU.mult), r=[tgb, tpg], w=[tgb])
                        P.op("dve", lambda e, cc=cc: e.tensor_tensor(out=m_b[:, cc, :], in0=gb, in1=h_s, op=ALU.mult), r=[tgb, th], w=[tm])
            if mode == "B":
                self.mixer_out(li, ti, m_b, tm, 8, w_out, sc)
        if mode == "A":
            stl = A.alloc([16], F32)
            P.op("dve", lambda e: e.tensor_copy(out=stl[:, 0:8], in_=hst), r=[thst], w=[th])
            P.op("dve", lambda e: e.tensor_copy(out=stl[:, 8:16], in_=ptot), r=[thst], w=[th])
            self.exchange_out(0, stl, th, 16)
        if self.fused:
            self.carry_save("rg_h", hst, [thst], 8)
            self.carry_save("rg_halo", halo.rearrange("p a b -> p (a b)"), [thalo], 24)
        self.phase_end()

    def gla_alloc(self, H, dv, mode):
        A, P = self.A, self.P
        c = dict(H=H, dv=dv, dvc=dv // 128, mode=mode)
        c["reset"] = A.alloc([NT], F32)
        c["maskT"] = A.alloc([128], F32)
        c["rowmask"] = A.alloc([4], F32)
        c["ttab"] = P.tok("gtab")
        P.dma("sp", c["reset"], self.din("tab_reset", [128, NT]), w=[c["ttab"]])
        P.dma("sp", c["maskT"], self.din("tab_maskT", [128, 128]), w=[c["ttab"]])
        P.dma("sp", c["rowmask"], self.din("tab_rowmask", [128, 4]), w=[c["ttab"]])
        c["S"] = A.alloc([H, dv], F32); c["tS"] = P.toks(H, "S")
        c["slg"] = A.alloc([H], F32); c["tslg"] = P.tok("slg")
        c["cum"] = A.alloc([NT], F32); c["E"] = A.alloc([NT], F32); c["E2"] = A.alloc([NT], F32)
        c["tcum"] = P.tok("cum"); c["tE"] = P.tok("E"); c["tE2"] = P.tok("E2")
        c["kd_b"] = A.alloc([NT], BF16); c["tkd"] = P.tok("kd")
        c["kdm"] = A.alloc([4, 4, 128], BF16); c["tkdm"] = P.toks(4, "kdm")
        c["dec"] = A.alloc([16], F32); c["tdec"] = P.tok("dec")
        c["red"] = A.alloc([1], F32)
        c["Sr"] = [A.alloc([dv], F32) for _ in range(4)]; c["tSr"] = P.toks(4, "Sr")
        if mode == "B":
            c["qe_b"] = A.alloc([NT], BF16); c["ke_b"] = A.alloc([NT], BF16); c["qi_f"] = A.alloc([NT], F32)
            c["tqk"] = P.tok("qk")
            c["PT"] = Ring(P, [A.alloc([128], BF16) for _ in range(2)], "PT")
            c["o_s"] = A.alloc([c["dvc"], NT], F32); c["to"] = P.tok("o")
            c["osq"] = A.alloc([c["dvc"], NT], BF16); c["tosq"] = P.tok("osq")
            c["rn"] = A.alloc([NT], F32); c["trn"] = P.tok("rn")
        return c

    def gla_init_state(self, c, li):
        P, A = self.P, self.A
        H, dv = c["H"], c["dv"]
        W = H * dv + H
        c["W"] = W
        S, tS = c["S"], c["tS"]
        P.op("dve", lambda e: e.memset(c["slg"], 0.0), w=[c["tslg"]])
        if self.fused:
            self.carry_load(f"S{li}", S.rearrange("p a b -> p (a b)"), tS, H * dv)
            return
        for h in range(H):
            P.op("dve", lambda e, h=h: e.memset(S[:, h, :], 0.0), w=[tS[h]])
        if c["mode"] == "A":
            return
        st_all = self.din(f"st_all{li}", [8, 128, W])
        m = A.mark()
        sta = A.alloc([W], F32); tsta = P.tok("sta")
        dsel = A.alloc([H], F32); tds = P.tok("dsel")
        for r in range(8):
            sr = self.sel[:, r:r + 1]
            P.dma("sp", sta, st_all[r], w=[tsta])
            P.op("act", lambda e: e.activation(out=dsel, in_=sta[:, H * dv:H * dv + H], func=AF.Exp), r=[tsta], w=[tds])
            P.op("dve", lambda e, sr=sr: e.tensor_scalar(out=dsel, in0=dsel, scalar1=-1.0, scalar2=sr, op0=ALU.add, op1=ALU.mult),
                 r=[tds, self.tconst], w=[tds])
            P.op("dve", lambda e: e.tensor_scalar(out=dsel, in0=dsel, scalar1=1.0, scalar2=None, op0=ALU.add), r=[tds], w=[tds])
            P.op("dve", lambda e, sr=sr: e.tensor_scalar(out=sta[:, 0:H * dv], in0=sta[:, 0:H * dv], scalar1=sr, scalar2=None, op0=ALU.mult),
                 r=[tsta, self.tconst], w=[tsta])
            for h in range(H):
                P.op("dve", lambda e, h=h: e.scalar_tensor_tensor(out=S[:, h, :], in0=S[:, h, :], scalar=dsel[:, h:h + 1],
                                                                  in1=sta[:, h * dv:(h + 1) * dv], op0=ALU.mult, op1=ALU.add),
                     r=[tds, tsta, tS[h]], w=[tS[h]])
        P.barrier()
        A.reset(m)

    def gla_finish(self, c, li):
        if self.fused:
            self.carry_save(f"S{li}", c["S"].rearrange("p a b -> p (a b)"), c["tS"], c["H"] * c["dv"])
        elif c["mode"] == "A":
            self.gla_finish_A(c, li)

    def gla_finish_A(self, c, li):
        P, A = self.P, self.A
        H, dv = c["H"], c["dv"]
        stl = A.alloc([H * dv + H], F32); tst = P.tok("stl")
        for h in range(H):
            P.op("dve", lambda e, h=h: e.tensor_copy(out=stl[:, h * dv:(h + 1) * dv], in_=c["S"][:, h, :]), r=[c["tS"][h]], w=[tst])
        P.op("dve", lambda e: e.tensor_copy(out=stl[:, H * dv:H * dv + H], in_=c["slg"]), r=[c["tslg"]], w=[tst])
        self.exchange_out(li, stl, tst, H * dv + H)

    def gla_head(self, c, h, q_f, k_f, lg_f, tin, v_tok, tv):
        P = self.P
        mode, dv, dvc = c["mode"], c["dv"], c["dvc"]
        cum, E, E2 = c["cum"], c["E"], c["E2"]
        tcum, tE, tE2 = c["tcum"], c["tE"], c["tE2"]
        S, tS = c["S"][:, h, :], c["tS"][h]
        if not c.get("pinned_by_front"):
            self.pin_lnexp()
        P.op("dve", lambda e: e.tensor_tensor_scan(out=cum, data0=c["reset"], data1=lg_f, initial=0.0, op0=ALU.mult, op1=ALU.add),
             r=[tin, c["ttab"]], w=[tcum])
        P.op("dve", lambda e: e.tensor_reduce(out=c["red"], in_=lg_f, axis=AX.X, op=ALU.add), r=[tin], w=[tE2])
        P.op("dve", lambda e: e.tensor_tensor(out=c["slg"][:, h:h + 1], in0=c["slg"][:, h:h + 1], in1=c["red"], op=ALU.add),
             r=[tE2, c["tslg"]], w=[c["tslg"]])
        cum3 = cum.rearrange("p (c t) -> p c t", t=32)
        E3 = E.rearrange("p (c t) -> p c t", t=32)
        lastb = cum3[:, :, 31:32].to_broadcast([128, 16, 32])
        refb = cum3[:, :, 16:17].to_broadcast([128, 16, 32])
        P.op("dve", lambda e: e.tensor_tensor(out=E3, in0=lastb, in1=cum3, op=ALU.subtract), r=[tcum], w=[tE])
        P.op("act", lambda e: e.activation(out=E, in_=E, func=AF.Exp), r=[tE], w=[tE])
        P.op("dve", lambda e: e.tensor_tensor(out=c["kd_b"], in0=k_f, in1=E, op=ALU.mult), r=[tE, tin], w=[c["tkd"]])
        P.op("act", lambda e: e.activation(out=c["dec"], in_=cum3[:, :, 31], func=AF.Exp), r=[tcum], w=[c["tdec"]])
        pT4 = self.pbT[:, 0:512].rearrange("p (b d) -> p b d", b=4)
        for blk in range(4):
            P.op("pe", lambda e, blk=blk: e.transpose(out=pT4[:, blk, :], in_=c["kd_b"][:, blk * 128:(blk + 1) * 128], identity=self.ident_b),
                 r=[c["tkd"], self.tconst], w=[self.tpbT], skip_self=True)
        for blk in range(4):
            P.op("dve", lambda e, blk=blk: e.tensor_tensor(
                out=c["kdm"][:, blk], in0=pT4[:, blk, :].unsqueeze(1).to_broadcast([128, 4, 128]),
                in1=c["rowmask"].unsqueeze(2).to_broadcast([128, 4, 128]), op=ALU.mult),
                r=[self.tpbT, c["ttab"]], w=[c["tkdm"][blk]])
        if mode == "B":
            qe_b, ke_b, qi_f, tqk = c["qe_b"], c["ke_b"], c["qi_f"], c["tqk"]
            E23 = E2.rearrange("p (c t) -> p c t", t=32)
            P.op("act", lambda e: e.activation(out=qi_f, in_=cum, func=AF.Exp), r=[tcum], w=[tqk])
            P.op("dve", lambda e: e.tensor_tensor(out=qi_f, in0=qi_f, in1=q_f, op=ALU.mult), r=[tqk, tin], w=[tqk])
            P.op("dve", lambda e: e.tensor_tensor(out=E23, in0=cum3, in1=refb, op=ALU.subtract), r=[tcum], w=[tE2])
            P.op("act", lambda e: e.activation(out=E, in_=E2, func=AF.Exp), r=[tE2, c["tkd"]], w=[tE])
            P.op("dve", lambda e: e.tensor_tensor(out=qe_b, in0=q_f, in1=E, op=ALU.mult), r=[tE, tin], w=[tqk])
            P.op("act", lambda e: e.activation(out=E2, in_=E2, func=AF.Exp, scale=-1.0), r=[tE2], w=[tE2])
            P.op("dve", lambda e: e.tensor_tensor(out=ke_b, in0=k_f, in1=E2, op=ALU.mult), r=[tE2, tin], w=[tqk])
        for blk in range(4):
            bs = slice(blk * 128, (blk + 1) * 128)
            if mode == "B":
                pss, tss = self.pb[2], self.tpb[2]
                P.op("pe", lambda e, bs=bs: e.matmul(pss[:, 0:128], lhsT=c["ke_b"][:, bs], rhs=c["qe_b"][:, bs], start=True, stop=True),
                     r=[c["tqk"]], w=[tss], skip_self=True)
                PT, tPT = c["PT"].next()
                P.op("dve", lambda e, PT=PT: e.tensor_tensor(out=PT, in0=pss[:, 0:128], in1=c["maskT"], op=ALU.mult),
                     r=[tss, c["ttab"]], w=[tPT])
                pso, tpo = self.pb[3 + blk % 2], self.tpb[3 + blk % 2]
                pso3 = pso[:, 0:dvc * 128].rearrange("p (a b) -> p a b", a=dvc)
                for ec in range(dvc):
                    P.op("pe", lambda e, ec=ec, blk=blk, PT=PT, pso3=pso3: e.matmul(
                        pso3[:, ec, :], lhsT=v_tok[:, blk, ec * 128:(ec + 1) * 128], rhs=PT, start=(ec == 0), stop=False, skip_group_check=True),
                        r=[tv, tPT], w=[tpo], skip_self=True)
            for i in range(4):
                ch = blk * 4 + i
                Ssrc, tSsrc = (S, tS) if ch == 0 else (c["Sr"][(ch - 1) % 4], c["tSr"][(ch - 1) % 4])
                Sdst, tSdst = (S, tS) if ch == 15 else (c["Sr"][ch % 4], c["tSr"][ch % 4])
                psS, tpS = self.pb[5 + ch % 2], self.tpb[5 + ch % 2]
                P.op("pe", lambda e, blk=blk, i=i, psS=psS: e.matmul(psS[:, 0:dv], lhsT=c["kdm"][:, blk, i, :], rhs=v_tok[:, blk, :],
                                                                     start=True, stop=True),
                     r=[c["tkdm"][blk], tv], w=[tpS], skip_self=True)
                if mode == "B":
                    for ec in range(dvc):
                        P.op("pe", lambda e, ec=ec, i=i, ch=ch, pso3=pso3, Ssrc=Ssrc: e.matmul(
                            pso3[:, ec, i * 32:(i + 1) * 32], lhsT=Ssrc[:, ec * 128:(ec + 1) * 128], rhs=c["qi_f"][:, ch * 32:(ch + 1) * 32],
                            start=False, stop=(i == 3), skip_group_check=True),
                            r=[tSsrc, c["tqk"]], w=[tpo], skip_self=True)
                P.op("dve", lambda e, ch=ch, psS=psS, Ssrc=Ssrc, Sdst=Sdst: e.scalar_tensor_tensor(
                    out=Sdst, in0=Ssrc, scalar=c["dec"][:, ch:ch + 1], in1=psS[:, 0:dv], op0=ALU.mult, op1=ALU.add),
                     r=[tpS, c["tdec"], tSsrc], w=[tSdst])
            if mode == "B":
                P.op("act", lambda e, bs=bs, pso3=pso3: e.copy(out=c["o_s"][:, :, bs], in_=pso3), r=[tpo], w=[c["to"]])

    def head_rms_gate(self, c, ps_g_list, m_b, tm, vc0):
        P = self.P
        dv, dvc = c["dv"], c["dvc"]
        o_s, to, osq, tosq, rn, trn = c["o_s"], c["to"], c["osq"], c["tosq"], c["rn"], c["trn"]
        epsc, tcc = self._eps, self.tcc
        P.op("act", lambda e: e.activation(out=osq, in_=o_s, func=AF.Square), r=[to], w=[tosq])
        psn, tpn = self.pb[2], self.tpb[2]
        for ec in range(dvc):
            P.op("pe", lambda e, ec=ec: e.matmul(psn, lhsT=self.ones_b, rhs=osq[:, ec, :], start=(ec == 0), stop=(ec == dvc - 1)),
                 r=[tosq, self.tconst], w=[tpn], skip_self=True)
        P.op("act", lambda e: e.activation(out=rn, in_=psn, func=AF.Ln, bias=epsc, scale=1.0 / dv), r=[tpn, tcc], w=[trn])
        P.op("act", lambda e: e.activation(out=rn, in_=rn, func=AF.Exp, scale=-0.5), r=[trn], w=[trn])
        for ec in range(dvc):
            psg, tpg = ps_g_list[ec]
            P.op("dve", lambda e, ec=ec: e.tensor_tensor(out=o_s[:, ec, :], in0=o_s[:, ec, :], in1=rn, op=ALU.mult), r=[trn, to], w=[to])
            sgt = c["E"]
            P.op("act", lambda e, psg=psg: e.activation(out=sgt, in_=psg, func=AF.Silu), r=[tpg, c["tkd"]], w=[c["tE"]])
            P.op("dve", lambda e, ec=ec: e.tensor_tensor(out=m_b[:, vc0 + ec, :], in0=o_s[:, ec, :], in1=sgt, op=ALU.mult),
                 r=[to, c["tE"]], w=[tm])

    def hgrn2_pass(self, mode):
        P, A = self.P, self.A
        li = 1
        self.new_consts()
        one_c = self._one
        w_in = self.din("hg_w_in", [D, 4 * D]).rearrange("(c p) n -> p c n", p=128)
        c = self.gla_alloc(8, 128, mode)
        lb = A.alloc([8], F32); oml = A.alloc([8], F32); tlb = P.tok("lb")
        et = A.alloc([4, 8], F32)
        for k in range(4):
            P.op("act", lambda e, k=k: e.activation(out=et[:, k, :], in_=self.vcol(f"hg_lb{k}", 0, 8), func=AF.Exp), r=[self.tvec], w=[tlb])
        P.op("dve", lambda e: e.tensor_tensor(out=oml, in0=et[:, 0, :], in1=et[:, 1, :], op=ALU.add), r=[tlb], w=[tlb])
        P.op("dve", lambda e: e.tensor_tensor(out=oml, in0=oml, in1=et[:, 2, :], op=ALU.add), r=[tlb], w=[tlb])
        P.op("dve", lambda e: e.tensor_tensor(out=oml, in0=oml, in1=et[:, 3, :], op=ALU.add), r=[tlb], w=[tlb])
        P.op("dve", lambda e: e.reciprocal(out=oml, in_=oml), r=[tlb], w=[tlb])
        P.op("dve", lambda e: e.tensor_copy(out=lb, in_=et[:, 1, :]), r=[tlb], w=[tlb])
        for k in range(2, li + 1):
            P.op("dve", lambda e, k=k: e.tensor_tensor(out=lb, in0=lb, in1=et[:, k, :], op=ALU.add), r=[tlb], w=[tlb])
        P.op("dve", lambda e: e.tensor_tensor(out=lb, in0=lb, in1=oml, op=ALU.mult), r=[tlb], w=[tlb])
        P.op("dve", lambda e: e.tensor_scalar(out=oml, in0=lb, scalar1=-1.0, scalar2=1.0, op0=ALU.mult, op1=ALU.add), r=[tlb], w=[tlb])
        hl = A.alloc([8], F32); bl = A.alloc([8], F32)
        P.op("dve", lambda e: e.tensor_scalar(out=hl, in0=oml, scalar1=0.5, scalar2=None, op0=ALU.mult), r=[tlb], w=[tlb])
        P.op("dve", lambda e: e.tensor_tensor(out=bl, in0=lb, in1=hl, op=ALU.add), r=[tlb], w=[tlb])
        c["pinned_by_front"] = True
        self.gla_init_state(c, li)
        x_b = A.alloc([8, NT], BF16); txb = P.tok("xb")
        wv_ring = Ring(P, [A.alloc([8, 256], BF16) for _ in range(2)], "wv")
        v_tok = A.alloc([4, D], BF16); tv = P.tok("v")
        ncol = 3 if mode == "B" else 1
        wh_ring = Ring(P, [A.alloc([8, ncol, 256], BF16) for _ in range(2)], "wh")
        q_f = A.alloc([NT], F32); k_f = A.alloc([NT], F32); lg_f = A.alloc([NT], F32); tin = P.tok("qkl")
        if mode == "B":
            m_b = A.alloc([8, NT], BF16); tm = P.tok("m")
            sc = self.mixer_scratch(8, x_b, txb, m_b, tm)
            w_out = self.din("hg_w_out", [D, D])
        for ti in range(NTILE):
            xt = self.x_f[:, :, ti * NT:(ti + 1) * NT]
            P.op("act", lambda e, xt=xt: e.copy(out=x_b, in_=xt), r=[self.tx[ti]], w=[txb])
            for qt in range(4):
                ws, tw = wv_ring.next()
                self.load_w(ws, w_in[:, :, 2048 + qt * 256:2048 + (qt + 1) * 256], tw)
                for blk in range(4):
                    ps, tps = self.pb[blk % 2], self.tpb[blk % 2]
                    for kc in range(8):
                        P.op("pe", lambda e, kc=kc, blk=blk, ps=ps, ws=ws: e.matmul(
                            ps[:, 0:256], lhsT=x_b[:, kc, blk * 128:(blk + 1) * 128], rhs=ws[:, kc, :], start=(kc == 0), stop=(kc == 7)),
                            r=[txb, tw], w=[tps], skip_self=True)
                    P.op("act", lambda e, blk=blk, qt=qt, ps=ps: e.copy(out=v_tok[:, blk, qt * 256:(qt + 1) * 256], in_=ps[:, 0:256]),
                         r=[tps], w=[tv])
            for hp in range(4):
                ws, tw = wh_ring.next()
                self.load_w(ws[:, :, 0, :], w_in[:, :, 1024 + hp * 256:1024 + (hp + 1) * 256], tw)
                if mode == "B":
                    self.load_w(ws[:, :, 1, :], w_in[:, :, hp * 256:(hp + 1) * 256], tw)
                    self.load_w(ws[:, :, 2, :], w_in[:, :, 3072 + hp * 256:3072 + (hp + 1) * 256], tw)
                for h2 in range(2):
                    h = hp * 2 + h2
                    cs = slice(h2 * 128, (h2 + 1) * 128)
                    psf, tpf = self.pb[0], self.tpb[0]
                    self.proj(psf, tpf, ws[:, :, 0, :], tw, cs, x_b, txb)
                    if mode == "B":
                        psq, tpq = self.pb[1], self.tpb[1]
                        self.proj(psq, tpq, ws[:, :, 1, :], tw, cs, x_b, txb)
                    P.op("act", lambda e: e.activation(out=k_f, in_=psf, func=AF.Tanh, scale=0.5), r=[tpf], w=[tin])
                    if mode == "B":
                        P.op("act", lambda e: e.activation(out=q_f, in_=psq, func=AF.Silu), r=[tpq], w=[tin])
                    self.pin_lnexp()
                    P.op("act", lambda e, h=h: e.activation(out=k_f, in_=k_f, func=AF.Identity, scale=hl[:, h:h + 1], bias=bl[:, h:h + 1]),
                         r=[tin, tlb], w=[tin])
                    P.op("act", lambda e: e.activation(out=lg_f, in_=k_f, func=AF.Ln), r=[tin], w=[tin])
                    P.op("dve", lambda e: e.tensor_scalar(out=k_f, in0=k_f, scalar1=-1.0, scalar2=1.0, op0=ALU.mult, op1=ALU.add),
                         r=[tin], w=[tin])
                    self.gla_head(c, h, q_f, k_f, lg_f, tin, v_tok[:, :, h * 128:(h + 1) * 128], tv)
                    if mode == "B" and DEBUG and ti == DEBUG_TI and h == 0:
                        self.dbg("d_q", q_f, tin, [128, NT]); self.dbg("d_k", k_f, tin, [128, NT]); self.dbg("d_lg", lg_f, tin, [128, NT])
                        self.dbg("d_cum", c["cum"], c["tcum"], [128, NT]); self.dbg("d_o", c["o_s"][:, 0, :], c["to"], [128, NT])
                        self.dbg("d_S", c["S"][:, 0, :], c["tS"][0], [128, 128])
                        self.dbg("d_v", v_tok, tv, [128, 4, D], ) if False else None
                    if mode == "B":
                        psg, tpg = self.pb[0], self.tpb[0]
                        self.proj(psg, tpg, ws[:, :, 2, :], tw, cs, x_b, txb)
                        self.head_rms_gate(c, [(psg, tpg)], m_b, tm, h)
            if mode == "B":
                self.mixer_out(li, ti, m_b, tm, 8, w_out, sc)
        self.gla_finish(c, li)
        self.phase_end()

    def gla_pass(self, mode):
        P, A = self.P, self.A
        li = 3
        self.new_consts()
        one_c = self._one
        w_in = self.din("gla_w_in", [D, 3088]).rearrange("(c p) n -> p c n", p=128)
        c = self.gla_alloc(4, 256, mode)
        nbg = A.alloc([4], F32); tnb = P.tok("nbg")
        P.op("dve", lambda e: e.tensor_scalar(out=nbg, in0=self.vcol("gla_b_gate", 0, 4), scalar1=-1.0, scalar2=None, op0=ALU.mult),
             r=[self.tvec], w=[tnb])
        wgl = A.alloc([8, 16], BF16); twgl = P.tok("wgl")
        self.load_w(wgl, w_in[:, :, 3072:3088], twgl)
        wgate = A.alloc([512], BF16, parts=16)
        self.load_w(wgate, self.din("gla_w_gate", [16, 512]), twgl)
        gl_b = A.alloc([NT], BF16, parts=16); tgl = P.tok("gl")
        self.gla_init_state(c, li)
        x_b = A.alloc([8, NT], BF16); txb = P.tok("xb")
        wring = Ring(P, [A.alloc([8, 256], BF16) for _ in range(4)], "wr")
        v_tok = A.alloc([4, D], BF16); tv = P.tok("v")
        q_f = A.alloc([NT], F32); k_f = A.alloc([NT], F32); lg_f = A.alloc([NT], F32); tin = P.tok("qkl")
        if mode == "B":
            m_b = A.alloc([8, NT], BF16); tm = P.tok("m")
            sc = self.mixer_scratch(8, x_b, txb, m_b, tm)
            w_out = self.din("gla_w_out", [D, D])
        for ti in range(NTILE):
            xt = self.x_f[:, :, ti * NT:(ti + 1) * NT]
            P.op("act", lambda e, xt=xt: e.copy(out=x_b, in_=xt), r=[self.tx[ti]], w=[txb])
            ps, tps = self.pb[0], self.tpb[0]
            for kc in range(8):
                P.op("pe", lambda e, kc=kc: e.matmul(ps[0:16, :], lhsT=wgl[:, kc, :], rhs=x_b[:, kc, :], start=(kc == 0), stop=(kc == 7)),
                     r=[twgl, txb], w=[tps], skip_self=True)
            P.op("act", lambda e: e.copy(out=gl_b, in_=ps[0:16, :]), r=[tps], w=[tgl])
            for qt in range(4):
                ws, tw = wring.next()
                self.load_w(ws, w_in[:, :, 1024 + qt * 256:1024 + (qt + 1) * 256], tw)
                for blk in range(4):
                    ps, tps = self.pb[blk % 2], self.tpb[blk % 2]
                    for kc in range(8):
                        P.op("pe", lambda e, kc=kc, blk=blk, ps=ps, ws=ws: e.matmul(
                            ps[:, 0:256], lhsT=x_b[:, kc, blk * 128:(blk + 1) * 128], rhs=ws[:, kc, :], start=(kc == 0), stop=(kc == 7)),
                            r=[txb, tw], w=[tps], skip_self=True)
                    P.op("act", lambda e, blk=blk, qt=qt, ps=ps: e.copy(out=v_tok[:, blk, qt * 256:(qt + 1) * 256], in_=ps[:, 0:256]),
                         r=[tps], w=[tv])
            for hp in range(2):
                wk, twk = wring.next()
                self.load_w(wk, w_in[:, :, 512 + hp * 256:512 + (hp + 1) * 256], twk)
                if mode == "B":
                    wq, twq = wring.next()
                    self.load_w(wq, w_in[:, :, hp * 256:(hp + 1) * 256], twq)
                for h2 in range(2):
                    h = hp * 2 + h2
                    cs = slice(h2 * 128, (h2 + 1) * 128)
                    psz, tpz = self.pb[0], self.tpb[0]
                    P.op("pe", lambda e, h=h: e.matmul(psz, lhsT=wgate[:, h * 128:(h + 1) * 128], rhs=gl_b, start=True, stop=True),
                         r=[twgl, tgl], w=[tpz], skip_self=True)
                    P.op("act", lambda e, h=h: e.activation(out=lg_f, in_=psz, func=AF.Exp, scale=-1.0, bias=nbg[:, h:h + 1]),
                         r=[tpz, tnb], w=[tin])
                    P.op("act", lambda e: e.activation(out=lg_f, in_=lg_f, func=AF.Ln, bias=one_c, scale=1.0), r=[tin, self.tcc], w=[tin])
                    P.op("dve", lambda e: e.tensor_scalar(out=lg_f, in0=lg_f, scalar1=-1.0 / 16.0, scalar2=None, op0=ALU.mult), r=[tin], w=[tin])
                    psk, tpk = self.pb[1], self.tpb[1]
                    self.proj(psk, tpk, wk, twk, cs, x_b, txb)
                    P.op("act", lambda e: e.copy(out=k_f, in_=psk), r=[tpk], w=[tin])
                    if mode == "B":
                        psq, tpq = self.pb[0], self.tpb[0]
                        self.proj(psq, tpq, wq, twq, cs, x_b, txb)
                        P.op("act", lambda e: e.mul(out=q_f, in_=psq, mul=128.0 ** -0.5), r=[tpq], w=[tin])
                    self.gla_head(c, h, q_f, k_f, lg_f, tin, v_tok[:, :, h * 256:(h + 1) * 256], tv)
                    if mode == "B" and DEBUG and ti == DEBUG_TI and h == 0:
                        self.dbg("d_q", q_f, tin, [128, NT]); self.dbg("d_k", k_f, tin, [128, NT]); self.dbg("d_lg", lg_f, tin, [128, NT])
                        self.dbg("d_o", c["o_s"], c["to"], [128, 2, NT])
                        self.dbg("d_S", c["S"][:, 0, :], c["tS"][0], [128, 256])
                    if mode == "B":
                        wr_, twr_ = wring.next()
                        self.load_w(wr_, w_in[:, :, 2048 + h * 256:2048 + (h + 1) * 256], twr_)
                        pl = []
                        for ec in range(2):
                            psg, tpg = self.pb[ec], self.tpb[ec]
                            self.proj(psg, tpg, wr_, twr_, slice(ec * 128, (ec + 1) * 128), x_b, txb)
                            pl.append((psg, tpg))
                        self.head_rms_gate(c, pl, m_b, tm, h * 2)
            if mode == "B":
                self.mixer_out(li, ti, m_b, tm, 8, w_out, sc)
        self.gla_finish(c, li)
        self.phase_end()

    def ret_pass(self, mode):
        P, A = self.P, self.A
        li = 2
        self.new_consts()
        epsc, tcc = self._eps, self.tcc
        H = 4
        gam = [1.0 - 2.0 ** (-5.0 - h) for h in range(H)]
        w_in = self.din("ret_w_in", [D, 6144]).rearrange("(c p) n -> p c n", p=128)
        S = A.alloc([2, H, 512], F32); tS = P.toks(H, "S")
        if self.fused:
            self.carry_load("S2", S.rearrange("p a b c -> p (a b c)"), tS, 4096)
        else:
            for h in range(H):
                P.op("dve", lambda e, h=h: e.memset(S[:, :, h, :], 0.0), w=[tS[h]])
        if mode == "B" and not self.fused:
            st_all = self.din("st_all2", [8, 128, 4096])
            m = A.mark()
            sring = Ring(P, [A.alloc([512], F32) for _ in range(3)], "sta")
            dsel = A.alloc([8, H], F32); tds = P.tok("dsel")
            for r in range(8):
                for h in range(H):
                    P.op("dve", lambda e, r=r, h=h: e.tensor_scalar(out=dsel[:, r, h:h + 1], in0=self.sel[:, r:r + 1],
                                                                   scalar1=float(gam[h] ** T - 1.0), scalar2=1.0, op0=ALU.mult, op1=ALU.add),
                         r=[self.tconst], w=[tds])
            for r in range(8):
                for h in range(H):
                    for dc in range(2):
                        sb, tsb = sring.next()
                        o0 = (dc * H + h) * 512
                        P.dma("sp", sb, st_all[r][:, o0:o0 + 512], w=[tsb])
                        P.op("dve", lambda e, r=r, sb=sb: e.tensor_scalar(out=sb, in0=sb, scalar1=self.sel[:, r:r + 1], scalar2=None, op0=ALU.mult),
                             r=[tsb, self.tconst], w=[tsb])
                        P.op("dve", lambda e, r=r, h=h, dc=dc, sb=sb: e.scalar_tensor_tensor(
                            out=S[:, dc, h, :], in0=S[:, dc, h, :], scalar=dsel[:, r, h:h + 1], in1=sb, op0=ALU.mult, op1=ALU.add),
                            r=[tsb, tds, tS[h]], w=[tS[h]])
            P.barrier()
            A.reset(m)
        zeta = A.alloc([H, 128], F32); ttab = P.tok("rtab")
        P.dma("sp", zeta, self.din("tab_zeta", [128, H, 128]), w=[ttab])
        cos_t = A.alloc([NT], F32); sin_t = A.alloc([NT], F32); tcs = P.tok("cs")
        sfx = f"_{self.cur_q}" if self.fused else ""
        cosd = self.din("tabc_cos" + sfx, [128, T]); sind = self.din("tabc_sin" + sfx, [128, T])
        if mode == "B":
            dmat = A.alloc([H, 128], F32); xi = A.alloc([H, 128], F32)
            P.dma("sp", dmat, self.din("tab_dmat", [128, H, 128]), w=[ttab])
            P.dma("sp", xi, self.din("tab_xi", [128, H, 128]), w=[ttab])
            S_b = A.alloc([2, 512], BF16); tSb = P.tok("Sb")
        x_b = A.alloc([8, NT], BF16); txb = P.tok("xb")
        wring = Ring(P, [A.alloc([8, 256], BF16) for _ in range(4)], "wr")
        v_tok = A.alloc([4, 512], BF16); tv = P.tok("v")
        A1 = A.alloc([NT], F32); A2 = A.alloc([NT], F32); tA = P.tok("A12")
        k_r = A.alloc([2, NT], BF16); kz = A.alloc([2, NT], BF16); tkr = P.tok("kr"); tkz = P.tok("kz")
        kz_tok = A.alloc([4, 256], BF16); tkzt = P.tok("kzt")
        if mode == "B":
            q_r = A.alloc([2, NT], BF16); qx = A.alloc([2, NT], BF16); tqr = P.tok("qr"); tqx = P.tok("qx")
            PTr = Ring(P, [A.alloc([128], BF16) for _ in range(2)], "PT")
            o_s = A.alloc([4, NT], F32); to = P.tok("o")
            m_b = A.alloc([16, NT], BF16); tm = P.tok("m")
            sc = self.mixer_scratch(16, x_b, txb, m_b[:, 0:8], tm, sw=128)
            zreg = sc["z"].rearrange("p a b -> p (a b)").bitcast(BF16)
            o_b = zreg[:, 0:4 * NT].rearrange("p (a b) -> p a b", a=4)
            osq = zreg[:, 4 * NT:8 * NT].rearrange("p (a b) -> p a b", a=4)
            tz = sc["tz"]
            sm = sc["sm"]
            w_out = self.din("ret_w_out", [2 * D, D])

        def rotary(ps1, tp1, ps2, tp2, out_b, tout):
            P.op("dve", lambda e: e.tensor_tensor(out=A1, in0=ps1, in1=cos_t, op=ALU.mult), r=[tp1, tcs], w=[tA])
            P.op("dve", lambda e: e.tensor_tensor(out=A2, in0=ps2, in1=sin_t, op=ALU.mult), r=[tp2, tcs], w=[tA])
            P.op("dve", lambda e: e.tensor_tensor(out=out_b[:, 0, :], in0=A1, in1=A2, op=ALU.subtract), r=[tA], w=[tout])
            P.op("dve", lambda e: e.tensor_tensor(out=A1, in0=ps1, in1=sin_t, op=ALU.mult), r=[tp1, tcs, tout], w=[tA])
            P.op("dve", lambda e: e.tensor_tensor(out=A2, in0=ps2, in1=cos_t, op=ALU.mult), r=[tp2, tcs], w=[tA])
            P.op("dve", lambda e: e.tensor_tensor(out=out_b[:, 1, :], in0=A1, in1=A2, op=ALU.add), r=[tA], w=[tout])

        for ti in range(NTILE):
            xt = self.x_f[:, :, ti * NT:(ti + 1) * NT]
            P.op("act", lambda e, xt=xt: e.copy(out=x_b, in_=xt), r=[self.tx[ti]], w=[txb])
            P.dma("sp", cos_t, cosd[:, ti * NT:(ti + 1) * NT], w=[tcs])
            P.dma("sp", sin_t, sind[:, ti * NT:(ti + 1) * NT], w=[tcs])
            for h in range(H):
                for qt in range(2):
                    ws, tw = wring.next()
                    self.load_w(ws, w_in[:, :, 2048 + h * 512 + qt * 256:2048 + h * 512 + (qt + 1) * 256], tw)
                    for blk in range(4):
                        ps, tps = self.pb[blk % 2], self.tpb[blk % 2]
                        for kc in range(8):
                            P.op("pe", lambda e, kc=kc, blk=blk, ps=ps, ws=ws: e.matmul(
                                ps[:, 0:256], lhsT=x_b[:, kc, blk * 128:(blk + 1) * 128], rhs=ws[:, kc, :], start=(kc == 0), stop=(kc == 7)),
                                r=[txb, tw], w=[tps], skip_self=True)
                        P.op("act", lambda e, blk=blk, qt=qt, ps=ps: e.copy(out=v_tok[:, blk, qt * 256:(qt + 1) * 256], in_=ps[:, 0:256]),
                             r=[tps], w=[tv])
                wk, twk = wring.next()
                self.load_w(wk, w_in[:, :, 1024 + h * 256:1024 + (h + 1) * 256], twk)
                self.proj(self.pb[0], self.tpb[0], wk, twk, slice(0, 128), x_b, txb)
                self.proj(self.pb[1], self.tpb[1], wk, twk, slice(128, 256), x_b, txb)
                rotary(self.pb[0], self.tpb[0], self.pb[1], self.tpb[1], k_r, tkr)
                kz4 = kz.rearrange("p a (b c) -> p a b c", b=4)
                kr4 = k_r.rearrange("p a (b c) -> p a b c", b=4)
                for dc in range(2):
                    P.op("dve", lambda e, dc=dc, h=h: e.tensor_tensor(out=kz4[:, dc], in0=kr4[:, dc],
                                                                      in1=zeta[:, h, :].unsqueeze(1).to_broadcast([128, 4, 128]), op=ALU.mult),
                         r=[tkr, ttab], w=[tkz])
                pT = self.pbT.rearrange("p (b d) -> p b d", b=4)
                for blk in range(4):
                    for dc in range(2):
                        P.op("pe", lambda e, blk=blk, dc=dc: e.transpose(out=pT[:, blk, dc * 128:(dc + 1) * 128],
                                                                         in_=kz[:, dc, blk * 128:(blk + 1) * 128], identity=self.ident_b),
                             r=[tkz, self.tconst], w=[self.tpbT], skip_self=True)
                P.op("act", lambda e: e.copy(out=kz_tok, in_=pT), r=[self.tpbT], w=[tkzt])
                if mode == "B":
                    wq, twq = wring.next()
                    self.load_w(wq, w_in[:, :, h * 256:(h + 1) * 256], twq)
                    self.proj(self.pb[0], self.tpb[0], wq, twq, slice(0, 128), x_b, txb)
                    self.proj(self.pb[1], self.tpb[1], wq, twq, slice(128, 256), x_b, txb)
                    rotary(self.pb[0], self.tpb[0], self.pb[1], self.tpb[1], q_r, tqr)
                    qx4 = qx.rearrange("p a (b c) -> p a b c", b=4)
                    qr4 = q_r.rearrange("p a (b c) -> p a b c", b=4)
                    for dc in range(2):
                        P.op("dve", lambda e, dc=dc, h=h: e.tensor_tensor(out=qx4[:, dc], in0=qr4[:, dc],
                                                                          in1=xi[:, h, :].unsqueeze(1).to_broadcast([128, 4, 128]), op=ALU.mult),
                             r=[tqr, ttab], w=[tqx])
                    P.op("act", lambda e, h=h: e.copy(out=S_b, in_=S[:, :, h, :]), r=[tS[h]], w=[tSb])
                for blk in range(4):
                    bs = slice(blk * 128, (blk + 1) * 128)
                    if mode == "B":
                        pss, tss = self.pb[2], self.tpb[2]
                        for dc in range(2):
                            P.op("pe", lambda e, dc=dc, bs=bs: e.matmul(pss[:, 0:128], lhsT=k_r[:, dc, bs], rhs=q_r[:, dc, bs],
                                                                        start=(dc == 0), stop=(dc == 1)),
                                 r=[tkr, tqr], w=[tss], skip_self=True)
                        PT, tPT = PTr.next()
                        P.op("dve", lambda e, PT=PT, h=h: e.tensor_tensor(out=PT, in0=pss[:, 0:128], in1=dmat[:, h, :], op=ALU.mult),
                             r=[tss, ttab], w=[tPT])
                        pso, tpo = self.pb[3 + blk % 2], self.tpb[3 + blk % 2]
                        pso3 = pso.rearrange("p (a b) -> p a b", a=4)
                        for ec in range(4):
                            P.op("pe", lambda e, ec=ec, blk=blk, PT=PT, pso3=pso3: e.matmul(
                                pso3[:, ec, :], lhsT=v_tok[:, blk, ec * 128:(ec + 1) * 128], rhs=PT, start=(ec == 0), stop=False, skip_group_check=True),
                                r=[tv, tPT], w=[tpo], skip_self=True)
                            for dc in range(2):
                                P.op("pe", lambda e, ec=ec, dc=dc, bs=bs, pso3=pso3: e.matmul(
                                    pso3[:, ec, :], lhsT=S_b[:, dc, ec * 128:(ec + 1) * 128], rhs=qx[:, dc, bs],
                                    start=False, stop=(dc == 1), skip_group_check=True),
                                    r=[tSb, tqx], w=[tpo], skip_self=True)
                        P.op("act", lambda e, bs=bs, pso3=pso3: e.copy(out=o_s[:, :, bs], in_=pso3), r=[tpo], w=[to])
                    for dc in range(2):
                        psS, tpS = self.pb[5 + dc], self.tpb[5 + dc]
                        P.op("pe", lambda e, dc=dc, blk=blk, psS=psS: e.matmul(psS, lhsT=kz_tok[:, blk, dc * 128:(dc + 1) * 128], rhs=v_tok[:, blk, :],
                                                                               start=True, stop=True),
                             r=[tkzt, tv], w=[tpS], skip_self=True)
                        P.op("dve", lambda e, dc=dc, h=h, psS=psS: e.scalar_tensor_tensor(
                            out=S[:, dc, h, :], in0=S[:, dc, h, :], scalar=float(gam[h] ** 128), in1=psS, op0=ALU.mult, op1=ALU.add),
                            r=[tpS, tS[h]], w=[tS[h]])
                    if mode == "B" and blk < 3:
                        P.op("act", lambda e, h=h: e.copy(out=S_b, in_=S[:, :, h, :]), r=[tS[h]], w=[tSb])
                if mode == "B":
                    mean, var, rstd, nmr, tsm = sm["mean"], sm["var"], sm["rstd"], sm["nmr"], sm["t"]
                    P.op("act", lambda e: e.copy(out=o_b, in_=o_s), r=[to], w=tz)
                    P.op("act", lambda e: e.activation(out=osq, in_=o_s, func=AF.Square), r=[to], w=tz)
                    ps_s, ts_s = self.pb[2], self.tpb[2]
                    ps_q, ts_q = self.pb[0], self.tpb[0]
                    for ec in range(4):
                        P.op("pe", lambda e, ec=ec: e.matmul(ps_s, lhsT=self.ones_b, rhs=o_b[:, ec, :], start=(ec == 0), stop=(ec == 3)),
                             r=tz + [self.tconst], w=[ts_s], skip_self=True)
                    for ec in range(4):
                        P.op("pe", lambda e, ec=ec: e.matmul(ps_q, lhsT=self.ones_b, rhs=osq[:, ec, :], start=(ec == 0), stop=(ec == 3)),
                             r=tz + [self.tconst], w=[ts_q], skip_self=True)
                    P.op("act", lambda e: e.mul(out=mean, in_=ps_s, mul=1.0 / 512), r=[ts_s], w=[tsm])
                    P.op("act", lambda e: e.activation(out=nmr, in_=ps_s, func=AF.Square, scale=1.0 / 512), r=[ts_s], w=[tsm])
                    P.op("dve", lambda e: e.scalar_tensor_tensor(out=var, in0=ps_q, scalar=1.0 / 512, in1=nmr, op0=ALU.mult, op1=ALU.subtract),
                         r=[ts_q, tsm], w=[tsm])
                    self.pin_lnexp()
                    P.op("act", lambda e: e.activation(out=var, in_=var, func=AF.Ln, bias=epsc, scale=1.0), r=[tsm, tcc], w=[tsm])
                    P.op("act", lambda e: e.activation(out=rstd, in_=var, func=AF.Exp, scale=-0.5), r=[tsm], w=[tsm])
                    P.op("dve", lambda e: e.scalar_tensor_tensor(out=nmr, in0=mean, scalar=-1.0, in1=rstd, op0=ALU.mult, op1=ALU.mult),
                         r=[tsm], w=[tsm])
                    P.op("dve", lambda e: e.tensor_tensor(out=o_s, in0=o_s, in1=rstd.unsqueeze(1).to_broadcast([128, 4, NT]), op=ALU.mult),
                         r=[tsm, to], w=[to])
                    P.op("dve", lambda e: e.tensor_tensor(out=o_s, in0=o_s, in1=nmr.unsqueeze(1).to_broadcast([128, 4, NT]), op=ALU.add),
                         r=[tsm, to], w=[to])
                    for e2 in range(2):
                        wg, twg = wring.next()
                        self.load_w(wg, w_in[:, :, 4096 + h * 512 + e2 * 256:4096 + h * 512 + (e2 + 1) * 256], twg)
                        for e1 in range(2):
                            ec = e2 * 2 + e1
                            psg, tpg = self.pb[e1], self.tpb[e1]
                            self.proj(psg, tpg, wg, twg, slice(e1 * 128, (e1 + 1) * 128), x_b, txb)
                            P.op("act", lambda e, psg=psg: e.activation(out=A1, in_=psg, func=AF.Silu), r=[tpg], w=[tA])
                            P.op("dve", lambda e, ec=ec, h=h: e.tensor_tensor(out=m_b[:, h * 4 + ec, :], in0=o_s[:, ec, :], in1=A1, op=ALU.mult),
                                 r=[to, tA], w=[tm])
            if mode == "B":
                self.mixer_out(li, ti, m_b, tm, 16, w_out, sc)
        if self.fused:
            self.carry_save("S2", S.rearrange("p a b c -> p (a b c)"), tS, 4096)
        elif mode == "A":
            st = self.dout("st_loc2", [128, 4096])
            for h in range(H):
                for dc in range(2):
                    o0 = (dc * H + h) * 512
                    P.dma("sp", st[:, o0:o0 + 512], S[:, dc, h, :], r=[tS[h]])
        self.phase_end()

    def finalize(self):
        self.P.finalize()
        return self.nc


class Host:
    def __init__(self, inputs):
        self.inp = {k: np.asarray(v) for k, v in inputs.items()}
        self.cache = {}
        v = np.zeros((128, NVEC), np.float32)

        def put(name, arr):
            o, n = VLAY[name]
            v[:, o:o + n] = _fm(arr)
        I = self.inp
        for j in range(4):
            put(f"rg_conv_w{j}", I["rg_conv_w"][0, j])
        put("rg_conv_b", I["rg_conv_b"][0])
        put("rg_b_a", I["rg_b_a"][0])
        put("rg_b_x", I["rg_b_x"][0])
        put("rg_lambda", I["rg_lambda"][0])
        for i in range(4):
            put(f"hg_lb{i}", I["hg_lb_logits"][i])
            put(f"ln_mix_g{i}", I["ln_mix_g"][i])
            put(f"ln_mix_b{i}", I["ln_mix_b"][i])
            put(f"ln_ffn_g{i}", I["ln_ffn_g"][i])
            put(f"ln_ffn_b{i}", I["ln_ffn_b"][i])
        put("gla_b_gate", I["gla_b_gate"][0])
        self.vecs = v
        self.ident = np.eye(128, dtype=np.float32)
        selE = np.zeros((8, 8, 128), np.float32)
        for e in range(8):
            selE[e, e, :] = 1.0
        self.selE = selE.reshape(8, 1024)

    @staticmethod
    def fm_act(a):
        t, c = a.shape
        return np.ascontiguousarray(a.T.reshape(c // 128, 128, t).transpose(1, 0, 2))

    def get(self, name, core, xcur=None, st_all=None):
        I = self.inp
        b, j = core // 4, core % 4
        t0 = j * T
        if name == "keep":
            k = np.zeros((128, 8), np.float32)
            for q in range(4):
                if q > 3 - j:
                    k[:, q] = 1.0
            return k
        if name[:-1].endswith("_") and name[-1].isdigit() and (name.startswith("xT_") or name.startswith("pT") or name.startswith("tabc_")):
            q = int(name[-1])
            ch = max(q - (3 - j), 0)
            t0 = ch * T
            base = name[:-2]
            if base == "xT":
                return self.fm_act(I["x"][b, t0:t0 + T])
            name = base
        if name == "xT":
            return xcur[core]
        if name == "vecs":
            return self.vecs
        if name == "ident_f":
            return self.ident
        if name == "selE":
            return self.selE
        if name == "tab_reset":
            r = np.ones((128, NT), np.float32)
            r[:, ::32] = 0.0
            return r
        if name == "tab_maskT":
            i = np.arange(128)
            return ((i[:, None] // 32 == i[None, :] // 32) & (i[:, None] <= i[None, :])).astype(np.float32)
        if name == "tab_rowmask":
            i = np.arange(128)
            return (i[:, None] // 32 == np.arange(4)[None, :]).astype(np.float32)
        if name in ("tab_zeta", "tab_xi", "tab_dmat"):
            out = np.zeros((128, 4, 128), np.float64)
            i = np.arange(128, dtype=np.float64)
            for h in range(4):
                g = 1.0 - 2.0 ** (-5.0 - h)
                if name == "tab_zeta":
                    out[:, h, :] = (g ** (127.0 - i))[None, :] / 16.0
                elif name == "tab_xi":
                    out[:, h, :] = (g ** (i + 1.0))[None, :]
                else:
                    rel = i[None, :] - i[:, None]
                    out[:, h, :] = np.where(rel >= 0, g ** np.maximum(rel, 0.0), 0.0) / 16.0
            return out.astype(np.float32)
        if name in ("tabc_cos", "tabc_sin"):
            inv = (np.float32(10000.0) ** (-(np.arange(0, 256, 2, dtype=np.float32)) / np.float32(256))).astype(np.float32)
            pos = np.arange(t0, t0 + T, dtype=np.float32)
            ang = (pos[None, :] * inv[:, None]).astype(np.float32).astype(np.float64)
            return (np.cos(ang) if name == "tabc_cos" else np.sin(ang)).astype(np.float32)
        if name == "sel":
            s = np.zeros((128, 8), np.float32)
            for r in range(8):
                if r // 4 == b and r % 4 < j:
                    s[:, r] = 1.0
            return s
        if name == "xh":
            h = np.zeros((4, D), np.float32)
            if j > 0:
                h[0:3] = I["x"][b, t0 - 3:t0]
            return self.fm_act(h)
        if name.startswith("pT"):
            li = int(name[2:])
            return self.fm_act(I["p"][li, b, t0:t0 + T])
        if name.startswith("st_all"):
            return st_all[name]
        if name in I and name not in ("x", "p"):
            a = I[name]
            return a[0] if a.shape[0] == 1 and name not in ("moe_w_in", "moe_w_out") else a
        for base in ("dense_w_in", "dense_w_out", "moe_w_router", "moe_w_in", "moe_w_out", "ple_w_proj", "ple_w_gate"):
            if name.startswith(base) and name[len(base):].isdigit():
                idx = int(name[len(base):])
                a = I[base]
                if base in ("dense_w_in", "dense_w_out"):
                    return a[idx:idx + 1]
                return a[idx]
        raise KeyError(name)


def run_launch(host, stage_list, xcur=None, st_all=None, trace=False, fused=False):
    kb = KB(stage_list, fused=fused)
    for st in stage_list:
        getattr(kb, st[0])(*st[1:])
    names = list(kb.inputs.keys())
    nc = kb.finalize()
    print("[kernel] instr counts", dict(kb.P.cnt), "sems", kb.P.n_sems, flush=True)
    in_maps = []
    for c in range(NCORES):
        m = {}
        for n in names:
            key = (n, c)
            per_core = n in ("xT", "sel", "xh", "keep") or n.startswith("xT_") or n.startswith("pT") or n.startswith("st_all") or n.startswith("tabc_")
            if per_core:
                m[n] = np.ascontiguousarray(host.get(n, c, xcur, st_all), dtype=np.float32)
            else:
                if n not in host.cache:
                    host.cache[n] = np.ascontiguousarray(host.get(n, 0, xcur, st_all), dtype=np.float32)
                m[n] = host.cache[n]
        in_maps.append(m)
    res = run_bass_kernel_spmd(nc, in_maps, core_ids=list(range(NCORES)), trace=trace)
    return res


PASSES = ["rglru_pass", "hgrn2_pass", "ret_pass", "gla_pass"]


def kernel(**inputs):
    host = Host(inputs)
    res = run_launch(host, fused_stages(), fused=True)
    out = np.zeros((2, SEQ, D), np.float32)
    for c in range(NCORES):
        out[c // 4, (c % 4) * T:(c % 4 + 1) * T] = res.results[c]["yT"].transpose(2, 1, 0).reshape(T, D)
    return out


def fused_stages():
    st = []
    for q in range(4):
        st += [("set_pass", q), ("load_x",), ("rglru_pass", "B"), ("ffn_phase", 0), ("hgrn2_pass", "B"), ("ffn_phase", 1),
               ("ret_pass", "B"), ("ffn_phase", 2)]
        if q == 3:
            st += [("gla_pass", "B"), ("ffn_phase", 3), ("store_x",)]
        else:
            st += [("gla_pass", "A")]
    return st


def kernel_unfused(**inputs):
    host = Host(inputs)
    x = np.asarray(inputs["x"], np.float32)
    xcur = [Host.fm_act(x[c // 4, (c % 4) * T:(c % 4 + 1) * T]) for c in range(NCORES)]
    res = run_launch(host, [("load_x",), (PASSES[0], "A")], xcur=xcur)
    st = np.stack([res.results[c]["st_loc0"] for c in range(NCORES)])
    for i in range(4):
        stages = [("load_x",), (PASSES[i], "B"), ("ffn_phase", i)]
        if i < 3:
            stages.append((PASSES[i + 1], "A"))
        stages.append(("store_x",))
        res = run_launch(host, stages, xcur=xcur, st_all={f"st_all{i}": st})
        xcur = [res.results[c]["yT"] for c in range(NCORES)]
        if i < 3:
            st = np.stack([res.results[c][f"st_loc{i + 1}"] for c in range(NCORES)])
    out = np.zeros((2, SEQ, D), np.float32)
    for c in range(NCORES):
        out[c // 4, (c % 4) * T:(c % 4 + 1) * T] = xcur[c].transpose(2, 1, 0).reshape(T, D)
    return out
```

```python
from contextlib import ExitStack
import math
import numpy as np
import ml_dtypes
import concourse.bass as bass
import concourse.mybir as mybir
from concourse.bass_utils import run_bass_kernel_spmd

F32 = mybir.dt.float32
BF16 = mybir.dt.bfloat16
AF = mybir.ActivationFunctionType
ALU = mybir.AluOpType
AX = mybir.AxisListType

NCORES = 8
D = 1024
SEQ = 8192
T = 2048
NT = 512
NTILE = T // NT
ALPHA = 8.0 ** 0.25
EPS = 1e-5
FFN_DENSE = 2816
FFN_EXPERT = 3584
ARENA_BYTES = 206 * 1024
DEBUG = False
CC_INC = 1
DEBUG_TI = 0


class Tok:
    __slots__ = ("name", "w", "r")

    def __init__(self, name):
        self.name = name
        self.w = None
        self.r = {}


class Prog:
    ENGS = ("pe", "act", "dve", "pool", "sp")

    def __init__(self, nc):
        self.nc = nc
        self.stack = ExitStack()
        self.streams = {e: [] for e in self.ENGS}
        self.cnt = {e: 0 for e in self.ENGS}
        self.waited = {e: {} for e in self.ENGS}
        self.needed = {e: set() for e in self.ENGS}
        self.slot_of = {}
        self.slot_total = []
        self.free_slots = []
        self.ntok = 0

    def _slot(self, key):
        if key not in self.slot_of:
            if self.free_slots:
                sl = self.free_slots.pop()
            else:
                sl = len(self.slot_total)
                self.slot_total.append(0)
            self.slot_of[key] = sl
        return self.slot_of[key]

    def release_keys(self):
        self.free_slots.extend(sorted(set(self.slot_of.values()), reverse=True))
        self.slot_of.clear()

    def tok(self, name=None):
        self.ntok += 1
        return Tok(name or f"t{self.ntok}")

    def toks(self, n, name="t"):
        return [self.tok(f"{name}{i}") for i in range(n)]

    def _deps(self, r, w):
        deps = []
        for t in r:
            if t.w is not None:
                deps.append(t.w)
        for t in w:
            if t.w is not None:
                deps.append(t.w)
            deps.extend(t.r.values())
        return deps

    def _emit_waits(self, eng, deps, skip_self=False):
        best = {}
        for d in deps:
            k = (d[0], d[1])
            if skip_self and d[0] == "e" and d[1] == eng:
                continue
            if best.get(k, 0) < d[2]:
                best[k] = d[2]
        wd = self.waited[eng]
        for k, v in best.items():
            if wd.get(k, 0) < v:
                wd[k] = v
                if k[0] == "e":
                    self.needed[k[1]].add(v)
                self.streams[eng].append(("wait", (k[0], k[1], v)))

    def op(self, eng, fn, r=(), w=(), skip_self=False):
        self._emit_waits(eng, self._deps(r, w), skip_self=skip_self)
        self.cnt[eng] += 1
        idx = self.cnt[eng]
        self.streams[eng].append(("op", fn, idx))
        me = ("e", eng, idx)
        for t in r:
            t.r[("e", eng)] = me
        for t in w:
            t.w = me
            t.r = {}
        return idx

    def raw(self, eng, fn):
        self.streams[eng].append(("raw", fn))

    def dma(self, q, out, in_, r=(), w=(), key=None):
        self._emit_waits(q, self._deps(r, w))
        if key is None:
            key = (w[0] if len(w) else r[0])
        sl = self._slot(key)
        self.slot_total[sl] += 16
        c = self.slot_total[sl]
        self.streams[q].append(("dma", (out, in_), sl))
        me = ("d", sl, c)
        for t in r:
            t.r[("d", sl)] = me
        for t in w:
            t.w = me
            t.r = {}

    def collective(self, ins_ap, outs_ap, r, w):
        self._emit_waits("pool", self._deps(r, w))
        sl = self._slot(w[0])
        self.slot_total[sl] += CC_INC
        c = self.slot_total[sl]
        self.streams["pool"].append(("cc", (ins_ap, outs_ap), sl))
        me = ("d", sl, c)
        for t in r:
            t.r[("d", sl)] = me
        for t in w:
            t.w = me
            t.r = {}

    def barrier(self):
        for e in self.ENGS:
            deps = [("e", f, self.cnt[f]) for f in self.ENGS if f != e and self.cnt[f] > 0]
            deps += [("d", k, c) for k, c in enumerate(self.slot_total) if c > 0]
            self._emit_waits(e, deps)

    def finalize(self):
        nc = self.nc
        self.barrier()
        esem = {}
        for e in self.ENGS:
            if self.needed[e]:
                esem[e] = self.stack.enter_context(nc.semaphore(f"es_{e}"))
        dsem = {}
        for i in range(len(self.slot_total)):
            dsem[i] = self.stack.enter_context(nc.semaphore(f"ds_{i}"))
        rank = {}
        for e in self.ENGS:
            s = sorted(self.needed[e])
            rank[e] = {v: i + 1 for i, v in enumerate(s)}
        self.n_sems = len(esem) + len(dsem)

        def replay(e, h):
            for ent in self.streams[e]:
                if ent[0] == "wait":
                    kind, k, v = ent[1]
                    if kind == "e":
                        h.wait_ge(esem[k], rank[k][v])
                    else:
                        h.wait_ge(dsem[k], v)
                elif ent[0] == "op":
                    ins = ent[1](h)
                    if ent[2] in rank[e]:
                        ins.then_inc(esem[e], 1)
                elif ent[0] == "raw":
                    ent[1](h)
                elif ent[0] == "cc":
                    ins_ap, outs_ap = ent[1]
                    h.collective_compute("AllGather", ALU.bypass, replica_groups=[list(range(NCORES))],
                                         ins=[ins_ap], outs=[outs_ap]).then_inc(dsem[ent[2]], CC_INC)
                else:
                    out, in_ = ent[1]
                    h.dma_start(out=out, in_=in_).then_inc(dsem[ent[2]], 16)

        with nc.Block() as block:
            @block.tensor
            def _(h):
                replay("pe", h)

            @block.scalar
            def _(h):
                replay("act", h)

            @block.vector
            def _(h):
                replay("dve", h)

            @block.gpsimd
            def _(h):
                replay("pool", h)

            @block.sync
            def _(h):
                replay("sp", h)
        self.stack.close()


class Arena:
    def __init__(self, P, nbytes):
        self.t = P.stack.enter_context(P.nc.sbuf_tensor("arena", [128, nbytes // 4], F32))
        self.n = nbytes // 4
        self.off = 0

    def alloc(self, shape, dtype=F32, parts=128):
        n = 1
        for s in shape:
            n *= s
        words = (n + 1) // 2 if dtype == BF16 else n
        words = (words + 7) // 8 * 8
        assert self.off + words <= self.n, f"arena overflow: need {words*4}B at {self.off*4}B"
        v = self.t[0:parts, self.off:self.off + words]
        self.off += words
        if dtype == BF16:
            v = v.bitcast(BF16)
        v = v[:, 0:n]
        if len(shape) == 2:
            v = v.rearrange("p (a b) -> p a b", a=shape[0])
        elif len(shape) == 3:
            v = v.rearrange("p (a b c) -> p a b c", a=shape[0], b=shape[1])
        elif len(shape) == 4:
            v = v.rearrange("p (a b c d) -> p a b c d", a=shape[0], b=shape[1], c=shape[2])
        return v

    def mark(self):
        return self.off

    def reset(self, m):
        self.off = m


class Ring:
    def __init__(self, P, bufs, name):
        self.bufs = bufs
        self.toks = P.toks(len(bufs), name)
        self.i = 0

    def next(self):
        b, t = self.bufs[self.i], self.toks[self.i]
        self.i = (self.i + 1) % len(self.bufs)
        return b, t


def _vec_layout():
    lay = {}
    off = 0

    def add(name, n):
        nonlocal off
        lay[name] = (off, n)
        off += n
    for j in range(4):
        add(f"rg_conv_w{j}", 8)
    add("rg_conv_b", 8)
    add("rg_b_a", 8)
    add("rg_b_x", 8)
    add("rg_lambda", 8)
    for i in range(4):
        add(f"hg_lb{i}", 8)
    add("gla_b_gate", 4)
    for i in range(4):
        add(f"ln_mix_g{i}", 8)
        add(f"ln_mix_b{i}", 8)
        add(f"ln_ffn_g{i}", 8)
        add(f"ln_ffn_b{i}", 8)
    return lay, off


VLAY, NVEC = _vec_layout()


def _fm(v):
    v = np.asarray(v, np.float32).reshape(-1)
    return np.ascontiguousarray(v.reshape(-1, 128).T)


class KB:
    def __init__(self, stages, fused=False):
        self.nc = bass.Bass("TRN2", target_bir_lowering=False)
        self.P = Prog(self.nc)
        self.stages = stages
        self.fused = fused
        self.cur_q = 0
        self.scr = {}
        self.inputs = {}
        self.outputs = {}
        P = self.P
        self.A = Arena(P, ARENA_BYTES)
        A = self.A
        self.pb = [P.stack.enter_context(self.nc.psum_tensor(f"pb{i}", [128, 512], F32))[:] for i in range(7)]
        self.pbT = P.stack.enter_context(self.nc.psum_tensor("pbT", [128, 1024], BF16))[:]
        self.tpb = P.toks(7, "pb")
        self.tpbT = P.tok("pbT")
        self.x_f = A.alloc([8, T], F32)
        self.tx = P.toks(NTILE, "x")
        self.vecs = A.alloc([NVEC], F32)
        self.tvec = P.tok("vecs")
        self.ident_b = A.alloc([128], BF16)
        self.ident_f = A.alloc([128], F32)
        self.ones_b = A.alloc([128], BF16)
        self.sel = A.alloc([8], F32)
        self.tconst = P.tok("const")
        P.dma("sp", self.vecs, self.din("vecs", [128, NVEC]), w=[self.tvec])
        P.dma("sp", self.ident_f, self.din("ident_f", [128, 128]), w=[self.tconst])
        P.dma("sp", self.sel, self.din("keep" if fused else "sel", [128, 8]), w=[self.tconst])
        P.dma("pool", self.ident_b, self.inputs["ident_f"], w=[self.tconst])
        P.op("dve", lambda e: e.memset(self.ones_b, 1.0), w=[self.tconst])
        self.base_mark = A.mark()

    def din(self, name, shape, dtype=F32):
        if name not in self.inputs:
            self.inputs[name] = self.nc.dram_tensor(name, list(shape), dtype, kind="ExternalInput").ap()
        return self.inputs[name]

    def dout(self, name, shape, dtype=F32):
        if name not in self.outputs:
            self.outputs[name] = self.nc.dram_tensor(name, list(shape), dtype, kind="ExternalOutput").ap()
        return self.outputs[name]

    def vcol(self, name, i=0, n=1):
        o, _ = VLAY[name]
        return self.vecs[:, o + i:o + i + n]

    def pin_lnexp(self):
        return

    def set_pass(self, q):
        self.cur_q = q

    def carry_load(self, name, dst, toks, n):
        P = self.P
        if self.cur_q == 0:
            P.op("dve", lambda e: e.memset(dst, 0.0), w=list(toks))
            return
        sc, tsc = self.scr[name]
        P.dma("sp", dst, sc, r=[tsc], w=list(toks), key=toks[0])
        kq = self.sel[:, self.cur_q:self.cur_q + 1]
        P.op("dve", lambda e: e.tensor_scalar(out=dst, in0=dst, scalar1=kq, scalar2=None, op0=ALU.mult),
             r=[self.tconst], w=list(toks))

    def carry_save(self, name, src, toks, n):
        P = self.P
        if name not in self.scr:
            self.scr[name] = (self.nc.dram_tensor("scr_" + name, [128, n], F32).ap(), P.tok("scr_" + name))
        sc, tsc = self.scr[name]
        P.dma("sp", sc, src, r=list(toks), w=[tsc], key=tsc)

    def dbg(self, name, ap, tok, shape):
        o = self.dout(name, shape)
        self.P.dma("sp", o, ap, r=[tok])

    def phase_end(self):
        self.P.barrier()
        self.P.release_keys()
        self.A.reset(self.base_mark)

    def load_w(self, dst, src, tok):
        self.P.dma("pool", dst, src, w=[tok])

    def proj(self, ps, tps, wslot, tw, cols, xb, txb, n=NT, kcs=8, extra_r=()):
        P = self.P
        for kc in range(kcs):
            P.op("pe", lambda e, kc=kc: e.matmul(ps, lhsT=wslot[:, kc, cols], rhs=xb[:, kc, :],
                                                 start=(kc == 0), stop=(kc == kcs - 1)),
                 r=[tw, txb] + list(extra_r), w=[tps], skip_self=True)

    def load_x(self):
        xT = self.din(f"xT_{self.cur_q}" if self.fused else "xT", [128, 8, T])
        for ti in range(NTILE):
            self.P.dma("sp", self.x_f[:, :, ti * NT:(ti + 1) * NT], xT[:, :, ti * NT:(ti + 1) * NT], w=[self.tx[ti]])

    def store_x(self):
        yT = self.dout("yT", [128, 8, T])
        for ti in range(NTILE):
            self.P.dma("sp", yT[:, :, ti * NT:(ti + 1) * NT], self.x_f[:, :, ti * NT:(ti + 1) * NT], r=[self.tx[ti]])

    def ln_alloc(self):
        A, P = self.A, self.P
        d = dict(mean=A.alloc([NT]), var=A.alloc([NT]), rstd=A.alloc([NT]), nmr=A.alloc([NT]), t=P.tok("lnsm"))
        return d

    def layer_norm(self, z, tz, zb, tzb, zsq, tzsq, sm, gname, bname, out_f, tout, out_b=None, tout_b=None,
                   pbs=(5, 6)):
        P = self.P
        tz = list(tz)
        ps_s, ts_s = self.pb[pbs[0]], self.tpb[pbs[0]]
        ps_q, ts_q = self.pb[pbs[1]], self.tpb[pbs[1]]
        P.op("dve", lambda e: e.tensor_copy(out=zb, in_=z), r=tz, w=[tzb])
        P.op("act", lambda e: e.activation(out=zsq, in_=z, func=AF.Square), r=tz, w=[tzsq])
        for kc in range(8):
            P.op("pe", lambda e, kc=kc: e.matmul(ps_s, lhsT=self.ones_b, rhs=zb[:, kc, :], start=(kc == 0), stop=(kc == 7)),
                 r=[tzb, self.tconst], w=[ts_s], skip_self=True)
        for kc in range(8):
            P.op("pe", lambda e, kc=kc: e.matmul(ps_q, lhsT=self.ones_b, rhs=zsq[:, kc, :], start=(kc == 0), stop=(kc == 7)),
                 r=[tzsq, self.tconst], w=[ts_q], skip_self=True)
        mean, var, rstd, nmr, tsm = sm["mean"], sm["var"], sm["rstd"], sm["nmr"], sm["t"]
        epsc, tcc = self._eps, self.tcc
        P.op("act", lambda e: e.mul(out=mean, in_=ps_s, mul=1.0 / D), r=[ts_s], w=[tsm])
        P.op("act", lambda e: e.activation(out=nmr, in_=ps_s, func=AF.Square, scale=1.0 / D), r=[ts_s], w=[tsm])
        P.op("dve", lambda e: e.scalar_tensor_tensor(out=var, in0=ps_q, scalar=1.0 / D, in1=nmr, op0=ALU.mult, op1=ALU.subtract),
             r=[ts_q, tsm], w=[tsm])
        self.pin_lnexp()
        P.op("act", lambda e: e.activation(out=var, in_=var, func=AF.Ln, bias=epsc, scale=1.0), r=[tsm, tcc], w=[tsm])
        P.op("act", lambda e: e.activation(out=rstd, in_=var, func=AF.Exp, scale=-0.5), r=[tsm], w=[tsm])
        P.op("dve", lambda e: e.scalar_tensor_tensor(out=nmr, in0=mean, scalar=-1.0, in1=rstd, op0=ALU.mult, op1=ALU.mult),
             r=[tsm], w=[tsm])
        for kc in range(8):
            P.op("dve", lambda e, kc=kc: e.tensor_tensor(out=z[:, kc, :], in0=z[:, kc, :], in1=rstd, op=ALU.mult),
                 r=[tsm, tz[kc]], w=[tz[kc]])
            P.op("dve", lambda e, kc=kc: e.tensor_tensor(out=z[:, kc, :], in0=z[:, kc, :], in1=nmr, op=ALU.add),
                 r=[tsm, tz[kc]], w=[tz[kc]])
            P.op("act", lambda e, kc=kc: e.activation(out=out_f[:, kc, :], in_=z[:, kc, :], func=AF.Identity,
                                                      scale=self.vcol(gname, kc), bias=self.vcol(bname, kc)),
                 r=[tz[kc], self.tvec], w=[tout])
            if out_b is not None:
                if kc % 2 == 0:
                    P.op("dve", lambda e, kc=kc: e.tensor_scalar(out=out_b[:, kc, :], in0=z[:, kc, :], scalar1=self.vcol(gname, kc),
                                                                 scalar2=self.vcol(bname, kc), op0=ALU.mult, op1=ALU.add),
                         r=[tz[kc], self.tvec], w=[tout_b])
                else:
                    P.op("act", lambda e, kc=kc: e.activation(out=out_b[:, kc, :], in_=z[:, kc, :], func=AF.Identity,
                                                              scale=self.vcol(gname, kc), bias=self.vcol(bname, kc)),
                         r=[tz[kc], self.tvec], w=[tout_b])

    def mixer_out(self, li, ti, m_b, tm, nvc, w_out, sc):
        P = self.P
        z, tz = sc["z"], sc["tz"]
        xt = self.x_f[:, :, ti * NT:(ti + 1) * NT]
        wv = w_out.rearrange("(c p) n -> p c n", p=128)
        sw = 128 if nvc > 8 else 256
        for half in range(1024 // sw):
            slot, tw = sc["wout_ring"].next()
            self.load_w(slot[:, 0:nvc, :], wv[:, :, half * sw:(half + 1) * sw], tw)
            for m2 in range(sw // 128):
                mo = half * (sw // 128) + m2
                pbi = mo % 2
                ps, tps = self.pb[pbi], self.tpb[pbi]
                for vc in range(nvc):
                    P.op("pe", lambda e, vc=vc, m2=m2, ps=ps, slot=slot: e.matmul(
                        ps, lhsT=slot[:, vc, m2 * 128:(m2 + 1) * 128], rhs=m_b[:, vc, :], start=(vc == 0), stop=(vc == nvc - 1)),
                        r=[tw, tm], w=[tps], skip_self=True)
                P.op("dve", lambda e, mo=mo, ps=ps: e.scalar_tensor_tensor(
                    out=z[:, mo, :], in0=xt[:, mo, :], scalar=ALPHA, in1=ps, op0=ALU.mult, op1=ALU.add),
                    r=[tps, self.tx[ti]], w=[tz[mo]])
        self.layer_norm(z, tz, sc["zb"], sc["tzb"], sc["zsq"], sc["tzsq"], sc["sm"], f"ln_mix_g{li}", f"ln_mix_b{li}",
                        xt, self.tx[ti])

    def mixer_scratch(self, nvc_max, zb, tzb, zsq, tzsq, sw=256):
        A, P = self.A, self.P
        sc = {}
        sc["z"] = A.alloc([8, NT]); sc["tz"] = P.toks(8, "z")
        sc["zb"] = zb; sc["tzb"] = tzb
        sc["zsq"] = zsq; sc["tzsq"] = tzsq
        sc["sm"] = self.ln_alloc()
        sc["wout_ring"] = Ring(P, [A.alloc([nvc_max, sw], BF16) for _ in range(2)], "wout")
        return sc

    def new_consts(self):
        P = self.P
        c = self.A.alloc([4], F32)
        tc = P.tok("cc")
        P.op("dve", lambda e: e.memset(c[:, 0:1], EPS), w=[tc])
        P.op("dve", lambda e: e.memset(c[:, 1:2], 1.0), w=[tc])
        self._eps = c[:, 0:1]
        self._one = c[:, 1:2]
        self.tcc = tc

    def ffn_phase(self, li):
        P, A = self.P, self.A
        moe = (li % 2 == 1)
        NE = 8 if moe else 1
        F = FFN_EXPERT if moe else FFN_DENSE
        NF = F // 128
        G = 4
        self.new_consts()
        if moe:
            w_in_all = self.din(f"moe_w_in{li // 2}", [8, D, 2 * F])
            w_out_all = self.din(f"moe_w_out{li // 2}", [8, F, D])
            w_r = self.din(f"moe_w_router{li // 2}", [D, 8])
        else:
            w_in_all = self.din(f"dense_w_in{li // 2}", [1, D, 2 * F])
            w_out_all = self.din(f"dense_w_out{li // 2}", [1, F, D])
        wg_d = self.din(f"ple_w_gate{li}", [D, D]).rearrange("(c p) n -> p c n", p=128)
        wp_d = self.din(f"ple_w_proj{li}", [256, D]).rearrange("(c p) n -> p c n", p=128)
        pT = self.din(f"pT{li}_{self.cur_q}" if self.fused else f"pT{li}", [128, 2, T])

        TT = 1024
        xn_b = A.alloc([2, 8, NT], BF16)
        txn = P.toks(2, "xn")
        a_b = A.alloc([G, 2, NT], BF16)
        ta = P.tok("a")
        y = A.alloc([2, 8, NT], F32)
        ty = [P.toks(8, f"y{st}") for st in range(2)]
        win_ring = Ring(P, [A.alloc([8, 2, 256], BF16) for _ in range(3)], "win")
        wout_ring = Ring(P, [A.alloc([G, D], BF16) for _ in range(2)], "wo")
        sg_ring = Ring(P, [A.alloc([NT], F32) for _ in range(2)], "sg")
        sm = self.ln_alloc()
        wg_ring = Ring(P, [A.alloc([8, 256], BF16) for _ in range(2)], "wg")
        wp_b = A.alloc([2, D], BF16)
        twp = P.tok("wp")
        p_b = A.alloc([2, NT], BF16)
        tp = P.tok("p")
        tmp_ring = Ring(P, [A.alloc([NT], F32) for _ in range(2)], "tmp")
        self.load_w(wp_b, wp_d, twp)
        if moe:
            wr_f = A.alloc([8, 8], F32)
            twr = P.tok("wr")
            P.dma("sp", wr_f, w_r.rearrange("(c p) n -> p c n", p=128), w=[twr])
            ones8 = A.alloc([128], F32, parts=8)
            P.op("dve", lambda e: e.memset(ones8, 1.0), w=[twr])
            gm = A.alloc([NT], F32, parts=8)
            tgm = P.tok("gm")
            lg_s = A.alloc([NT], F32, parts=8)
            tlg = P.tok("lg")
            lt = A.alloc([4, 8], F32)
            mx = A.alloc([4, 8], F32)
            ex = A.alloc([4, 8], F32)
            msk = A.alloc([4, 8], F32)
            den = A.alloc([4], F32)
            trt = P.tok("rt")
            g_fm = A.alloc([2, NT], F32, parts=8)
            tgf = P.tok("gfm")
            gate_b = A.alloc([2, NT], BF16)
            tgb = P.tok("gb")

        groups = [(f0, min(G, NF - f0)) for f0 in range(0, NF, G)]
        for tt in range(2):
            tiles = [tt * 2, tt * 2 + 1]
            for st in range(2):
                ti = tiles[st]
                P.op("act", lambda e, st=st, ti=ti: e.copy(out=xn_b[:, st], in_=self.x_f[:, :, ti * NT:(ti + 1) * NT]),
                     r=[self.tx[ti]], w=[txn[st]])
            if moe:
                for st in range(2):
                    ti = tiles[st]
                    xt = self.x_f[:, :, ti * NT:(ti + 1) * NT]
                    ps, tps = self.pb[6], self.tpb[6]
                    for kc in range(8):
                        P.op("pe", lambda e, kc=kc, xt=xt: e.matmul(ps[0:8, :], lhsT=wr_f[:, kc, :], rhs=xt[:, kc, :],
                                                                    start=(kc == 0), stop=(kc == 7)),
                             r=[twr, self.tx[ti]], w=[tps], skip_self=True)
                    P.op("act", lambda e: e.copy(out=lg_s, in_=ps[0:8, :]), r=[tps], w=[tlg])
                    pst = self.pb[6][:, 0:32].rearrange("p (a b) -> p a b", a=4)
                    for blk in range(4):
                        P.op("pe", lambda e, blk=blk: e.transpose(out=pst[:, blk, :], in_=lg_s[:, blk * 128:(blk + 1) * 128],
                                                                  identity=self.ident_f[0:8, 0:8]),
                             r=[tlg, self.tconst], w=[tps], skip_self=True)
                    P.op("dve", lambda e: e.tensor_copy(out=lt, in_=pst), r=[tps], w=[trt])
                    for blk in range(4):
                        P.op("dve", lambda e, blk=blk: e.max(out=mx[:, blk, :], in_=lt[:, blk, :]), r=[trt], w=[trt])
                    P.op("dve", lambda e: e.tensor_tensor(out=ex, in0=lt, in1=mx[:, :, 0:1].to_broadcast([128, 4, 8]), op=ALU.subtract),
                         r=[trt], w=[trt])
                    P.op("act", lambda e: e.activation(out=ex, in_=ex, func=AF.Exp), r=[trt], w=[trt])
                    P.op("dve", lambda e: e.tensor_tensor(out=msk, in0=lt, in1=mx[:, :, 1:2].to_broadcast([128, 4, 8]), op=ALU.is_ge),
                         r=[trt], w=[trt])
                    P.op("dve", lambda e: e.tensor_tensor(out=ex, in0=ex, in1=msk, op=ALU.mult), r=[trt], w=[trt])
                    P.op("dve", lambda e: e.tensor_reduce(out=den, in_=ex, axis=AX.X, op=ALU.add), r=[trt], w=[trt])
                    P.op("dve", lambda e: e.reciprocal(out=den, in_=den), r=[trt], w=[trt])
                    P.op("dve", lambda e: e.tensor_tensor(out=ex, in0=ex, in1=den.unsqueeze(2).to_broadcast([128, 4, 8]), op=ALU.mult),
                         r=[trt], w=[trt])
                    for blk in range(4):
                        P.op("pe", lambda e, blk=blk: e.transpose(out=ps[0:8, blk * 128:(blk + 1) * 128], in_=ex[:, blk, :],
                                                                  identity=self.ident_f),
                             r=[trt, self.tconst], w=[tps], skip_self=True)
                    P.op("act", lambda e, st=st: e.copy(out=g_fm[:, st, :], in_=ps[0:8, :]), r=[tps], w=[tgf])
            for ei in range(NE):
                w_in = w_in_all[ei].rearrange("(c p) n -> p c n", p=128)
                w_out = w_out_all[ei]
                if moe:
                    for st in range(2):
                        ps, tps = self.pb[6], self.tpb[6]
                        P.op("dve", lambda e, st=st, ei=ei: e.tensor_scalar(out=gm, in0=g_fm[:, st, :], scalar1=self.ident_f[0:8, ei:ei + 1],
                                                                          scalar2=None, op0=ALU.mult), r=[tgf, self.tconst], w=[tgm])
                        P.op("pe", lambda e: e.matmul(ps, lhsT=ones8, rhs=gm, start=True, stop=True),
                             r=[tgm, twr], w=[tps], skip_self=True)
                        P.op("act", lambda e, st=st: e.copy(out=gate_b[:, st, :], in_=ps), r=[tps], w=[tgb])
                for gi, (f0, g) in enumerate(groups):
                    for pr in range(0, g, 2):
                        npair = min(2, g - pr)
                        slot, tw = win_ring.next()
                        c0 = (f0 + pr) * 128
                        self.load_w(slot[:, :, 0, 0:npair * 128], w_in[:, :, c0:c0 + npair * 128], tw)
                        self.load_w(slot[:, :, 1, 0:npair * 128], w_in[:, :, F + c0:F + c0 + npair * 128], tw)
                        for ff in range(npair):
                            fi = pr + ff
                            for st in range(2):
                                bi = 2 * st
                                psg, tg_ = self.pb[bi], self.tpb[bi]
                                psu, tu_ = self.pb[bi + 1], self.tpb[bi + 1]
                                for kc in range(8):
                                    P.op("pe", lambda e, kc=kc, ff=ff, st=st, psg=psg, slot=slot: e.matmul(
                                        psg, lhsT=slot[:, kc, 0, ff * 128:(ff + 1) * 128], rhs=xn_b[:, st, kc, :],
                                        start=(kc == 0), stop=(kc == 7)), r=[tw, txn[st]], w=[tg_], skip_self=True)
                                for kc in range(8):
                                    P.op("pe", lambda e, kc=kc, ff=ff, st=st, psu=psu, slot=slot: e.matmul(
                                        psu, lhsT=slot[:, kc, 1, ff * 128:(ff + 1) * 128], rhs=xn_b[:, st, kc, :],
                                        start=(kc == 0), stop=(kc == 7)), r=[tw, txn[st]], w=[tu_], skip_self=True)
                                sg, tsg = sg_ring.next()
                                P.op("act", lambda e, sg=sg, psg=psg: e.activation(out=sg, in_=psg, func=AF.Silu), r=[tg_], w=[tsg])
                                if moe:
                                    P.op("dve", lambda e, sg=sg, st=st: e.tensor_tensor(out=sg, in0=sg, in1=gate_b[:, st, :], op=ALU.mult),
                                         r=[tsg, tgb], w=[tsg])
                                P.op("dve", lambda e, sg=sg, psu=psu, fi=fi, st=st: e.tensor_tensor(
                                    out=a_b[:, fi, st, :], in0=sg, in1=psu, op=ALU.mult), r=[tsg, tu_], w=[ta])
                    wslot, two = wout_ring.next()
                    self.load_w(wslot[:, 0:g, :], w_out[f0 * 128:(f0 + g) * 128, :].rearrange("(g p) n -> p g n", p=128), two)
                    first = (ei == 0 and gi == 0)
                    for st in range(2):
                        for mo in range(8):
                            bi = 4 + (mo % 2)
                            ps, tps = self.pb[bi], self.tpb[bi]
                            for fi in range(g):
                                P.op("pe", lambda e, fi=fi, mo=mo, st=st, ps=ps, wslot=wslot: e.matmul(
                                    ps, lhsT=wslot[:, fi, mo * 128:(mo + 1) * 128], rhs=a_b[:, fi, st, :],
                                    start=(fi == 0), stop=(fi == g - 1)), r=[two, ta], w=[tps], skip_self=True)
                            if first:
                                P.op("act", lambda e, mo=mo, st=st, ps=ps: e.copy(out=y[:, st, mo, :], in_=ps), r=[tps], w=[ty[st][mo]])
                            else:
                                P.op("dve", lambda e, mo=mo, st=st, ps=ps: e.tensor_tensor(
                                    out=y[:, st, mo, :], in0=y[:, st, mo, :], in1=ps, op=ALU.add), r=[tps, ty[st][mo]], w=[ty[st][mo]])
            for st in range(2):
                ti = tiles[st]
                xt = self.x_f[:, :, ti * NT:(ti + 1) * NT]
                yz = y[:, st]
                P.op("dve", lambda e, yz=yz, xt=xt: e.scalar_tensor_tensor(out=yz, in0=xt, scalar=ALPHA, in1=yz,
                                                                           op0=ALU.mult, op1=ALU.add),
                     r=[self.tx[ti]] + ty[st], w=ty[st])
                zb, zsq = xn_b[:, 0], xn_b[:, 1]
                self.layer_norm(yz, ty[st], zb, txn[0], zsq, txn[1], sm, f"ln_ffn_g{li}", f"ln_ffn_b{li}",
                                xt, self.tx[ti], out_b=zb, tout_b=txn[0], pbs=(5, 6))
                xb = zb
                self.load_w(p_b, pT[:, :, ti * NT:(ti + 1) * NT], tp)
                for q4 in range(4):
                    wgs, twg = wg_ring.next()
                    self.load_w(wgs, wg_d[:, :, q4 * 256:(q4 + 1) * 256], twg)
                    for m2 in range(2):
                        mo = q4 * 2 + m2
                        psg, tg_ = self.pb[0 + 2 * m2], self.tpb[0 + 2 * m2]
                        psp, tp_ = self.pb[1 + 2 * m2], self.tpb[1 + 2 * m2]
                        for kc in range(8):
                            P.op("pe", lambda e, kc=kc, m2=m2, psg=psg, wgs=wgs: e.matmul(
                                psg, lhsT=wgs[:, kc, m2 * 128:(m2 + 1) * 128], rhs=xb[:, kc, :], start=(kc == 0), stop=(kc == 7)),
                                r=[twg, txn[0]], w=[tg_], skip_self=True)
                        for k2 in range(2):
                            P.op("pe", lambda e, k2=k2, mo=mo, psp=psp: e.matmul(
                                psp, lhsT=wp_b[:, k2, mo * 128:(mo + 1) * 128], rhs=p_b[:, k2, :], start=(k2 == 0), stop=(k2 == 1)),
                                r=[twp, tp], w=[tp_], skip_self=True)
                        sg, tsg = sg_ring.next()
                        P.op("act", lambda e, sg=sg, psg=psg: e.activation(out=sg, in_=psg, func=AF.Sigmoid), r=[tg_], w=[tsg])
                        tmp, ttmp = tmp_ring.next()
                        P.op("dve", lambda e, sg=sg, psp=psp, tmp=tmp: e.tensor_tensor(out=tmp, in0=sg, in1=psp, op=ALU.mult),
                             r=[tsg, tp_], w=[ttmp])
                        P.op("dve", lambda e, mo=mo, xt=xt, tmp=tmp: e.tensor_tensor(out=xt[:, mo, :], in0=xt[:, mo, :], in1=tmp, op=ALU.add),
                             r=[ttmp, self.tx[ti]], w=[self.tx[ti]])
        self.phase_end()

    def exchange_out(self, li, src, tsrc, W):
        st = self.dout(f"st_loc{li}", [128, W])
        self.P.dma("sp", st, src, r=[tsrc])

    def rglru_pass(self, mode):
        P, A = self.P, self.A
        li = 0
        self.new_consts()
        w_in = self.din("rg_w_in", [D, 2 * D]).rearrange("(c p) n -> p c n", p=128)
        w_a = self.din("rg_w_a", [4, 256, 256])
        w_x = self.din("rg_w_x", [4, 256, 256])
        x_b = A.alloc([8, NT], BF16); txb = P.tok("xb")
        if not self.fused:
            xh = self.din("xh", [128, 8, 4])
            xh_b = A.alloc([8, 4], BF16); txh = P.tok("xh")
            self.load_w(xh_b, xh, txh)
        wa_b = A.alloc([4, 2, 256], BF16); wx_b = A.alloc([4, 2, 256], BF16); twax = P.tok("wax")
        for n in range(4):
            self.load_w(wa_b[:, n], w_a[n].rearrange("(c p) n -> p c n", p=128), twax)
            self.load_w(wx_b[:, n], w_x[n].rearrange("(c p) n -> p c n", p=128), twax)
        w_ring = Ring(P, [A.alloc([8, 256], BF16) for _ in range(3)], "wi")
        rec = A.alloc([2, NT + 3], F32); trec = P.tok("rec")
        halo = A.alloc([8, 3], F32); thalo = P.tok("halo")
        u = A.alloc([2, NT], F32); tu = P.tok("u")
        u_b = A.alloc([2, NT], BF16); tub = P.tok("ub")
        r_s2 = [A.alloc([NT], F32) for _ in range(2)]; i_s2 = [A.alloc([NT], F32) for _ in range(2)]
        a_s2 = [A.alloc([NT], F32) for _ in range(2)]; q_s2 = [A.alloc([NT], F32) for _ in range(2)]
        h_s2 = [A.alloc([NT], F32) for _ in range(2)]
        tg2 = P.toks(2, "gates"); th2 = P.toks(2, "h")
        th = th2[0]
        hst = A.alloc([8], F32); thst = P.tok("hst")
        cl = A.alloc([8], F32); cl2 = A.alloc([8], F32); tcl = P.tok("cl")
        lam = self.vcol("rg_lambda", 0, 8)
        one_c = self._one
        P.op("act", lambda e: e.activation(out=cl, in_=lam, func=AF.Exp, scale=-1.0), r=[self.tvec], w=[tcl])
        P.op("act", lambda e: e.activation(out=cl, in_=cl, func=AF.Ln, bias=one_c.to_broadcast([128, 8]) if False else one_c, scale=1.0), r=[tcl, self.tcc], w=[tcl])
        P.op("dve", lambda e: e.tensor_scalar(out=cl2, in0=cl, scalar1=-16.0, scalar2=None, op0=ALU.mult), r=[tcl], w=[tcl])
        P.op("dve", lambda e: e.tensor_scalar(out=cl, in0=cl, scalar1=-8.0, scalar2=None, op0=ALU.mult), r=[tcl], w=[tcl])
        if mode == "A":
            P.op("dve", lambda e: e.memset(hst, 0.0), w=[thst])
            ptot = A.alloc([8], F32)
            P.op("dve", lambda e: e.memset(ptot, 1.0), w=[thst])
            pt_s = A.alloc([NT], F32)
            zeros = A.alloc([NT], F32)
            P.op("dve", lambda e: e.memset(zeros, 0.0), w=[thst])
        elif self.fused:
            self.carry_load("rg_h", hst, [thst], 8)
            self.carry_load("rg_halo", halo.rearrange("p a b -> p (a b)"), [thalo], 24)
            m_b = A.alloc([8, NT], BF16); tm = P.tok("m")
            gb2 = [A.alloc([NT], F32) for _ in range(2)]; g22 = [A.alloc([NT], F32) for _ in range(2)]; tgb2 = P.toks(2, "gb")
            sc = self.mixer_scratch(8, x_b, txb, m_b, tm)
            w_out = self.din("rg_w_out", [D, D])
        else:
            st_all = self.din("st_all0", [8, 128, 16])
            sta = A.alloc([8, 16], F32); tsta = P.tok("sta")
            P.dma("sp", sta, st_all.rearrange("r p w -> p r w"), w=[tsta])
            P.op("dve", lambda e: e.memset(hst, 0.0), w=[thst])
            dsel = A.alloc([8], F32); hl = A.alloc([8], F32)
            for r in range(8):
                sr = self.sel[:, r:r + 1]
                P.op("dve", lambda e, r=r, sr=sr: e.tensor_scalar(out=dsel, in0=sta[:, r, 8:16], scalar1=-1.0, scalar2=sr,
                                                                 op0=ALU.add, op1=ALU.mult), r=[tsta, self.tconst], w=[thst])
                P.op("dve", lambda e: e.tensor_scalar(out=dsel, in0=dsel, scalar1=1.0, scalar2=None, op0=ALU.add), r=[thst], w=[thst])
                P.op("dve", lambda e, r=r, sr=sr: e.tensor_scalar(out=hl, in0=sta[:, r, 0:8], scalar1=sr, scalar2=None, op0=ALU.mult),
                     r=[tsta, self.tconst], w=[thst])
                P.op("dve", lambda e: e.tensor_tensor(out=hst, in0=hst, in1=dsel, op=ALU.mult), r=[thst], w=[thst])
                P.op("dve", lambda e: e.tensor_tensor(out=hst, in0=hst, in1=hl, op=ALU.add), r=[thst], w=[thst])
            m_b = A.alloc([8, NT], BF16); tm = P.tok("m")
            gb2 = [A.alloc([NT], F32) for _ in range(2)]; g22 = [A.alloc([NT], F32) for _ in range(2)]; tgb2 = P.toks(2, "gb")
            sc = self.mixer_scratch(8, x_b, txb, m_b, tm)
            w_out = self.din("rg_w_out", [D, D])
        for n in range(4 if not self.fused else 0):
            slot, tw = w_ring.next()
            self.load_w(slot, w_in[:, :, D + n * 256:D + (n + 1) * 256], tw)
            for c2 in range(2):
                cc = 2 * n + c2
                ps, tps = self.pb[c2], self.tpb[c2]
                self.proj(ps[:, 0:4], tps, slot, tw, slice(c2 * 128, (c2 + 1) * 128), xh_b, txh)
                P.op("act", lambda e, cc=cc, ps=ps: e.copy(out=halo[:, cc, :], in_=ps[:, 0:3]), r=[tps], w=[thalo])
        for ti in range(NTILE):
            xt = self.x_f[:, :, ti * NT:(ti + 1) * NT]
            P.op("act", lambda e, xt=xt: e.copy(out=x_b, in_=xt), r=[self.tx[ti]], w=[txb])
            for n in range(4):
                slot, tw = w_ring.next()
                self.load_w(slot, w_in[:, :, D + n * 256:D + (n + 1) * 256], tw)
                if mode == "B":
                    gslot, tgw = w_ring.next()
                    self.load_w(gslot, w_in[:, :, n * 256:(n + 1) * 256], tgw)
                for c2 in range(2):
                    cc = 2 * n + c2
                    ps, tps = self.pb[c2], self.tpb[c2]
                    self.proj(ps, tps, slot, tw, slice(c2 * 128, (c2 + 1) * 128), x_b, txb)
                    P.op("dve", lambda e, cc=cc, c2=c2: e.tensor_copy(out=rec[:, c2, 0:3], in_=halo[:, cc, :]), r=[thalo], w=[trec])
                    P.op("act", lambda e, c2=c2, ps=ps: e.copy(out=rec[:, c2, 3:NT + 3], in_=ps), r=[tps], w=[trec])
                    P.op("dve", lambda e, cc=cc, c2=c2: e.tensor_copy(out=halo[:, cc, :], in_=rec[:, c2, NT:NT + 3]), r=[trec], w=[thalo])
                    P.op("act", lambda e, cc=cc, c2=c2: e.activation(out=u[:, c2, :], in_=rec[:, c2, 0:NT], func=AF.Identity,
                                                                     scale=self.vcol("rg_conv_w0", cc), bias=self.vcol("rg_conv_b", cc)),
                         r=[trec, self.tvec], w=[tu])
                    for j in range(1, 4):
                        P.op("dve", lambda e, cc=cc, c2=c2, j=j: e.scalar_tensor_tensor(
                            out=u[:, c2, :], in0=rec[:, c2, j:j + NT], scalar=self.vcol(f"rg_conv_w{j}", cc), in1=u[:, c2, :],
                            op0=ALU.mult, op1=ALU.add), r=[trec, tu, self.tvec], w=[tu])
                    P.op("act", lambda e, c2=c2: e.copy(out=u_b[:, c2, :], in_=u[:, c2, :]), r=[tu], w=[tub])
                def chain(c2, n=n):
                    cc = 2 * n + c2
                    r_s, i_s, a_s, q_s, h_s = r_s2[c2], i_s2[c2], a_s2[c2], q_s2[c2], h_s2[c2]
                    tg, th = tg2[c2], th2[c2]
                    psr, tpr = (self.pb[2], self.tpb[2]) if c2 == 0 else (self.pb[5], self.tpb[5])
                    psi, tpi = (self.pb[3], self.tpb[3]) if c2 == 0 else (self.pb[6], self.tpb[6])
                    for k2 in range(2):
                        P.op("pe", lambda e, k2=k2: e.matmul(psr, lhsT=wa_b[:, n, k2, c2 * 128:(c2 + 1) * 128], rhs=u_b[:, k2, :],
                                                             start=(k2 == 0), stop=(k2 == 1)), r=[twax, tub], w=[tpr], skip_self=True)
                    for k2 in range(2):
                        P.op("pe", lambda e, k2=k2: e.matmul(psi, lhsT=wx_b[:, n, k2, c2 * 128:(c2 + 1) * 128], rhs=u_b[:, k2, :],
                                                             start=(k2 == 0), stop=(k2 == 1)), r=[twax, tub], w=[tpi], skip_self=True)
                    if mode == "B":
                        psg, tpg = self.pb[4 + c2] if c2 == 0 else self.pb[1], self.tpb[4 + c2] if c2 == 0 else self.tpb[1]
                        psg, tpg = (self.pb[4], self.tpb[4]) if c2 == 0 else (self.pb[1], self.tpb[1])
                        self.proj(psg, tpg, gslot, tgw, slice(c2 * 128, (c2 + 1) * 128), x_b, txb)
                    yield
                    P.op("act", lambda e: e.activation(out=r_s, in_=psr, func=AF.Sigmoid, bias=self.vcol("rg_b_a", cc), scale=1.0),
                         r=[tpr, self.tvec], w=[tg])
                    P.op("act", lambda e: e.activation(out=i_s, in_=psi, func=AF.Sigmoid, bias=self.vcol("rg_b_x", cc), scale=1.0),
                         r=[tpi, self.tvec], w=[tg])
                    yield
                    P.op("act", lambda e: e.activation(out=a_s, in_=r_s, func=AF.Exp, scale=cl[:, cc:cc + 1]), r=[tg, tcl], w=[tg])
                    P.op("act", lambda e: e.activation(out=q_s, in_=r_s, func=AF.Exp, scale=cl2[:, cc:cc + 1]), r=[tg, tcl], w=[tg])
                    yield
                    P.op("act", lambda e: e.activation(out=q_s, in_=q_s, func=AF.Sqrt, bias=one_c, scale=-1.0), r=[tg, self.tcc], w=[tg])
                    yield
                    P.op("dve", lambda e: e.tensor_tensor(out=q_s, in0=q_s, in1=i_s, op=ALU.mult), r=[tg], w=[tg])
                    P.op("dve", lambda e: e.tensor_tensor(out=q_s, in0=q_s, in1=u[:, c2, :], op=ALU.mult), r=[tg, tu], w=[tg])
                    P.op("dve", lambda e: e.tensor_tensor_scan(out=h_s, data0=a_s, data1=q_s, initial=hst[:, cc:cc + 1],
                                                               op0=ALU.mult, op1=ALU.add), r=[tg, thst], w=[th])
                    P.op("dve", lambda e: e.tensor_copy(out=hst[:, cc:cc + 1], in_=h_s[:, NT - 1:NT]), r=[th], w=[thst])
                    yield
                    if mode == "A":
                        P.op("dve", lambda e: e.tensor_tensor_scan(out=pt_s, data0=a_s, data1=zeros, initial=ptot[:, cc:cc + 1],
                                                                   op0=ALU.mult, op1=ALU.add), r=[tg, thst, self.tconst], w=[th])
                        P.op("dve", lambda e: e.tensor_copy(out=ptot[:, cc:cc + 1], in_=pt_s[:, NT - 1:NT]), r=[th], w=[thst])
                    else:
                        g2, gb, tgb = g22[c2], gb2[c2], tgb2[c2]
                        P.op("act", lambda e: e.activation(out=g2, in_=psg, func=AF.Square), r=[tpg], w=[tgb])
                        yield
                        P.op("dve", lambda e: e.tensor_scalar(out=g2, in0=g2, scalar1=0.044715, scalar2=1.0, op0=ALU.mult, op1=ALU.add),
                             r=[tgb], w=[tgb])
                        P.op("dve", lambda e: e.tensor_tensor(out=g2, in0=g2, in1=psg, op=ALU.mult), r=[tgb, tpg], w=[tgb])
                        yield
                        P.op("act", lambda e: e.activation(out=g2, in_=g2, func=AF.Sigmoid, scale=1.5957691216057308), r=[tgb], w=[tgb])
                        yield
                        P.op("dve", lambda e: e.tensor_tensor(out=gb, in0=g2, in1=psg, op=ALU.mult), r=[tgb, tpg], w=[tgb])
                        P.op("dve", lambda e: e.tensor_tensor(out=m_b[:, cc, :], in0=gb, in1=h_s, op=ALU.mult), r=[tgb, th], w=[tm])

                gens = [chain(0), chain(1)]
                while gens:
                    for g in list(gens):
                        try:
                            next(g)
                        except StopIteration:
                            gens.remove(g)
            if mode == "B":
                self.mixer_out(li, ti, m_b, tm, 8, w_out, sc)
        if mode == "A":
            stl = A.alloc([16], F32)
            P.op("dve", lambda e: e.tensor_copy(out=stl[:, 0:8], in_=hst), r=[thst], w=[th])
            P.op("dve", lambda e: e.tensor_copy(out=stl[:, 8:16], in_=ptot), r=[thst], w=[th])
            self.exchange_out(0, stl, th, 16)
        if self.fused:
            self.carry_save("rg_h", hst, [thst], 8)
            self.carry_save("rg_halo", halo.rearrange("p a b -> p (a b)"), [thalo], 24)
        self.phase_end()

    def gla_alloc(self, H, dv, mode):
        A, P = self.A, self.P
        c = dict(H=H, dv=dv, dvc=dv // 128, mode=mode)
        c["reset"] = A.alloc([NT], F32)
        c["maskT"] = A.alloc([128], F32)
        c["rowmask"] = A.alloc([4], F32)
        c["ttab"] = P.tok("gtab")
        P.dma("sp", c["reset"], self.din("tab_reset", [128, NT]), w=[c["ttab"]])
        P.dma("sp", c["maskT"], self.din("tab_maskT", [128, 128]), w=[c["ttab"]])
        P.dma("sp", c["rowmask"], self.din("tab_rowmask", [128, 4]), w=[c["ttab"]])
        c["S"] = A.alloc([H, dv], F32); c["tS"] = P.toks(H, "S")
        c["slg"] = A.alloc([H], F32); c["tslg"] = P.tok("slg")
        c["cum"] = A.alloc([NT], F32); c["E"] = A.alloc([NT], F32); c["E2"] = A.alloc([NT], F32)
        c["tcum"] = P.tok("cum"); c["tE"] = P.tok("E"); c["tE2"] = P.tok("E2")
        c["kd_b"] = A.alloc([NT], BF16); c["tkd"] = P.tok("kd")
        c["kdm"] = A.alloc([4, 4, 128], BF16); c["tkdm"] = P.toks(4, "kdm")
        c["dec"] = A.alloc([16], F32); c["tdec"] = P.tok("dec")
        c["red"] = A.alloc([1], F32)
        c["Sr"] = [A.alloc([dv], F32) for _ in range(4)]; c["tSr"] = P.toks(4, "Sr")
        if mode == "B":
            c["qe_b"] = A.alloc([NT], BF16); c["ke_b"] = A.alloc([NT], BF16); c["qi_f"] = A.alloc([NT], F32)
            c["tqk"] = P.tok("qk")
            c["PT"] = Ring(P, [A.alloc([128], BF16) for _ in range(2)], "PT")
            c["o_s"] = A.alloc([c["dvc"], NT], F32); c["to"] = P.tok("o")
            c["osq"] = A.alloc([c["dvc"], NT], BF16); c["tosq"] = P.tok("osq")
            c["rn"] = A.alloc([NT], F32); c["trn"] = P.tok("rn")
        return c

    def gla_init_state(self, c, li):
        P, A = self.P, self.A
        H, dv = c["H"], c["dv"]
        W = H * dv + H
        c["W"] = W
        S, tS = c["S"], c["tS"]
        P.op("dve", lambda e: e.memset(c["slg"], 0.0), w=[c["tslg"]])
        if self.fused:
            self.carry_load(f"S{li}", S.rearrange("p a b -> p (a b)"), tS, H * dv)
            return
        for h in range(H):
            P.op("dve", lambda e, h=h: e.memset(S[:, h, :], 0.0), w=[tS[h]])
        if c["mode"] == "A":
            return
        st_all = self.din(f"st_all{li}", [8, 128, W])
        m = A.mark()
        sta = A.alloc([W], F32); tsta = P.tok("sta")
        dsel = A.alloc([H], F32); tds = P.tok("dsel")
        for r in range(8):
            sr = self.sel[:, r:r + 1]
            P.dma("sp", sta, st_all[r], w=[tsta])
            P.op("act", lambda e: e.activation(out=dsel, in_=sta[:, H * dv:H * dv + H], func=AF.Exp), r=[tsta], w=[tds])
            P.op("dve", lambda e, sr=sr: e.tensor_scalar(out=dsel, in0=dsel, scalar1=-1.0, scalar2=sr, op0=ALU.add, op1=ALU.mult),
                 r=[tds, self.tconst], w=[tds])
            P.op("dve", lambda e: e.tensor_scalar(out=dsel, in0=dsel, scalar1=1.0, scalar2=None, op0=ALU.add), r=[tds], w=[tds])
            P.op("dve", lambda e, sr=sr: e.tensor_scalar(out=sta[:, 0:H * dv], in0=sta[:, 0:H * dv], scalar1=sr, scalar2=None, op0=ALU.mult),
                 r=[tsta, self.tconst], w=[tsta])
            for h in range(H):
                P.op("dve", lambda e, h=h: e.scalar_tensor_tensor(out=S[:, h, :], in0=S[:, h, :], scalar=dsel[:, h:h + 1],
                                                                  in1=sta[:, h * dv:(h + 1) * dv], op0=ALU.mult, op1=ALU.add),
                     r=[tds, tsta, tS[h]], w=[tS[h]])
        P.barrier()
        A.reset(m)

    def gla_finish(self, c, li):
        if self.fused:
            self.carry_save(f"S{li}", c["S"].rearrange("p a b -> p (a b)"), c["tS"], c["H"] * c["dv"])
        elif c["mode"] == "A":
            self.gla_finish_A(c, li)

    def gla_finish_A(self, c, li):
        P, A = self.P, self.A
        H, dv = c["H"], c["dv"]
        stl = A.alloc([H * dv + H], F32); tst = P.tok("stl")
        for h in range(H):
            P.op("dve", lambda e, h=h: e.tensor_copy(out=stl[:, h * dv:(h + 1) * dv], in_=c["S"][:, h, :]), r=[c["tS"][h]], w=[tst])
        P.op("dve", lambda e: e.tensor_copy(out=stl[:, H * dv:H * dv + H], in_=c["slg"]), r=[c["tslg"]], w=[tst])
        self.exchange_out(li, stl, tst, H * dv + H)

    def gla_head(self, c, h, q_f, k_f, lg_f, tin, v_tok, tv, mid_hook=None):
        P = self.P
        mode, dv, dvc = c["mode"], c["dv"], c["dvc"]
        cum, E, E2 = c["cum"], c["E"], c["E2"]
        tcum, tE, tE2 = c["tcum"], c["tE"], c["tE2"]
        S, tS = c["S"][:, h, :], c["tS"][h]
        if not c.get("pinned_by_front"):
            self.pin_lnexp()
        P.op("dve", lambda e: e.tensor_tensor_scan(out=cum, data0=c["reset"], data1=lg_f, initial=0.0, op0=ALU.mult, op1=ALU.add),
             r=[tin, c["ttab"]], w=[tcum])
        P.op("dve", lambda e: e.tensor_reduce(out=c["red"], in_=lg_f, axis=AX.X, op=ALU.add), r=[tin], w=[tE2])
        P.op("dve", lambda e: e.tensor_tensor(out=c["slg"][:, h:h + 1], in0=c["slg"][:, h:h + 1], in1=c["red"], op=ALU.add),
             r=[tE2, c["tslg"]], w=[c["tslg"]])
        cum3 = cum.rearrange("p (c t) -> p c t", t=32)
        E3 = E.rearrange("p (c t) -> p c t", t=32)
        lastb = cum3[:, :, 31:32].to_broadcast([128, 16, 32])
        refb = cum3[:, :, 16:17].to_broadcast([128, 16, 32])
        P.op("dve", lambda e: e.tensor_tensor(out=E3, in0=lastb, in1=cum3, op=ALU.subtract), r=[tcum], w=[tE])
        P.op("act", lambda e: e.activation(out=E, in_=E, func=AF.Exp), r=[tE], w=[tE])
        P.op("dve", lambda e: e.tensor_tensor(out=c["kd_b"], in0=k_f, in1=E, op=ALU.mult), r=[tE, tin], w=[c["tkd"]])
        P.op("act", lambda e: e.activation(out=c["dec"], in_=cum3[:, :, 31], func=AF.Exp), r=[tcum], w=[c["tdec"]])
        pT4 = self.pbT[:, 0:512].rearrange("p (b d) -> p b d", b=4)
        for blk in range(4):
            P.op("pe", lambda e, blk=blk: e.transpose(out=pT4[:, blk, :], in_=c["kd_b"][:, blk * 128:(blk + 1) * 128], identity=self.ident_b),
                 r=[c["tkd"], self.tconst], w=[self.tpbT], skip_self=True)
        for blk in range(4):
            P.op("dve", lambda e, blk=blk: e.tensor_tensor(
                out=c["kdm"][:, blk], in0=pT4[:, blk, :].unsqueeze(1).to_broadcast([128, 4, 128]),
                in1=c["rowmask"].unsqueeze(2).to_broadcast([128, 4, 128]), op=ALU.mult),
                r=[self.tpbT, c["ttab"]], w=[c["tkdm"][blk]])
        if mode == "B":
            qe_b, ke_b, qi_f, tqk = c["qe_b"], c["ke_b"], c["qi_f"], c["tqk"]
            E23 = E2.rearrange("p (c t) -> p c t", t=32)
            P.op("act", lambda e: e.activation(out=qi_f, in_=cum, func=AF.Exp), r=[tcum], w=[tqk])
            P.op("dve", lambda e: e.tensor_tensor(out=qi_f, in0=qi_f, in1=q_f, op=ALU.mult), r=[tqk, tin], w=[tqk])
            P.op("dve", lambda e: e.tensor_tensor(out=E23, in0=cum3, in1=refb, op=ALU.subtract), r=[tcum], w=[tE2])
            P.op("act", lambda e: e.activation(out=E, in_=E2, func=AF.Exp), r=[tE2, c["tkd"]], w=[tE])
            P.op("dve", lambda e: e.tensor_tensor(out=qe_b, in0=q_f, in1=E, op=ALU.mult), r=[tE, tin], w=[tqk])
            P.op("act", lambda e: e.activation(out=E2, in_=E2, func=AF.Exp, scale=-1.0), r=[tE2], w=[tE2])
            P.op("dve", lambda e: e.tensor_tensor(out=ke_b, in0=k_f, in1=E2, op=ALU.mult), r=[tE2, tin], w=[tqk])
        for blk in range(4):
            bs = slice(blk * 128, (blk + 1) * 128)
            if mode == "B":
                pso, tpo = self.pb[3 + blk % 2], self.tpb[3 + blk % 2]
                pss, tss = pso[:, 384:512], tpo
                P.op("pe", lambda e, bs=bs, pss=pss: e.matmul(pss, lhsT=c["ke_b"][:, bs], rhs=c["qe_b"][:, bs], start=True, stop=True,
                                                              skip_group_check=True),
                     r=[c["tqk"]], w=[tss], skip_self=True)
                PT, tPT = c["PT"].next()
                P.op("dve", lambda e, PT=PT, pss=pss: e.tensor_tensor(out=PT, in0=pss, in1=c["maskT"], op=ALU.mult),
                     r=[tss, c["ttab"]], w=[tPT])
                pso3 = pso[:, 0:dvc * 128].rearrange("p (a b) -> p a b", a=dvc)
                for ec in range(dvc):
                    P.op("pe", lambda e, ec=ec, blk=blk, PT=PT, pso3=pso3: e.matmul(
                        pso3[:, ec, :], lhsT=v_tok[:, blk, ec * 128:(ec + 1) * 128], rhs=PT, start=(ec == 0), stop=False, skip_group_check=True),
                        r=[tv, tPT], w=[tpo], skip_self=True)
            if mid_hook is not None and blk == 1:
                mid_hook()
            pviews = []
            for i in range(4):
                if dv <= 128:
                    bk = 5 + blk % 2
                    pv = self.pb[bk][:, i * 128:i * 128 + dv]
                else:
                    bk = 5 + i // 2
                    pv = self.pb[bk][:, (i % 2) * 256:(i % 2) * 256 + dv]
                pviews.append((pv, self.tpb[bk]))
                P.op("pe", lambda e, blk=blk, i=i, pv=pv: e.matmul(pv, lhsT=c["kdm"][:, blk, i, :], rhs=v_tok[:, blk, :],
                                                                   start=True, stop=True, skip_group_check=True),
                     r=[c["tkdm"][blk], tv], w=[self.tpb[bk]], skip_self=True)
            for i in range(4):
                ch = blk * 4 + i
                Ssrc, tSsrc = (S, tS) if ch == 0 else (c["Sr"][(ch - 1) % 4], c["tSr"][(ch - 1) % 4])
                Sdst, tSdst = (S, tS) if ch == 15 else (c["Sr"][ch % 4], c["tSr"][ch % 4])
                psS, tpS = pviews[i]
                if mode == "B":
                    for ec in range(dvc):
                        P.op("pe", lambda e, ec=ec, i=i, ch=ch, pso3=pso3, Ssrc=Ssrc: e.matmul(
                            pso3[:, ec, i * 32:(i + 1) * 32], lhsT=Ssrc[:, ec * 128:(ec + 1) * 128], rhs=c["qi_f"][:, ch * 32:(ch + 1) * 32],
                            start=False, stop=(i == 3), skip_group_check=True),
                            r=[tSsrc, c["tqk"]], w=[tpo], skip_self=True)
                P.op("dve", lambda e, ch=ch, psS=psS, Ssrc=Ssrc, Sdst=Sdst: e.scalar_tensor_tensor(
                    out=Sdst, in0=Ssrc, scalar=c["dec"][:, ch:ch + 1], in1=psS, op0=ALU.mult, op1=ALU.add),
                     r=[tpS, c["tdec"], tSsrc], w=[tSdst])
            if mode == "B":
                P.op("act", lambda e, bs=bs, pso3=pso3: e.copy(out=c["o_s"][:, :, bs], in_=pso3), r=[tpo], w=[c["to"]])

    def head_rms_gate(self, c, ps_g_list, m_b, tm, vc0, pre_silu=False):
        P = self.P
        dv, dvc = c["dv"], c["dvc"]
        o_s, to, osq, tosq, rn, trn = c["o_s"], c["to"], c["osq"], c["tosq"], c["rn"], c["trn"]
        epsc, tcc = self._eps, self.tcc
        P.op("act", lambda e: e.activation(out=osq, in_=o_s, func=AF.Square), r=[to], w=[tosq])
        psn, tpn = self.pb[3], self.tpb[3]
        for ec in range(dvc):
            P.op("pe", lambda e, ec=ec: e.matmul(psn, lhsT=self.ones_b, rhs=osq[:, ec, :], start=(ec == 0), stop=(ec == dvc - 1)),
                 r=[tosq, self.tconst], w=[tpn], skip_self=True)
        P.op("act", lambda e: e.activation(out=rn, in_=psn, func=AF.Ln, bias=epsc, scale=1.0 / dv), r=[tpn, tcc], w=[trn])
        P.op("act", lambda e: e.activation(out=rn, in_=rn, func=AF.Exp, scale=-0.5), r=[trn], w=[trn])
        for ec in range(dvc):
            psg, tpg = ps_g_list[ec]
            P.op("dve", lambda e, ec=ec: e.tensor_tensor(out=o_s[:, ec, :], in0=o_s[:, ec, :], in1=rn, op=ALU.mult), r=[trn, to], w=[to])
            if pre_silu:
                P.op("dve", lambda e, ec=ec, psg=psg: e.tensor_tensor(out=m_b[:, vc0 + ec, :], in0=o_s[:, ec, :], in1=psg, op=ALU.mult),
                     r=[to, tpg], w=[tm])
                continue
            sgt = c["E"]
            P.op("act", lambda e, psg=psg: e.activation(out=sgt, in_=psg, func=AF.Silu), r=[tpg, c["tkd"]], w=[c["tE"]])
            P.op("dve", lambda e, ec=ec: e.tensor_tensor(out=m_b[:, vc0 + ec, :], in0=o_s[:, ec, :], in1=sgt, op=ALU.mult),
                 r=[to, c["tE"]], w=[tm])

    def hgrn2_pass(self, mode):
        P, A = self.P, self.A
        li = 1
        self.new_consts()
        one_c = self._one
        w_in = self.din("hg_w_in", [D, 4 * D]).rearrange("(c p) n -> p c n", p=128)
        c = self.gla_alloc(8, 128, mode)
        lb = A.alloc([8], F32); oml = A.alloc([8], F32); tlb = P.tok("lb")
        et = A.alloc([4, 8], F32)
        for k in range(4):
            P.op("act", lambda e, k=k: e.activation(out=et[:, k, :], in_=self.vcol(f"hg_lb{k}", 0, 8), func=AF.Exp), r=[self.tvec], w=[tlb])
        P.op("dve", lambda e: e.tensor_tensor(out=oml, in0=et[:, 0, :], in1=et[:, 1, :], op=ALU.add), r=[tlb], w=[tlb])
        P.op("dve", lambda e: e.tensor_tensor(out=oml, in0=oml, in1=et[:, 2, :], op=ALU.add), r=[tlb], w=[tlb])
        P.op("dve", lambda e: e.tensor_tensor(out=oml, in0=oml, in1=et[:, 3, :], op=ALU.add), r=[tlb], w=[tlb])
        P.op("dve", lambda e: e.reciprocal(out=oml, in_=oml), r=[tlb], w=[tlb])
        P.op("dve", lambda e: e.tensor_copy(out=lb, in_=et[:, 1, :]), r=[tlb], w=[tlb])
        for k in range(2, li + 1):
            P.op("dve", lambda e, k=k: e.tensor_tensor(out=lb, in0=lb, in1=et[:, k, :], op=ALU.add), r=[tlb], w=[tlb])
        P.op("dve", lambda e: e.tensor_tensor(out=lb, in0=lb, in1=oml, op=ALU.mult), r=[tlb], w=[tlb])
        P.op("dve", lambda e: e.tensor_scalar(out=oml, in0=lb, scalar1=-1.0, scalar2=1.0, op0=ALU.mult, op1=ALU.add), r=[tlb], w=[tlb])
        hl = A.alloc([8], F32); bl = A.alloc([8], F32)
        P.op("dve", lambda e: e.tensor_scalar(out=hl, in0=oml, scalar1=0.5, scalar2=None, op0=ALU.mult), r=[tlb], w=[tlb])
        P.op("dve", lambda e: e.tensor_tensor(out=bl, in0=lb, in1=hl, op=ALU.add), r=[tlb], w=[tlb])
        c["pinned_by_front"] = True
        self.gla_init_state(c, li)
        x_b = A.alloc([8, NT], BF16); txb = P.tok("xb")
        wv_ring = Ring(P, [A.alloc([8, 256], BF16) for _ in range(2)], "wv")
        v_tok = A.alloc([4, D], BF16); tv = P.tok("v")
        ncol = 3 if mode == "B" else 1
        wh_ring = Ring(P, [A.alloc([8, ncol, 256], BF16) for _ in range(2)], "wh")
        q_f = A.alloc([NT], F32); k_f = A.alloc([NT], F32); lg_f = A.alloc([NT], F32); tin = P.tok("qkl")
        if mode == "B":
            sg = A.alloc([2, NT], F32); tsg = P.toks(2, "sg")
            m_b = A.alloc([8, NT], BF16); tm = P.tok("m")
            sc = self.mixer_scratch(8, x_b, txb, m_b, tm)
            w_out = self.din("hg_w_out", [D, D])
        for ti in range(NTILE):
            xt = self.x_f[:, :, ti * NT:(ti + 1) * NT]
            P.op("act", lambda e, xt=xt: e.copy(out=x_b, in_=xt), r=[self.tx[ti]], w=[txb])
            for qt in range(4):
                ws, tw = wv_ring.next()
                self.load_w(ws, w_in[:, :, 2048 + qt * 256:2048 + (qt + 1) * 256], tw)
                for blk in range(4):
                    ps, tps = self.pb[blk % 2], self.tpb[blk % 2]
                    for kc in range(8):
                        P.op("pe", lambda e, kc=kc, blk=blk, ps=ps, ws=ws: e.matmul(
                            ps[:, 0:256], lhsT=x_b[:, kc, blk * 128:(blk + 1) * 128], rhs=ws[:, kc, :], start=(kc == 0), stop=(kc == 7)),
                            r=[txb, tw], w=[tps], skip_self=True)
                    P.op("act", lambda e, blk=blk, qt=qt, ps=ps: e.copy(out=v_tok[:, blk, qt * 256:(qt + 1) * 256], in_=ps[:, 0:256]),
                         r=[tps], w=[tv])
            wstate = {}

            def front(h):
                hp, h2 = h // 2, h % 2
                if h2 == 0:
                    ws, tw = wh_ring.next()
                    self.load_w(ws[:, :, 0, :], w_in[:, :, 1024 + hp * 256:1024 + (hp + 1) * 256], tw)
                    if mode == "B":
                        self.load_w(ws[:, :, 1, :], w_in[:, :, hp * 256:(hp + 1) * 256], tw)
                        self.load_w(ws[:, :, 2, :], w_in[:, :, 3072 + hp * 256:3072 + (hp + 1) * 256], tw)
                    wstate["ws"], wstate["tw"] = ws, tw
                ws, tw = wstate["ws"], wstate["tw"]
                cs = slice(h2 * 128, (h2 + 1) * 128)
                psf, tpf = self.pb[0], self.tpb[0]
                self.proj(psf, tpf, ws[:, :, 0, :], tw, cs, x_b, txb)
                if mode == "B":
                    psq, tpq = self.pb[1], self.tpb[1]
                    self.proj(psq, tpq, ws[:, :, 1, :], tw, cs, x_b, txb)
                    psg, tpg = self.pb[2], self.tpb[2]
                    self.proj(psg, tpg, ws[:, :, 2, :], tw, cs, x_b, txb)
                P.op("act", lambda e: e.activation(out=k_f, in_=psf, func=AF.Tanh, scale=0.5), r=[tpf], w=[tin])
                if mode == "B":
                    P.op("act", lambda e: e.activation(out=q_f, in_=psq, func=AF.Silu), r=[tpq], w=[tin])
                    P.op("act", lambda e, h=h: e.activation(out=sg[:, h % 2, :], in_=psg, func=AF.Silu), r=[tpg], w=[tsg[h % 2]])
                P.op("act", lambda e, h=h: e.activation(out=k_f, in_=k_f, func=AF.Identity, scale=hl[:, h:h + 1], bias=bl[:, h:h + 1]),
                     r=[tin, tlb], w=[tin])
                P.op("act", lambda e: e.activation(out=lg_f, in_=k_f, func=AF.Ln), r=[tin], w=[tin])
                P.op("dve", lambda e: e.tensor_scalar(out=k_f, in0=k_f, scalar1=-1.0, scalar2=1.0, op0=ALU.mult, op1=ALU.add),
                     r=[tin], w=[tin])

            front(0)
            for h in range(8):
                hook = (lambda h=h: front(h + 1)) if h + 1 < 8 else None
                self.gla_head(c, h, q_f, k_f, lg_f, tin, v_tok[:, :, h * 128:(h + 1) * 128], tv, mid_hook=hook)
                if mode == "B":
                    self.head_rms_gate(c, [(sg[:, h % 2, :], tsg[h % 2])], m_b, tm, h, pre_silu=True)
            if mode == "B":
                self.mixer_out(li, ti, m_b, tm, 8, w_out, sc)
        self.gla_finish(c, li)
        self.phase_end()

    def gla_pass(self, mode):
        P, A = self.P, self.A
        li = 3
        self.new_consts()
        one_c = self._one
        w_in = self.din("gla_w_in", [D, 3088]).rearrange("(c p) n -> p c n", p=128)
        c = self.gla_alloc(4, 256, mode)
        nbg = A.alloc([4], F32); tnb = P.tok("nbg")
        P.op("dve", lambda e: e.tensor_scalar(out=nbg, in0=self.vcol("gla_b_gate", 0, 4), scalar1=-1.0, scalar2=None, op0=ALU.mult),
             r=[self.tvec], w=[tnb])
        wgl = A.alloc([8, 16], BF16); twgl = P.tok("wgl")
        self.load_w(wgl, w_in[:, :, 3072:3088], twgl)
        wgate = A.alloc([512], BF16, parts=16)
        self.load_w(wgate, self.din("gla_w_gate", [16, 512]), twgl)
        gl_b = A.alloc([NT], BF16, parts=16); tgl = P.tok("gl")
        self.gla_init_state(c, li)
        x_b = A.alloc([8, NT], BF16); txb = P.tok("xb")
        wring = Ring(P, [A.alloc([8, 256], BF16) for _ in range(4)], "wr")
        v_tok = A.alloc([4, D], BF16); tv = P.tok("v")
        q_f = A.alloc([NT], F32); k_f = A.alloc([NT], F32); lg_f = A.alloc([NT], F32); tin = P.tok("qkl")
        if mode == "B":
            m_b = A.alloc([8, NT], BF16); tm = P.tok("m")
            sc = self.mixer_scratch(8, x_b, txb, m_b, tm)
            w_out = self.din("gla_w_out", [D, D])
        for ti in range(NTILE):
            xt = self.x_f[:, :, ti * NT:(ti + 1) * NT]
            P.op("act", lambda e, xt=xt: e.copy(out=x_b, in_=xt), r=[self.tx[ti]], w=[txb])
            ps, tps = self.pb[0], self.tpb[0]
            for kc in range(8):
                P.op("pe", lambda e, kc=kc: e.matmul(ps[0:16, :], lhsT=wgl[:, kc, :], rhs=x_b[:, kc, :], start=(kc == 0), stop=(kc == 7)),
                     r=[twgl, txb], w=[tps], skip_self=True)
            P.op("act", lambda e: e.copy(out=gl_b, in_=ps[0:16, :]), r=[tps], w=[tgl])
            for qt in range(4):
                ws, tw = wring.next()
                self.load_w(ws, w_in[:, :, 1024 + qt * 256:1024 + (qt + 1) * 256], tw)
                for blk in range(4):
                    ps, tps = self.pb[blk % 2], self.tpb[blk % 2]
                    for kc in range(8):
                        P.op("pe", lambda e, kc=kc, blk=blk, ps=ps, ws=ws: e.matmul(
                            ps[:, 0:256], lhsT=x_b[:, kc, blk * 128:(blk + 1) * 128], rhs=ws[:, kc, :], start=(kc == 0), stop=(kc == 7)),
                            r=[txb, tw], w=[tps], skip_self=True)
                    P.op("act", lambda e, blk=blk, qt=qt, ps=ps: e.copy(out=v_tok[:, blk, qt * 256:(qt + 1) * 256], in_=ps[:, 0:256]),
                         r=[tps], w=[tv])
            for hp in range(2):
                wk, twk = wring.next()
                self.load_w(wk, w_in[:, :, 512 + hp * 256:512 + (hp + 1) * 256], twk)
                if mode == "B":
                    wq, twq = wring.next()
                    self.load_w(wq, w_in[:, :, hp * 256:(hp + 1) * 256], twq)
                for h2 in range(2):
                    h = hp * 2 + h2
                    cs = slice(h2 * 128, (h2 + 1) * 128)
                    psz, tpz = self.pb[0], self.tpb[0]
                    P.op("pe", lambda e, h=h: e.matmul(psz, lhsT=wgate[:, h * 128:(h + 1) * 128], rhs=gl_b, start=True, stop=True),
                         r=[twgl, tgl], w=[tpz], skip_self=True)
                    P.op("act", lambda e, h=h: e.activation(out=lg_f, in_=psz, func=AF.Exp, scale=-1.0, bias=nbg[:, h:h + 1]),
                         r=[tpz, tnb], w=[tin])
                    P.op("act", lambda e: e.activation(out=lg_f, in_=lg_f, func=AF.Ln, bias=one_c, scale=1.0), r=[tin, self.tcc], w=[tin])
                    P.op("dve", lambda e: e.tensor_scalar(out=lg_f, in0=lg_f, scalar1=-1.0 / 16.0, scalar2=None, op0=ALU.mult), r=[tin], w=[tin])
                    psk, tpk = self.pb[1], self.tpb[1]
                    self.proj(psk, tpk, wk, twk, cs, x_b, txb)
                    P.op("act", lambda e: e.copy(out=k_f, in_=psk), r=[tpk], w=[tin])
                    if mode == "B":
                        psq, tpq = self.pb[0], self.tpb[0]
                        self.proj(psq, tpq, wq, twq, cs, x_b, txb)
                        P.op("act", lambda e: e.mul(out=q_f, in_=psq, mul=128.0 ** -0.5), r=[tpq], w=[tin])
                    self.gla_head(c, h, q_f, k_f, lg_f, tin, v_tok[:, :, h * 256:(h + 1) * 256], tv)
                    if mode == "B" and DEBUG and ti == DEBUG_TI and h == 0:
                        self.dbg("d_q", q_f, tin, [128, NT]); self.dbg("d_k", k_f, tin, [128, NT]); self.dbg("d_lg", lg_f, tin, [128, NT])
                        self.dbg("d_o", c["o_s"], c["to"], [128, 2, NT])
                        self.dbg("d_S", c["S"][:, 0, :], c["tS"][0], [128, 256])
                    if mode == "B":
                        wr_, twr_ = wring.next()
                        self.load_w(wr_, w_in[:, :, 2048 + h * 256:2048 + (h + 1) * 256], twr_)
                        pl = []
                        for ec in range(2):
                            psg, tpg = self.pb[ec], self.tpb[ec]
                            self.proj(psg, tpg, wr_, twr_, slice(ec * 128, (ec + 1) * 128), x_b, txb)
                            pl.append((psg, tpg))
                        self.head_rms_gate(c, pl, m_b, tm, h * 2)
            if mode == "B":
                self.mixer_out(li, ti, m_b, tm, 8, w_out, sc)
        self.gla_finish(c, li)
        self.phase_end()

    def ret_pass(self, mode):
        P, A = self.P, self.A
        li = 2
        self.new_consts()
        epsc, tcc = self._eps, self.tcc
        H = 4
        gam = [1.0 - 2.0 ** (-5.0 - h) for h in range(H)]
        w_in = self.din("ret_w_in", [D, 6144]).rearrange("(c p) n -> p c n", p=128)
        S = A.alloc([2, H, 512], F32); tS = P.toks(H, "S")
        if self.fused:
            self.carry_load("S2", S.rearrange("p a b c -> p (a b c)"), tS, 4096)
        else:
            for h in range(H):
                P.op("dve", lambda e, h=h: e.memset(S[:, :, h, :], 0.0), w=[tS[h]])
        if mode == "B" and not self.fused:
            st_all = self.din("st_all2", [8, 128, 4096])
            m = A.mark()
            sring = Ring(P, [A.alloc([512], F32) for _ in range(3)], "sta")
            dsel = A.alloc([8, H], F32); tds = P.tok("dsel")
            for r in range(8):
                for h in range(H):
                    P.op("dve", lambda e, r=r, h=h: e.tensor_scalar(out=dsel[:, r, h:h + 1], in0=self.sel[:, r:r + 1],
                                                                   scalar1=float(gam[h] ** T - 1.0), scalar2=1.0, op0=ALU.mult, op1=ALU.add),
                         r=[self.tconst], w=[tds])
            for r in range(8):
                for h in range(H):
                    for dc in range(2):
                        sb, tsb = sring.next()
                        o0 = (dc * H + h) * 512
                        P.dma("sp", sb, st_all[r][:, o0:o0 + 512], w=[tsb])
                        P.op("dve", lambda e, r=r, sb=sb: e.tensor_scalar(out=sb, in0=sb, scalar1=self.sel[:, r:r + 1], scalar2=None, op0=ALU.mult),
                             r=[tsb, self.tconst], w=[tsb])
                        P.op("dve", lambda e, r=r, h=h, dc=dc, sb=sb: e.scalar_tensor_tensor(
                            out=S[:, dc, h, :], in0=S[:, dc, h, :], scalar=dsel[:, r, h:h + 1], in1=sb, op0=ALU.mult, op1=ALU.add),
                            r=[tsb, tds, tS[h]], w=[tS[h]])
            P.barrier()
            A.reset(m)
        zeta = A.alloc([H, 128], F32); ttab = P.tok("rtab")
        P.dma("sp", zeta, self.din("tab_zeta", [128, H, 128]), w=[ttab])
        cos_t = A.alloc([NT], F32); sin_t = A.alloc([NT], F32); tcs = P.tok("cs")
        sfx = f"_{self.cur_q}" if self.fused else ""
        cosd = self.din("tabc_cos" + sfx, [128, T]); sind = self.din("tabc_sin" + sfx, [128, T])
        if mode == "B":
            dmat = A.alloc([H, 128], F32); xi = A.alloc([H, 128], F32)
            P.dma("sp", dmat, self.din("tab_dmat", [128, H, 128]), w=[ttab])
            P.dma("sp", xi, self.din("tab_xi", [128, H, 128]), w=[ttab])
            S_b = A.alloc([2, 512], BF16); tSb = P.tok("Sb")
        x_b = A.alloc([8, NT], BF16); txb = P.tok("xb")
        wring = Ring(P, [A.alloc([8, 256], BF16) for _ in range(4)], "wr")
        v_tok = A.alloc([4, 512], BF16); tv = P.tok("v")
        A1 = A.alloc([NT], F32); A2 = A.alloc([NT], F32); tA = P.tok("A12")
        k_r = A.alloc([2, NT], BF16); kz = A.alloc([2, NT], BF16); tkr = P.tok("kr"); tkz = P.tok("kz")
        kz_tok = A.alloc([4, 256], BF16); tkzt = P.tok("kzt")
        if mode == "B":
            q_r = A.alloc([2, NT], BF16); qx = A.alloc([2, NT], BF16); tqr = P.tok("qr"); tqx = P.tok("qx")
            PTr = Ring(P, [A.alloc([128], BF16) for _ in range(2)], "PT")
            o_s = A.alloc([4, NT], F32); to = P.tok("o")
            m_b = A.alloc([16, NT], BF16); tm = P.tok("m")
            sc = self.mixer_scratch(16, x_b, txb, m_b[:, 0:8], tm, sw=128)
            zreg = sc["z"].rearrange("p a b -> p (a b)").bitcast(BF16)
            o_b = zreg[:, 0:4 * NT].rearrange("p (a b) -> p a b", a=4)
            osq = zreg[:, 4 * NT:8 * NT].rearrange("p (a b) -> p a b", a=4)
            tz = sc["tz"]
            sm = sc["sm"]
            w_out = self.din("ret_w_out", [2 * D, D])

        def rotary(ps1, tp1, ps2, tp2, out_b, tout):
            P.op("dve", lambda e: e.tensor_tensor(out=A1, in0=ps1, in1=cos_t, op=ALU.mult), r=[tp1, tcs], w=[tA])
            P.op("dve", lambda e: e.tensor_tensor(out=A2, in0=ps2, in1=sin_t, op=ALU.mult), r=[tp2, tcs], w=[tA])
            P.op("dve", lambda e: e.tensor_tensor(out=out_b[:, 0, :], in0=A1, in1=A2, op=ALU.subtract), r=[tA], w=[tout])
            P.op("dve", lambda e: e.tensor_tensor(out=A1, in0=ps1, in1=sin_t, op=ALU.mult), r=[tp1, tcs, tout], w=[tA])
            P.op("dve", lambda e: e.tensor_tensor(out=A2, in0=ps2, in1=cos_t, op=ALU.mult), r=[tp2, tcs], w=[tA])
            P.op("dve", lambda e: e.tensor_tensor(out=out_b[:, 1, :], in0=A1, in1=A2, op=ALU.add), r=[tA], w=[tout])

        for ti in range(NTILE):
            xt = self.x_f[:, :, ti * NT:(ti + 1) * NT]
            P.op("act", lambda e, xt=xt: e.copy(out=x_b, in_=xt), r=[self.tx[ti]], w=[txb])
            P.dma("sp", cos_t, cosd[:, ti * NT:(ti + 1) * NT], w=[tcs])
            P.dma("sp", sin_t, sind[:, ti * NT:(ti + 1) * NT], w=[tcs])
            for h in range(H):
                for qt in range(2):
                    ws, tw = wring.next()
                    self.load_w(ws, w_in[:, :, 2048 + h * 512 + qt * 256:2048 + h * 512 + (qt + 1) * 256], tw)
                    for blk in range(4):
                        ps, tps = self.pb[blk % 2], self.tpb[blk % 2]
                        for kc in range(8):
                            P.op("pe", lambda e, kc=kc, blk=blk, ps=ps, ws=ws: e.matmul(
                                ps[:, 0:256], lhsT=x_b[:, kc, blk * 128:(blk + 1) * 128], rhs=ws[:, kc, :], start=(kc == 0), stop=(kc == 7)),
                                r=[txb, tw], w=[tps], skip_self=True)
                        P.op("act", lambda e, blk=blk, qt=qt, ps=ps: e.copy(out=v_tok[:, blk, qt * 256:(qt + 1) * 256], in_=ps[:, 0:256]),
                             r=[tps], w=[tv])
                wk, twk = wring.next()
                self.load_w(wk, w_in[:, :, 1024 + h * 256:1024 + (h + 1) * 256], twk)
                self.proj(self.pb[0], self.tpb[0], wk, twk, slice(0, 128), x_b, txb)
                self.proj(self.pb[1], self.tpb[1], wk, twk, slice(128, 256), x_b, txb)
                rotary(self.pb[0], self.tpb[0], self.pb[1], self.tpb[1], k_r, tkr)
                kz4 = kz.rearrange("p a (b c) -> p a b c", b=4)
                kr4 = k_r.rearrange("p a (b c) -> p a b c", b=4)
                for dc in range(2):
                    P.op("dve", lambda e, dc=dc, h=h: e.tensor_tensor(out=kz4[:, dc], in0=kr4[:, dc],
                                                                      in1=zeta[:, h, :].unsqueeze(1).to_broadcast([128, 4, 128]), op=ALU.mult),
                         r=[tkr, ttab], w=[tkz])
                pT = self.pbT.rearrange("p (b d) -> p b d", b=4)
                for blk in range(4):
                    for dc in range(2):
                        P.op("pe", lambda e, blk=blk, dc=dc: e.transpose(out=pT[:, blk, dc * 128:(dc + 1) * 128],
                                                                         in_=kz[:, dc, blk * 128:(blk + 1) * 128], identity=self.ident_b),
                             r=[tkz, self.tconst], w=[self.tpbT], skip_self=True)
                P.op("act", lambda e: e.copy(out=kz_tok, in_=pT), r=[self.tpbT], w=[tkzt])
                if mode == "B":
                    wq, twq = wring.next()
                    self.load_w(wq, w_in[:, :, h * 256:(h + 1) * 256], twq)
                    self.proj(self.pb[0], self.tpb[0], wq, twq, slice(0, 128), x_b, txb)
                    self.proj(self.pb[1], self.tpb[1], wq, twq, slice(128, 256), x_b, txb)
                    rotary(self.pb[0], self.tpb[0], self.pb[1], self.tpb[1], q_r, tqr)
                    qx4 = qx.rearrange("p a (b c) -> p a b c", b=4)
                    qr4 = q_r.rearrange("p a (b c) -> p a b c", b=4)
                    for dc in range(2):
                        P.op("dve", lambda e, dc=dc, h=h: e.tensor_tensor(out=qx4[:, dc], in0=qr4[:, dc],
                                                                          in1=xi[:, h, :].unsqueeze(1).to_broadcast([128, 4, 128]), op=ALU.mult),
                             r=[tqr, ttab], w=[tqx])
                    P.op("act", lambda e, h=h: e.copy(out=S_b, in_=S[:, :, h, :]), r=[tS[h]], w=[tSb])
                for blk in range(4):
                    bs = slice(blk * 128, (blk + 1) * 128)
                    if mode == "B":
                        pss, tss = self.pb[2], self.tpb[2]
                        for dc in range(2):
                            P.op("pe", lambda e, dc=dc, bs=bs: e.matmul(pss[:, 0:128], lhsT=k_r[:, dc, bs], rhs=q_r[:, dc, bs],
                                                                        start=(dc == 0), stop=(dc == 1)),
                                 r=[tkr, tqr], w=[tss], skip_self=True)
                        PT, tPT = PTr.next()
                        P.op("dve", lambda e, PT=PT, h=h: e.tensor_tensor(out=PT, in0=pss[:, 0:128], in1=dmat[:, h, :], op=ALU.mult),
                             r=[tss, ttab], w=[tPT])
                        pso, tpo = self.pb[3 + blk % 2], self.tpb[3 + blk % 2]
                        pso3 = pso.rearrange("p (a b) -> p a b", a=4)
                        for ec in range(4):
                            P.op("pe", lambda e, ec=ec, blk=blk, PT=PT, pso3=pso3: e.matmul(
                                pso3[:, ec, :], lhsT=v_tok[:, blk, ec * 128:(ec + 1) * 128], rhs=PT, start=(ec == 0), stop=False, skip_group_check=True),
                                r=[tv, tPT], w=[tpo], skip_self=True)
                            for dc in range(2):
                                P.op("pe", lambda e, ec=ec, dc=dc, bs=bs, pso3=pso3: e.matmul(
                                    pso3[:, ec, :], lhsT=S_b[:, dc, ec * 128:(ec + 1) * 128], rhs=qx[:, dc, bs],
                                    start=False, stop=(dc == 1), skip_group_check=True),
                                    r=[tSb, tqx], w=[tpo], skip_self=True)
                        P.op("act", lambda e, bs=bs, pso3=pso3: e.copy(out=o_s[:, :, bs], in_=pso3), r=[tpo], w=[to])
                    for dc in range(2):
                        psS, tpS = self.pb[5 + dc], self.tpb[5 + dc]
                        P.op("pe", lambda e, dc=dc, blk=blk, psS=psS: e.matmul(psS, lhsT=kz_tok[:, blk, dc * 128:(dc + 1) * 128], rhs=v_tok[:, blk, :],
                                                                               start=True, stop=True),
                             r=[tkzt, tv], w=[tpS], skip_self=True)
                        P.op("dve", lambda e, dc=dc, h=h, psS=psS: e.scalar_tensor_tensor(
                            out=S[:, dc, h, :], in0=S[:, dc, h, :], scalar=float(gam[h] ** 128), in1=psS, op0=ALU.mult, op1=ALU.add),
                            r=[tpS, tS[h]], w=[tS[h]])
                    if mode == "B" and blk < 3:
                        P.op("act", lambda e, h=h: e.copy(out=S_b, in_=S[:, :, h, :]), r=[tS[h]], w=[tSb])
                    if mode == "B" and blk == 1:
                        for e2 in range(2):
                            wg, twg = wring.next()
                            self.load_w(wg, w_in[:, :, 4096 + h * 512 + e2 * 256:4096 + h * 512 + (e2 + 1) * 256], twg)
                            for e1 in range(2):
                                ec = e2 * 2 + e1
                                psg, tpg = self.pb[e1], self.tpb[e1]
                                self.proj(psg, tpg, wg, twg, slice(e1 * 128, (e1 + 1) * 128), x_b, txb)
                                P.op("act", lambda e, psg=psg, ec=ec, h=h: e.activation(out=m_b[:, h * 4 + ec, :], in_=psg, func=AF.Silu),
                                     r=[tpg], w=[tm])
                if mode == "B":
                    mean, var, rstd, nmr, tsm = sm["mean"], sm["var"], sm["rstd"], sm["nmr"], sm["t"]
                    P.op("act", lambda e: e.copy(out=o_b, in_=o_s), r=[to], w=tz)
                    P.op("act", lambda e: e.activation(out=osq, in_=o_s, func=AF.Square), r=[to], w=tz)
                    ps_s, ts_s = self.pb[2], self.tpb[2]
                    ps_q, ts_q = self.pb[0], self.tpb[0]
                    for ec in range(4):
                        P.op("pe", lambda e, ec=ec: e.matmul(ps_s, lhsT=self.ones_b, rhs=o_b[:, ec, :], start=(ec == 0), stop=(ec == 3)),
                             r=tz + [self.tconst], w=[ts_s], skip_self=True)
                    for ec in range(4):
                        P.op("pe", lambda e, ec=ec: e.matmul(ps_q, lhsT=self.ones_b, rhs=osq[:, ec, :], start=(ec == 0), stop=(ec == 3)),
                             r=tz + [self.tconst], w=[ts_q], skip_self=True)
                    P.op("act", lambda e: e.mul(out=mean, in_=ps_s, mul=1.0 / 512), r=[ts_s], w=[tsm])
                    P.op("act", lambda e: e.activation(out=nmr, in_=ps_s, func=AF.Square, scale=1.0 / 512), r=[ts_s], w=[tsm])
                    P.op("dve", lambda e: e.scalar_tensor_tensor(out=var, in0=ps_q, scalar=1.0 / 512, in1=nmr, op0=ALU.mult, op1=ALU.subtract),
                         r=[ts_q, tsm], w=[tsm])
                    self.pin_lnexp()
                    P.op("act", lambda e: e.activation(out=var, in_=var, func=AF.Ln, bias=epsc, scale=1.0), r=[tsm, tcc], w=[tsm])
                    P.op("act", lambda e: e.activation(out=rstd, in_=var, func=AF.Exp, scale=-0.5), r=[tsm], w=[tsm])
                    P.op("dve", lambda e: e.scalar_tensor_tensor(out=nmr, in0=mean, scalar=-1.0, in1=rstd, op0=ALU.mult, op1=ALU.mult),
                         r=[tsm], w=[tsm])
                    P.op("dve", lambda e: e.tensor_tensor(out=o_s, in0=o_s, in1=rstd.unsqueeze(1).to_broadcast([128, 4, NT]), op=ALU.mult),
                         r=[tsm, to], w=[to])
                    P.op("dve", lambda e: e.tensor_tensor(out=o_s, in0=o_s, in1=nmr.unsqueeze(1).to_broadcast([128, 4, NT]), op=ALU.add),
                         r=[tsm, to], w=[to])
                    for ec in range(4):
                        P.op("dve", lambda e, ec=ec, h=h: e.tensor_tensor(out=m_b[:, h * 4 + ec, :], in0=o_s[:, ec, :], in1=m_b[:, h * 4 + ec, :], op=ALU.mult),
                             r=[to, tm], w=[tm])
            if mode == "B":
                self.mixer_out(li, ti, m_b, tm, 16, w_out, sc)
        if self.fused:
            self.carry_save("S2", S.rearrange("p a b c -> p (a b c)"), tS, 4096)
        elif mode == "A":
            st = self.dout("st_loc2", [128, 4096])
            for h in range(H):
                for dc in range(2):
                    o0 = (dc * H + h) * 512
                    P.dma("sp", st[:, o0:o0 + 512], S[:, dc, h, :], r=[tS[h]])
        self.phase_end()

    def finalize(self):
        self.P.finalize()
        return self.nc


class Host:
    def __init__(self, inputs):
        self.inp = {k: np.asarray(v) for k, v in inputs.items()}
        self.cache = {}
        v = np.zeros((128, NVEC), np.float32)

        def put(name, arr):
            o, n = VLAY[name]
            v[:, o:o + n] = _fm(arr)
        I = self.inp
        for j in range(4):
            put(f"rg_conv_w{j}", I["rg_conv_w"][0, j])
        put("rg_conv_b", I["rg_conv_b"][0])
        put("rg_b_a", I["rg_b_a"][0])
        put("rg_b_x", I["rg_b_x"][0])
        put("rg_lambda", I["rg_lambda"][0])
        for i in range(4):
            put(f"hg_lb{i}", I["hg_lb_logits"][i])
            put(f"ln_mix_g{i}", I["ln_mix_g"][i])
            put(f"ln_mix_b{i}", I["ln_mix_b"][i])
            put(f"ln_ffn_g{i}", I["ln_ffn_g"][i])
            put(f"ln_ffn_b{i}", I["ln_ffn_b"][i])
        put("gla_b_gate", I["gla_b_gate"][0])
        self.vecs = v
        self.ident = np.eye(128, dtype=np.float32)
        selE = np.zeros((8, 8, 128), np.float32)
        for e in range(8):
            selE[e, e, :] = 1.0
        self.selE = selE.reshape(8, 1024)

    @staticmethod
    def fm_act(a):
        t, c = a.shape
        return np.ascontiguousarray(a.T.reshape(c // 128, 128, t).transpose(1, 0, 2))

    def get(self, name, core, xcur=None, st_all=None):
        I = self.inp
        b, j = core // 4, core % 4
        t0 = j * T
        if name == "keep":
            k = np.zeros((128, 8), np.float32)
            for q in range(4):
                if q > 3 - j:
                    k[:, q] = 1.0
            return k
        if name[:-1].endswith("_") and name[-1].isdigit() and (name.startswith("xT_") or name.startswith("pT") or name.startswith("tabc_")):
            q = int(name[-1])
            ch = max(q - (3 - j), 0)
            t0 = ch * T
            base = name[:-2]
            if base == "xT":
                return self.fm_act(I["x"][b, t0:t0 + T])
            name = base
        if name == "xT":
            return xcur[core]
        if name == "vecs":
            return self.vecs
        if name == "ident_f":
            return self.ident
        if name == "selE":
            return self.selE
        if name == "tab_reset":
            r = np.ones((128, NT), np.float32)
            r[:, ::32] = 0.0
            return r
        if name == "tab_maskT":
            i = np.arange(128)
            return ((i[:, None] // 32 == i[None, :] // 32) & (i[:, None] <= i[None, :])).astype(np.float32)
        if name == "tab_rowmask":
            i = np.arange(128)
            return (i[:, None] // 32 == np.arange(4)[None, :]).astype(np.float32)
        if name in ("tab_zeta", "tab_xi", "tab_dmat"):
            out = np.zeros((128, 4, 128), np.float64)
            i = np.arange(128, dtype=np.float64)
            for h in range(4):
                g = 1.0 - 2.0 ** (-5.0 - h)
                if name == "tab_zeta":
                    out[:, h, :] = (g ** (127.0 - i))[None, :] / 16.0
                elif name == "tab_xi":
                    out[:, h, :] = (g ** (i + 1.0))[None, :]
                else:
                    rel = i[None, :] - i[:, None]
                    out[:, h, :] = np.where(rel >= 0, g ** np.maximum(rel, 0.0), 0.0) / 16.0
            return out.astype(np.float32)
        if name in ("tabc_cos", "tabc_sin"):
            inv = (np.float32(10000.0) ** (-(np.arange(0, 256, 2, dtype=np.float32)) / np.float32(256))).astype(np.float32)
            pos = np.arange(t0, t0 + T, dtype=np.float32)
            ang = (pos[None, :] * inv[:, None]).astype(np.float32).astype(np.float64)
            return (np.cos(ang) if name == "tabc_cos" else np.sin(ang)).astype(np.float32)
        if name == "sel":
            s = np.zeros((128, 8), np.float32)
            for r in range(8):
                if r // 4 == b and r % 4 < j:
                    s[:, r] = 1.0
            return s
        if name == "xh":
            h = np.zeros((4, D), np.float32)
            if j > 0:
                h[0:3] = I["x"][b, t0 - 3:t0]
            return self.fm_act(h)
        if name.startswith("pT"):
            li = int(name[2:])
            return self.fm_act(I["p"][li, b, t0:t0 + T])
        if name.startswith("st_all"):
            return st_all[name]
        if name in I and name not in ("x", "p"):
            a = I[name]
            return a[0] if a.shape[0] == 1 and name not in ("moe_w_in", "moe_w_out") else a
        for base in ("dense_w_in", "dense_w_out", "moe_w_router", "moe_w_in", "moe_w_out", "ple_w_proj", "ple_w_gate"):
            if name.startswith(base) and name[len(base):].isdigit():
                idx = int(name[len(base):])
                a = I[base]
                if base in ("dense_w_in", "dense_w_out"):
                    return a[idx:idx + 1]
                return a[idx]
        raise KeyError(name)


def run_launch(host, stage_list, xcur=None, st_all=None, trace=False, fused=False):
    kb = KB(stage_list, fused=fused)
    for st in stage_list:
        getattr(kb, st[0])(*st[1:])
    names = list(kb.inputs.keys())
    nc = kb.finalize()
    print("[kernel] instr counts", dict(kb.P.cnt), "sems", kb.P.n_sems, flush=True)
    in_maps = []
    for c in range(NCORES):
        m = {}
        for n in names:
            key = (n, c)
            per_core = n in ("xT", "sel", "xh", "keep") or n.startswith("xT_") or n.startswith("pT") or n.startswith("st_all") or n.startswith("tabc_")
            if per_core:
                m[n] = np.ascontiguousarray(host.get(n, c, xcur, st_all), dtype=np.float32)
            else:
                if n not in host.cache:
                    host.cache[n] = np.ascontiguousarray(host.get(n, 0, xcur, st_all), dtype=np.float32)
                m[n] = host.cache[n]
        in_maps.append(m)
    res = run_bass_kernel_spmd(nc, in_maps, core_ids=list(range(NCORES)), trace=trace)
    return res


PASSES = ["rglru_pass", "hgrn2_pass", "ret_pass", "gla_pass"]


def kernel(**inputs):
    host = Host(inputs)
    res = run_launch(host, fused_stages(), fused=True)
    out = np.zeros((2, SEQ, D), np.float32)
    for c in range(NCORES):
        out[c // 4, (c % 4) * T:(c % 4 + 1) * T] = res.results[c]["yT"].transpose(2, 1, 0).reshape(T, D)
    return out


def fused_stages():
    st = []
    for q in range(4):
        st += [("set_pass", q), ("load_x",), ("rglru_pass", "B"), ("ffn_phase", 0), ("hgrn2_pass", "B"), ("ffn_phase", 1),
               ("ret_pass", "B"), ("ffn_phase", 2)]
        if q == 3:
            st += [("gla_pass", "B"), ("ffn_phase", 3), ("store_x",)]
        else:
            st += [("gla_pass", "A")]
    return st


def kernel_unfused(**inputs):
    host = Host(inputs)
    x = np.asarray(inputs["x"], np.float32)
    xcur = [Host.fm_act(x[c // 4, (c % 4) * T:(c % 4 + 1) * T]) for c in range(NCORES)]
    res = run_launch(host, [("load_x",), (PASSES[0], "A")], xcur=xcur)
    st = np.stack([res.results[c]["st_loc0"] for c in range(NCORES)])
    for i in range(4):
        stages = [("load_x",), (PASSES[i], "B"), ("ffn_phase", i)]
        if i < 3:
            stages.append((PASSES[i + 1], "A"))
        stages.append(("store_x",))
        res = run_launch(host, stages, xcur=xcur, st_all={f"st_all{i}": st})
        xcur = [res.results[c]["yT"] for c in range(NCORES)]
        if i < 3:
            st = np.stack([res.results[c][f"st_loc{i + 1}"] for c in range(NCORES)])
    out = np.zeros((2, SEQ, D), np.float32)
    for c in range(NCORES):
        out[c // 4, (c % 4) * T:(c % 4 + 1) * T] = xcur[c].transpose(2, 1, 0).reshape(T, D)
    return out
```

```python
from contextlib import ExitStack
import math
import numpy as np
import ml_dtypes
import concourse.bass as bass
import concourse.mybir as mybir
from concourse.bass_utils import run_bass_kernel_spmd

F32 = mybir.dt.float32
BF16 = mybir.dt.bfloat16
AF = mybir.ActivationFunctionType
ALU = mybir.AluOpType
AX = mybir.AxisListType

NCORES = 8
D = 1024
SEQ = 8192
T = 2048
NT = 512
NTILE = T // NT
ALPHA = 8.0 ** 0.25
EPS = 1e-5
FFN_DENSE = 2816
FFN_EXPERT = 3584
ARENA_BYTES = 206 * 1024
DEBUG = False
CC_INC = 1
DEBUG_TI = 0


class Tok:
    __slots__ = ("name", "w", "r")

    def __init__(self, name):
        self.name = name
        self.w = None
        self.r = {}


class Prog:
    ENGS = ("pe", "act", "dve", "pool", "sp")

    def __init__(self, nc):
        self.nc = nc
        self.stack = ExitStack()
        self.streams = {e: [] for e in self.ENGS}
        self.cnt = {e: 0 for e in self.ENGS}
        self.waited = {e: {} for e in self.ENGS}
        self.needed = {e: set() for e in self.ENGS}
        self.slot_of = {}
        self.slot_total = []
        self.free_slots = []
        self.ntok = 0

    def _slot(self, key):
        if key not in self.slot_of:
            if self.free_slots:
                sl = self.free_slots.pop()
            else:
                sl = len(self.slot_total)
                self.slot_total.append(0)
            self.slot_of[key] = sl
        return self.slot_of[key]

    def release_keys(self):
        self.free_slots.extend(sorted(set(self.slot_of.values()), reverse=True))
        self.slot_of.clear()

    def tok(self, name=None):
        self.ntok += 1
        return Tok(name or f"t{self.ntok}")

    def toks(self, n, name="t"):
        return [self.tok(f"{name}{i}") for i in range(n)]

    def _deps(self, r, w):
        deps = []
        for t in r:
            if t.w is not None:
                deps.append(t.w)
        for t in w:
            if t.w is not None:
                deps.append(t.w)
            deps.extend(t.r.values())
        return deps

    def _emit_waits(self, eng, deps, skip_self=False):
        best = {}
        for d in deps:
            k = (d[0], d[1])
            if skip_self and d[0] == "e" and d[1] == eng:
                continue
            if best.get(k, 0) < d[2]:
                best[k] = d[2]
        wd = self.waited[eng]
        for k, v in best.items():
            if wd.get(k, 0) < v:
                wd[k] = v
                if k[0] == "e":
                    self.needed[k[1]].add(v)
                self.streams[eng].append(("wait", (k[0], k[1], v)))

    def op(self, eng, fn, r=(), w=(), skip_self=False):
        self._emit_waits(eng, self._deps(r, w), skip_self=skip_self)
        self.cnt[eng] += 1
        idx = self.cnt[eng]
        self.streams[eng].append(("op", fn, idx))
        me = ("e", eng, idx)
        for t in r:
            t.r[("e", eng)] = me
        for t in w:
            t.w = me
            t.r = {}
        return idx

    def raw(self, eng, fn):
        self.streams[eng].append(("raw", fn))

    def dma(self, q, out, in_, r=(), w=(), key=None):
        self._emit_waits(q, self._deps(r, w))
        if key is None:
            key = (w[0] if len(w) else r[0])
        sl = self._slot(key)
        self.slot_total[sl] += 16
        c = self.slot_total[sl]
        self.streams[q].append(("dma", (out, in_), sl))
        me = ("d", sl, c)
        for t in r:
            t.r[("d", sl)] = me
        for t in w:
            t.w = me
            t.r = {}

    def collective(self, ins_ap, outs_ap, r, w):
        self._emit_waits("pool", self._deps(r, w))
        sl = self._slot(w[0])
        self.slot_total[sl] += CC_INC
        c = self.slot_total[sl]
        self.streams["pool"].append(("cc", (ins_ap, outs_ap), sl))
        me = ("d", sl, c)
        for t in r:
            t.r[("d", sl)] = me
        for t in w:
            t.w = me
            t.r = {}

    def barrier(self):
        for e in self.ENGS:
            deps = [("e", f, self.cnt[f]) for f in self.ENGS if f != e and self.cnt[f] > 0]
            deps += [("d", k, c) for k, c in enumerate(self.slot_total) if c > 0]
            self._emit_waits(e, deps)

    def finalize(self):
        nc = self.nc
        self.barrier()
        esem = {}
        for e in self.ENGS:
            if self.needed[e]:
                esem[e] = self.stack.enter_context(nc.semaphore(f"es_{e}"))
        dsem = {}
        for i in range(len(self.slot_total)):
            dsem[i] = self.stack.enter_context(nc.semaphore(f"ds_{i}"))
        rank = {}
        for e in self.ENGS:
            s = sorted(self.needed[e])
            rank[e] = {v: i + 1 for i, v in enumerate(s)}
        self.n_sems = len(esem) + len(dsem)

        def replay(e, h):
            for ent in self.streams[e]:
                if ent[0] == "wait":
                    kind, k, v = ent[1]
                    if kind == "e":
                        h.wait_ge(esem[k], rank[k][v])
                    else:
                        h.wait_ge(dsem[k], v)
                elif ent[0] == "op":
                    ins = ent[1](h)
                    if ent[2] in rank[e]:
                        ins.then_inc(esem[e], 1)
                elif ent[0] == "raw":
                    ent[1](h)
                elif ent[0] == "cc":
                    ins_ap, outs_ap = ent[1]
                    h.collective_compute("AllGather", ALU.bypass, replica_groups=[list(range(NCORES))],
                                         ins=[ins_ap], outs=[outs_ap]).then_inc(dsem[ent[2]], CC_INC)
                else:
                    out, in_ = ent[1]
                    h.dma_start(out=out, in_=in_).then_inc(dsem[ent[2]], 16)

        with nc.Block() as block:
            @block.tensor
            def _(h):
                replay("pe", h)

            @block.scalar
            def _(h):
                replay("act", h)

            @block.vector
            def _(h):
                replay("dve", h)

            @block.gpsimd
            def _(h):
                replay("pool", h)

            @block.sync
            def _(h):
                replay("sp", h)
        self.stack.close()


class Arena:
    def __init__(self, P, nbytes):
        self.t = P.stack.enter_context(P.nc.sbuf_tensor("arena", [128, nbytes // 4], F32))
        self.n = nbytes // 4
        self.off = 0

    def alloc(self, shape, dtype=F32, parts=128):
        n = 1
        for s in shape:
            n *= s
        words = (n + 1) // 2 if dtype == BF16 else n
        words = (words + 7) // 8 * 8
        assert self.off + words <= self.n, f"arena overflow: need {words*4}B at {self.off*4}B"
        v = self.t[0:parts, self.off:self.off + words]
        self.off += words
        if dtype == BF16:
            v = v.bitcast(BF16)
        v = v[:, 0:n]
        if len(shape) == 2:
            v = v.rearrange("p (a b) -> p a b", a=shape[0])
        elif len(shape) == 3:
            v = v.rearrange("p (a b c) -> p a b c", a=shape[0], b=shape[1])
        elif len(shape) == 4:
            v = v.rearrange("p (a b c d) -> p a b c d", a=shape[0], b=shape[1], c=shape[2])
        return v

    def mark(self):
        return self.off

    def reset(self, m):
        self.off = m


class Ring:
    def __init__(self, P, bufs, name):
        self.bufs = bufs
        self.toks = P.toks(len(bufs), name)
        self.i = 0

    def next(self):
        b, t = self.bufs[self.i], self.toks[self.i]
        self.i = (self.i + 1) % len(self.bufs)
        return b, t


def _vec_layout():
    lay = {}
    off = 0

    def add(name, n):
        nonlocal off
        lay[name] = (off, n)
        off += n
    for j in range(4):
        add(f"rg_conv_w{j}", 8)
    add("rg_conv_b", 8)
    add("rg_b_a", 8)
    add("rg_b_x", 8)
    add("rg_lambda", 8)
    for i in range(4):
        add(f"hg_lb{i}", 8)
    add("gla_b_gate", 4)
    for i in range(4):
        add(f"ln_mix_g{i}", 8)
        add(f"ln_mix_b{i}", 8)
        add(f"ln_ffn_g{i}", 8)
        add(f"ln_ffn_b{i}", 8)
    return lay, off


VLAY, NVEC = _vec_layout()


def _fm(v):
    v = np.asarray(v, np.float32).reshape(-1)
    return np.ascontiguousarray(v.reshape(-1, 128).T)


class KB:
    def __init__(self, stages, fused=False):
        self.nc = bass.Bass("TRN2", target_bir_lowering=False)
        self.P = Prog(self.nc)
        self.stages = stages
        self.fused = fused
        self.cur_q = 0
        self.scr = {}
        self.inputs = {}
        self.outputs = {}
        P = self.P
        self.A = Arena(P, ARENA_BYTES)
        A = self.A
        self.pb = [P.stack.enter_context(self.nc.psum_tensor(f"pb{i}", [128, 512], F32))[:] for i in range(7)]
        self.pbT = P.stack.enter_context(self.nc.psum_tensor("pbT", [128, 1024], BF16))[:]
        self.tpb = P.toks(7, "pb")
        self.tpbT = P.tok("pbT")
        self.x_f = A.alloc([8, T], F32)
        self.tx = P.toks(NTILE, "x")
        self.vecs = A.alloc([NVEC], F32)
        self.tvec = P.tok("vecs")
        self.ident_b = A.alloc([128], BF16)
        self.ident_f = A.alloc([128], F32)
        self.ones_b = A.alloc([128], BF16)
        self.sel = A.alloc([8], F32)
        self.tconst = P.tok("const")
        P.dma("sp", self.vecs, self.din("vecs", [128, NVEC]), w=[self.tvec])
        P.dma("sp", self.ident_f, self.din("ident_f", [128, 128]), w=[self.tconst])
        P.dma("sp", self.sel, self.din("keep" if fused else "sel", [128, 8]), w=[self.tconst])
        P.dma("pool", self.ident_b, self.inputs["ident_f"], w=[self.tconst])
        P.op("dve", lambda e: e.memset(self.ones_b, 1.0), w=[self.tconst])
        self.base_mark = A.mark()

    def din(self, name, shape, dtype=F32):
        if name not in self.inputs:
            self.inputs[name] = self.nc.dram_tensor(name, list(shape), dtype, kind="ExternalInput").ap()
        return self.inputs[name]

    def dout(self, name, shape, dtype=F32):
        if name not in self.outputs:
            self.outputs[name] = self.nc.dram_tensor(name, list(shape), dtype, kind="ExternalOutput").ap()
        return self.outputs[name]

    def vcol(self, name, i=0, n=1):
        o, _ = VLAY[name]
        return self.vecs[:, o + i:o + i + n]

    def pin_lnexp(self):
        return

    def set_pass(self, q):
        self.cur_q = q

    def carry_load(self, name, dst, toks, n):
        P = self.P
        if self.cur_q == 0:
            P.op("dve", lambda e: e.memset(dst, 0.0), w=list(toks))
            return
        sc, tsc = self.scr[name]
        P.dma("sp", dst, sc, r=[tsc], w=list(toks), key=toks[0])
        kq = self.sel[:, self.cur_q:self.cur_q + 1]
        P.op("dve", lambda e: e.tensor_scalar(out=dst, in0=dst, scalar1=kq, scalar2=None, op0=ALU.mult),
             r=[self.tconst], w=list(toks))

    def carry_save(self, name, src, toks, n):
        P = self.P
        if name not in self.scr:
            self.scr[name] = (self.nc.dram_tensor("scr_" + name, [128, n], F32).ap(), P.tok("scr_" + name))
        sc, tsc = self.scr[name]
        P.dma("sp", sc, src, r=list(toks), w=[tsc], key=tsc)

    def dbg(self, name, ap, tok, shape):
        o = self.dout(name, shape)
        self.P.dma("sp", o, ap, r=[tok])

    def phase_end(self):
        self.P.barrier()
        self.P.release_keys()
        self.A.reset(self.base_mark)

    def load_w(self, dst, src, tok):
        self.P.dma("pool", dst, src, w=[tok])

    def proj(self, ps, tps, wslot, tw, cols, xb, txb, n=NT, kcs=8, extra_r=()):
        P = self.P
        for kc in range(kcs):
            P.op("pe", lambda e, kc=kc: e.matmul(ps, lhsT=wslot[:, kc, cols], rhs=xb[:, kc, :],
                                                 start=(kc == 0), stop=(kc == kcs - 1)),
                 r=[tw, txb] + list(extra_r), w=[tps], skip_self=True)

    def load_x(self):
        xT = self.din(f"xT_{self.cur_q}" if self.fused else "xT", [128, 8, T])
        for ti in range(NTILE):
            self.P.dma("sp", self.x_f[:, :, ti * NT:(ti + 1) * NT], xT[:, :, ti * NT:(ti + 1) * NT], w=[self.tx[ti]])

    def store_x(self):
        yT = self.dout("yT", [128, 8, T])
        for ti in range(NTILE):
            self.P.dma("sp", yT[:, :, ti * NT:(ti + 1) * NT], self.x_f[:, :, ti * NT:(ti + 1) * NT], r=[self.tx[ti]])

    def ln_alloc(self):
        A, P = self.A, self.P
        d = dict(mean=A.alloc([NT]), var=A.alloc([NT]), rstd=A.alloc([NT]), nmr=A.alloc([NT]), t=P.tok("lnsm"))
        return d

    def layer_norm(self, z, tz, zb, tzb, zsq, tzsq, sm, gname, bname, out_f, tout, out_b=None, tout_b=None,
                   pbs=(5, 6)):
        P = self.P
        tz = list(tz)
        ps_s, ts_s = self.pb[pbs[0]], self.tpb[pbs[0]]
        ps_q, ts_q = self.pb[pbs[1]], self.tpb[pbs[1]]
        P.op("dve", lambda e: e.tensor_copy(out=zb, in_=z), r=tz, w=[tzb])
        P.op("act", lambda e: e.activation(out=zsq, in_=z, func=AF.Square), r=tz, w=[tzsq])
        for kc in range(8):
            P.op("pe", lambda e, kc=kc: e.matmul(ps_s, lhsT=self.ones_b, rhs=zb[:, kc, :], start=(kc == 0), stop=(kc == 7)),
                 r=[tzb, self.tconst], w=[ts_s], skip_self=True)
        for kc in range(8):
            P.op("pe", lambda e, kc=kc: e.matmul(ps_q, lhsT=self.ones_b, rhs=zsq[:, kc, :], start=(kc == 0), stop=(kc == 7)),
                 r=[tzsq, self.tconst], w=[ts_q], skip_self=True)
        mean, var, rstd, nmr, tsm = sm["mean"], sm["var"], sm["rstd"], sm["nmr"], sm["t"]
        epsc, tcc = self._eps, self.tcc
        P.op("act", lambda e: e.mul(out=mean, in_=ps_s, mul=1.0 / D), r=[ts_s], w=[tsm])
        P.op("act", lambda e: e.activation(out=nmr, in_=ps_s, func=AF.Square, scale=1.0 / D), r=[ts_s], w=[tsm])
        P.op("dve", lambda e: e.scalar_tensor_tensor(out=var, in0=ps_q, scalar=1.0 / D, in1=nmr, op0=ALU.mult, op1=ALU.subtract),
             r=[ts_q, tsm], w=[tsm])
        self.pin_lnexp()
        P.op("act", lambda e: e.activation(out=var, in_=var, func=AF.Ln, bias=epsc, scale=1.0), r=[tsm, tcc], w=[tsm])
        P.op("act", lambda e: e.activation(out=rstd, in_=var, func=AF.Exp, scale=-0.5), r=[tsm], w=[tsm])
        P.op("dve", lambda e: e.scalar_tensor_tensor(out=nmr, in0=mean, scalar=-1.0, in1=rstd, op0=ALU.mult, op1=ALU.mult),
             r=[tsm], w=[tsm])
        for kc in range(8):
            P.op("dve", lambda e, kc=kc: e.tensor_tensor(out=z[:, kc, :], in0=z[:, kc, :], in1=rstd, op=ALU.mult),
                 r=[tsm, tz[kc]], w=[tz[kc]])
            P.op("dve", lambda e, kc=kc: e.tensor_tensor(out=z[:, kc, :], in0=z[:, kc, :], in1=nmr, op=ALU.add),
                 r=[tsm, tz[kc]], w=[tz[kc]])
            P.op("act", lambda e, kc=kc: e.activation(out=out_f[:, kc, :], in_=z[:, kc, :], func=AF.Identity,
                                                      scale=self.vcol(gname, kc), bias=self.vcol(bname, kc)),
                 r=[tz[kc], self.tvec], w=[tout])
            if out_b is not None:
                if kc % 2 == 0:
                    P.op("dve", lambda e, kc=kc: e.tensor_scalar(out=out_b[:, kc, :], in0=z[:, kc, :], scalar1=self.vcol(gname, kc),
                                                                 scalar2=self.vcol(bname, kc), op0=ALU.mult, op1=ALU.add),
                         r=[tz[kc], self.tvec], w=[tout_b])
                else:
                    P.op("act", lambda e, kc=kc: e.activation(out=out_b[:, kc, :], in_=z[:, kc, :], func=AF.Identity,
                                                              scale=self.vcol(gname, kc), bias=self.vcol(bname, kc)),
                         r=[tz[kc], self.tvec], w=[tout_b])

    def mixer_out(self, li, ti, m_b, tm, nvc, w_out, sc):
        P = self.P
        z, tz = sc["z"], sc["tz"]
        xt = self.x_f[:, :, ti * NT:(ti + 1) * NT]
        wv = w_out.rearrange("(c p) n -> p c n", p=128)
        sw = 128 if nvc > 8 else 256
        for half in range(1024 // sw):
            slot, tw = sc["wout_ring"].next()
            self.load_w(slot[:, 0:nvc, :], wv[:, :, half * sw:(half + 1) * sw], tw)
            for m2 in range(sw // 128):
                mo = half * (sw // 128) + m2
                pbi = mo % 2
                ps, tps = self.pb[pbi], self.tpb[pbi]
                for vc in range(nvc):
                    P.op("pe", lambda e, vc=vc, m2=m2, ps=ps, slot=slot: e.matmul(
                        ps, lhsT=slot[:, vc, m2 * 128:(m2 + 1) * 128], rhs=m_b[:, vc, :], start=(vc == 0), stop=(vc == nvc - 1)),
                        r=[tw, tm], w=[tps], skip_self=True)
                P.op("dve", lambda e, mo=mo, ps=ps: e.scalar_tensor_tensor(
                    out=z[:, mo, :], in0=xt[:, mo, :], scalar=ALPHA, in1=ps, op0=ALU.mult, op1=ALU.add),
                    r=[tps, self.tx[ti]], w=[tz[mo]])
        self.layer_norm(z, tz, sc["zb"], sc["tzb"], sc["zsq"], sc["tzsq"], sc["sm"], f"ln_mix_g{li}", f"ln_mix_b{li}",
                        xt, self.tx[ti])

    def mixer_scratch(self, nvc_max, zb, tzb, zsq, tzsq, sw=256):
        A, P = self.A, self.P
        sc = {}
        sc["z"] = A.alloc([8, NT]); sc["tz"] = P.toks(8, "z")
        sc["zb"] = zb; sc["tzb"] = tzb
        sc["zsq"] = zsq; sc["tzsq"] = tzsq
        sc["sm"] = self.ln_alloc()
        sc["wout_ring"] = Ring(P, [A.alloc([nvc_max, sw], BF16) for _ in range(2)], "wout")
        return sc

    def new_consts(self):
        P = self.P
        c = self.A.alloc([4], F32)
        tc = P.tok("cc")
        P.op("dve", lambda e: e.memset(c[:, 0:1], EPS), w=[tc])
        P.op("dve", lambda e: e.memset(c[:, 1:2], 1.0), w=[tc])
        self._eps = c[:, 0:1]
        self._one = c[:, 1:2]
        self.tcc = tc

    def ffn_phase(self, li):
        P, A = self.P, self.A
        moe = (li % 2 == 1)
        NE = 8 if moe else 1
        F = FFN_EXPERT if moe else FFN_DENSE
        NF = F // 128
        G = 4
        self.new_consts()
        if moe:
            w_in_all = self.din(f"moe_w_in{li // 2}", [8, D, 2 * F])
            w_out_all = self.din(f"moe_w_out{li // 2}", [8, F, D])
            w_r = self.din(f"moe_w_router{li // 2}", [D, 8])
        else:
            w_in_all = self.din(f"dense_w_in{li // 2}", [1, D, 2 * F])
            w_out_all = self.din(f"dense_w_out{li // 2}", [1, F, D])
        wg_d = self.din(f"ple_w_gate{li}", [D, D]).rearrange("(c p) n -> p c n", p=128)
        wp_d = self.din(f"ple_w_proj{li}", [256, D]).rearrange("(c p) n -> p c n", p=128)
        pT = self.din(f"pT{li}_{self.cur_q}" if self.fused else f"pT{li}", [128, 2, T])

        TT = 1024
        xn_b = A.alloc([2, 8, NT], BF16)
        txn = P.toks(2, "xn")
        a_b = A.alloc([G, 2, NT], BF16)
        ta = P.tok("a")
        y = A.alloc([2, 8, NT], F32)
        ty = [P.toks(8, f"y{st}") for st in range(2)]
        win_ring = Ring(P, [A.alloc([8, 2, 256], BF16) for _ in range(3)], "win")
        wout_ring = Ring(P, [A.alloc([G, D], BF16) for _ in range(2)], "wo")
        sg_ring = Ring(P, [A.alloc([NT], F32) for _ in range(2)], "sg")
        sm = self.ln_alloc()
        wg_ring = Ring(P, [A.alloc([8, 256], BF16) for _ in range(2)], "wg")
        wp_b = A.alloc([2, D], BF16)
        twp = P.tok("wp")
        p_b = A.alloc([2, NT], BF16)
        tp = P.tok("p")
        tmp_ring = Ring(P, [A.alloc([NT], F32) for _ in range(2)], "tmp")
        self.load_w(wp_b, wp_d, twp)
        if moe:
            wr_f = A.alloc([8, 8], F32)
            twr = P.tok("wr")
            P.dma("sp", wr_f, w_r.rearrange("(c p) n -> p c n", p=128), w=[twr])
            ones8 = A.alloc([128], F32, parts=8)
            P.op("dve", lambda e: e.memset(ones8, 1.0), w=[twr])
            gm = A.alloc([NT], F32, parts=8)
            tgm = P.tok("gm")
            lg_s = A.alloc([NT], F32, parts=8)
            tlg = P.tok("lg")
            lt = A.alloc([4, 8], F32)
            mx = A.alloc([4, 8], F32)
            ex = A.alloc([4, 8], F32)
            msk = A.alloc([4, 8], F32)
            den = A.alloc([4], F32)
            trt = P.tok("rt")
            g_fm = A.alloc([2, NT], F32, parts=8)
            tgf = P.tok("gfm")
            gate_b = A.alloc([2, NT], BF16)
            tgb = P.tok("gb")

        groups = [(f0, min(G, NF - f0)) for f0 in range(0, NF, G)]
        for tt in range(2):
            tiles = [tt * 2, tt * 2 + 1]
            for st in range(2):
                ti = tiles[st]
                P.op("act", lambda e, st=st, ti=ti: e.copy(out=xn_b[:, st], in_=self.x_f[:, :, ti * NT:(ti + 1) * NT]),
                     r=[self.tx[ti]], w=[txn[st]])
            if moe:
                for st in range(2):
                    ti = tiles[st]
                    xt = self.x_f[:, :, ti * NT:(ti + 1) * NT]
                    ps, tps = self.pb[6], self.tpb[6]
                    for kc in range(8):
                        P.op("pe", lambda e, kc=kc, xt=xt: e.matmul(ps[0:8, :], lhsT=wr_f[:, kc, :], rhs=xt[:, kc, :],
                                                                    start=(kc == 0), stop=(kc == 7)),
                             r=[twr, self.tx[ti]], w=[tps], skip_self=True)
                    P.op("act", lambda e: e.copy(out=lg_s, in_=ps[0:8, :]), r=[tps], w=[tlg])
                    pst = self.pb[6][:, 0:32].rearrange("p (a b) -> p a b", a=4)
                    for blk in range(4):
                        P.op("pe", lambda e, blk=blk: e.transpose(out=pst[:, blk, :], in_=lg_s[:, blk * 128:(blk + 1) * 128],
                                                                  identity=self.ident_f[0:8, 0:8]),
                             r=[tlg, self.tconst], w=[tps], skip_self=True)
                    P.op("dve", lambda e: e.tensor_copy(out=lt, in_=pst), r=[tps], w=[trt])
                    for blk in range(4):
                        P.op("dve", lambda e, blk=blk: e.max(out=mx[:, blk, :], in_=lt[:, blk, :]), r=[trt], w=[trt])
                    P.op("dve", lambda e: e.tensor_tensor(out=ex, in0=lt, in1=mx[:, :, 0:1].to_broadcast([128, 4, 8]), op=ALU.subtract),
                         r=[trt], w=[trt])
                    P.op("act", lambda e: e.activation(out=ex, in_=ex, func=AF.Exp), r=[trt], w=[trt])
                    P.op("dve", lambda e: e.tensor_tensor(out=msk, in0=lt, in1=mx[:, :, 1:2].to_broadcast([128, 4, 8]), op=ALU.is_ge),
                         r=[trt], w=[trt])
                    P.op("dve", lambda e: e.tensor_tensor(out=ex, in0=ex, in1=msk, op=ALU.mult), r=[trt], w=[trt])
                    P.op("dve", lambda e: e.tensor_reduce(out=den, in_=ex, axis=AX.X, op=ALU.add), r=[trt], w=[trt])
                    P.op("dve", lambda e: e.reciprocal(out=den, in_=den), r=[trt], w=[trt])
                    P.op("dve", lambda e: e.tensor_tensor(out=ex, in0=ex, in1=den.unsqueeze(2).to_broadcast([128, 4, 8]), op=ALU.mult),
                         r=[trt], w=[trt])
                    for blk in range(4):
                        P.op("pe", lambda e, blk=blk: e.transpose(out=ps[0:8, blk * 128:(blk + 1) * 128], in_=ex[:, blk, :],
                                                                  identity=self.ident_f),
                             r=[trt, self.tconst], w=[tps], skip_self=True)
                    P.op("act", lambda e, st=st: e.copy(out=g_fm[:, st, :], in_=ps[0:8, :]), r=[tps], w=[tgf])
            for ei in range(NE):
                w_in = w_in_all[ei].rearrange("(c p) n -> p c n", p=128)
                w_out = w_out_all[ei]
                if moe:
                    for st in range(2):
                        ps, tps = self.pb[6], self.tpb[6]
                        P.op("dve", lambda e, st=st, ei=ei: e.tensor_scalar(out=gm, in0=g_fm[:, st, :], scalar1=self.ident_f[0:8, ei:ei + 1],
                                                                          scalar2=None, op0=ALU.mult), r=[tgf, self.tconst], w=[tgm])
                        P.op("pe", lambda e: e.matmul(ps, lhsT=ones8, rhs=gm, start=True, stop=True),
                             r=[tgm, twr], w=[tps], skip_self=True)
                        P.op("act", lambda e, st=st: e.copy(out=gate_b[:, st, :], in_=ps), r=[tps], w=[tgb])
                for gi, (f0, g) in enumerate(groups):
                    for pr in range(0, g, 2):
                        npair = min(2, g - pr)
                        slot, tw = win_ring.next()
                        c0 = (f0 + pr) * 128
                        self.load_w(slot[:, :, 0, 0:npair * 128], w_in[:, :, c0:c0 + npair * 128], tw)
                        self.load_w(slot[:, :, 1, 0:npair * 128], w_in[:, :, F + c0:F + c0 + npair * 128], tw)
                        for ff in range(npair):
                            fi = pr + ff
                            for st in range(2):
                                bi = 2 * st
                                psg, tg_ = self.pb[bi], self.tpb[bi]
                                psu, tu_ = self.pb[bi + 1], self.tpb[bi + 1]
                                for kc in range(8):
                                    P.op("pe", lambda e, kc=kc, ff=ff, st=st, psg=psg, slot=slot: e.matmul(
                                        psg, lhsT=slot[:, kc, 0, ff * 128:(ff + 1) * 128], rhs=xn_b[:, st, kc, :],
                                        start=(kc == 0), stop=(kc == 7)), r=[tw, txn[st]], w=[tg_], skip_self=True)
                                for kc in range(8):
                                    P.op("pe", lambda e, kc=kc, ff=ff, st=st, psu=psu, slot=slot: e.matmul(
                                        psu, lhsT=slot[:, kc, 1, ff * 128:(ff + 1) * 128], rhs=xn_b[:, st, kc, :],
                                        start=(kc == 0), stop=(kc == 7)), r=[tw, txn[st]], w=[tu_], skip_self=True)
                                sg, tsg = sg_ring.next()
                                P.op("act", lambda e, sg=sg, psg=psg: e.activation(out=sg, in_=psg, func=AF.Silu), r=[tg_], w=[tsg])
                                if moe:
                                    P.op("dve", lambda e, sg=sg, st=st: e.tensor_tensor(out=sg, in0=sg, in1=gate_b[:, st, :], op=ALU.mult),
                                         r=[tsg, tgb], w=[tsg])
                                P.op("dve", lambda e, sg=sg, psu=psu, fi=fi, st=st: e.tensor_tensor(
                                    out=a_b[:, fi, st, :], in0=sg, in1=psu, op=ALU.mult), r=[tsg, tu_], w=[ta])
                    wslot, two = wout_ring.next()
                    self.load_w(wslot[:, 0:g, :], w_out[f0 * 128:(f0 + g) * 128, :].rearrange("(g p) n -> p g n", p=128), two)
                    first = (ei == 0 and gi == 0)
                    for st in range(2):
                        for mo in range(8):
                            bi = 4 + (mo % 2)
                            ps, tps = self.pb[bi], self.tpb[bi]
                            for fi in range(g):
                                P.op("pe", lambda e, fi=fi, mo=mo, st=st, ps=ps, wslot=wslot: e.matmul(
                                    ps, lhsT=wslot[:, fi, mo * 128:(mo + 1) * 128], rhs=a_b[:, fi, st, :],
                                    start=(fi == 0), stop=(fi == g - 1)), r=[two, ta], w=[tps], skip_self=True)
                            if first:
                                P.op("act", lambda e, mo=mo, st=st, ps=ps: e.copy(out=y[:, st, mo, :], in_=ps), r=[tps], w=[ty[st][mo]])
                            else:
                                P.op("dve", lambda e, mo=mo, st=st, ps=ps: e.tensor_tensor(
                                    out=y[:, st, mo, :], in0=y[:, st, mo, :], in1=ps, op=ALU.add), r=[tps, ty[st][mo]], w=[ty[st][mo]])
            for st in range(2):
                ti = tiles[st]
                xt = self.x_f[:, :, ti * NT:(ti + 1) * NT]
                yz = y[:, st]
                P.op("dve", lambda e, yz=yz, xt=xt: e.scalar_tensor_tensor(out=yz, in0=xt, scalar=ALPHA, in1=yz,
                                                                           op0=ALU.mult, op1=ALU.add),
                     r=[self.tx[ti]] + ty[st], w=ty[st])
                zb, zsq = xn_b[:, 0], xn_b[:, 1]
                self.layer_norm(yz, ty[st], zb, txn[0], zsq, txn[1], sm, f"ln_ffn_g{li}", f"ln_ffn_b{li}",
                                xt, self.tx[ti], out_b=zb, tout_b=txn[0], pbs=(5, 6))
                xb = zb
                self.load_w(p_b, pT[:, :, ti * NT:(ti + 1) * NT], tp)
                for q4 in range(4):
                    wgs, twg = wg_ring.next()
                    self.load_w(wgs, wg_d[:, :, q4 * 256:(q4 + 1) * 256], twg)
                    for m2 in range(2):
                        mo = q4 * 2 + m2
                        psg, tg_ = self.pb[0 + 2 * m2], self.tpb[0 + 2 * m2]
                        psp, tp_ = self.pb[1 + 2 * m2], self.tpb[1 + 2 * m2]
                        for kc in range(8):
                            P.op("pe", lambda e, kc=kc, m2=m2, psg=psg, wgs=wgs: e.matmul(
                                psg, lhsT=wgs[:, kc, m2 * 128:(m2 + 1) * 128], rhs=xb[:, kc, :], start=(kc == 0), stop=(kc == 7)),
                                r=[twg, txn[0]], w=[tg_], skip_self=True)
                        for k2 in range(2):
                            P.op("pe", lambda e, k2=k2, mo=mo, psp=psp: e.matmul(
                                psp, lhsT=wp_b[:, k2, mo * 128:(mo + 1) * 128], rhs=p_b[:, k2, :], start=(k2 == 0), stop=(k2 == 1)),
                                r=[twp, tp], w=[tp_], skip_self=True)
                        sg, tsg = sg_ring.next()
                        P.op("act", lambda e, sg=sg, psg=psg: e.activation(out=sg, in_=psg, func=AF.Sigmoid), r=[tg_], w=[tsg])
                        tmp, ttmp = tmp_ring.next()
                        P.op("dve", lambda e, sg=sg, psp=psp, tmp=tmp: e.tensor_tensor(out=tmp, in0=sg, in1=psp, op=ALU.mult),
                             r=[tsg, tp_], w=[ttmp])
                        P.op("dve", lambda e, mo=mo, xt=xt, tmp=tmp: e.tensor_tensor(out=xt[:, mo, :], in0=xt[:, mo, :], in1=tmp, op=ALU.add),
                             r=[ttmp, self.tx[ti]], w=[self.tx[ti]])
        self.phase_end()

    def exchange_out(self, li, src, tsrc, W):
        st = self.dout(f"st_loc{li}", [128, W])
        self.P.dma("sp", st, src, r=[tsrc])

    def rglru_pass(self, mode):
        P, A = self.P, self.A
        li = 0
        self.new_consts()
        w_in = self.din("rg_w_in", [D, 2 * D]).rearrange("(c p) n -> p c n", p=128)
        w_a = self.din("rg_w_a", [4, 256, 256])
        w_x = self.din("rg_w_x", [4, 256, 256])
        x_b = A.alloc([8, NT], BF16); txb = P.tok("xb")
        if not self.fused:
            xh = self.din("xh", [128, 8, 4])
            xh_b = A.alloc([8, 4], BF16); txh = P.tok("xh")
            self.load_w(xh_b, xh, txh)
        wa_b = A.alloc([4, 2, 256], BF16); wx_b = A.alloc([4, 2, 256], BF16); twax = P.tok("wax")
        for n in range(4):
            self.load_w(wa_b[:, n], w_a[n].rearrange("(c p) n -> p c n", p=128), twax)
            self.load_w(wx_b[:, n], w_x[n].rearrange("(c p) n -> p c n", p=128), twax)
        w_ring = Ring(P, [A.alloc([8, 256], BF16) for _ in range(3)], "wi")
        rec = A.alloc([2, NT + 3], F32); trec = P.tok("rec")
        halo = A.alloc([8, 3], F32); thalo = P.tok("halo")
        u = A.alloc([2, NT], F32); tu = P.tok("u")
        u_b = A.alloc([2, NT], BF16); tub = P.tok("ub")
        r_s2 = [A.alloc([NT], F32) for _ in range(2)]; i_s2 = [A.alloc([NT], F32) for _ in range(2)]
        a_s2 = [A.alloc([NT], F32) for _ in range(2)]; q_s2 = [A.alloc([NT], F32) for _ in range(2)]
        h_s2 = [A.alloc([NT], F32) for _ in range(2)]
        tg2 = P.toks(2, "gates"); th2 = P.toks(2, "h")
        th = th2[0]
        hst = A.alloc([8], F32); thst = P.tok("hst")
        cl = A.alloc([8], F32); cl2 = A.alloc([8], F32); tcl = P.tok("cl")
        lam = self.vcol("rg_lambda", 0, 8)
        one_c = self._one
        P.op("act", lambda e: e.activation(out=cl, in_=lam, func=AF.Exp, scale=-1.0), r=[self.tvec], w=[tcl])
        P.op("act", lambda e: e.activation(out=cl, in_=cl, func=AF.Ln, bias=one_c.to_broadcast([128, 8]) if False else one_c, scale=1.0), r=[tcl, self.tcc], w=[tcl])
        P.op("dve", lambda e: e.tensor_scalar(out=cl2, in0=cl, scalar1=-16.0, scalar2=None, op0=ALU.mult), r=[tcl], w=[tcl])
        P.op("dve", lambda e: e.tensor_scalar(out=cl, in0=cl, scalar1=-8.0, scalar2=None, op0=ALU.mult), r=[tcl], w=[tcl])
        if mode == "A":
            P.op("dve", lambda e: e.memset(hst, 0.0), w=[thst])
            ptot = A.alloc([8], F32)
            P.op("dve", lambda e: e.memset(ptot, 1.0), w=[thst])
            pt_s = A.alloc([NT], F32)
            zeros = A.alloc([NT], F32)
            P.op("dve", lambda e: e.memset(zeros, 0.0), w=[thst])
        elif self.fused:
            self.carry_load("rg_h", hst, [thst], 8)
            self.carry_load("rg_halo", halo.rearrange("p a b -> p (a b)"), [thalo], 24)
            m_b = A.alloc([8, NT], BF16); tm = P.tok("m")
            gb2 = [A.alloc([NT], F32) for _ in range(2)]; g22 = [A.alloc([NT], F32) for _ in range(2)]; tgb2 = P.toks(2, "gb")
            sc = self.mixer_scratch(8, x_b, txb, m_b, tm)
            w_out = self.din("rg_w_out", [D, D])
        else:
            st_all = self.din("st_all0", [8, 128, 16])
            sta = A.alloc([8, 16], F32); tsta = P.tok("sta")
            P.dma("sp", sta, st_all.rearrange("r p w -> p r w"), w=[tsta])
            P.op("dve", lambda e: e.memset(hst, 0.0), w=[thst])
            dsel = A.alloc([8], F32); hl = A.alloc([8], F32)
            for r in range(8):
                sr = self.sel[:, r:r + 1]
                P.op("dve", lambda e, r=r, sr=sr: e.tensor_scalar(out=dsel, in0=sta[:, r, 8:16], scalar1=-1.0, scalar2=sr,
                                                                 op0=ALU.add, op1=ALU.mult), r=[tsta, self.tconst], w=[thst])
                P.op("dve", lambda e: e.tensor_scalar(out=dsel, in0=dsel, scalar1=1.0, scalar2=None, op0=ALU.add), r=[thst], w=[thst])
                P.op("dve", lambda e, r=r, sr=sr: e.tensor_scalar(out=hl, in0=sta[:, r, 0:8], scalar1=sr, scalar2=None, op0=ALU.mult),
                     r=[tsta, self.tconst], w=[thst])
                P.op("dve", lambda e: e.tensor_tensor(out=hst, in0=hst, in1=dsel, op=ALU.mult), r=[thst], w=[thst])
                P.op("dve", lambda e: e.tensor_tensor(out=hst, in0=hst, in1=hl, op=ALU.add), r=[thst], w=[thst])
            m_b = A.alloc([8, NT], BF16); tm = P.tok("m")
            gb2 = [A.alloc([NT], F32) for _ in range(2)]; g22 = [A.alloc([NT], F32) for _ in range(2)]; tgb2 = P.toks(2, "gb")
            sc = self.mixer_scratch(8, x_b, txb, m_b, tm)
            w_out = self.din("rg_w_out", [D, D])
        for n in range(4 if not self.fused else 0):
            slot, tw = w_ring.next()
            self.load_w(slot, w_in[:, :, D + n * 256:D + (n + 1) * 256], tw)
            for c2 in range(2):
                cc = 2 * n + c2
                ps, tps = self.pb[c2], self.tpb[c2]
                self.proj(ps[:, 0:4], tps, slot, tw, slice(c2 * 128, (c2 + 1) * 128), xh_b, txh)
                P.op("act", lambda e, cc=cc, ps=ps: e.copy(out=halo[:, cc, :], in_=ps[:, 0:3]), r=[tps], w=[thalo])
        for ti in range(NTILE):
            xt = self.x_f[:, :, ti * NT:(ti + 1) * NT]
            P.op("act", lambda e, xt=xt: e.copy(out=x_b, in_=xt), r=[self.tx[ti]], w=[txb])
            for n in range(4):
                slot, tw = w_ring.next()
                self.load_w(slot, w_in[:, :, D + n * 256:D + (n + 1) * 256], tw)
                if mode == "B":
                    gslot, tgw = w_ring.next()
                    self.load_w(gslot, w_in[:, :, n * 256:(n + 1) * 256], tgw)
                for c2 in range(2):
                    cc = 2 * n + c2
                    ps, tps = self.pb[c2], self.tpb[c2]
                    self.proj(ps, tps, slot, tw, slice(c2 * 128, (c2 + 1) * 128), x_b, txb)
                    P.op("dve", lambda e, cc=cc, c2=c2: e.tensor_copy(out=rec[:, c2, 0:3], in_=halo[:, cc, :]), r=[thalo], w=[trec])
                    P.op("act", lambda e, c2=c2, ps=ps: e.copy(out=rec[:, c2, 3:NT + 3], in_=ps), r=[tps], w=[trec])
                    P.op("dve", lambda e, cc=cc, c2=c2: e.tensor_copy(out=halo[:, cc, :], in_=rec[:, c2, NT:NT + 3]), r=[trec], w=[thalo])
                    P.op("act", lambda e, cc=cc, c2=c2: e.activation(out=u[:, c2, :], in_=rec[:, c2, 0:NT], func=AF.Identity,
                                                                     scale=self.vcol("rg_conv_w0", cc), bias=self.vcol("rg_conv_b", cc)),
                         r=[trec, self.tvec], w=[tu])
                    for j in range(1, 4):
                        P.op("dve", lambda e, cc=cc, c2=c2, j=j: e.scalar_tensor_tensor(
                            out=u[:, c2, :], in0=rec[:, c2, j:j + NT], scalar=self.vcol(f"rg_conv_w{j}", cc), in1=u[:, c2, :],
                            op0=ALU.mult, op1=ALU.add), r=[trec, tu, self.tvec], w=[tu])
                    P.op("act", lambda e, c2=c2: e.copy(out=u_b[:, c2, :], in_=u[:, c2, :]), r=[tu], w=[tub])
                def chain(c2, n=n):
                    cc = 2 * n + c2
                    r_s, i_s, a_s, q_s, h_s = r_s2[c2], i_s2[c2], a_s2[c2], q_s2[c2], h_s2[c2]
                    tg, th = tg2[c2], th2[c2]
                    psr, tpr = (self.pb[2], self.tpb[2]) if c2 == 0 else (self.pb[5], self.tpb[5])
                    psi, tpi = (self.pb[3], self.tpb[3]) if c2 == 0 else (self.pb[6], self.tpb[6])
                    for k2 in range(2):
                        P.op("pe", lambda e, k2=k2: e.matmul(psr, lhsT=wa_b[:, n, k2, c2 * 128:(c2 + 1) * 128], rhs=u_b[:, k2, :],
                                                             start=(k2 == 0), stop=(k2 == 1)), r=[twax, tub], w=[tpr], skip_self=True)
                    for k2 in range(2):
                        P.op("pe", lambda e, k2=k2: e.matmul(psi, lhsT=wx_b[:, n, k2, c2 * 128:(c2 + 1) * 128], rhs=u_b[:, k2, :],
                                                             start=(k2 == 0), stop=(k2 == 1)), r=[twax, tub], w=[tpi], skip_self=True)
                    if mode == "B":
                        psg, tpg = self.pb[4 + c2] if c2 == 0 else self.pb[1], self.tpb[4 + c2] if c2 == 0 else self.tpb[1]
                        psg, tpg = (self.pb[4], self.tpb[4]) if c2 == 0 else (self.pb[1], self.tpb[1])
                        self.proj(psg, tpg, gslot, tgw, slice(c2 * 128, (c2 + 1) * 128), x_b, txb)
                    yield
                    P.op("act", lambda e: e.activation(out=r_s, in_=psr, func=AF.Sigmoid, bias=self.vcol("rg_b_a", cc), scale=1.0),
                         r=[tpr, self.tvec], w=[tg])
                    P.op("act", lambda e: e.activation(out=i_s, in_=psi, func=AF.Sigmoid, bias=self.vcol("rg_b_x", cc), scale=1.0),
                         r=[tpi, self.tvec], w=[tg])
                    yield
                    P.op("act", lambda e: e.activation(out=a_s, in_=r_s, func=AF.Exp, scale=cl[:, cc:cc + 1]), r=[tg, tcl], w=[tg])
                    P.op("act", lambda e: e.activation(out=q_s, in_=r_s, func=AF.Exp, scale=cl2[:, cc:cc + 1]), r=[tg, tcl], w=[tg])
                    yield
                    P.op("act", lambda e: e.activation(out=q_s, in_=q_s, func=AF.Sqrt, bias=one_c, scale=-1.0), r=[tg, self.tcc], w=[tg])
                    yield
                    P.op("dve", lambda e: e.tensor_tensor(out=q_s, in0=q_s, in1=i_s, op=ALU.mult), r=[tg], w=[tg])
                    P.op("dve", lambda e: e.tensor_tensor(out=q_s, in0=q_s, in1=u[:, c2, :], op=ALU.mult), r=[tg, tu], w=[tg])
                    P.op("dve", lambda e: e.tensor_tensor_scan(out=h_s, data0=a_s, data1=q_s, initial=hst[:, cc:cc + 1],
                                                               op0=ALU.mult, op1=ALU.add), r=[tg, thst], w=[th])
                    P.op("dve", lambda e: e.tensor_copy(out=hst[:, cc:cc + 1], in_=h_s[:, NT - 1:NT]), r=[th], w=[thst])
                    yield
                    if mode == "A":
                        P.op("dve", lambda e: e.tensor_tensor_scan(out=pt_s, data0=a_s, data1=zeros, initial=ptot[:, cc:cc + 1],
                                                                   op0=ALU.mult, op1=ALU.add), r=[tg, thst, self.tconst], w=[th])
                        P.op("dve", lambda e: e.tensor_copy(out=ptot[:, cc:cc + 1], in_=pt_s[:, NT - 1:NT]), r=[th], w=[thst])
                    else:
                        g2, gb, tgb = g22[c2], gb2[c2], tgb2[c2]
                        P.op("act", lambda e: e.activation(out=g2, in_=psg, func=AF.Square), r=[tpg], w=[tgb])
                        yield
                        P.op("dve", lambda e: e.tensor_scalar(out=g2, in0=g2, scalar1=0.044715, scalar2=1.0, op0=ALU.mult, op1=ALU.add),
                             r=[tgb], w=[tgb])
                        P.op("dve", lambda e: e.tensor_tensor(out=g2, in0=g2, in1=psg, op=ALU.mult), r=[tgb, tpg], w=[tgb])
                        yield
                        P.op("act", lambda e: e.activation(out=g2, in_=g2, func=AF.Sigmoid, scale=1.5957691216057308), r=[tgb], w=[tgb])
                        yield
                        P.op("dve", lambda e: e.tensor_tensor(out=gb, in0=g2, in1=psg, op=ALU.mult), r=[tgb, tpg], w=[tgb])
                        P.op("dve", lambda e: e.tensor_tensor(out=m_b[:, cc, :], in0=gb, in1=h_s, op=ALU.mult), r=[tgb, th], w=[tm])

                gens = [chain(0), chain(1)]
                while gens:
                    for g in list(gens):
                        try:
                            next(g)
                        except StopIteration:
                            gens.remove(g)
            if mode == "B":
                self.mixer_out(li, ti, m_b, tm, 8, w_out, sc)
        if mode == "A":
            stl = A.alloc([16], F32)
            P.op("dve", lambda e: e.tensor_copy(out=stl[:, 0:8], in_=hst), r=[thst], w=[th])
            P.op("dve", lambda e: e.tensor_copy(out=stl[:, 8:16], in_=ptot), r=[thst], w=[th])
            self.exchange_out(0, stl, th, 16)
        if self.fused:
            self.carry_save("rg_h", hst, [thst], 8)
            self.carry_save("rg_halo", halo.rearrange("p a b -> p (a b)"), [thalo], 24)
        self.phase_end()

    def gla_alloc(self, H, dv, mode):
        A, P = self.A, self.P
        c = dict(H=H, dv=dv, dvc=dv // 128, mode=mode)
        c["reset"] = A.alloc([NT], F32)
        c["maskT"] = A.alloc([128], F32)
        c["rowmask"] = A.alloc([4], F32)
        c["ttab"] = P.tok("gtab")
        P.dma("sp", c["reset"], self.din("tab_reset", [128, NT]), w=[c["ttab"]])
        P.dma("sp", c["maskT"], self.din("tab_maskT", [128, 128]), w=[c["ttab"]])
        P.dma("sp", c["rowmask"], self.din("tab_rowmask", [128, 4]), w=[c["ttab"]])
        c["S"] = A.alloc([H, dv], F32); c["tS"] = P.toks(H, "S")
        c["slg"] = A.alloc([H], F32); c["tslg"] = P.tok("slg")
        c["cum"] = A.alloc([NT], F32); c["E"] = A.alloc([NT], F32); c["E2"] = A.alloc([NT], F32)
        c["tcum"] = P.tok("cum"); c["tE"] = P.tok("E"); c["tE2"] = P.tok("E2")
        c["kd_b"] = A.alloc([NT], BF16); c["tkd"] = P.tok("kd")
        c["kdm"] = A.alloc([4, 4, 128], BF16); c["tkdm"] = P.toks(4, "kdm")
        c["dec"] = A.alloc([16], F32); c["tdec"] = P.tok("dec")
        c["red"] = A.alloc([1], F32)
        c["Sr"] = [A.alloc([dv], F32) for _ in range(4)]; c["tSr"] = P.toks(4, "Sr")
        if mode == "B":
            c["qe_b"] = A.alloc([NT], BF16); c["ke_b"] = A.alloc([NT], BF16); c["qi_f"] = A.alloc([NT], F32)
            c["tqk"] = P.tok("qk")
            c["PT"] = Ring(P, [A.alloc([128], BF16) for _ in range(2)], "PT")
            c["o_s"] = A.alloc([c["dvc"], NT], F32); c["to"] = P.tok("o")
            c["osq"] = A.alloc([c["dvc"], NT], BF16); c["tosq"] = P.tok("osq")
            c["rn"] = A.alloc([NT], F32); c["trn"] = P.tok("rn")
        return c

    def gla_init_state(self, c, li):
        P, A = self.P, self.A
        H, dv = c["H"], c["dv"]
        W = H * dv + H
        c["W"] = W
        S, tS = c["S"], c["tS"]
        P.op("dve", lambda e: e.memset(c["slg"], 0.0), w=[c["tslg"]])
        if self.fused:
            self.carry_load(f"S{li}", S.rearrange("p a b -> p (a b)"), tS, H * dv)
            return
        for h in range(H):
            P.op("dve", lambda e, h=h: e.memset(S[:, h, :], 0.0), w=[tS[h]])
        if c["mode"] == "A":
            return
        st_all = self.din(f"st_all{li}", [8, 128, W])
        m = A.mark()
        sta = A.alloc([W], F32); tsta = P.tok("sta")
        dsel = A.alloc([H], F32); tds = P.tok("dsel")
        for r in range(8):
            sr = self.sel[:, r:r + 1]
            P.dma("sp", sta, st_all[r], w=[tsta])
            P.op("act", lambda e: e.activation(out=dsel, in_=sta[:, H * dv:H * dv + H], func=AF.Exp), r=[tsta], w=[tds])
            P.op("dve", lambda e, sr=sr: e.tensor_scalar(out=dsel, in0=dsel, scalar1=-1.0, scalar2=sr, op0=ALU.add, op1=ALU.mult),
                 r=[tds, self.tconst], w=[tds])
            P.op("dve", lambda e: e.tensor_scalar(out=dsel, in0=dsel, scalar1=1.0, scalar2=None, op0=ALU.add), r=[tds], w=[tds])
            P.op("dve", lambda e, sr=sr: e.tensor_scalar(out=sta[:, 0:H * dv], in0=sta[:, 0:H * dv], scalar1=sr, scalar2=None, op0=ALU.mult),
                 r=[tsta, self.tconst], w=[tsta])
            for h in range(H):
                P.op("dve", lambda e, h=h: e.scalar_tensor_tensor(out=S[:, h, :], in0=S[:, h, :], scalar=dsel[:, h:h + 1],
                                                                  in1=sta[:, h * dv:(h + 1) * dv], op0=ALU.mult, op1=ALU.add),
                     r=[tds, tsta, tS[h]], w=[tS[h]])
        P.barrier()
        A.reset(m)

    def gla_finish(self, c, li):
        if self.fused:
            self.carry_save(f"S{li}", c["S"].rearrange("p a b -> p (a b)"), c["tS"], c["H"] * c["dv"])
        elif c["mode"] == "A":
            self.gla_finish_A(c, li)

    def gla_finish_A(self, c, li):
        P, A = self.P, self.A
        H, dv = c["H"], c["dv"]
        stl = A.alloc([H * dv + H], F32); tst = P.tok("stl")
        for h in range(H):
            P.op("dve", lambda e, h=h: e.tensor_copy(out=stl[:, h * dv:(h + 1) * dv], in_=c["S"][:, h, :]), r=[c["tS"][h]], w=[tst])
        P.op("dve", lambda e: e.tensor_copy(out=stl[:, H * dv:H * dv + H], in_=c["slg"]), r=[c["tslg"]], w=[tst])
        self.exchange_out(li, stl, tst, H * dv + H)

    def gla_head(self, c, h, q_f, k_f, lg_f, tin, v_tok, tv, mid_hook=None):
        P = self.P
        mode, dv, dvc = c["mode"], c["dv"], c["dvc"]
        cum, E, E2 = c["cum"], c["E"], c["E2"]
        tcum, tE, tE2 = c["tcum"], c["tE"], c["tE2"]
        S, tS = c["S"][:, h, :], c["tS"][h]
        if not c.get("pinned_by_front"):
            self.pin_lnexp()
        P.op("dve", lambda e: e.tensor_tensor_scan(out=cum, data0=c["reset"], data1=lg_f, initial=0.0, op0=ALU.mult, op1=ALU.add),
             r=[tin, c["ttab"]], w=[tcum])
        P.op("dve", lambda e: e.tensor_reduce(out=c["red"], in_=lg_f, axis=AX.X, op=ALU.add), r=[tin], w=[tE2])
        P.op("dve", lambda e: e.tensor_tensor(out=c["slg"][:, h:h + 1], in0=c["slg"][:, h:h + 1], in1=c["red"], op=ALU.add),
             r=[tE2, c["tslg"]], w=[c["tslg"]])
        cum3 = cum.rearrange("p (c t) -> p c t", t=32)
        E3 = E.rearrange("p (c t) -> p c t", t=32)
        lastb = cum3[:, :, 31:32].to_broadcast([128, 16, 32])
        refb = cum3[:, :, 16:17].to_broadcast([128, 16, 32])
        P.op("dve", lambda e: e.tensor_tensor(out=E3, in0=lastb, in1=cum3, op=ALU.subtract), r=[tcum], w=[tE])
        P.op("act", lambda e: e.activation(out=E, in_=E, func=AF.Exp), r=[tE], w=[tE])
        P.op("dve", lambda e: e.tensor_tensor(out=c["kd_b"], in0=k_f, in1=E, op=ALU.mult), r=[tE, tin], w=[c["tkd"]])
        P.op("act", lambda e: e.activation(out=c["dec"], in_=cum3[:, :, 31], func=AF.Exp), r=[tcum], w=[c["tdec"]])
        pT4 = self.pbT[:, 0:512].rearrange("p (b d) -> p b d", b=4)
        for blk in range(4):
            P.op("pe", lambda e, blk=blk: e.transpose(out=pT4[:, blk, :], in_=c["kd_b"][:, blk * 128:(blk + 1) * 128], identity=self.ident_b),
                 r=[c["tkd"], self.tconst], w=[self.tpbT], skip_self=True)
        for blk in range(4):
            P.op("dve", lambda e, blk=blk: e.tensor_tensor(
                out=c["kdm"][:, blk], in0=pT4[:, blk, :].unsqueeze(1).to_broadcast([128, 4, 128]),
                in1=c["rowmask"].unsqueeze(2).to_broadcast([128, 4, 128]), op=ALU.mult),
                r=[self.tpbT, c["ttab"]], w=[c["tkdm"][blk]])
        if mode == "B":
            qe_b, ke_b, qi_f, tqk = c["qe_b"], c["ke_b"], c["qi_f"], c["tqk"]
            E23 = E2.rearrange("p (c t) -> p c t", t=32)
            P.op("act", lambda e: e.activation(out=qi_f, in_=cum, func=AF.Exp), r=[tcum], w=[tqk])
            P.op("dve", lambda e: e.tensor_tensor(out=qi_f, in0=qi_f, in1=q_f, op=ALU.mult), r=[tqk, tin], w=[tqk])
            P.op("dve", lambda e: e.tensor_tensor(out=E23, in0=cum3, in1=refb, op=ALU.subtract), r=[tcum], w=[tE2])
            P.op("act", lambda e: e.activation(out=E, in_=E2, func=AF.Exp), r=[tE2, c["tkd"]], w=[tE])
            P.op("dve", lambda e: e.tensor_tensor(out=qe_b, in0=q_f, in1=E, op=ALU.mult), r=[tE, tin], w=[tqk])
            P.op("act", lambda e: e.activation(out=E2, in_=E2, func=AF.Exp, scale=-1.0), r=[tE2], w=[tE2])
            P.op("dve", lambda e: e.tensor_tensor(out=ke_b, in0=k_f, in1=E2, op=ALU.mult), r=[tE2, tin], w=[tqk])
        for blk in range(4):
            bs = slice(blk * 128, (blk + 1) * 128)
            if mode == "B":
                pso, tpo = self.pb[3 + blk % 2], self.tpb[3 + blk % 2]
                pss, tss = pso[:, 384:512], tpo
                P.op("pe", lambda e, bs=bs, pss=pss: e.matmul(pss, lhsT=c["ke_b"][:, bs], rhs=c["qe_b"][:, bs], start=True, stop=True,
                                                              skip_group_check=True),
                     r=[c["tqk"]], w=[tss], skip_self=True)
                PT, tPT = c["PT"].next()
                P.op("dve", lambda e, PT=PT, pss=pss: e.tensor_tensor(out=PT, in0=pss, in1=c["maskT"], op=ALU.mult),
                     r=[tss, c["ttab"]], w=[tPT])
                pso3 = pso[:, 0:dvc * 128].rearrange("p (a b) -> p a b", a=dvc)
                for ec in range(dvc):
                    P.op("pe", lambda e, ec=ec, blk=blk, PT=PT, pso3=pso3: e.matmul(
                        pso3[:, ec, :], lhsT=v_tok[:, blk, ec * 128:(ec + 1) * 128], rhs=PT, start=(ec == 0), stop=False, skip_group_check=True),
                        r=[tv, tPT], w=[tpo], skip_self=True)
            if mid_hook is not None and blk == 1:
                mid_hook()
            pviews = []
            for i in range(4):
                if dv <= 128:
                    bk = 5 + blk % 2
                    pv = self.pb[bk][:, i * 128:i * 128 + dv]
                else:
                    bk = 5 + i // 2
                    pv = self.pb[bk][:, (i % 2) * 256:(i % 2) * 256 + dv]
                pviews.append((pv, self.tpb[bk]))
                P.op("pe", lambda e, blk=blk, i=i, pv=pv: e.matmul(pv, lhsT=c["kdm"][:, blk, i, :], rhs=v_tok[:, blk, :],
                                                                   start=True, stop=True, skip_group_check=True),
                     r=[c["tkdm"][blk], tv], w=[self.tpb[bk]], skip_self=True)
            for i in range(4):
                ch = blk * 4 + i
                Ssrc, tSsrc = (S, tS) if ch == 0 else (c["Sr"][(ch - 1) % 4], c["tSr"][(ch - 1) % 4])
                Sdst, tSdst = (S, tS) if ch == 15 else (c["Sr"][ch % 4], c["tSr"][ch % 4])
                psS, tpS = pviews[i]
                if mode == "B":
                    for ec in range(dvc):
                        P.op("pe", lambda e, ec=ec, i=i, ch=ch, pso3=pso3, Ssrc=Ssrc: e.matmul(
                            pso3[:, ec, i * 32:(i + 1) * 32], lhsT=Ssrc[:, ec * 128:(ec + 1) * 128], rhs=c["qi_f"][:, ch * 32:(ch + 1) * 32],
                            start=False, stop=(i == 3), skip_group_check=True),
                            r=[tSsrc, c["tqk"]], w=[tpo], skip_self=True)
                P.op("dve", lambda e, ch=ch, psS=psS, Ssrc=Ssrc, Sdst=Sdst: e.scalar_tensor_tensor(
                    out=Sdst, in0=Ssrc, scalar=c["dec"][:, ch:ch + 1], in1=psS, op0=ALU.mult, op1=ALU.add),
                     r=[tpS, c["tdec"], tSsrc], w=[tSdst])
            if mode == "B":
                P.op("act", lambda e, bs=bs, pso3=pso3: e.copy(out=c["o_s"][:, :, bs], in_=pso3), r=[tpo], w=[c["to"]])

    def head_rms_gate(self, c, ps_g_list, m_b, tm, vc0, pre_silu=False):
        P = self.P
        dv, dvc = c["dv"], c["dvc"]
        o_s, to, osq, tosq, rn, trn = c["o_s"], c["to"], c["osq"], c["tosq"], c["rn"], c["trn"]
        epsc, tcc = self._eps, self.tcc
        P.op("act", lambda e: e.activation(out=osq, in_=o_s, func=AF.Square), r=[to], w=[tosq])
        psn, tpn = self.pb[3], self.tpb[3]
        for ec in range(dvc):
            P.op("pe", lambda e, ec=ec: e.matmul(psn, lhsT=self.ones_b, rhs=osq[:, ec, :], start=(ec == 0), stop=(ec == dvc - 1)),
                 r=[tosq, self.tconst], w=[tpn], skip_self=True)
        P.op("act", lambda e: e.activation(out=rn, in_=psn, func=AF.Ln, bias=epsc, scale=1.0 / dv), r=[tpn, tcc], w=[trn])
        P.op("act", lambda e: e.activation(out=rn, in_=rn, func=AF.Exp, scale=-0.5), r=[trn], w=[trn])
        for ec in range(dvc):
            psg, tpg = ps_g_list[ec]
            P.op("dve", lambda e, ec=ec: e.tensor_tensor(out=o_s[:, ec, :], in0=o_s[:, ec, :], in1=rn, op=ALU.mult), r=[trn, to], w=[to])
            if pre_silu:
                P.op("dve", lambda e, ec=ec, psg=psg: e.tensor_tensor(out=m_b[:, vc0 + ec, :], in0=o_s[:, ec, :], in1=psg, op=ALU.mult),
                     r=[to, tpg], w=[tm])
                continue
            sgt = c["E"]
            P.op("act", lambda e, psg=psg: e.activation(out=sgt, in_=psg, func=AF.Silu), r=[tpg, c["tkd"]], w=[c["tE"]])
            P.op("dve", lambda e, ec=ec: e.tensor_tensor(out=m_b[:, vc0 + ec, :], in0=o_s[:, ec, :], in1=sgt, op=ALU.mult),
                 r=[to, c["tE"]], w=[tm])

    def hgrn2_pass(self, mode):
        P, A = self.P, self.A
        li = 1
        self.new_consts()
        one_c = self._one
        w_in = self.din("hg_w_in", [D, 4 * D]).rearrange("(c p) n -> p c n", p=128)
        c = self.gla_alloc(8, 128, mode)
        lb = A.alloc([8], F32); oml = A.alloc([8], F32); tlb = P.tok("lb")
        et = A.alloc([4, 8], F32)
        for k in range(4):
            P.op("act", lambda e, k=k: e.activation(out=et[:, k, :], in_=self.vcol(f"hg_lb{k}", 0, 8), func=AF.Exp), r=[self.tvec], w=[tlb])
        P.op("dve", lambda e: e.tensor_tensor(out=oml, in0=et[:, 0, :], in1=et[:, 1, :], op=ALU.add), r=[tlb], w=[tlb])
        P.op("dve", lambda e: e.tensor_tensor(out=oml, in0=oml, in1=et[:, 2, :], op=ALU.add), r=[tlb], w=[tlb])
        P.op("dve", lambda e: e.tensor_tensor(out=oml, in0=oml, in1=et[:, 3, :], op=ALU.add), r=[tlb], w=[tlb])
        P.op("dve", lambda e: e.reciprocal(out=oml, in_=oml), r=[tlb], w=[tlb])
        P.op("dve", lambda e: e.tensor_copy(out=lb, in_=et[:, 1, :]), r=[tlb], w=[tlb])
        for k in range(2, li + 1):
            P.op("dve", lambda e, k=k: e.tensor_tensor(out=lb, in0=lb, in1=et[:, k, :], op=ALU.add), r=[tlb], w=[tlb])
        P.op("dve", lambda e: e.tensor_tensor(out=lb, in0=lb, in1=oml, op=ALU.mult), r=[tlb], w=[tlb])
        P.op("dve", lambda e: e.tensor_scalar(out=oml, in0=lb, scalar1=-1.0, scalar2=1.0, op0=ALU.mult, op1=ALU.add), r=[tlb], w=[tlb])
        hl = A.alloc([8], F32); bl = A.alloc([8], F32)
        P.op("dve", lambda e: e.tensor_scalar(out=hl, in0=oml, scalar1=0.5, scalar2=None, op0=ALU.mult), r=[tlb], w=[tlb])
        P.op("dve", lambda e: e.tensor_tensor(out=bl, in0=lb, in1=hl, op=ALU.add), r=[tlb], w=[tlb])
        c["pinned_by_front"] = True
        self.gla_init_state(c, li)
        x_b = A.alloc([8, NT], BF16); txb = P.tok("xb")
        wv_ring = Ring(P, [A.alloc([8, 256], BF16) for _ in range(2)], "wv")
        v_tok = A.alloc([4, D], BF16); tv = P.tok("v")
        ncol = 3 if mode == "B" else 1
        wh_ring = Ring(P, [A.alloc([8, ncol, 256], BF16) for _ in range(2)], "wh")
        q_f = A.alloc([NT], F32); k_f = A.alloc([NT], F32); lg_f = A.alloc([NT], F32); tin = P.tok("qkl")
        if mode == "B":
            sg = A.alloc([2, NT], F32); tsg = P.toks(2, "sg")
            m_b = A.alloc([8, NT], BF16); tm = P.tok("m")
            sc = self.mixer_scratch(8, x_b, txb, m_b, tm)
            w_out = self.din("hg_w_out", [D, D])
        for ti in range(NTILE):
            xt = self.x_f[:, :, ti * NT:(ti + 1) * NT]
            P.op("act", lambda e, xt=xt: e.copy(out=x_b, in_=xt), r=[self.tx[ti]], w=[txb])
            for qt in range(4):
                ws, tw = wv_ring.next()
                self.load_w(ws, w_in[:, :, 2048 + qt * 256:2048 + (qt + 1) * 256], tw)
                for blk in range(4):
                    ps, tps = self.pb[blk % 2], self.tpb[blk % 2]
                    for kc in range(8):
                        P.op("pe", lambda e, kc=kc, blk=blk, ps=ps, ws=ws: e.matmul(
                            ps[:, 0:256], lhsT=x_b[:, kc, blk * 128:(blk + 1) * 128], rhs=ws[:, kc, :], start=(kc == 0), stop=(kc == 7)),
                            r=[txb, tw], w=[tps], skip_self=True)
                    P.op("act", lambda e, blk=blk, qt=qt, ps=ps: e.copy(out=v_tok[:, blk, qt * 256:(qt + 1) * 256], in_=ps[:, 0:256]),
                         r=[tps], w=[tv])
            wstate = {}

            def front(h):
                hp, h2 = h // 2, h % 2
                if h2 == 0:
                    ws, tw = wh_ring.next()
                    self.load_w(ws[:, :, 0, :], w_in[:, :, 1024 + hp * 256:1024 + (hp + 1) * 256], tw)
                    if mode == "B":
                        self.load_w(ws[:, :, 1, :], w_in[:, :, hp * 256:(hp + 1) * 256], tw)
                        self.load_w(ws[:, :, 2, :], w_in[:, :, 3072 + hp * 256:3072 + (hp + 1) * 256], tw)
                    wstate["ws"], wstate["tw"] = ws, tw
                ws, tw = wstate["ws"], wstate["tw"]
                cs = slice(h2 * 128, (h2 + 1) * 128)
                psf, tpf = self.pb[0], self.tpb[0]
                self.proj(psf, tpf, ws[:, :, 0, :], tw, cs, x_b, txb)
                if mode == "B":
                    psq, tpq = self.pb[1], self.tpb[1]
                    self.proj(psq, tpq, ws[:, :, 1, :], tw, cs, x_b, txb)
                    psg, tpg = self.pb[2], self.tpb[2]
                    self.proj(psg, tpg, ws[:, :, 2, :], tw, cs, x_b, txb)
                P.op("act", lambda e: e.activation(out=k_f, in_=psf, func=AF.Tanh, scale=0.5), r=[tpf], w=[tin])
                if mode == "B":
                    P.op("act", lambda e: e.activation(out=q_f, in_=psq, func=AF.Silu), r=[tpq], w=[tin])
                    P.op("act", lambda e, h=h: e.activation(out=sg[:, h % 2, :], in_=psg, func=AF.Silu), r=[tpg], w=[tsg[h % 2]])
                P.op("act", lambda e, h=h: e.activation(out=k_f, in_=k_f, func=AF.Identity, scale=hl[:, h:h + 1], bias=bl[:, h:h + 1]),
                     r=[tin, tlb], w=[tin])
                P.op("act", lambda e: e.activation(out=lg_f, in_=k_f, func=AF.Ln), r=[tin], w=[tin])
                P.op("dve", lambda e: e.tensor_scalar(out=k_f, in0=k_f, scalar1=-1.0, scalar2=1.0, op0=ALU.mult, op1=ALU.add),
                     r=[tin], w=[tin])

            front(0)
            for h in range(8):
                hook = (lambda h=h: front(h + 1)) if h + 1 < 8 else None
                self.gla_head(c, h, q_f, k_f, lg_f, tin, v_tok[:, :, h * 128:(h + 1) * 128], tv, mid_hook=hook)
                if mode == "B":
                    self.head_rms_gate(c, [(sg[:, h % 2, :], tsg[h % 2])], m_b, tm, h, pre_silu=True)
            if mode == "B":
                self.mixer_out(li, ti, m_b, tm, 8, w_out, sc)
        self.gla_finish(c, li)
        self.phase_end()

    def gla_pass(self, mode):
        P, A = self.P, self.A
        li = 3
        self.new_consts()
        one_c = self._one
        w_in = self.din("gla_w_in", [D, 3088]).rearrange("(c p) n -> p c n", p=128)
        c = self.gla_alloc(4, 256, mode)
        nbg = A.alloc([4], F32); tnb = P.tok("nbg")
        P.op("dve", lambda e: e.tensor_scalar(out=nbg, in0=self.vcol("gla_b_gate", 0, 4), scalar1=-1.0, scalar2=None, op0=ALU.mult),
             r=[self.tvec], w=[tnb])
        wgl = A.alloc([8, 16], BF16); twgl = P.tok("wgl")
        self.load_w(wgl, w_in[:, :, 3072:3088], twgl)
        wgate = A.alloc([512], BF16, parts=16)
        self.load_w(wgate, self.din("gla_w_gate", [16, 512]), twgl)
        gl_b = A.alloc([NT], BF16, parts=16); tgl = P.tok("gl")
        self.gla_init_state(c, li)
        x_b = A.alloc([8, NT], BF16); txb = P.tok("xb")
        wring = Ring(P, [A.alloc([8, 256], BF16) for _ in range(4)], "wr")
        v_tok = A.alloc([4, D], BF16); tv = P.tok("v")
        q_f = A.alloc([NT], F32); k_f = A.alloc([NT], F32); lg_f = A.alloc([NT], F32); tin = P.tok("qkl")
        if mode == "B":
            m_b = A.alloc([8, NT], BF16); tm = P.tok("m")
            sc = self.mixer_scratch(8, x_b, txb, m_b, tm)
            w_out = self.din("gla_w_out", [D, D])
        for ti in range(NTILE):
            xt = self.x_f[:, :, ti * NT:(ti + 1) * NT]
            P.op("act", lambda e, xt=xt: e.copy(out=x_b, in_=xt), r=[self.tx[ti]], w=[txb])
            ps, tps = self.pb[0], self.tpb[0]
            for kc in range(8):
                P.op("pe", lambda e, kc=kc: e.matmul(ps[0:16, :], lhsT=wgl[:, kc, :], rhs=x_b[:, kc, :], start=(kc == 0), stop=(kc == 7)),
                     r=[twgl, txb], w=[tps], skip_self=True)
            P.op("act", lambda e: e.copy(out=gl_b, in_=ps[0:16, :]), r=[tps], w=[tgl])
            for qt in range(4):
                ws, tw = wring.next()
                self.load_w(ws, w_in[:, :, 1024 + qt * 256:1024 + (qt + 1) * 256], tw)
                for blk in range(4):
                    ps, tps = self.pb[blk % 2], self.tpb[blk % 2]
                    for kc in range(8):
                        P.op("pe", lambda e, kc=kc, blk=blk, ps=ps, ws=ws: e.matmul(
                            ps[:, 0:256], lhsT=x_b[:, kc, blk * 128:(blk + 1) * 128], rhs=ws[:, kc, :], start=(kc == 0), stop=(kc == 7)),
                            r=[txb, tw], w=[tps], skip_self=True)
                    P.op("act", lambda e, blk=blk, qt=qt, ps=ps: e.copy(out=v_tok[:, blk, qt * 256:(qt + 1) * 256], in_=ps[:, 0:256]),
                         r=[tps], w=[tv])
            for hp in range(2):
                wk, twk = wring.next()
                self.load_w(wk, w_in[:, :, 512 + hp * 256:512 + (hp + 1) * 256], twk)
                if mode == "B":
                    wq, twq = wring.next()
                    self.load_w(wq, w_in[:, :, hp * 256:(hp + 1) * 256], twq)
                for h2 in range(2):
                    h = hp * 2 + h2
                    cs = slice(h2 * 128, (h2 + 1) * 128)
                    psz, tpz = self.pb[0], self.tpb[0]
                    P.op("pe", lambda e, h=h: e.matmul(psz, lhsT=wgate[:, h * 128:(h + 1) * 128], rhs=gl_b, start=True, stop=True),
                         r=[twgl, tgl], w=[tpz], skip_self=True)
                    P.op("act", lambda e, h=h: e.activation(out=lg_f, in_=psz, func=AF.Exp, scale=-1.0, bias=nbg[:, h:h + 1]),
                         r=[tpz, tnb], w=[tin])
                    P.op("act", lambda e: e.activation(out=lg_f, in_=lg_f, func=AF.Ln, bias=one_c, scale=1.0), r=[tin, self.tcc], w=[tin])
                    P.op("dve", lambda e: e.tensor_scalar(out=lg_f, in0=lg_f, scalar1=-1.0 / 16.0, scalar2=None, op0=ALU.mult), r=[tin], w=[tin])
                    psk, tpk = self.pb[1], self.tpb[1]
                    self.proj(psk, tpk, wk, twk, cs, x_b, txb)
                    P.op("act", lambda e: e.copy(out=k_f, in_=psk), r=[tpk], w=[tin])
                    if mode == "B":
                        psq, tpq = self.pb[0], self.tpb[0]
                        self.proj(psq, tpq, wq, twq, cs, x_b, txb)
                        P.op("act", lambda e: e.mul(out=q_f, in_=psq, mul=128.0 ** -0.5), r=[tpq], w=[tin])
                    self.gla_head(c, h, q_f, k_f, lg_f, tin, v_tok[:, :, h * 256:(h + 1) * 256], tv)
                    if mode == "B" and DEBUG and ti == DEBUG_TI and h == 0:
                        self.dbg("d_q", q_f, tin, [128, NT]); self.dbg("d_k", k_f, tin, [128, NT]); self.dbg("d_lg", lg_f, tin, [128, NT])
                        self.dbg("d_o", c["o_s"], c["to"], [128, 2, NT])
                        self.dbg("d_S", c["S"][:, 0, :], c["tS"][0], [128, 256])
                    if mode == "B":
                        wr_, twr_ = wring.next()
                        self.load_w(wr_, w_in[:, :, 2048 + h * 256:2048 + (h + 1) * 256], twr_)
                        pl = []
                        for ec in range(2):
                            psg, tpg = self.pb[ec], self.tpb[ec]
                            self.proj(psg, tpg, wr_, twr_, slice(ec * 128, (ec + 1) * 128), x_b, txb)
                            pl.append((psg, tpg))
                        self.head_rms_gate(c, pl, m_b, tm, h * 2)
            if mode == "B":
                self.mixer_out(li, ti, m_b, tm, 8, w_out, sc)
        self.gla_finish(c, li)
        self.phase_end()

    def ret_pass(self, mode):
        P, A = self.P, self.A
        li = 2
        self.new_consts()
        epsc, tcc = self._eps, self.tcc
        H = 4
        gam = [1.0 - 2.0 ** (-5.0 - h) for h in range(H)]
        w_in = self.din("ret_w_in", [D, 6144]).rearrange("(c p) n -> p c n", p=128)
        S = A.alloc([2, H, 512], F32); tS = P.toks(H, "S")
        if self.fused:
            self.carry_load("S2", S.rearrange("p a b c -> p (a b c)"), tS, 4096)
        else:
            for h in range(H):
                P.op("dve", lambda e, h=h: e.memset(S[:, :, h, :], 0.0), w=[tS[h]])
        if mode == "B" and not self.fused:
            st_all = self.din("st_all2", [8, 128, 4096])
            m = A.mark()
            sring = Ring(P, [A.alloc([512], F32) for _ in range(3)], "sta")
            dsel = A.alloc([8, H], F32); tds = P.tok("dsel")
            for r in range(8):
                for h in range(H):
                    P.op("dve", lambda e, r=r, h=h: e.tensor_scalar(out=dsel[:, r, h:h + 1], in0=self.sel[:, r:r + 1],
                                                                   scalar1=float(gam[h] ** T - 1.0), scalar2=1.0, op0=ALU.mult, op1=ALU.add),
                         r=[self.tconst], w=[tds])
            for r in range(8):
                for h in range(H):
                    for dc in range(2):
                        sb, tsb = sring.next()
                        o0 = (dc * H + h) * 512
                        P.dma("sp", sb, st_all[r][:, o0:o0 + 512], w=[tsb])
                        P.op("dve", lambda e, r=r, sb=sb: e.tensor_scalar(out=sb, in0=sb, scalar1=self.sel[:, r:r + 1], scalar2=None, op0=ALU.mult),
                             r=[tsb, self.tconst], w=[tsb])
                        P.op("dve", lambda e, r=r, h=h, dc=dc, sb=sb: e.scalar_tensor_tensor(
                            out=S[:, dc, h, :], in0=S[:, dc, h, :], scalar=dsel[:, r, h:h + 1], in1=sb, op0=ALU.mult, op1=ALU.add),
                            r=[tsb, tds, tS[h]], w=[tS[h]])
            P.barrier()
            A.reset(m)
        zeta = A.alloc([H, 128], F32); ttab = P.tok("rtab")
        P.dma("sp", zeta, self.din("tab_zeta", [128, H, 128]), w=[ttab])
        cos_t = A.alloc([NT], F32); sin_t = A.alloc([NT], F32); tcs = P.tok("cs")
        sfx = f"_{self.cur_q}" if self.fused else ""
        cosd = self.din("tabc_cos" + sfx, [128, T]); sind = self.din("tabc_sin" + sfx, [128, T])
        if mode == "B":
            dmat = A.alloc([H, 128], F32); xi = A.alloc([H, 128], F32)
            P.dma("sp", dmat, self.din("tab_dmat", [128, H, 128]), w=[ttab])
            P.dma("sp", xi, self.din("tab_xi", [128, H, 128]), w=[ttab])
            S_b = A.alloc([2, 512], BF16); tSb = P.tok("Sb")
        x_b = A.alloc([8, NT], BF16); txb = P.tok("xb")
        wring = Ring(P, [A.alloc([8, 256], BF16) for _ in range(4)], "wr")
        v_tok = A.alloc([4, 512], BF16); tv = P.tok("v")
        A1 = A.alloc([NT], F32); A2 = A.alloc([NT], F32); tA = P.tok("A12")
        k_r = A.alloc([2, NT], BF16); kz = A.alloc([2, NT], BF16); tkr = P.tok("kr"); tkz = P.tok("kz")
        kz_tok = A.alloc([4, 256], BF16); tkzt = P.tok("kzt")
        if mode == "B":
            q_r = A.alloc([2, NT], BF16); qx = A.alloc([2, NT], BF16); tqr = P.tok("qr"); tqx = P.tok("qx")
            PTr = Ring(P, [A.alloc([128], BF16) for _ in range(2)], "PT")
            o_s = A.alloc([4, NT], F32); to = P.tok("o")
            m_b = A.alloc([16, NT], BF16); tm = P.tok("m")
            sc = self.mixer_scratch(16, x_b, txb, m_b[:, 0:8], tm, sw=128)
            zreg = sc["z"].rearrange("p a b -> p (a b)").bitcast(BF16)
            o_b = zreg[:, 0:4 * NT].rearrange("p (a b) -> p a b", a=4)
            osq = zreg[:, 4 * NT:8 * NT].rearrange("p (a b) -> p a b", a=4)
            tz = sc["tz"]
            sm = sc["sm"]
            w_out = self.din("ret_w_out", [2 * D, D])

        def rotary(ps1, tp1, ps2, tp2, out_b, tout):
            P.op("dve", lambda e: e.tensor_tensor(out=A1, in0=ps1, in1=cos_t, op=ALU.mult), r=[tp1, tcs], w=[tA])
            P.op("dve", lambda e: e.tensor_tensor(out=A2, in0=ps2, in1=sin_t, op=ALU.mult), r=[tp2, tcs], w=[tA])
            P.op("dve", lambda e: e.tensor_tensor(out=out_b[:, 0, :], in0=A1, in1=A2, op=ALU.subtract), r=[tA], w=[tout])
            P.op("dve", lambda e: e.tensor_tensor(out=A1, in0=ps1, in1=sin_t, op=ALU.mult), r=[tp1, tcs, tout], w=[tA])
            P.op("dve", lambda e: e.tensor_tensor(out=A2, in0=ps2, in1=cos_t, op=ALU.mult), r=[tp2, tcs], w=[tA])
            P.op("dve", lambda e: e.tensor_tensor(out=out_b[:, 1, :], in0=A1, in1=A2, op=ALU.add), r=[tA], w=[tout])

        for ti in range(NTILE):
            xt = self.x_f[:, :, ti * NT:(ti + 1) * NT]
            P.op("act", lambda e, xt=xt: e.copy(out=x_b, in_=xt), r=[self.tx[ti]], w=[txb])
            P.dma("sp", cos_t, cosd[:, ti * NT:(ti + 1) * NT], w=[tcs])
            P.dma("sp", sin_t, sind[:, ti * NT:(ti + 1) * NT], w=[tcs])
            for h in range(H):
                for qt in range(2):
                    ws, tw = wring.next()
                    self.load_w(ws, w_in[:, :, 2048 + h * 512 + qt * 256:2048 + h * 512 + (qt + 1) * 256], tw)
                    for blk in range(4):
                        ps, tps = self.pb[blk % 2], self.tpb[blk % 2]
                        for kc in range(8):
                            P.op("pe", lambda e, kc=kc, blk=blk, ps=ps, ws=ws: e.matmul(
                                ps[:, 0:256], lhsT=x_b[:, kc, blk * 128:(blk + 1) * 128], rhs=ws[:, kc, :], start=(kc == 0), stop=(kc == 7)),
                                r=[txb, tw], w=[tps], skip_self=True)
                        P.op("act", lambda e, blk=blk, qt=qt, ps=ps: e.copy(out=v_tok[:, blk, qt * 256:(qt + 1) * 256], in_=ps[:, 0:256]),
                             r=[tps], w=[tv])
                wk, twk = wring.next()
                self.load_w(wk, w_in[:, :, 1024 + h * 256:1024 + (h + 1) * 256], twk)
                self.proj(self.pb[0], self.tpb[0], wk, twk, slice(0, 128), x_b, txb)
                self.proj(self.pb[1], self.tpb[1], wk, twk, slice(128, 256), x_b, txb)
                rotary(self.pb[0], self.tpb[0], self.pb[1], self.tpb[1], k_r, tkr)
                kz4 = kz.rearrange("p a (b c) -> p a b c", b=4)
                kr4 = k_r.rearrange("p a (b c) -> p a b c", b=4)
                for dc in range(2):
                    P.op("dve", lambda e, dc=dc, h=h: e.tensor_tensor(out=kz4[:, dc], in0=kr4[:, dc],
                                                                      in1=zeta[:, h, :].unsqueeze(1).to_broadcast([128, 4, 128]), op=ALU.mult),
                         r=[tkr, ttab], w=[tkz])
                pT = self.pbT.rearrange("p (b d) -> p b d", b=4)
                for blk in range(4):
                    for dc in range(2):
                        P.op("pe", lambda e, blk=blk, dc=dc: e.transpose(out=pT[:, blk, dc * 128:(dc + 1) * 128],
                                                                         in_=kz[:, dc, blk * 128:(blk + 1) * 128], identity=self.ident_b),
                             r=[tkz, self.tconst], w=[self.tpbT], skip_self=True)
                P.op("act", lambda e: e.copy(out=kz_tok, in_=pT), r=[self.tpbT], w=[tkzt])
                if mode == "B":
                    wq, twq = wring.next()
                    self.load_w(wq, w_in[:, :, h * 256:(h + 1) * 256], twq)
                    self.proj(self.pb[0], self.tpb[0], wq, twq, slice(0, 128), x_b, txb)
                    self.proj(self.pb[1], self.tpb[1], wq, twq, slice(128, 256), x_b, txb)
                    rotary(self.pb[0], self.tpb[0], self.pb[1], self.tpb[1], q_r, tqr)
                    qx4 = qx.rearrange("p a (b c) -> p a b c", b=4)
                    qr4 = q_r.rearrange("p a (b c) -> p a b c", b=4)
                    for dc in range(2):
                        P.op("dve", lambda e, dc=dc, h=h: e.tensor_tensor(out=qx4[:, dc], in0=qr4[:, dc],
                                                                          in1=xi[:, h, :].unsqueeze(1).to_broadcast([128, 4, 128]), op=ALU.mult),
                             r=[tqr, ttab], w=[tqx])
                    P.op("act", lambda e, h=h: e.copy(out=S_b, in_=S[:, :, h, :]), r=[tS[h]], w=[tSb])
                for blk in range(4):
                    bs = slice(blk * 128, (blk + 1) * 128)
                    if mode == "B":
                        pss, tss = self.pb[2], self.tpb[2]
                        for dc in range(2):
                            P.op("pe", lambda e, dc=dc, bs=bs: e.matmul(pss[:, 0:128], lhsT=k_r[:, dc, bs], rhs=q_r[:, dc, bs],
                                                                        start=(dc == 0), stop=(dc == 1)),
                                 r=[tkr, tqr], w=[tss], skip_self=True)
                        PT, tPT = PTr.next()
                        P.op("dve", lambda e, PT=PT, h=h: e.tensor_tensor(out=PT, in0=pss[:, 0:128], in1=dmat[:, h, :], op=ALU.mult),
                             r=[tss, ttab], w=[tPT])
                        pso, tpo = self.pb[3 + blk % 2], self.tpb[3 + blk % 2]
                        pso3 = pso.rearrange("p (a b) -> p a b", a=4)
                        for ec in range(4):
                            P.op("pe", lambda e, ec=ec, blk=blk, PT=PT, pso3=pso3: e.matmul(
                                pso3[:, ec, :], lhsT=v_tok[:, blk, ec * 128:(ec + 1) * 128], rhs=PT, start=(ec == 0), stop=False, skip_group_check=True),
                                r=[tv, tPT], w=[tpo], skip_self=True)
                            for dc in range(2):
                                P.op("pe", lambda e, ec=ec, dc=dc, bs=bs, pso3=pso3: e.matmul(
                                    pso3[:, ec, :], lhsT=S_b[:, dc, ec * 128:(ec + 1) * 128], rhs=qx[:, dc, bs],
                                    start=False, stop=(dc == 1), skip_group_check=True),
                                    r=[tSb, tqx], w=[tpo], skip_self=True)
                        P.op("act", lambda e, bs=bs, pso3=pso3: e.copy(out=o_s[:, :, bs], in_=pso3), r=[tpo], w=[to])
                    for dc in range(2):
                        psS, tpS = self.pb[5 + dc], self.tpb[5 + dc]
                        P.op("pe", lambda e, dc=dc, blk=blk, psS=psS: e.matmul(psS, lhsT=kz_tok[:, blk, dc * 128:(dc + 1) * 128], rhs=v_tok[:, blk, :],
                                                                               start=True, stop=True),
                             r=[tkzt, tv], w=[tpS], skip_self=True)
                        P.op("dve", lambda e, dc=dc, h=h, psS=psS: e.scalar_tensor_tensor(
                            out=S[:, dc, h, :], in0=S[:, dc, h, :], scalar=float(gam[h] ** 128), in1=psS, op0=ALU.mult, op1=ALU.add),
                            r=[tpS, tS[h]], w=[tS[h]])
                    if mode == "B" and blk < 3:
                        P.op("act", lambda e, h=h: e.copy(out=S_b, in_=S[:, :, h, :]), r=[tS[h]], w=[tSb])
                    if mode == "B" and blk == 1:
                        for e2 in range(2):
                            wg, twg = wring.next()
                            self.load_w(wg, w_in[:, :, 4096 + h * 512 + e2 * 256:4096 + h * 512 + (e2 + 1) * 256], twg)
                            for e1 in range(2):
                                ec = e2 * 2 + e1
                                psg, tpg = self.pb[e1], self.tpb[e1]
                                self.proj(psg, tpg, wg, twg, slice(e1 * 128, (e1 + 1) * 128), x_b, txb)
                                P.op("act", lambda e, psg=psg, ec=ec, h=h: e.activation(out=m_b[:, h * 4 + ec, :], in_=psg, func=AF.Silu),
                                     r=[tpg], w=[tm])
                if mode == "B":
                    mean, var, rstd, nmr, tsm = sm["mean"], sm["var"], sm["rstd"], sm["nmr"], sm["t"]
                    P.op("act", lambda e: e.copy(out=o_b, in_=o_s), r=[to], w=tz)
                    P.op("act", lambda e: e.activation(out=osq, in_=o_s, func=AF.Square), r=[to], w=tz)
                    ps_s, ts_s = self.pb[2], self.tpb[2]
                    ps_q, ts_q = self.pb[0], self.tpb[0]
                    for ec in range(4):
                        P.op("pe", lambda e, ec=ec: e.matmul(ps_s, lhsT=self.ones_b, rhs=o_b[:, ec, :], start=(ec == 0), stop=(ec == 3)),
                             r=tz + [self.tconst], w=[ts_s], skip_self=True)
                    for ec in range(4):
                        P.op("pe", lambda e, ec=ec: e.matmul(ps_q, lhsT=self.ones_b, rhs=osq[:, ec, :], start=(ec == 0), stop=(ec == 3)),
                             r=tz + [self.tconst], w=[ts_q], skip_self=True)
                    P.op("act", lambda e: e.mul(out=mean, in_=ps_s, mul=1.0 / 512), r=[ts_s], w=[tsm])
                    P.op("act", lambda e: e.activation(out=nmr, in_=ps_s, func=AF.Square, scale=1.0 / 512), r=[ts_s], w=[tsm])
                    P.op("dve", lambda e: e.scalar_tensor_tensor(out=var, in0=ps_q, scalar=1.0 / 512, in1=nmr, op0=ALU.mult, op1=ALU.subtract),
                         r=[ts_q, tsm], w=[tsm])
                    self.pin_lnexp()
                    P.op("act", lambda e: e.activation(out=var, in_=var, func=AF.Ln, bias=epsc, scale=1.0), r=[tsm, tcc], w=[tsm])
                    P.op("act", lambda e: e.activation(out=rstd, in_=var, func=AF.Exp, scale=-0.5), r=[tsm], w=[tsm])
                    P.op("dve", lambda e: e.scalar_tensor_tensor(out=nmr, in0=mean, scalar=-1.0, in1=rstd, op0=ALU.mult, op1=ALU.mult),
                         r=[tsm], w=[tsm])
                    P.op("dve", lambda e: e.tensor_tensor(out=o_s, in0=o_s, in1=rstd.unsqueeze(1).to_broadcast([128, 4, NT]), op=ALU.mult),
                         r=[tsm, to], w=[to])
                    P.op("dve", lambda e: e.tensor_tensor(out=o_s, in0=o_s, in1=nmr.unsqueeze(1).to_broadcast([128, 4, NT]), op=ALU.add),
                         r=[tsm, to], w=[to])
                    for ec in range(4):
                        P.op("dve", lambda e, ec=ec, h=h: e.tensor_tensor(out=m_b[:, h * 4 + ec, :], in0=o_s[:, ec, :], in1=m_b[:, h * 4 + ec, :], op=ALU.mult),
                             r=[to, tm], w=[tm])
            if mode == "B":
                self.mixer_out(li, ti, m_b, tm, 16, w_out, sc)
        if self.fused:
            self.carry_save("S2", S.rearrange("p a b c -> p (a b c)"), tS, 4096)
        elif mode == "A":
            st = self.dout("st_loc2", [128, 4096])
            for h in range(H):
                for dc in range(2):
                    o0 = (dc * H + h) * 512
                    P.dma("sp", st[:, o0:o0 + 512], S[:, dc, h, :], r=[tS[h]])
        self.phase_end()

    def finalize(self):
        self.P.finalize()
        return self.nc


class Host:
    def __init__(self, inputs):
        self.inp = {k: np.asarray(v) for k, v in inputs.items()}
        self.cache = {}
        v = np.zeros((128, NVEC), np.float32)

        def put(name, arr):
            o, n = VLAY[name]
            v[:, o:o + n] = _fm(arr)
        I = self.inp
        for j in range(4):
            put(f"rg_conv_w{j}", I["rg_conv_w"][0, j])
        put("rg_conv_b", I["rg_conv_b"][0])
        put("rg_b_a", I["rg_b_a"][0])
        put("rg_b_x", I["rg_b_x"][0])
        put("rg_lambda", I["rg_lambda"][0])
        for i in range(4):
            put(f"hg_lb{i}", I["hg_lb_logits"][i])
            put(f"ln_mix_g{i}", I["ln_mix_g"][i])
            put(f"ln_mix_b{i}", I["ln_mix_b"][i])
            put(f"ln_ffn_g{i}", I["ln_ffn_g"][i])
            put(f"ln_ffn_b{i}", I["ln_ffn_b"][i])
        put("gla_b_gate", I["gla_b_gate"][0])
        self.vecs = v
        self.ident = np.eye(128, dtype=np.float32)
        selE = np.zeros((8, 8, 128), np.float32)
        for e in range(8):
            selE[e, e, :] = 1.0
        self.selE = selE.reshape(8, 1024)

    @staticmethod
    def fm_act(a):
        t, c = a.shape
        return np.ascontiguousarray(a.T.reshape(c // 128, 128, t).transpose(1, 0, 2))

    def get(self, name, core, xcur=None, st_all=None):
        I = self.inp
        b, j = core // 4, core % 4
        t0 = j * T
        if name == "keep":
            k = np.zeros((128, 8), np.float32)
            for q in range(4):
                if q > 3 - j:
                    k[:, q] = 1.0
            return k
        if name[:-1].endswith("_") and name[-1].isdigit() and (name.startswith("xT_") or name.startswith("pT") or name.startswith("tabc_")):
            q = int(name[-1])
            ch = max(q - (3 - j), 0)
            t0 = ch * T
            base = name[:-2]
            if base == "xT":
                return self.fm_act(I["x"][b, t0:t0 + T])
            name = base
        if name == "xT":
            return xcur[core]
        if name == "vecs":
            return self.vecs
        if name == "ident_f":
            return self.ident
        if name == "selE":
            return self.selE
        if name == "tab_reset":
            r = np.ones((128, NT), np.float32)
            r[:, ::32] = 0.0
            return r
        if name == "tab_maskT":
            i = np.arange(128)
            return ((i[:, None] // 32 == i[None, :] // 32) & (i[:, None] <= i[None, :])).astype(np.float32)
        if name == "tab_rowmask":
            i = np.arange(128)
            return (i[:, None] // 32 == np.arange(4)[None, :]).astype(np.float32)
        if name in ("tab_zeta", "tab_xi", "tab_dmat"):
            out = np.zeros((128, 4, 128), np.float64)
            i = np.arange(128, dtype=np.float64)
            for h in range(4):
                g = 1.0 - 2.0 ** (-5.0 - h)
                if name == "tab_zeta":
                    out[:, h, :] = (g ** (127.0 - i))[None, :] / 16.0
                elif name == "tab_xi":
                    out[:, h, :] = (g ** (i + 1.0))[None, :]
                else:
                    rel = i[None, :] - i[:, None]
                    out[:, h, :] = np.where(rel >= 0, g ** np.maximum(rel, 0.0), 0.0) / 16.0
            return out.astype(np.float32)
        if name in ("tabc_cos", "tabc_sin"):
            inv = (np.float32(10000.0) ** (-(np.arange(0, 256, 2, dtype=np.float32)) / np.float32(256))).astype(np.float32)
            pos = np.arange(t0, t0 + T, dtype=np.float32)
            ang = (pos[None, :] * inv[:, None]).astype(np.float32).astype(np.float64)
            return (np.cos(ang) if name == "tabc_cos" else np.sin(ang)).astype(np.float32)
        if name == "sel":
            s = np.zeros((128, 8), np.float32)
            for r in range(8):
                if r // 4 == b and r % 4 < j:
                    s[:, r] = 1.0
            return s
        if name == "xh":
            h = np.zeros((4, D), np.float32)
            if j > 0:
                h[0:3] = I["x"][b, t0 - 3:t0]
            return self.fm_act(h)
        if name.startswith("pT"):
            li = int(name[2:])
            return self.fm_act(I["p"][li, b, t0:t0 + T])
        if name.startswith("st_all"):
            return st_all[name]
        if name in I and name not in ("x", "p"):
            a = I[name]
            return a[0] if a.shape[0] == 1 and name not in ("moe_w_in", "moe_w_out") else a
        for base in ("dense_w_in", "dense_w_out", "moe_w_router", "moe_w_in", "moe_w_out", "ple_w_proj", "ple_w_gate"):
            if name.startswith(base) and name[len(base):].isdigit():
                idx = int(name[len(base):])
                a = I[base]
                if base in ("dense_w_in", "dense_w_out"):
                    return a[idx:idx + 1]
                return a[idx]
        raise KeyError(name)


def run_launch(host, stage_list, xcur=None, st_all=None, trace=False, fused=False):
    kb = KB(stage_list, fused=fused)
    for st in stage_list:
        getattr(kb, st[0])(*st[1:])
    names = list(kb.inputs.keys())
    nc = kb.finalize()
    print("[kernel] instr counts", dict(kb.P.cnt), "sems", kb.P.n_sems, flush=True)
    in_maps = []
    for c in range(NCORES):
        m = {}
        for n in names:
            key = (n, c)
            per_core = n in ("xT", "sel", "xh", "keep") or n.startswith("xT_") or n.startswith("pT") or n.startswith("st_all") or n.startswith("tabc_")
            if per_core:
                m[n] = np.ascontiguousarray(host.get(n, c, xcur, st_all), dtype=np.float32)
            else:
                if n not in host.cache:
                    host.cache[n] = np.ascontiguousarray(host.get(n, 0, xcur, st_all), dtype=np.float32)
                m[n] = host.cache[n]
        in_maps.append(m)
    res = run_bass_kernel_spmd(nc, in_maps, core_ids=list(range(NCORES)), trace=trace)
    return res


PASSES = ["rglru_pass", "hgrn2_pass", "ret_pass", "gla_pass"]


def kernel_fused(**inputs):
    host = Host(inputs)
    res = run_launch(host, fused_stages(), fused=True)
    out = np.zeros((2, SEQ, D), np.float32)
    for c in range(NCORES):
        out[c // 4, (c % 4) * T:(c % 4 + 1) * T] = res.results[c]["yT"].transpose(2, 1, 0).reshape(T, D)
    return out


def fused_stages():
    st = []
    for q in range(4):
        st += [("set_pass", q), ("load_x",), ("rglru_pass", "B"), ("ffn_phase", 0), ("hgrn2_pass", "B"), ("ffn_phase", 1),
               ("ret_pass", "B"), ("ffn_phase", 2)]
        if q == 3:
            st += [("gla_pass", "B"), ("ffn_phase", 3), ("store_x",)]
        else:
            st += [("gla_pass", "A")]
    return st


def kernel(**inputs):
    host = Host(inputs)
    x = np.asarray(inputs["x"], np.float32)
    xcur = [Host.fm_act(x[c // 4, (c % 4) * T:(c % 4 + 1) * T]) for c in range(NCORES)]
    res = run_launch(host, [("load_x",), (PASSES[0], "A")], xcur=xcur)
    st = np.stack([res.results[c]["st_loc0"] for c in range(NCORES)])
    for i in range(4):
        stages = [("load_x",), (PASSES[i], "B"), ("ffn_phase", i)]
        if i < 3:
            stages.append((PASSES[i + 1], "A"))
        stages.append(("store_x",))
        res = run_launch(host, stages, xcur=xcur, st_all={f"st_all{i}": st})
        xcur = [res.results[c]["yT"] for c in range(NCORES)]
        if i < 3:
            st = np.stack([res.results[c][f"st_loc{i + 1}"] for c in range(NCORES)])
    out = np.zeros((2, SEQ, D), np.float32)
    for c in range(NCORES):
        out[c // 4, (c % 4) * T:(c % 4 + 1) * T] = xcur[c].transpose(2, 1, 0).reshape(T, D)
    return out
```
